# Optimizing a Trainium2 kernel written in Bass

```python
import jax, jax.numpy as jnp
from jax import lax
import numpy as np

D_MODEL = 1024
BATCH = 16
SEQ = 4096
DEPTH = 2

GRID_W = 64
CTX_LEN = 256
MIX_WIDTH = D_MODEL
GLA_WIDTH = MIX_WIDTH // 2
GLA_HEADS = 4
GLA_DV = GLA_WIDTH // GLA_HEADS
GLA_DK = GLA_DV // 2
GLA_KDIM = GLA_HEADS * GLA_DK
GLA_GATE_RANK = 16
GLA_GATE_NORM = 16.0
GLA_CHUNK = 64
NA_WIDTH = MIX_WIDTH - GLA_WIDTH
NA_HEADS = 8
NA_DH = NA_WIDTH // NA_HEADS
NA_WIN_H = 8
NA_WIN_W = 16
ROPE_BASE = 10000.0
D_FF = 11 * D_MODEL // 4
N_EXPERTS = 8
TOP_K = 2
D_FF_EXPERT = 7 * D_MODEL // 2
N_DENSE = (DEPTH + 1) // 2
N_MOE = DEPTH // 2
EPS = 1e-6
PROJ_SIZES = (GLA_KDIM, GLA_KDIM, GLA_WIDTH, GLA_WIDTH, 2 * GLA_GATE_RANK, NA_WIDTH, NA_WIDTH, NA_WIDTH)
PROJ_TOTAL = 2 * GLA_KDIM + 2 * GLA_WIDTH + 2 * GLA_GATE_RANK + 3 * NA_WIDTH

kernel_name = "hybrid_gla_natten_moe_dit"


def rmsnorm(x, g):
    xf = x.astype(jnp.float32)
    y = xf * lax.rsqrt(jnp.mean(xf * xf, axis=-1, keepdims=True) + EPS)
    return (y * g.astype(jnp.float32)).astype(x.dtype)


def modulation(cond, w, b):
    return jnp.split(jax.nn.silu(cond) @ w + b, 6, axis=-1)


def rope_1d(x, pos):
    half = x.shape[-1] // 2
    inv = ROPE_BASE ** (-jnp.arange(half, dtype=jnp.float32) / half)
    ang = pos.astype(jnp.float32)[:, None] * inv[None, :]
    cos = jnp.cos(ang)[:, None, :].astype(x.dtype)
    sin = jnp.sin(ang)[:, None, :].astype(x.dtype)
    x1, x2 = x[..., :half], x[..., half:]
    return jnp.concatenate([x1 * cos - x2 * sin, x1 * sin + x2 * cos], axis=-1)


def rope_2d(x, row, col):
    h = x.shape[-1] // 2
    return jnp.concatenate([rope_1d(x[..., :h], row), rope_1d(x[..., h:], col)], axis=-1)


def split_proj(p):
    return jnp.split(p, np.cumsum(PROJ_SIZES)[:-1].tolist(), axis=-1)


def gla_chunk(q, k, v, g, s0):
    B, H, L, dk = q.shape
    dv = v.shape[-1]
    C = GLA_CHUNK
    n = L // C
    q = q.reshape(B, H, n, C, dk)
    k = k.reshape(B, H, n, C, dk)
    v = v.reshape(B, H, n, C, dv)
    b = jnp.cumsum(g.reshape(B, H, n, C, dk), axis=3)
    b_ref = b[:, :, :, C // 2:C // 2 + 1, :]
    a = jnp.einsum('bhncd,bhnsd->bhncs', q * jnp.exp(b - b_ref), k * jnp.exp(b_ref - b))
    lower = jnp.tril(jnp.ones((C, C), dtype=bool))
    a = jnp.where(lower, a, 0.0)
    o = jnp.einsum('bhncs,bhnsv->bhncv', a, v)
    b_last = b[:, :, :, -1:, :]
    ds = jnp.einsum('bhnsd,bhnsv->bhndv', k * jnp.exp(b_last - b), v)
    decay = jnp.exp(b_last[:, :, :, 0, :])

    def step(s, inp):
        dec, d = inp
        return dec[..., None] * s + d, s

    s_fin, s_prev = lax.scan(step, s0, (jnp.moveaxis(decay, 2, 0), jnp.moveaxis(ds, 2, 0)))
    o = o + jnp.einsum('bhncd,nbhdv->bhncv', q * jnp.exp(b), s_prev)
    return o.reshape(B, H, L, dv), s_fin


def flip_seq(t):
    return jnp.flip(t, axis=2)


def bidir_gla(q, k, v, g_fwd, g_bwd, s_fwd0, s_bwd0):
    o_f, s_f = gla_chunk(q, k, v, g_fwd, s_fwd0)
    o_b, s_b = gla_chunk(flip_seq(q), flip_seq(k), flip_seq(v), flip_seq(g_bwd), s_bwd0)
    return o_f + flip_seq(o_b), s_f, s_b


def gla_inputs(q, k, v, lr, w_gate, b_gate, row, col):
    B, L, _ = q.shape
    q = q.reshape(B, L, GLA_HEADS, GLA_DK)
    k = k.reshape(B, L, GLA_HEADS, GLA_DK)
    if row is not None:
        q = rope_2d(q, row, col)
        k = rope_2d(k, row, col)
    q = q * GLA_DK ** -0.5
    v = v.reshape(B, L, GLA_HEADS, GLA_DV)
    lr_f, lr_b = jnp.split(lr, 2, axis=-1)

    def gate(z, w, bias):
        logit = (z @ w + bias).astype(jnp.float32)
        return (jax.nn.log_sigmoid(logit) / GLA_GATE_NORM).reshape(B, L, GLA_HEADS, GLA_DK)

    g_f = gate(lr_f, w_gate[0], b_gate[0])
    g_b = gate(lr_b, w_gate[1], b_gate[1])
    to_bhl = lambda t: jnp.swapaxes(t, 1, 2).astype(jnp.float32)
    return to_bhl(q), to_bhl(k), to_bhl(v), to_bhl(g_f), to_bhl(g_b)


def gla_output(o, r, gain):
    B, H, L, dv = o.shape
    o = rmsnorm(jnp.swapaxes(o, 1, 2), gain).reshape(B, L, H * dv).astype(r.dtype)
    return o * jax.nn.silu(r)


def neighbourhood_attention(q, k, v, kc, vc, rpb):
    B, L, H, dh = q.shape
    rows = L // GRID_W
    kh = min(NA_WIN_H, rows)
    cols = np.arange(GRID_W)
    cidx = np.clip(cols - NA_WIN_W // 2, 0, GRID_W - NA_WIN_W)[:, None] + np.arange(NA_WIN_W)[None, :]
    coff = cidx - cols[:, None] + NA_WIN_W - 1
    kg = k.reshape(B, rows, GRID_W, H, dh)
    vg = v.reshape(B, rows, GRID_W, H, dh)
    qg = jnp.moveaxis(q.reshape(B, rows, GRID_W, H, dh), 1, 0) * dh ** -0.5
    n_loc = kh * NA_WIN_W

    def row_block(args):
        q_r, r = args
        rs = jnp.clip(r - kh // 2, 0, rows - kh)
        kb = lax.dynamic_slice_in_dim(kg, rs, kh, axis=1)[:, :, cidx]
        vb = lax.dynamic_slice_in_dim(vg, rs, kh, axis=1)[:, :, cidx]
        roff = rs + jnp.arange(kh) - r + NA_WIN_H - 1
        bias = jnp.transpose(rpb[:, roff[:, None, None], coff[None]], (0, 2, 1, 3))
        s_loc = jnp.einsum('bqhd,biqjhd->bhqij', q_r, kb) + bias
        s_ctx = jnp.einsum('bqhd,bchd->bhqc', q_r, kc)
        s = jnp.concatenate([s_loc.reshape(B, H, GRID_W, n_loc), s_ctx], axis=-1).astype(jnp.float32)
        p = jax.nn.softmax(s, axis=-1).astype(v.dtype)
        p_loc = p[..., :n_loc].reshape(B, H, GRID_W, kh, NA_WIN_W)
        return (jnp.einsum('bhqij,biqjhd->bqhd', p_loc, vb)
                + jnp.einsum('bhqc,bchd->bqhd', p[..., n_loc:], vc))

    out = lax.map(row_block, (qg, jnp.arange(rows)))
    return jnp.moveaxis(out, 0, 1).reshape(B, L, H * dh)


def context_attention(q, k, v):
    B, Lc, H, dh = q.shape
    s = jnp.einsum('bqhd,bkhd->bhqk', q, k).astype(jnp.float32) * dh ** -0.5
    p = jax.nn.softmax(s, axis=-1).astype(v.dtype)
    return jnp.einsum('bhqk,bkhd->bqhd', p, v).reshape(B, Lc, H * dh)


def mixer(h, hc, w_in, w_out, gla_w_gate, gla_b_gate, gla_g_norm, na_rpb, with_ctx_out):
    B, L, _ = h.shape
    pos = jnp.arange(L)
    row, col = pos // GRID_W, pos % GRID_W
    gq, gk, gv, gr, glr, nq, nk, nv = split_proj(h @ w_in)
    cq, ck, cv, cr, clr, cnq, cnk, cnv = split_proj(hc @ w_in)
    s0 = jnp.zeros((B, GLA_HEADS, GLA_DK, GLA_DV), jnp.float32)
    oc, s_fwd, s_bwd = bidir_gla(*gla_inputs(cq, ck, cv, clr, gla_w_gate, gla_b_gate, None, None), s0, s0)
    ol, _, _ = bidir_gla(*gla_inputs(gq, gk, gv, glr, gla_w_gate, gla_b_gate, row, col), s_fwd, s_bwd)
    heads = lambda t: t.reshape(t.shape[0], t.shape[1], NA_HEADS, NA_DH)
    na_lat = neighbourhood_attention(heads(nq), heads(nk), heads(nv), heads(cnk), heads(cnv), na_rpb)
    y = jnp.concatenate([gla_output(ol, gr, gla_g_norm), na_lat], axis=-1) @ w_out
    if not with_ctx_out:
        return y, None
    na_ctx = context_attention(heads(cnq), heads(cnk), heads(cnv))
    yc = jnp.concatenate([gla_output(oc, cr, gla_g_norm), na_ctx], axis=-1) @ w_out
    return y, yc


def swiglu(h, wg, wu, wd):
    return (jax.nn.silu(h @ wg) * (h @ wu)) @ wd


def moe_swiglu(h, w_router, wg, wu, wd):
    def per_sample(t):
        logits = (t @ w_router).astype(jnp.float32)
        top_v, top_i = lax.top_k(logits, TOP_K)
        wts = jax.nn.softmax(top_v, axis=-1)
        combine = jnp.einsum('tk,tke->te', wts, jax.nn.one_hot(top_i, N_EXPERTS, dtype=jnp.float32)).astype(t.dtype)
        hid = jax.nn.silu(jnp.einsum('td,edf->tef', t, wg)) * jnp.einsum('td,edf->tef', t, wu)
        return jnp.einsum('tef,efd->td', hid * combine[:, :, None], wd)
    return lax.map(per_sample, h)


def setup_inputs(seed: int = 0) -> dict:
    key = jax.random.key(seed)
    ks = jax.random.split(key, 24)
    nrm = lambda k, shape, scale: jax.random.normal(k, shape, jnp.float32) * scale
    D = D_MODEL
    return {
        "x": nrm(ks[0], (BATCH, SEQ, D), 1.0),
        "c": nrm(ks[1], (BATCH, D), 1.0),
        "ctx": nrm(ks[2], (BATCH, CTX_LEN, D), 1.0),
        "c_ctx": nrm(ks[3], (D,), 1.0),
        "w_ada": nrm(ks[4], (DEPTH, D, 6 * D), 0.5 * D ** -0.5),
        "b_ada": nrm(ks[5], (DEPTH, 6 * D), 0.02),
        "g_pre_mix": 1.0 + nrm(ks[6], (DEPTH, D), 0.02),
        "g_post_mix": 1.0 + nrm(ks[7], (DEPTH, D), 0.02),
        "g_pre_ffn": 1.0 + nrm(ks[8], (DEPTH, D), 0.02),
        "g_post_ffn": 1.0 + nrm(ks[9], (DEPTH, D), 0.02),
        "w_in": nrm(ks[10], (DEPTH, D, PROJ_TOTAL), D ** -0.5),
        "gla_w_gate": nrm(ks[11], (DEPTH, 2, GLA_GATE_RANK, GLA_KDIM), GLA_GATE_RANK ** -0.5),
        "gla_b_gate": nrm(ks[12], (DEPTH, 2, GLA_KDIM), 0.1),
        "gla_g_norm": 1.0 + nrm(ks[13], (DEPTH, GLA_DV), 0.02),
        "na_rpb": nrm(ks[14], (DEPTH, NA_HEADS, 2 * NA_WIN_H - 1, 2 * NA_WIN_W - 1), 0.1),
        "w_out": nrm(ks[15], (DEPTH, MIX_WIDTH, D), MIX_WIDTH ** -0.5),
        "ffn_w_gate": nrm(ks[16], (N_DENSE, D, D_FF), D ** -0.5),
        "ffn_w_up": nrm(ks[17], (N_DENSE, D, D_FF), D ** -0.5),
        "ffn_w_down": nrm(ks[18], (N_DENSE, D_FF, D), D_FF ** -0.5),
        "moe_w_router": nrm(ks[19], (N_MOE, D, N_EXPERTS), D ** -0.5),
        "moe_w_gate": nrm(ks[20], (N_MOE, N_EXPERTS, D, D_FF_EXPERT), D ** -0.5),
        "moe_w_up": nrm(ks[21], (N_MOE, N_EXPERTS, D, D_FF_EXPERT), D ** -0.5),
        "moe_w_down": nrm(ks[22], (N_MOE, N_EXPERTS, D_FF_EXPERT, D), D_FF_EXPERT ** -0.5),
    }


def reference(x, c, ctx, c_ctx, w_ada, b_ada, g_pre_mix, g_post_mix, g_pre_ffn, g_post_ffn, w_in,
              gla_w_gate, gla_b_gate, gla_g_norm, na_rpb, w_out, ffn_w_gate, ffn_w_up, ffn_w_down,
              moe_w_router, moe_w_gate, moe_w_up, moe_w_down):
    def channel_mixer(i, h):
        j = i // 2
        if i % 2 == 0:
            return swiglu(h, ffn_w_gate[j], ffn_w_up[j], ffn_w_down[j])
        return moe_swiglu(h, moe_w_router[j], moe_w_gate[j], moe_w_up[j], moe_w_down[j])

    for i in range(DEPTH):
        last = i == DEPTH - 1
        sh1, sc1, gt1, sh2, sc2, gt2 = [m[:, None, :] for m in modulation(c, w_ada[i], b_ada[i])]
        csh1, csc1, cgt1, csh2, csc2, cgt2 = modulation(c_ctx, w_ada[i], b_ada[i])
        h = rmsnorm(x, g_pre_mix[i]) * (1.0 + sc1) + sh1
        hc = rmsnorm(ctx, g_pre_mix[i]) * (1.0 + csc1) + csh1
        y, yc = mixer(h, hc, w_in[i], w_out[i], gla_w_gate[i], gla_b_gate[i], gla_g_norm[i], na_rpb[i], not last)
        x = x + gt1 * rmsnorm(y, g_post_mix[i])
        h = rmsnorm(x, g_pre_ffn[i]) * (1.0 + sc2) + sh2
        x = x + gt2 * rmsnorm(channel_mixer(i, h), g_post_ffn[i])
        if not last:
            ctx = ctx + cgt1 * rmsnorm(yc, g_post_mix[i])
            hc = rmsnorm(ctx, g_pre_ffn[i]) * (1.0 + csc2) + csh2
            ctx = ctx + cgt2 * rmsnorm(channel_mixer(i, hc), g_post_ffn[i])
    return x
```

```python
import numpy as np
from contextlib import ExitStack
import concourse.bass as bass
import concourse.mybir as mybir
from concourse.bass_utils import run_bass_kernel_spmd

F32 = mybir.dt.float32
BF16 = mybir.dt.bfloat16
AF = mybir.ActivationFunctionType
ALU = mybir.AluOpType
AX = mybir.AxisListType

D = 1024
NB = 2
LCTX = 256
LLAT = 4096
LT = LCTX + LLAT
NT = LT // 128
PROJ = 3104
EPS = 1e-6
NEG = -30000.0

ENGS = ['pe', 'act', 'dve', 'pool', 'sp']
EPOCH = 30000


class Buf:
    __slots__ = ('name', 'w', 'r', 'dsem')

    def __init__(self, name=''):
        self.name = name
        self.w = None
        self.r = {}
        self.dsem = None


class Sched:
    def __init__(self, nc, stack):
        self.nc = nc
        self.stack = stack
        self.cnt = {e: 0 for e in ENGS}
        self.esems = {e: [] for e in ENGS}
        self.items = {e: [] for e in ENGS}
        self.waited = {e: {} for e in ENGS}
        self.free_dsems = []
        self.phase_bufs = []
        self.outstanding = {}
        self.nsem = 0
        self.ninstr = 0
        self.cregs = {}
        self.bregs = {}
        self.bounds = []
        self._cond = None

    def _newsem(self, name):
        self.nsem += 1
        return self.stack.enter_context(self.nc.semaphore(name))

    def _esem(self, e, seq):
        ep = (seq - 1) // EPOCH
        while len(self.esems[e]) <= ep:
            self.esems[e].append(self._newsem(f"s_{e}_{len(self.esems[e])}"))
        return self.esems[e][ep], (seq - 1) % EPOCH + 1

    def buf(self, name=''):
        b = Buf(name)
        self.phase_bufs.append(b)
        return b

    def bufs(self, n, name=''):
        return [self.buf(f"{name}{i}") for i in range(n)]

    def op(self, eng, fn, reads=(), writes=(), dma=None, self_ok=False, track=True):
        deps = {}

        def add(p):
            if p is None:
                return
            sem, val, peng = p
            if self_ok and peng == eng:
                return
            k = sem.num
            if k not in deps or deps[k][1] < val:
                deps[k] = (sem, val)

        for b in reads:
            add(b.w)
        for b in writes:
            add(b.w)
            for p in b.r.values():
                add(p)
        if dma is None:
            self.cnt[eng] += 1
            sem, val = self._esem(eng, self.cnt[eng])
            inc = 1
            tok = (sem, val, eng)
        else:
            if dma.dsem is None:
                if self.free_dsems:
                    dma.dsem = self.free_dsems.pop()
                else:
                    dma.dsem = [self._newsem(f"d{self.nsem}"), 0]
            dma.dsem[1] += 16
            sem, val = dma.dsem[0], dma.dsem[1]
            inc = 16
            tok = (sem, val, 'dma')
        waits = []
        wd = self.waited[eng]
        for k, (s, v) in deps.items():
            if wd.get(k, 0) >= v:
                continue
            wd[k] = v
            waits.append((s, v))
        self.items[eng].append((fn, waits, sem, inc, val))
        self.ninstr += 1
        for b in writes:
            b.w = tok
            b.r = {}
        for b in reads:
            if b not in writes:
                b.r[sem.num] = tok
        if track:
            self.outstanding[sem.num] = (sem, val)
        return tok

    def breg(self, engine, value):
        if value not in self.bregs:
            r = engine.alloc_register(f"bnd_{value}")
            engine.reg_mov(r, value)
            self.bregs[value] = r
        return self.bregs[value]

    def cond_begin(self, flag_ap):
        self._cond = {'flag': flag_ap, 'start': {e: len(self.items[e]) for e in ENGS},
                      'waited': {e: dict(self.waited[e]) for e in ENGS}}
        for e in ENGS:
            self.items[e].append(('cond_begin', flag_ap))

    def cond_end(self):
        c = self._cond
        for e in ENGS:
            body = self.items[e][c['start'][e] + 1:]
            agg = {}
            for it in body:
                fn, waits, sem, inc = it[0], it[1], it[2], it[3]
                if fn is None or sem is None:
                    continue
                k = sem.num
                if k not in agg:
                    agg[k] = [sem, it[4] - inc, 0]
                agg[k][2] += inc
            self.items[e].append(('cond_end', list(agg.values())))
            self.waited[e] = c['waited'][e]
        self._cond = None

    def barrier(self):
        for e in ENGS:
            waits = []
            for k, (s, v) in self.outstanding.items():
                if self.waited[e].get(k, 0) >= v:
                    continue
                self.waited[e][k] = v
                waits.append((s, v))
            self.items[e].append((None, waits, None, 0, 0))
        self.outstanding = {}

    def flush(self):
        self.barrier()
        nc = self.nc
        with nc.Block() as block:
            regs = {'pe': block.tensor, 'act': block.scalar, 'dve': block.vector,
                    'pool': block.gpsimd, 'sp': block.sync}
            for e in ENGS:
                items = self.items[e]

                def body(engine, items=items, e=e):
                    guard = None
                    if e == 'pool':
                        for v in self.bounds:
                            self.breg(engine, v)
                    for it in items:
                        if it[0] == 'cond_begin':
                            if e not in self.cregs:
                                self.cregs[e] = engine.alloc_register(f"creg_{e}")
                            reg = self.cregs[e]
                            engine.reg_load(reg, it[1])
                            guard = engine.If_ne(reg, 0)
                            guard.__enter__()
                            continue
                        if it[0] == 'cond_end':
                            guard.__exit__(None, None, None)
                            eg = engine.Else()
                            eg.__enter__()
                            for sem, pre, tot in it[1]:
                                if pre > 0:
                                    engine.wait_ge(sem, pre)
                                engine.sem_inc(sem, tot)
                            eg.__exit__(None, None, None)
                            guard = None
                            continue
                        fn, waits, sem, inc, _ = it
                        for s, v in waits:
                            engine.wait_ge(s, v)
                        if fn is not None:
                            ins = fn(engine)
                            ins.then_inc(sem, inc)

                regs[e](body)
        self.items = {e: [] for e in ENGS}
        for b in self.phase_bufs:
            if b.dsem is not None:
                self.free_dsems.append(b.dsem)
                b.dsem = None
        self.phase_bufs = []


class Rot:
    def __init__(self, S, mk, n, name):
        self.t = [mk(f"{name}{i}") for i in range(n)]
        self.b = [S.buf(f"{name}{i}") for i in range(n)]
        self.i = 0

    def next(self):
        k = self.i % len(self.t)
        self.i += 1
        return self.t[k], self.b[k]


class Phase:
    _uid = [0]

    def __init__(self, C, name):
        self.C = C
        self.nc = C.nc
        self.S = C.S
        self.st = ExitStack()
        self.name = name

    def _nm(self, nm):
        Phase._uid[0] += 1
        return f"{self.name}_{nm}_{Phase._uid[0]}"

    def sb(self, shape, dt, nm='t'):
        return self.st.enter_context(self.nc.sbuf_tensor(self._nm(nm), list(shape), dt))

    def ps(self, shape, dt, nm='p'):
        return self.st.enter_context(self.nc.psum_tensor(self._nm(nm), list(shape), dt))

    def sbb(self, shape, dt, nm='t'):
        return self.sb(shape, dt, nm), self.S.buf(nm)

    def rot(self, n, shape, dt, nm, psum=False):
        f = self.ps if psum else self.sb
        return Rot(self.S, lambda s: f(shape, dt, nm), n, nm)

    def close(self):
        self.S.flush()
        self.st.close()


class Ctx:
    pass


def _dram_in(nc, name, shape, dt=F32):
    return nc.dram_tensor(name, list(shape), dt, kind="ExternalInput").ap()


def _dram_tmp(nc, name, shape, dt, dbg=False):
    return nc.dram_tensor(name, list(shape), dt, kind="ExternalOutput" if dbg else "Internal").ap()


import os as _os
USE_TTR = False

O_Q, O_K, O_V, O_R, O_LR, O_NQ, O_NK, O_NV = 0, 256, 512, 1024, 1536, 1568, 2080, 2592
F_FFN = 2816
F_MOE = 3584
NE = 8


def prenorm_tile(Ph, K, xt, bx, hT, bhT, c0, Gt, St, bGS, j, h32=None, bh32=None):
    S = Ph.S
    C = Ph.C
    junk, bj = K['junk'].next()
    st, bst = K['stat'].next()
    xn, bxn = K['xn'].next()
    ptr, bptr = K['ptr'].next()
    S.op('act', lambda e: e.activation(out=junk[:], in_=xt[:], func=AF.Square, accum_out=st[:, 0:1]),
         reads=[bx], writes=[bj, bst])
    S.op('dve', lambda e: e.tensor_scalar(out=st[:, 1:2], in0=st[:, 0:1], scalar1=1.0 / D, scalar2=EPS,
                                          op0=ALU.mult, op1=ALU.add), reads=[bst], writes=[bst])
    S.op('pool', lambda e: e.tensor_tensor(out=st[:, 2:3], in0=st[:, 1:2], in1=C.neghalf[:, 0:1], op=ALU.pow),
         reads=[bst], writes=[bst])
    S.op('act', lambda e: e.activation(out=xn[:], in_=xt[:], func=AF.Copy, scale=st[:, 2:3]),
         reads=[bx, bst], writes=[bxn])

    def tr(e):
        for k in range(8):
            ins = e.transpose(out=ptr[:, k, :], in_=xn[:, k * 128:(k + 1) * 128], identity=C.ident[:])
        return ins
    S.op('pe', tr, reads=[bxn], writes=[bptr], self_ok=True)
    for k in range(8):
        if k % 2 == 0:
            S.op('dve', lambda e, k=k: e.tensor_scalar(out=hT[:, k, c0:c0 + 128], in0=ptr[:, k, :],
                                                       scalar1=Gt[:, k, j:j + 1], scalar2=St[:, k, j:j + 1],
                                                       op0=ALU.mult, op1=ALU.add),
                 reads=[bptr, bGS], writes=[bhT])
        else:
            S.op('act', lambda e, k=k: e.activation(out=hT[:, k, c0:c0 + 128], in_=ptr[:, k, :], func=AF.Identity,
                                                    scale=Gt[:, k, j:j + 1], bias=St[:, k, j:j + 1]),
                 reads=[bptr, bGS], writes=[bhT])
    if h32 is not None:
        xn32, bxn32 = K['xn32'].next()
        p32, bp32 = K['p32'].next()
        S.op('act', lambda e: e.activation(out=xn32[:], in_=xt[:], func=AF.Copy, scale=st[:, 2:3]),
             reads=[bx, bst], writes=[bxn32])

        def tr32(e):
            for k in range(8):
                ins = e.transpose(out=p32[:, k, :], in_=xn32[:, k * 128:(k + 1) * 128], identity=C.ident32[:])
            return ins
        S.op('pe', tr32, reads=[bxn32], writes=[bp32], self_ok=True)
        for k in range(8):
            S.op('dve', lambda e, k=k: e.tensor_scalar(out=h32[:, k, :], in0=p32[:, k, :],
                                                       scalar1=Gt[:, k, j:j + 1], scalar2=St[:, k, j:j + 1],
                                                       op0=ALU.mult, op1=ALU.add),
                 reads=[bp32, bGS], writes=[bh32])


def prenorm_kit(Ph, with32=False):
    K = {
        'junk': Ph.rot(1, [128, D], BF16, 'junk'),
        'stat': Ph.rot(4, [128, 4], F32, 'stat'),
        'xn': Ph.rot(2, [128, D], BF16, 'xn'),
        'ptr': Ph.rot(2, [128, 8, 128], BF16, 'ptr', psum=True),
    }
    if with32:
        K['xn32'] = Ph.rot(2, [128, D], F32, 'xn32')
        K['p32'] = Ph.rot(1, [128, 8, 128], F32, 'p32', psum=True)
    return K


def res_src(C, l, stage, b, tile):
    if l == 0 and stage == 0:
        if tile < 2:
            return C.ctx_in[b, tile * 128:(tile + 1) * 128, :]
        return C.x_in[b, (tile - 2) * 128:(tile - 1) * 128, :]
    return C.xs[b, tile * 128:(tile + 1) * 128, :]


def phase_mod(C, l, LP):
    nc, S = C.nc, C.S
    Ph = Phase(C, f"mod{l}")
    C.G1, C.bG1 = LP.sbb([128, 8, 3], F32, 'G1')
    C.SH1 = LP.sb([128, 8, 3], F32, 'SH1')
    C.G2, C.bG2 = LP.sbb([128, 8, 3], F32, 'G2')
    C.SH2 = LP.sb([128, 8, 3], F32, 'SH2')
    C.GG1, C.bGG1 = LP.sbb([128, 3, D], F32, 'GG1')
    C.GG2, C.bGG2 = LP.sbb([128, 3, D], F32, 'GG2')
    if getattr(C, 'want_rows', False):
        C.G2row, C.bG2row = LP.sbb([128, 2, D], F32, 'G2row')
        C.S2row = LP.sb([128, 2, D], F32, 'S2row')
    scT, bscT = Ph.sbb([128, 8, 3], F32, 'scT')
    scB, bscB = Ph.sbb([128, 3, 8, 128], F32, 'scB')
    bada, bbada = Ph.sbb([128, 48, 3], F32, 'bada')
    gpre, bgpre = Ph.sbb([128, 2, 8, 3], F32, 'gpre')
    rowc, browc = Ph.sbb([128, 7, D], F32, 'rowc')
    sc1, bsc1 = Ph.sbb([128, 8, 3], F32, 'sc1')
    sc2, bsc2 = Ph.sbb([128, 8, 3], F32, 'sc2')
    wblk = Ph.rot(2, [128, 8, 1024], F32, 'wblk')
    pm = Ph.rot(2, [128, 8, 3], F32, 'pm', psum=True)
    pg = Ph.rot(2, [128, 512], F32, 'pg', psum=True)
    S.op('sp', lambda e: e.dma_start(out=scT[:], in_=C.cT[:, :, :]), writes=[bscT], dma=bscT)
    S.op('sp', lambda e: e.dma_start(out=bada[:], in_=C.badaT3[l]), writes=[bbada], dma=bbada)
    S.op('sp', lambda e: e.dma_start(out=gpre[:], in_=C.gpre3[l]), writes=[bgpre], dma=bgpre)
    S.op('sp', lambda e: e.dma_start(out=rowc[:], in_=C.rowc[l]), writes=[browc], dma=browc)
    S.op('act', lambda e: e.activation(out=scT[:], in_=scT[:], func=AF.Silu), reads=[bscT], writes=[bscT])
    for j in range(3):
        for kc in range(8):
            S.op('act', lambda e, j=j, kc=kc: e.activation(out=scB[:, j, kc, :], in_=C.ones[:], func=AF.Copy,
                                                           scale=scT[:, kc, j:j + 1]),
                 reads=[bscT], writes=[bscB])
    fm = [(0, C.SH1, C.bG1), (1, sc1, bsc1), (3, C.SH2, C.bG2), (4, sc2, bsc2)]
    for blk, dst, bdst in fm:
        wt, bw = wblk.next()
        S.op('sp', lambda e, wt=wt, blk=blk: e.dma_start(
            out=wt[:], in_=C.w_ada[l, :, blk * 1024:(blk + 1) * 1024].rearrange("(kc p) n -> p kc n", p=128)),
            writes=[bw], dma=bw)
        pmt, bpm = pm.next()

        def mm(e, wt=wt, pmt=pmt):
            for ch in range(8):
                for kc in range(8):
                    ins = e.matmul(pmt[:, ch, :], lhsT=wt[:, kc, ch * 128:(ch + 1) * 128], rhs=scT[:, kc, :],
                                   start=(kc == 0), stop=(kc == 7))
            return ins
        S.op('pe', mm, reads=[bw, bscT], writes=[bpm], self_ok=True)
        S.op('dve', lambda e, dst=dst, pmt=pmt, blk=blk: e.tensor_tensor(
            out=dst[:], in0=pmt[:], in1=bada[:, blk * 8:(blk + 1) * 8, :], op=ALU.add),
            reads=[bpm, bbada], writes=[bdst])
    S.op('dve', lambda e: e.scalar_tensor_tensor(out=C.G1[:], in0=sc1[:], scalar=1.0, in1=gpre[:, 0], op0=ALU.add,
                                                 op1=ALU.mult), reads=[bsc1, bgpre], writes=[C.bG1])
    S.op('dve', lambda e: e.scalar_tensor_tensor(out=C.G2[:], in0=sc2[:], scalar=1.0, in1=gpre[:, 1], op0=ALU.add,
                                                 op1=ALU.mult), reads=[bsc2, bgpre], writes=[C.bG2])
    for gi, blk, GG, bGG in [(0, 2, C.GG1, C.bGG1), (1, 5, C.GG2, C.bGG2)]:
        wt, bw = wblk.next()
        S.op('sp', lambda e, wt=wt, blk=blk: e.dma_start(
            out=wt[:], in_=C.w_ada[l, :, blk * 1024:(blk + 1) * 1024].rearrange("(kc p) n -> p kc n", p=128)),
            writes=[bw], dma=bw)
        for j in range(3):
            for half in range(2):
                pgt, bpg = pg.next()
                hs = slice(half * 512, (half + 1) * 512)

                def mm(e, wt=wt, pgt=pgt, j=j, hs=hs):
                    for kc in range(8):
                        ins = e.matmul(pgt[:], lhsT=scB[:, j, kc, :], rhs=wt[:, kc, hs], start=(kc == 0),
                                       stop=(kc == 7))
                    return ins
                S.op('pe', mm, reads=[bw, bscB], writes=[bpg], self_ok=True)
                S.op('dve', lambda e, GG=GG, pgt=pgt, j=j, hs=hs, gi=gi: e.tensor_tensor(
                    out=GG[:, j, hs], in0=pgt[:], in1=rowc[:, gi, hs], op=ALU.add), reads=[bpg, browc], writes=[bGG])
                S.op('pool', lambda e, GG=GG, j=j, hs=hs, gi=gi: e.tensor_tensor(
                    out=GG[:, j, hs], in0=GG[:, j, hs], in1=rowc[:, 2 + gi, hs], op=ALU.mult),
                    reads=[bGG, browc], writes=[bGG])
    if getattr(C, 'want_rows', False):
        for blk, dst, ri in [(3, C.S2row, 4), (4, C.G2row, 5)]:
            wt, bw = wblk.next()
            S.op('sp', lambda e, wt=wt, blk=blk: e.dma_start(
                out=wt[:], in_=C.w_ada[l, :, blk * 1024:(blk + 1) * 1024].rearrange("(kc p) n -> p kc n", p=128)),
                writes=[bw], dma=bw)
            for j in range(2):
                for half in range(2):
                    pgt, bpg = pg.next()
                    hs = slice(half * 512, (half + 1) * 512)

                    def mm(e, wt=wt, pgt=pgt, j=j, hs=hs):
                        for kc in range(8):
                            ins = e.matmul(pgt[:], lhsT=scB[:, j, kc, :], rhs=wt[:, kc, hs], start=(kc == 0),
                                           stop=(kc == 7))
                        return ins
                    S.op('pe', mm, reads=[bw, bscB], writes=[bpg], self_ok=True)
                    S.op('dve', lambda e, dst=dst, pgt=pgt, j=j, hs=hs, ri=ri: e.tensor_tensor(
                        out=dst[:, j, hs], in0=pgt[:], in1=rowc[:, ri, hs], op=ALU.add), reads=[bpg, browc],
                        writes=[C.bG2row])
                    if blk == 4:
                        S.op('dve', lambda e, dst=dst, j=j, hs=hs: e.scalar_tensor_tensor(
                            out=dst[:, j, hs], in0=dst[:, j, hs], scalar=1.0, in1=rowc[:, 6, hs], op0=ALU.add,
                            op1=ALU.mult), reads=[C.bG2row, browc], writes=[C.bG2row])
    Ph.close()


def phase_proj(C, l):
    nc, S = C.nc, C.S
    Ph = Phase(C, f"proj{l}")
    K = prenorm_kit(Ph)
    win, bwin = Ph.sbb([128, 8, PROJ], BF16, 'win')
    cos, bcos = Ph.sbb([128, 32, 64], F32, 'cos')
    sin, bsin = Ph.sbb([128, 32, 64], F32, 'sin')
    npc = 4
    pw = PROJ // npc
    for i in range(npc):
        S.op('pool', lambda e, i=i: e.dma_start(
            out=win[:, :, i * pw:(i + 1) * pw],
            in_=C.w_in[l, :, i * pw:(i + 1) * pw].rearrange("(kc p) n -> p kc n", p=128)), writes=[bwin], dma=bwin)
    S.op('sp', lambda e: e.dma_start(out=cos[:], in_=C.rope_cos[:, :, :]), writes=[bcos], dma=bcos)
    S.op('sp', lambda e: e.dma_start(out=sin[:], in_=C.rope_sin[:, :, :]), writes=[bsin], dma=bsin)
    S.op('dve', lambda e: e.tensor_scalar(out=win[:, :, O_Q:O_Q + 256], in0=win[:, :, O_Q:O_Q + 256], scalar1=0.125,
                                          scalar2=None, op0=ALU.mult), reads=[bwin], writes=[bwin])
    S.op('dve', lambda e: e.tensor_scalar(out=win[:, :, O_NQ:O_NQ + 512], in0=win[:, :, O_NQ:O_NQ + 512],
                                          scalar1=0.125, scalar2=None, op0=ALU.mult), reads=[bwin], writes=[bwin])
    xr = Ph.rot(3, [128, D], F32, 'x')
    hTr = Ph.rot(2, [128, 8, 256], BF16, 'hT')
    stg = Ph.rot(2, [128, 2048], BF16, 'stg')
    fstg = Ph.rot(2, [128, 8, 256], BF16, 'fstg')
    lstg = Ph.rot(2, [16, 2, 256], F32, 'lstg')
    t1r = Ph.rot(2, [128, 512], F32, 't1')
    t2r = Ph.rot(2, [128, 512], F32, 't2')
    ptok = Ph.rot(2, [128, 512], F32, 'ptok', psum=True)
    pfe = Ph.rot(2, [128, 512], F32, 'pfe', psum=True)
    tokcols = [(O_Q, O_Q + 512), (O_V, O_V + 512), (O_R, O_R + 512), (O_NV, O_NV + 512)]
    for b in range(NB):
        for g in range(LT // 256):
            j = 2 if g == 0 else b
            hT, bhT = hTr.next()
            for t in range(2):
                tile = g * 2 + t
                xt, bx = xr.next()
                S.op('sp', lambda e, xt=xt, tile=tile, b=b: e.dma_start(out=xt[:], in_=res_src(C, l, 0, b, tile)),
                     writes=[bx], dma=bx)
                prenorm_tile(Ph, K, xt, bx, hT, bhT, t * 128, C.G1, C.SH1, C.bG1, j)
            for t in range(2):
                tile = g * 2 + t
                st, bst = stg.next()
                for cb in range(4):
                    pt_, bpt = ptok.next()
                    c0, c1 = tokcols[cb]

                    def mm(e, pt_=pt_, t=t, c0=c0, c1=c1, hT=hT):
                        for kc in range(8):
                            ins = e.matmul(pt_[:], lhsT=hT[:, kc, t * 128:(t + 1) * 128], rhs=win[:, kc, c0:c1],
                                           start=(kc == 0), stop=(kc == 7))
                        return ins
                    S.op('pe', mm, reads=[bhT, bwin], writes=[bpt], self_ok=True)
                    so = st[:, cb * 512:(cb + 1) * 512]
                    if cb == 0 and g > 0:
                        lt = tile - 2
                        t1, bt1 = t1r.next()
                        t2, bt2 = t2r.next()
                        cb_ = cos[:, lt, :].unsqueeze(1).to_broadcast([128, 8, 64])
                        p3 = pt_[:].rearrange("p (a d) -> p a d", a=8)
                        S.op('dve', lambda e, t1=t1, p3=p3, cb_=cb_: e.tensor_tensor(
                            out=t1[:].rearrange("p (a d) -> p a d", a=8), in0=p3, in1=cb_, op=ALU.mult),
                            reads=[bpt, bcos], writes=[bt1])
                        p5 = pt_[:].rearrange("p (a b c d) -> p a b c d", a=8, b=2, c=2)
                        s5 = sin[:, lt, :].rearrange("p (b c d) -> p b c d", b=2, c=2)
                        t25 = t2[:].rearrange("p (a b c d) -> p a b c d", a=8, b=2, c=2)
                        for hf in range(2):
                            sb_ = s5[:, :, hf, :].unsqueeze(1).to_broadcast([128, 8, 2, 16])
                            S.op('dve', lambda e, t25=t25, p5=p5, sb_=sb_, hf=hf: e.tensor_tensor(
                                out=t25[:, :, :, hf, :], in0=p5[:, :, :, 1 - hf, :], in1=sb_, op=ALU.mult),
                                reads=[bpt, bsin], writes=[bt2])
                        S.op('pool', lambda e, so=so, t1=t1, t2=t2: e.tensor_tensor(out=so, in0=t1[:], in1=t2[:],
                                                                                   op=ALU.add),
                             reads=[bt1, bt2], writes=[bst])
                    elif cb == 2:
                        S.op('act', lambda e, so=so, pt_=pt_: e.activation(out=so, in_=pt_[:], func=AF.Silu),
                             reads=[bpt], writes=[bst])
                    else:
                        S.op('act', lambda e, so=so, pt_=pt_: e.activation(out=so, in_=pt_[:], func=AF.Copy),
                             reads=[bpt], writes=[bst])
                S.op('sp', lambda e, st=st, tile=tile, b=b: e.dma_start(
                    out=C.tokmaj[b, tile * 128:(tile + 1) * 128, :], in_=st[:]), reads=[bst], dma=bst)
            ft, bft = fstg.next()
            for cc in range(8):
                pf, bpf = pfe.next()
                c0 = O_NQ + cc * 128

                def mmf(e, pf=pf, c0=c0, hT=hT):
                    for kc in range(8):
                        ins = e.matmul(pf[:, 0:256], lhsT=win[:, kc, c0:c0 + 128], rhs=hT[:, kc, :], start=(kc == 0),
                                       stop=(kc == 7))
                    return ins
                S.op('pe', mmf, reads=[bhT, bwin], writes=[bpf], self_ok=True)
                S.op('dve', lambda e, ft=ft, pf=pf, cc=cc: e.tensor_copy(out=ft[:, cc, :], in_=pf[:, 0:256]),
                     reads=[bpf], writes=[bft])
            S.op('sp', lambda e, ft=ft, g=g, b=b: e.dma_start(
                out=C.nqkT[b].rearrange("(cc p) t -> p cc t", p=128)[:, :, g * 256:(g + 1) * 256], in_=ft[:]),
                reads=[bft], dma=bft)
            lt_, blt = lstg.next()
            for d in range(2):
                pf, bpf = pfe.next()
                c0 = O_LR + 16 * d

                def mml(e, pf=pf, c0=c0, hT=hT):
                    for kc in range(8):
                        ins = e.matmul(pf[0:16, 0:256], lhsT=win[:, kc, c0:c0 + 16], rhs=hT[:, kc, :],
                                       start=(kc == 0), stop=(kc == 7))
                    return ins
                S.op('pe', mml, reads=[bhT, bwin], writes=[bpf], self_ok=True)
                S.op('dve', lambda e, lt_=lt_, pf=pf, d=d: e.tensor_copy(out=lt_[:, d, :], in_=pf[0:16, 0:256]),
                     reads=[bpf], writes=[blt])
            S.op('sp', lambda e, lt_=lt_, g=g, b=b: e.dma_start(
                out=C.lrT[b].rearrange("d r t -> r d t")[:, :, g * 256:(g + 1) * 256], in_=lt_[:]),
                reads=[blt], dma=blt)
    Ph.close()


def phase_gla(C, l, last):
    S = C.S
    Ph = Phase(C, f"gla{l}")
    tri, btri = Ph.sbb([128, 4, 128], F32, 'tri')
    wg, bwg = Ph.sbb([16, 2, 256], F32, 'wg')
    bg, bbg = Ph.sbb([1, 2, 256], F32, 'bg')
    gn, bgn = Ph.sbb([128, 512], F32, 'gn')
    S.op('sp', lambda e: e.dma_start(out=tri[:], in_=C.tri_in[:, :, :]), writes=[btri], dma=btri)
    S.op('sp', lambda e: e.dma_start(out=wg[:], in_=C.wgate[l].rearrange("d r n -> r d n")), writes=[bwg], dma=bwg)
    S.op('sp', lambda e: e.dma_start(out=bg[:], in_=C.bgate[l].rearrange("d o n -> o d n")), writes=[bbg], dma=bbg)
    S.op('sp', lambda e: e.dma_start(out=gn[:], in_=C.gnormB[l]), writes=[bgn], dma=bgn)
    lrr = Ph.rot(1, [16, 2, LT], F32, 'lr')
    ost = Ph.sb([128, NT, 512], F32, 'ost')
    bost = [S.buf(f"ost{i}") for i in range(NT)]
    Sst = [Ph.sbb([128, 2, 128], F32, 'Sst') for _ in range(2)]
    Sbf = [Ph.sbb([128, 2, 128], BF16, 'Sbf') for _ in range(2)]
    qkvr = Ph.rot(4, [128, 1024], BF16, 'qkv')
    rgr = Ph.rot(2, [128, 512], BF16, 'rg')
    e1r = Ph.rot(2, [128, 256], F32, 'e1')
    spr = Ph.rot(2, [128, 256], F32, 'sp')
    ebr = Ph.rot(2, [128, 2, 128], F32, 'eb')
    enbr = Ph.rot(2, [128, 2, 128], F32, 'enb')
    eEr = Ph.rot(2, [128, 256], F32, 'eE')
    qdr = Ph.rot(2, [128, 4, 128], BF16, 'qd')
    kdr = Ph.rot(2, [128, 4, 128], BF16, 'kd')
    for rr in (qdr, kdr):
        for t_, b_ in zip(rr.t, rr.b):
            S.op('pool', lambda e, t_=t_: e.memset(t_[:], 0.0), writes=[b_])
    ker = Ph.rot(2, [128, 256], BF16, 'kend')
    Amr = Ph.rot(2, [128, 4, 128], BF16, 'Am')
    osr = Ph.rot(2, [128, 512], F32, 'osum')
    sqr = Ph.rot(2, [128, 512], F32, 'sq')
    ogr = Ph.rot(2, [128, 512], BF16, 'og')
    str_ = Ph.rot(4, [128, 12], F32, 'gst')
    plr = Ph.rot(1, [128, 512], F32, 'pl', psum=True)
    pber = Ph.rot(1, [128, 512], F32, 'pbe', psum=True)
    pTr = Ph.rot(1, [128, 8, 128], BF16, 'pT', psum=True)
    pAr = Ph.rot(2, [128, 4, 128], F32, 'pA', psum=True)
    por = Ph.rot(2, [128, 4, 128], F32, 'po', psum=True)
    pdsr = Ph.rot(1, [128, 2, 256], F32, 'pds', psum=True)

    import os
    STG = int(os.environ.get('GLA_STG', '99'))
    NTL = int(os.environ.get('GLA_NT', str(NT)))

    def block(b, d, tile, first, lr):
        lrt, blr = lr
        rows_of = lambda hp: slice(hp * 64, hp * 64 + 64)
        r0 = tile * 128
        qkv, bqkv = qkvr.next()
        S.op('sp', lambda e: e.dma_start(out=qkv[:], in_=C.tokmaj[b, r0:r0 + 128, 0:1024]), writes=[bqkv], dma=bqkv)
        pl, bpl = plr.next()

        def mml(e):
            e.matmul(pl[:, 0:256], lhsT=lrt[:, d, r0:r0 + 128], rhs=wg[:, d, :], start=True, stop=False)
            return e.matmul(pl[:, 0:256], lhsT=C.ones[0:1, :], rhs=bg[0:1, d, :], start=False, stop=True)
        S.op('pe', mml, reads=[blr, bwg, bbg], writes=[bpl], self_ok=True)
        if STG < 2:
            return
        e1, be1 = e1r.next()
        sp_, bsp = spr.next()
        S.op('act', lambda e: e.activation(out=e1[:], in_=pl[:, 0:256], func=AF.Exp, scale=-1.0), reads=[bpl],
             writes=[be1])
        S.op('act', lambda e: e.activation(out=sp_[:], in_=e1[:], func=AF.Ln, bias=1.0), reads=[be1], writes=[bsp])
        if STG < 3:
            return
        Rm = tri[:, 0 if d == 0 else 1, :]
        Um = tri[:, 3 if d == 0 else 2, :]
        pbe, bpbe = pber.next()

        def mmb(e):
            for g in range(2):
                e.matmul(pbe[:, g * 128:(g + 1) * 128], lhsT=sp_[:, g * 128:(g + 1) * 128], rhs=Rm, start=True,
                         stop=True)
            return e.matmul(pbe[:, 256:512], lhsT=Um, rhs=sp_[:], start=True, stop=True)
        S.op('pe', mmb, reads=[bsp, btri], writes=[bpbe], self_ok=True)
        if STG < 4:
            return
        eb, beb = ebr.next()
        enb, benb = enbr.next()
        eE, beE = eEr.next()
        pb3 = pbe[:, 0:256].rearrange("p (g t) -> p g t", g=2)
        S.op('act', lambda e: e.activation(out=eb[:], in_=pb3, func=AF.Exp, scale=-1.0 / 16), reads=[bpbe],
             writes=[beb])
        S.op('act', lambda e: e.activation(out=enb[:], in_=pb3, func=AF.Exp, scale=1.0 / 16), reads=[bpbe],
             writes=[benb])
        S.op('act', lambda e: e.activation(out=eE[:], in_=pbe[:, 256:512], func=AF.Exp, scale=-1.0 / 16),
             reads=[bpbe], writes=[beE])
        if STG < 5:
            return
        pT, bpT = pTr.next()

        def tr(e):
            for i in range(4):
                ins = e.transpose(out=pT[:, i, :], in_=qkv[:, i * 128:(i + 1) * 128], identity=C.ident[:])
            return ins
        S.op('pe', tr, reads=[bqkv], writes=[bpT], self_ok=True)
        if STG < 6:
            return
        qd, bqd = qdr.next()
        kd, bkd = kdr.next()
        kend, bke = ker.next()
        for h in range(4):
            g, rs = h // 2, rows_of(h % 2)
            S.op('dve', lambda e, h=h, g=g, rs=rs: e.tensor_tensor(out=qd[rs, h, :], in0=pT[rs, g, :], in1=eb[rs, g, :],
                                                                   op=ALU.mult), reads=[bpT, beb], writes=[bqd])
            S.op('dve', lambda e, h=h, g=g, rs=rs: e.tensor_tensor(out=kd[rs, h, :], in0=pT[rs, 2 + g, :],
                                                                   in1=enb[rs, g, :], op=ALU.mult),
                 reads=[bpT, benb], writes=[bkd])
        S.op('pool', lambda e: e.tensor_tensor(out=kend[:], in0=qkv[:, 256:512], in1=eE[:], op=ALU.mult),
             reads=[bqkv, beE], writes=[bke])
        if STG < 7:
            return
        pA, bpA = pAr.next()

        def mmA(e):
            for h in range(4):
                ins = e.matmul(pA[:, h, :], lhsT=kd[:, h, :], rhs=qd[:, h, :], start=True, stop=True)
            return ins
        S.op('pe', mmA, reads=[bkd, bqd], writes=[bpA], self_ok=True)
        if STG < 8:
            return
        Am, bAm = Amr.next()
        mask = tri[:, 0 if d == 0 else 1, :].unsqueeze(1).to_broadcast([128, 4, 128])
        S.op('dve', lambda e: e.tensor_tensor(out=Am[:], in0=pA[:], in1=mask, op=ALU.mult), reads=[bpA, btri],
             writes=[bAm])
        if STG < 9:
            return
        po, bpo = por.next()
        sbf, bsbf = Sbf[d]

        def mmo(e):
            for h in range(4):
                g, rs = h // 2, rows_of(h % 2)
                e.matmul(po[:, h, :], lhsT=Am[:, h, :], rhs=qkv[:, 512 + h * 128:512 + (h + 1) * 128], start=True,
                         stop=False)
                ins = e.matmul(po[:, h, :], lhsT=qd[:, h, :], rhs=sbf[:, g, :], start=False, stop=True)
            return ins
        S.op('pe', mmo, reads=[bAm, bqkv, bqd, bsbf], writes=[bpo], self_ok=True)
        need_out = not (last and tile < 2)
        if STG < 10:
            return
        if need_out:
            if first:
                S.op('act', lambda e: e.activation(out=ost[:, tile, :], in_=po[:].rearrange("p h v -> p (h v)"),
                                                   func=AF.Copy), reads=[bpo], writes=[bost[tile]])
            else:
                osum, bos = osr.next()
                sq, bsq = sqr.next()
                og, bog = ogr.next()
                st, bst = str_.next()
                rg, brg = rgr.next()
                S.op('sp', lambda e: e.dma_start(out=rg[:], in_=C.tokmaj[b, r0:r0 + 128, 1024:1536]), writes=[brg],
                     dma=brg)
                S.op('dve', lambda e: e.tensor_tensor(out=osum[:], in0=po[:].rearrange("p h v -> p (h v)"),
                                                      in1=ost[:, tile, :], op=ALU.add), reads=[bpo, bost[tile]],
                     writes=[bos])
                S.op('pool', lambda e: e.tensor_tensor(out=sq[:], in0=osum[:], in1=osum[:], op=ALU.mult), reads=[bos],
                     writes=[bsq])
                S.op('dve', lambda e: e.tensor_reduce(out=st[:, 0:4], in_=sq[:].rearrange("p (h v) -> p h v", h=4),
                                                      axis=AX.X, op=ALU.add), reads=[bsq], writes=[bst])
                S.op('dve', lambda e: e.tensor_scalar(out=st[:, 4:8], in0=st[:, 0:4], scalar1=1.0 / 128, scalar2=EPS,
                                                      op0=ALU.mult, op1=ALU.add), reads=[bst], writes=[bst])
                S.op('pool', lambda e: e.tensor_tensor(out=st[:, 8:12], in0=st[:, 4:8], in1=C.neghalf[:, 0:4],
                                                       op=ALU.pow), reads=[bst], writes=[bst])
                S.op('dve', lambda e: e.tensor_tensor(
                    out=sq[:].rearrange("p (h v) -> p h v", h=4), in0=osum[:].rearrange("p (h v) -> p h v", h=4),
                    in1=st[:, 8:12].unsqueeze(2).to_broadcast([128, 4, 128]), op=ALU.mult), reads=[bos, bst],
                    writes=[bsq])
                S.op('pool', lambda e: e.tensor_tensor(out=sq[:], in0=sq[:], in1=gn[:], op=ALU.mult), reads=[bsq, bgn],
                     writes=[bsq])
                S.op('pool', lambda e: e.tensor_tensor(out=og[:], in0=sq[:], in1=rg[:], op=ALU.mult),
                     reads=[bsq, brg], writes=[bog])
                S.op('sp', lambda e: e.dma_start(out=C.cat[b, r0:r0 + 128, 0:512], in_=og[:]), reads=[bog], dma=bog)
        if STG < 11:
            return
        pds, bpds = pdsr.next()

        def mmds(e):
            for g in range(2):
                ins = e.matmul(pds[:, g, :], lhsT=kend[:, g * 128:(g + 1) * 128],
                               rhs=qkv[:, 512 + g * 256:512 + (g + 1) * 256], start=True, stop=True)
            return ins
        S.op('pe', mmds, reads=[bke, bqkv], writes=[bpds], self_ok=True)
        if STG < 12:
            return
        sst, bsst = Sst[d]
        dc = 127 if d == 0 else 0
        for g in range(2):
            for hp in range(2):
                rs = rows_of(hp)
                S.op('dve', lambda e, g=g, hp=hp, rs=rs: e.scalar_tensor_tensor(
                    out=sst[rs, g, :], in0=sst[rs, g, :], scalar=eb[rs, g, dc:dc + 1],
                    in1=pds[rs, g, hp * 128:(hp + 1) * 128], op0=ALU.mult, op1=ALU.add),
                    reads=[bsst, beb, bpds], writes=[bsst])
        S.op('act', lambda e: e.activation(out=sbf[:], in_=sst[:], func=AF.Copy), reads=[bsst], writes=[bsbf])

    for b in range(NB):
        lr = lrr.next()
        S.op('sp', lambda e, lr=lr, b=b: e.dma_start(out=lr[0][:], in_=C.lrT[b].rearrange("d r t -> r d t")),
             writes=[lr[1]], dma=lr[1])
        for d in range(2):
            S.op('pool', lambda e, d=d: e.memset(Sst[d][0][:], 0.0), writes=[Sst[d][1]])
            S.op('pool', lambda e, d=d: e.memset(Sbf[d][0][:], 0.0), writes=[Sbf[d][1]])
        orders = [list(range(NT)), [1, 0] + list(range(NT - 1, 1, -1))]
        done = set()
        for i in range(NTL):
            for d in range(2):
                tile = orders[d][i]
                block(b, d, tile, tile not in done, lr)
                done.add(tile)
    Ph.close()


NA_SHIFT = 0.0


def phase_na(C, l, last):
    S = C.S
    Ph = Phase(C, f"na{l}")
    kTr = Ph.rot(1, [128, 4, LT], BF16, 'kT')
    qTr = Ph.rot(2, [128, LT], BF16, 'qT')
    for t_, b_ in zip(qTr.t, qTr.b):
        S.op('pool', lambda e, t_=t_: e.memset(t_[:], 0.0), writes=[b_])
    Vr = Ph.rot(1, [128, NT, 8, 65], BF16, 'V')
    for t_, b_ in zip(Vr.t, Vr.b):
        S.op('pool', lambda e, t_=t_: e.memset(t_[:], 1.0), writes=[b_])
    vstr = Ph.rot(2, [128, 8, 512], BF16, 'vst')
    onar = Ph.rot(1, [128, NT, 512], BF16, 'ona')
    biasr = Ph.rot(1, [128, 5, 5, 128], F32, 'bias')
    ssr = Ph.rot(4, [128, 5, 128], F32, 's')
    pr = Ph.rot(4, [128, 7, 128], BF16, 'p')
    str_ = Ph.rot(8, [128, 4], F32, 'nst')
    negc, bnegc = Ph.sbb([128, 1], F32, 'negc')
    S.op('pool', lambda e: e.memset(negc[:], -NA_SHIFT), writes=[bnegc])
    psr = Ph.rot(3, [128, 8, 128], F32, 'ps', psum=True)
    po_t = Ph.ps([128, 512], F32, 'po')
    po_b = [S.buf(f"po{i}") for i in range(7)]
    po_i = [0]

    def unit(b, h, qt, kT, bkT, qT, bqT, V, bV, ona, bona, bias, bbias):
        g = h // 2
        q0 = qt * 128
        if qt >= 2:
            j = qt - 2
            ts = min(max(j - 2, 0), 27)
            pi = {0: 0, 1: 1, 30: 3, 31: 4}.get(j, 2)
            kcols = [256 + 128 * (ts + k) for k in range(5)] + [0, 128]
            vt = [2 + ts + k for k in range(5)] + [0, 1]
            nl = 5
        else:
            kcols = [0, 128]
            vt = [0, 1]
            nl = 0
        nblk = len(kcols)
        ps_, bps = psr.next()

        def mm(e):
            for kb in range(nblk):
                ins = e.matmul(ps_[:, kb, :], lhsT=kT[:, g, kcols[kb]:kcols[kb] + 128], rhs=qT[:, q0:q0 + 128],
                               start=True, stop=True)
            return ins
        S.op('pe', mm, reads=[bqT, bkT], writes=[bps], self_ok=True)
        p, bp = pr.next()
        if nl:
            s_, bs = ssr.next()
            S.op('dve', lambda e: e.tensor_tensor(out=s_[:], in0=ps_[:, 0:5, :], in1=bias[:, pi, :, :], op=ALU.add),
                 reads=[bps, bbias], writes=[bs])
            S.op('act', lambda e: e.activation(out=p[:, 0:5, :], in_=s_[:], func=AF.Exp, bias=negc[:, 0:1], scale=1.0),
                 reads=[bs, bnegc], writes=[bp])
        S.op('act', lambda e: e.activation(out=p[:, nl:nblk, :], in_=ps_[:, nl:nblk, :], func=AF.Exp,
                                           bias=negc[:, 0:1], scale=1.0), reads=[bps, bnegc], writes=[bp])
        slot = po_i[0] % 7
        po_i[0] += 1
        po = po_t[:, slot * 65:(slot + 1) * 65]
        bpo = po_b[slot]

        def mmpv(e):
            for kb in range(nblk):
                ins = e.matmul(po, lhsT=p[:, kb, :], rhs=V[:, vt[kb], h, :], start=(kb == 0), stop=(kb == nblk - 1))
            return ins
        S.op('pe', mmpv, reads=[bp, bV], writes=[bpo], self_ok=True)
        st, bst = str_.next()
        S.op('dve', lambda e: e.reciprocal(out=st[:, 0:1], in_=po[:, 64:65]), reads=[bpo], writes=[bst])
        S.op('act', lambda e: e.activation(out=ona[:, qt, h * 64:(h + 1) * 64], in_=po[:, 0:64], func=AF.Copy,
                                           scale=st[:, 0:1]), reads=[bpo, bst], writes=[bona])

    for b in range(NB):
        kT, bkT = kTr.next()
        V, bV = Vr.next()
        ona, bona = onar.next()
        for g in range(4):
            S.op('sp', lambda e, kT=kT, b=b, g=g: e.dma_start(
                out=kT[:, g, :], in_=C.nqkT[b, 512 + g * 128:512 + (g + 1) * 128, :]), writes=[bkT], dma=bkT)
        for t0 in range(0, NT, 8):
            t1 = min(NT, t0 + 8)
            vs, bvs = vstr.next()
            S.op('sp', lambda e, vs=vs, b=b, t0=t0, t1=t1: e.dma_start(
                out=vs[:, 0:t1 - t0, :],
                in_=C.tokmaj[b, t0 * 128:t1 * 128, 1536:2048].rearrange("(t p) c -> p t c", p=128)),
                writes=[bvs], dma=bvs)
            S.op('pool', lambda e, vs=vs, V=V, t0=t0, t1=t1: e.tensor_copy(
                out=V[:, t0:t1, :, 0:64], in_=vs[:, 0:t1 - t0, :].rearrange("p t (h d) -> p t h d", h=8)),
                reads=[bvs], writes=[bV])
        for h in range(8):
            bias, bbias = biasr.next()
            S.op('sp', lambda e, bias=bias, h=h: e.dma_start(out=bias[:], in_=C.natb[l, h]), writes=[bbias], dma=bbias)
            qT, bqT = qTr.next()
            hr = slice((h % 2) * 64, (h % 2) * 64 + 64)
            S.op('sp', lambda e, qT=qT, b=b, h=h, hr=hr: e.dma_start(
                out=qT[hr, :], in_=C.nqkT[b, h * 64:(h + 1) * 64, :]), writes=[bqT], dma=bqT)
            qts = list(range(2, NT)) + ([] if last else [0, 1])
            for qt in qts:
                unit(b, h, qt, kT, bkT, qT, bqT, V, bV, ona, bona, bias, bbias)
        for t0 in range(2 if last else 0, NT, 8):
            t1 = min(NT, t0 + 8)
            S.op('sp', lambda e, ona=ona, b=b, t0=t0, t1=t1: e.dma_start(
                out=C.cat[b, t0 * 128:t1 * 128, 512:1024].rearrange("(t p) c -> p t c", p=128), in_=ona[:, t0:t1, :]),
                reads=[bona], dma=bona)
    Ph.close()


def phase_outproj(C, l, last):
    S = C.S
    Ph = Phase(C, f"op{l}")
    wo, bwo = Ph.sbb([128, 8, D], BF16, 'wo')
    S.op('pool', lambda e: e.dma_start(out=wo[:], in_=C.w_out[l].rearrange("(kc p) n -> p kc n", p=128)),
         writes=[bwo], dma=bwo)
    ctr = Ph.rot(2, [128, D], BF16, 'ct')
    xr = Ph.rot(2, [128, D], F32, 'x')
    cTsr = Ph.rot(2, [128, 8, 128], BF16, 'cTs')
    ttr = Ph.rot(2, [128, D], F32, 'tt')
    junkr = Ph.rot(1, [128, D], BF16, 'junk')
    str_ = Ph.rot(4, [128, 4], F32, 'ost')
    pTr = Ph.rot(2, [128, 8, 128], BF16, 'pT', psum=True)
    pyr = Ph.rot(2, [128, D], F32, 'py', psum=True)
    for b in range(NB):
        for tile in (range(2, NT) if last else range(NT)):
            j = 2 if tile < 2 else b
            r0 = tile * 128
            ct, bct = ctr.next()
            xt, bx = xr.next()
            S.op('sp', lambda e, ct=ct, b=b, r0=r0: e.dma_start(out=ct[:], in_=C.cat[b, r0:r0 + 128, :]), writes=[bct],
                 dma=bct)
            S.op('sp', lambda e, xt=xt, b=b, tile=tile: e.dma_start(out=xt[:], in_=res_src(C, l, 0, b, tile)),
                 writes=[bx], dma=bx)
            pT, bpT = pTr.next()

            def tr(e, pT=pT, ct=ct):
                for k in range(8):
                    ins = e.transpose(out=pT[:, k, :], in_=ct[:, k * 128:(k + 1) * 128], identity=C.ident[:])
                return ins
            S.op('pe', tr, reads=[bct], writes=[bpT], self_ok=True)
            cTs, bcTs = cTsr.next()
            S.op('dve', lambda e, cTs=cTs, pT=pT: e.tensor_copy(out=cTs[:, 0:4, :], in_=pT[:, 0:4, :]), reads=[bpT],
                 writes=[bcTs])
            S.op('act', lambda e, cTs=cTs, pT=pT: e.activation(out=cTs[:, 4:8, :], in_=pT[:, 4:8, :], func=AF.Copy),
                 reads=[bpT], writes=[bcTs])
            py, bpy = pyr.next()

            def mm(e, py=py, cTs=cTs):
                for half in range(2):
                    for kc in range(8):
                        ins = e.matmul(py[:, half * 512:(half + 1) * 512], lhsT=cTs[:, kc, :],
                                       rhs=wo[:, kc, half * 512:(half + 1) * 512], start=(kc == 0), stop=(kc == 7))
                return ins
            S.op('pe', mm, reads=[bcTs, bwo], writes=[bpy], self_ok=True)
            post_norm_res(Ph, py[:], bpy, xt, bx, C.GG1, C.bGG1, j, junkr, str_, ttr,
                          C.xs[b, r0:r0 + 128, :])
    Ph.close()


def post_norm_res(Ph, y, by, xt, bx, GG, bGG, j, junkr, str_, ttr, dst):
    S = Ph.S
    C = Ph.C
    junk, bj = junkr.next()
    st, bst = str_.next()
    tt, btt = ttr.next()
    S.op('act', lambda e: e.activation(out=junk[:], in_=y, func=AF.Square, accum_out=st[:, 0:1]), reads=[by],
         writes=[bj, bst])
    S.op('dve', lambda e: e.tensor_scalar(out=st[:, 1:2], in0=st[:, 0:1], scalar1=1.0 / D, scalar2=EPS, op0=ALU.mult,
                                          op1=ALU.add), reads=[bst], writes=[bst])
    S.op('pool', lambda e: e.tensor_tensor(out=st[:, 2:3], in0=st[:, 1:2], in1=C.neghalf[:, 0:1], op=ALU.pow),
         reads=[bst], writes=[bst])
    S.op('dve', lambda e: e.scalar_tensor_tensor(out=tt[:], in0=y, scalar=st[:, 2:3], in1=GG[:, j, :], op0=ALU.mult,
                                                 op1=ALU.mult), reads=[by, bst, bGG], writes=[btt])
    S.op('pool', lambda e: e.tensor_tensor(out=tt[:], in0=tt[:], in1=xt[:], op=ALU.add), reads=[btt, bx],
         writes=[btt])
    S.op('sp', lambda e: e.dma_start(out=dst, in_=tt[:]), reads=[btt], dma=btt)


def phase_ffn_pre(C, l, moe, tiles):
    S = C.S
    Ph = Phase(C, f"fpre{l}")
    K = prenorm_kit(Ph, with32=moe)
    xr = Ph.rot(3, [128, D], F32, 'x')
    hTr = Ph.rot(2, [128, 8, 512], BF16, 'hT')
    if moe:
        wr, bwr = Ph.sbb([128, 8, NE], F32, 'wr')
        S.op('sp', lambda e: e.dma_start(out=wr[:], in_=C.moe_wr.rearrange("(kc p) n -> p kc n", p=128)),
             writes=[bwr], dma=bwr)
        h32r = Ph.rot(2, [128, 8, 128], F32, 'h32')
        plg = Ph.rot(1, [128, 512], F32, 'plg', psum=True)
        cmbr = Ph.rot(2, [128, 4, NE], F32, 'cmb')
        rsr = Ph.rot(4, [128, 48], F32, 'rst')
    for gi in range(len(tiles) // 4):
        hT, bhT = hTr.next()
        if moe:
            cmb, bcmb = cmbr.next()
        for t in range(4):
            b, tile = tiles[gi * 4 + t]
            j = 2 if tile < 2 else b
            xt, bx = xr.next()
            S.op('sp', lambda e, xt=xt, b=b, tile=tile: e.dma_start(out=xt[:], in_=res_src(C, l, 1, b, tile)),
                 writes=[bx], dma=bx)
            if not moe:
                prenorm_tile(Ph, K, xt, bx, hT, bhT, t * 128, C.G2, C.SH2, C.bG2, j)
                continue
            h32, bh32 = h32r.next()
            prenorm_tile(Ph, K, xt, bx, hT, bhT, t * 128, C.G2, C.SH2, C.bG2, j, h32, bh32)
            pl, bpl = plg.next()

            def mm(e, pl=pl, h32=h32):
                for kc in range(8):
                    ins = e.matmul(pl[:, 0:NE], lhsT=h32[:, kc, :], rhs=wr[:, kc, :], start=(kc == 0), stop=(kc == 7))
                return ins
            S.op('pe', mm, reads=[bh32, bwr], writes=[bpl], self_ok=True)
            st, bst = rsr.next()
            ops = [
                lambda e, st=st, pl=pl: e.tensor_copy(out=st[:, 0:8], in_=pl[:, 0:NE]),
                lambda e, st=st: e.reduce_max(out=st[:, 8:9], in_=st[:, 0:8], axis=AX.X),
                lambda e, st=st: e.tensor_scalar(out=st[:, 16:24], in0=st[:, 0:8], scalar1=st[:, 8:9], scalar2=-1e30,
                                                 op0=ALU.is_equal, op1=ALU.mult),
                lambda e, st=st: e.tensor_tensor(out=st[:, 16:24], in0=st[:, 16:24], in1=st[:, 0:8], op=ALU.add),
                lambda e, st=st: e.reduce_max(out=st[:, 9:10], in_=st[:, 16:24], axis=AX.X),
                lambda e, st=st: e.tensor_scalar(out=st[:, 24:32], in0=st[:, 0:8], scalar1=st[:, 9:10], scalar2=None,
                                                 op0=ALU.is_ge),
                lambda e, st=st: e.tensor_scalar(out=st[:, 10:11], in0=st[:, 8:9], scalar1=-1.0, scalar2=None,
                                                 op0=ALU.mult),
            ]
            for i, f_ in enumerate(ops):
                S.op('dve', f_, reads=[bst] + ([bpl] if i == 0 else []), writes=[bst])
            S.op('act', lambda e, st=st: e.activation(out=st[:, 32:40], in_=st[:, 0:8], func=AF.Exp, bias=st[:, 10:11],
                                                      scale=1.0), reads=[bst], writes=[bst])
            ops2 = [
                lambda e, st=st: e.tensor_tensor(out=st[:, 32:40], in0=st[:, 32:40], in1=st[:, 24:32], op=ALU.mult),
                lambda e, st=st: e.reduce_sum(out=st[:, 11:12], in_=st[:, 32:40], axis=AX.X),
                lambda e, st=st: e.reciprocal(out=st[:, 12:13], in_=st[:, 11:12]),
            ]
            for f_ in ops2:
                S.op('dve', f_, reads=[bst], writes=[bst])
            S.op('dve', lambda e, st=st, cmb=cmb, t=t: e.tensor_scalar(out=cmb[:, t, :], in0=st[:, 32:40],
                                                                       scalar1=st[:, 12:13], scalar2=None,
                                                                       op0=ALU.mult), reads=[bst], writes=[bcmb])
        S.op('sp', lambda e, hT=hT, gi=gi: e.dma_start(out=C.h2T[gi], in_=hT[:]), reads=[bhT], dma=bhT)
        if moe:
            S.op('sp', lambda e, cmb=cmb, gi=gi: e.dma_start(out=C.comb[gi], in_=cmb[:]), reads=[bcmb], dma=bcmb)
    Ph.close()


def phase_ffn(C, l, moe, tiles):
    S = C.S
    Ph = Phase(C, f"ffn{l}")
    E = NE if moe else 1
    F = F_MOE if moe else F_FFN
    NFC = F // 128
    NFB = F // 256
    wsrc = C.moe_bf if moe else C.ffn_bf
    bwg_ = C.bmoe if moe else C.bffn
    hTr = Ph.rot(2, [128, 8, 512], BF16, 'hT')
    hid, bhid = Ph.sbb([128, NFC, 512], BF16, 'hid')
    wd, bwd = Ph.sbb([128, NFC, D], BF16, 'wd')
    wgr = Ph.rot(3, [128, 8, 256], BF16, 'wg')
    wur = Ph.rot(3, [128, 8, 256], BF16, 'wu')
    yacc, byacc = Ph.sbb([128, 4, D], F32, 'yacc')
    sgr = Ph.rot(2, [128, 512], F32, 'sg')
    xr = Ph.rot(2, [128, D], F32, 'x')
    ttr = Ph.rot(2, [128, D], F32, 'tt')
    junkr = Ph.rot(1, [128, D], BF16, 'junk')
    str_ = Ph.rot(4, [128, 4], F32, 'fst')
    cmbr = Ph.rot(2, [128, 4, NE], F32, 'cmb')
    pgr = Ph.rot(2, [128, 512], F32, 'pg', psum=True)
    pur = Ph.rot(2, [128, 512], F32, 'pu', psum=True)
    pyr = Ph.rot(2, [128, 512], F32, 'py', psum=True)
    for gi in range(len(tiles) // 4):
        hT, bhT = hTr.next()
        S.op('sp', lambda e, hT=hT, gi=gi: e.dma_start(out=hT[:], in_=C.h2T[gi]), writes=[bhT], dma=bhT)
        if moe:
            cmb, bcmb = cmbr.next()
            S.op('sp', lambda e, cmb=cmb, gi=gi: e.dma_start(out=cmb[:], in_=C.comb[gi]), writes=[bcmb], dma=bcmb)
        for ex in range(E):
            hh = NFC // 2
            for (a0, a1) in [(0, hh), (hh, NFC)]:
                S.op('pool', lambda e, ex=ex, a0=a0, a1=a1: e.dma_start(
                    out=wd[:, a0:a1, :],
                    in_=wsrc[2][ex, a0 * 128:a1 * 128, :].rearrange("(fc p) n -> p fc n", p=128)),
                    reads=[bwg_], writes=[bwd], dma=bwd)
            for fb in range(NFB):
                wg, bwg = wgr.next()
                wu, bwu = wur.next()
                S.op('sp', lambda e, wg=wg, ex=ex, fb=fb: e.dma_start(
                    out=wg[:], in_=wsrc[0][ex, :, fb * 256:(fb + 1) * 256].rearrange("(kc p) f -> p kc f", p=128)),
                    reads=[bwg_], writes=[bwg], dma=bwg)
                S.op('sp', lambda e, wu=wu, ex=ex, fb=fb: e.dma_start(
                    out=wu[:], in_=wsrc[1][ex, :, fb * 256:(fb + 1) * 256].rearrange("(kc p) f -> p kc f", p=128)),
                    reads=[bwg_], writes=[bwu], dma=bwu)
                for fi in range(2):
                    fc = fb * 2 + fi
                    pg_, bpg = pgr.next()
                    pu_, bpu = pur.next()

                    def mmg(e, pg_=pg_, wg=wg, fi=fi, hT=hT):
                        for kc in range(8):
                            ins = e.matmul(pg_[:], lhsT=wg[:, kc, fi * 128:(fi + 1) * 128], rhs=hT[:, kc, :],
                                           start=(kc == 0), stop=(kc == 7))
                        return ins

                    def mmu(e, pu_=pu_, wu=wu, fi=fi, hT=hT):
                        for kc in range(8):
                            ins = e.matmul(pu_[:], lhsT=wu[:, kc, fi * 128:(fi + 1) * 128], rhs=hT[:, kc, :],
                                           start=(kc == 0), stop=(kc == 7))
                        return ins
                    S.op('pe', mmg, reads=[bwg, bhT], writes=[bpg], self_ok=True)
                    S.op('pe', mmu, reads=[bwu, bhT], writes=[bpu], self_ok=True)
                    sg, bsg = sgr.next()
                    S.op('act', lambda e, sg=sg, pg_=pg_: e.activation(out=sg[:], in_=pg_[:], func=AF.Silu),
                         reads=[bpg], writes=[bsg])
                    S.op('dve', lambda e, sg=sg, pu_=pu_, fc=fc: e.tensor_tensor(out=hid[:, fc, :], in0=pu_[:],
                                                                                 in1=sg[:], op=ALU.mult),
                         reads=[bsg, bpu], writes=[bhid])
            for t in range(4):
                for half in range(2):
                    py_, bpy = pyr.next()
                    hs = slice(half * 512, (half + 1) * 512)

                    def mmd(e, py_=py_, t=t, hs=hs):
                        for fc in range(NFC):
                            ins = e.matmul(py_[:], lhsT=hid[:, fc, t * 128:(t + 1) * 128], rhs=wd[:, fc, hs],
                                           start=(fc == 0), stop=(fc == NFC - 1))
                        return ins
                    S.op('pe', mmd, reads=[bhid, bwd], writes=[bpy], self_ok=True)
                    if not moe:
                        S.op('act', lambda e, py_=py_, t=t, hs=hs: e.activation(out=yacc[:, t, hs], in_=py_[:],
                                                                                func=AF.Copy),
                             reads=[bpy], writes=[byacc])
                    elif ex == 0:
                        S.op('act', lambda e, py_=py_, t=t, hs=hs, cmb=cmb: e.activation(
                            out=yacc[:, t, hs], in_=py_[:], func=AF.Copy, scale=cmb[:, t, 0:1]),
                            reads=[bpy, bcmb], writes=[byacc])
                    else:
                        S.op('dve', lambda e, py_=py_, t=t, hs=hs, cmb=cmb, ex=ex: e.scalar_tensor_tensor(
                            out=yacc[:, t, hs], in0=py_[:], scalar=cmb[:, t, ex:ex + 1], in1=yacc[:, t, hs],
                            op0=ALU.mult, op1=ALU.add), reads=[bpy, bcmb, byacc], writes=[byacc])
        for t in range(4):
            b, tile = tiles[gi * 4 + t]
            j = 2 if tile < 2 else b
            xt, bx = xr.next()
            S.op('sp', lambda e, xt=xt, b=b, tile=tile: e.dma_start(out=xt[:], in_=res_src(C, l, 1, b, tile)),
                 writes=[bx], dma=bx)
            if moe:
                dst = C.y_out[b, (tile - 2) * 128:(tile - 1) * 128, :]
            else:
                dst = C.xs[b, tile * 128:(tile + 1) * 128, :]

            post_norm_res(Ph, yacc[:, t, :], byacc, xt, bx, C.GG2, C.bGG2, j, junkr, str_, ttr, dst)
    Ph.close()


U32 = mybir.dt.uint32
I32 = mybir.dt.int32
NTOK = NB * LLAT
CAPE = NTOK
GS = 512
NGRP = CAPE // GS


def phase_moe_pre(C, l, tiles):
    S = C.S
    Ph = Phase(C, f"mpre{l}")
    xr = Ph.rot(2, [128, D], F32, 'x')
    junkr = Ph.rot(1, [128, D], BF16, 'junk')
    h32r = Ph.rot(2, [128, D], F32, 'h32')
    hbr = Ph.rot(2, [128, D], BF16, 'hb')
    hTr = Ph.rot(2, [128, 8, 128], F32, 'hT32')
    str_ = Ph.rot(4, [128, 4], F32, 'pst')
    rsr = Ph.rot(4, [128, 80], F32, 'rst')
    selr = Ph.rot(2, [128, NE], BF16, 'selb')
    recr = Ph.rot(8, [128, 4], U32, 'rec')
    slur = Ph.rot(4, [128, 2], U32, 'slu')
    wr, bwr = Ph.sbb([128, 8, NE], F32, 'wr')
    base, bbase = Ph.sbb([128, NE], F32, 'base')
    nid, bnid = Ph.sbb([128, 64], F32, 'nid')
    eoff, beoff = Ph.sbb([128, NE], F32, 'eoff')
    thr, bthr = Ph.sbb([128, NGRP], F32, 'thr')
    trif, btrif = Ph.sbb([128, 128], F32, 'trif')
    trib, btrib = Ph.sbb([128, 128], BF16, 'trib')
    oneb, boneb = Ph.sbb([128, 128], BF16, 'oneb')
    flf, bflf = Ph.sbb([128, NE, NGRP], F32, 'flf')
    fli, bfli = Ph.sbb([128, NE, NGRP], I32, 'fli')
    p32r = Ph.rot(2, [128, 8, 128], F32, 'p32', psum=True)
    plg = Ph.rot(2, [128, 512], F32, 'plg', psum=True)
    blst = S.buf('lst')
    S.op('sp', lambda e: e.dma_start(out=C.lst[:, :], in_=C.lst_init[:, :]), writes=[blst], dma=blst)
    S.op('sp', lambda e: e.dma_start(out=wr[:], in_=C.moe_wr.rearrange("(kc p) n -> p kc n", p=128)), writes=[bwr],
         dma=bwr)
    S.op('sp', lambda e: e.dma_start(out=nid[:], in_=C.nidf[:, :]), writes=[bnid], dma=bnid)
    S.op('sp', lambda e: e.dma_start(out=eoff[:], in_=C.eoff[:, :]), writes=[beoff], dma=beoff)
    S.op('sp', lambda e: e.dma_start(out=thr[:], in_=C.thr[:, :]), writes=[bthr], dma=bthr)
    S.op('sp', lambda e: e.dma_start(out=trif[:], in_=C.tri_in[:, 2, :]), writes=[btrif], dma=btrif)
    S.op('dve', lambda e: e.tensor_copy(out=trib[:], in_=trif[:]), reads=[btrif], writes=[btrib])
    S.op('pool', lambda e: e.memset(oneb[:], 1.0), writes=[boneb])
    S.op('pool', lambda e: e.memset(base[:], 0.0), writes=[bbase])
    for k, (b, tile) in enumerate(tiles):
        xt, bx = xr.next()
        S.op('sp', lambda e, xt=xt, b=b, tile=tile: e.dma_start(out=xt[:], in_=res_src(C, l, 1, b, tile)), writes=[bx],
             dma=bx)
        junk, bj = junkr.next()
        st, bst = str_.next()
        h32, bh32 = h32r.next()
        hb, bhb = hbr.next()
        S.op('act', lambda e, junk=junk, xt=xt, st=st: e.activation(out=junk[:], in_=xt[:], func=AF.Square,
                                                                     accum_out=st[:, 0:1]), reads=[bx], writes=[bj, bst])
        S.op('dve', lambda e, st=st: e.tensor_scalar(out=st[:, 1:2], in0=st[:, 0:1], scalar1=1.0 / D, scalar2=EPS,
                                                     op0=ALU.mult, op1=ALU.add), reads=[bst], writes=[bst])
        S.op('pool', lambda e, st=st: e.tensor_tensor(out=st[:, 2:3], in0=st[:, 1:2], in1=C.neghalf[:, 0:1],
                                                      op=ALU.pow), reads=[bst], writes=[bst])
        S.op('dve', lambda e, h32=h32, xt=xt, st=st, b=b: e.scalar_tensor_tensor(
            out=h32[:], in0=xt[:], scalar=st[:, 2:3], in1=C.G2row[:, b, :], op0=ALU.mult, op1=ALU.mult),
            reads=[bx, bst, C.bG2row], writes=[bh32])
        S.op('pool', lambda e, h32=h32, b=b: e.tensor_tensor(out=h32[:], in0=h32[:], in1=C.S2row[:, b, :], op=ALU.add),
             reads=[bh32, C.bG2row], writes=[bh32])
        S.op('act', lambda e, hb=hb, h32=h32: e.activation(out=hb[:], in_=h32[:], func=AF.Copy), reads=[bh32],
             writes=[bhb])
        S.op('sp', lambda e, hb=hb, k=k: e.dma_start(out=C.h2tok[k * 128:(k + 1) * 128, :], in_=hb[:]), reads=[bhb],
             dma=bhb)
        p32, bp32 = p32r.next()

        def tr32(e, p32=p32, h32=h32):
            for kc in range(8):
                ins = e.transpose(out=p32[:, kc, :], in_=h32[:, kc * 128:(kc + 1) * 128], identity=C.ident32[:])
            return ins
        S.op('pe', tr32, reads=[bh32], writes=[bp32], self_ok=True)
        hT, bhT = hTr.next()
        S.op('dve', lambda e, hT=hT, p32=p32: e.tensor_copy(out=hT[:, 0:4, :], in_=p32[:, 0:4, :]), reads=[bp32],
             writes=[bhT])
        S.op('act', lambda e, hT=hT, p32=p32: e.activation(out=hT[:, 4:8, :], in_=p32[:, 4:8, :], func=AF.Copy),
             reads=[bp32], writes=[bhT])
        pl, bpl = plg.next()

        def mm(e, pl=pl, hT=hT):
            for kc in range(8):
                ins = e.matmul(pl[:, 0:NE], lhsT=hT[:, kc, :], rhs=wr[:, kc, :], start=(kc == 0), stop=(kc == 7))
            return ins
        S.op('pe', mm, reads=[bhT, bwr], writes=[bpl], self_ok=True)
        r, br = rsr.next()
        LG, M1, M2, NM1, DEN, RDEN = r[:, 0:8], r[:, 8:9], r[:, 9:10], r[:, 10:11], r[:, 11:12], r[:, 12:13]
        EQ1, SEL, WN, MSK, OH1, SV, TMP = r[:, 16:24], r[:, 24:32], r[:, 32:40], r[:, 40:48], r[:, 48:56], r[:, 56:64], \
            r[:, 64:72]
        SL0, SL1, W0, W1, D1 = r[:, 72:73], r[:, 73:74], r[:, 74:75], r[:, 75:76], r[:, 76:77]
        selb, bselb = selr.next()
        dv = lambda f_, extra=(): S.op('dve', f_, reads=[br] + list(extra), writes=[br])
        dv(lambda e, LG=LG, pl=pl: e.tensor_copy(out=LG, in_=pl[:, 0:NE]), [bpl])
        dv(lambda e, LG=LG, M1=M1: e.reduce_max(out=M1, in_=LG, axis=AX.X))
        dv(lambda e, EQ1=EQ1, LG=LG, M1=M1: e.tensor_scalar(out=EQ1, in0=LG, scalar1=M1, scalar2=None,
                                                            op0=ALU.is_equal))
        dv(lambda e, MSK=MSK, EQ1=EQ1, LG=LG: e.scalar_tensor_tensor(out=MSK, in0=EQ1, scalar=-1e30, in1=LG,
                                                                     op0=ALU.mult, op1=ALU.add))
        dv(lambda e, MSK=MSK, M2=M2: e.reduce_max(out=M2, in_=MSK, axis=AX.X))
        dv(lambda e, SEL=SEL, LG=LG, M2=M2: e.tensor_scalar(out=SEL, in0=LG, scalar1=M2, scalar2=None, op0=ALU.is_ge))
        dv(lambda e, NM1=NM1, M1=M1: e.tensor_scalar(out=NM1, in0=M1, scalar1=-1.0, scalar2=None, op0=ALU.mult))
        S.op('act', lambda e, WN=WN, LG=LG, NM1=NM1: e.activation(out=WN, in_=LG, func=AF.Exp, bias=NM1, scale=1.0),
             reads=[br], writes=[br])
        dv(lambda e, WN=WN, SEL=SEL: e.tensor_tensor(out=WN, in0=WN, in1=SEL, op=ALU.mult))
        dv(lambda e, WN=WN, DEN=DEN: e.reduce_sum(out=DEN, in_=WN, axis=AX.X))
        dv(lambda e, DEN=DEN, RDEN=RDEN: e.reciprocal(out=RDEN, in_=DEN))
        dv(lambda e, WN=WN, RDEN=RDEN: e.tensor_scalar(out=WN, in0=WN, scalar1=RDEN, scalar2=None, op0=ALU.mult))
        dv(lambda e, OH1=OH1, SEL=SEL, EQ1=EQ1: e.tensor_tensor(out=OH1, in0=SEL, in1=EQ1, op=ALU.subtract))
        S.op('dve', lambda e, selb=selb, SEL=SEL: e.tensor_copy(out=selb[:], in_=SEL), reads=[br], writes=[bselb])
        pc, bpc = plg.next()

        def mmc(e, pc=pc, selb=selb):
            e.matmul(pc[:, 0:NE], lhsT=trib[:], rhs=selb[:], start=True, stop=True)
            return e.matmul(pc[:, 8:8 + NE], lhsT=oneb[:], rhs=selb[:], start=True, stop=True)
        S.op('pe', mmc, reads=[bselb, btrib, boneb], writes=[bpc], self_ok=True)
        dv(lambda e, SV=SV, pc=pc: e.tensor_tensor(out=SV, in0=pc[:, 0:NE], in1=base[:], op=ALU.add), [bpc, bbase])
        dv(lambda e, SV=SV: e.tensor_tensor(out=SV, in0=SV, in1=eoff[:], op=ALU.add), [beoff])
        S.op('dve', lambda e, pc=pc: e.tensor_tensor(out=base[:], in0=pc[:, 8:8 + NE], in1=base[:], op=ALU.add),
             reads=[bpc, br], writes=[bbase])
        for (OH, SL, W) in [(EQ1, SL0, W0), (OH1, SL1, W1)]:
            dv(lambda e, TMP=TMP, OH=OH, SV=SV: e.tensor_tensor(out=TMP, in0=OH, in1=SV, op=ALU.mult))
            dv(lambda e, TMP=TMP, SL=SL: e.reduce_sum(out=SL, in_=TMP, axis=AX.X))
            dv(lambda e, TMP=TMP, OH=OH, WN=WN: e.tensor_tensor(out=TMP, in0=OH, in1=WN, op=ALU.mult))
            dv(lambda e, TMP=TMP, W=W: e.reduce_sum(out=W, in_=TMP, axis=AX.X))
        dv(lambda e, D1=D1, k=k: e.tensor_scalar(out=D1, in0=nid[:, k:k + 1], scalar1=float(NTOK), scalar2=None,
                                                 op0=ALU.add), [bnid])
        slu, bslu = slur.next()
        S.op('dve', lambda e, slu=slu, SL0=SL0: e.tensor_copy(out=slu[:, 0:1], in_=SL0), reads=[br], writes=[bslu])
        S.op('dve', lambda e, slu=slu, SL1=SL1: e.tensor_copy(out=slu[:, 1:2], in_=SL1), reads=[br], writes=[bslu])
        for rk, (DD, WW) in enumerate([(None, W0), (D1, W1)]):
            rec, brec = recr.next()
            S.op('pool', lambda e, rec=rec: e.memset(rec[:], 0), writes=[brec])
            rw = lambda f_: S.op('dve', f_, reads=[br, bnid], writes=[brec])
            rw(lambda e, rec=rec, k=k: e.tensor_copy(out=rec[:, 0:1], in_=nid[:, k:k + 1]))
            if DD is None:
                rw(lambda e, rec=rec, k=k: e.tensor_copy(out=rec[:, 1:2], in_=nid[:, k:k + 1]))
            else:
                rw(lambda e, rec=rec, DD=DD: e.tensor_copy(out=rec[:, 1:2], in_=DD))
            rw(lambda e, rec=rec, WW=WW: e.tensor_copy(out=rec[:, 2:3].bitcast(F32), in_=WW))
            S.op('pool', lambda e, rec=rec, slu=slu, rk=rk: e.indirect_dma_start(
                out=C.lst[:, :], out_offset=bass.IndirectOffsetOnAxis(ap=slu[:, rk:rk + 1], axis=0),
                in_=rec[:], in_offset=None, bounds_check=S.breg(e, NE * CAPE - 1), oob_is_err=False),
                reads=[brec, bslu, blst], dma=brec)
    for ex in range(NE):
        S.op('dve', lambda e, ex=ex: e.tensor_scalar(out=flf[:, ex, :], in0=thr[:], scalar1=base[:, ex:ex + 1],
                                                     scalar2=None, op0=ALU.is_lt), reads=[bbase, bthr], writes=[bflf])
    S.op('dve', lambda e: e.tensor_copy(out=fli[:], in_=flf[:]), reads=[bflf], writes=[bfli])
    S.op('sp', lambda e: e.dma_start(out=C.flags[0:1, :], in_=fli[0:1, :, :].rearrange("p a b -> p (a b)")),
         reads=[bfli], dma=bfli)
    Ph.close()


def phase_moe_sparse(C, l):
    import os
    SPS = int(os.environ.get('SP_STG', '9'))
    S = C.S
    Ph = Phase(C, f"moe{l}")
    NFC = F_MOE // 128
    NFB = F_MOE // 256
    wsrc = C.moe_bf
    hid, bhid = Ph.sbb([128, NFC, 512], BF16, 'hid')
    wd, bwd = Ph.sbb([128, NFC, D], BF16, 'wd')
    wgr = Ph.rot(3, [128, 8, 256], BF16, 'wg')
    wur = Ph.rot(3, [128, 8, 256], BF16, 'wu')
    htr = Ph.rot(2, [128, 4, D], BF16, 'htok')
    hTr = Ph.rot(2, [128, 8, 512], BF16, 'hT')
    recr = Ph.rot(2, [128, 4, 4], U32, 'recs')
    sgr = Ph.rot(2, [128, 512], F32, 'sg')
    yscr = Ph.rot(2, [128, D], F32, 'ysc')
    ptrr = Ph.rot(2, [128, 8, 128], BF16, 'ptr', psum=True)
    pgr = Ph.rot(2, [128, 512], F32, 'pg', psum=True)
    pur = Ph.rot(1, [128, 512], F32, 'pu', psum=True)
    pyr = Ph.rot(2, [128, 512], F32, 'py', psum=True)
    for t_, b_ in zip(htr.t, htr.b):
        S.op('pool', lambda e, t_=t_: e.memset(t_[:], 0.0), writes=[b_])
    for ex in range(NE):
        hh = NFC // 2
        for (a0, a1) in [(0, hh), (hh, NFC)]:
            S.op('sp', lambda e, ex=ex, a0=a0, a1=a1: e.dma_start(
                out=wd[:, a0:a1, :], in_=wsrc[2][ex, a0 * 128:a1 * 128, :].rearrange("(fc p) n -> p fc n", p=128)),
                reads=[C.bmoe], writes=[bwd], dma=bwd)
        for g in range(NGRP):
            S.cond_begin(C.flags[0:1, ex * NGRP + g:ex * NGRP + g + 1])
            recs, brecs = recr.next()
            r0 = ex * CAPE + g * GS
            S.op('sp', lambda e, recs=recs, r0=r0: e.dma_start(
                out=recs[:], in_=C.lst[r0:r0 + GS, :].rearrange("(t p) c -> p t c", p=128)), writes=[brecs], dma=brecs)
            ht, bht = htr.next()
            for t in range(4):
                S.op('pool', lambda e, ht=ht, recs=recs, t=t: e.indirect_dma_start(
                    out=ht[:, t, :], out_offset=None, in_=C.h2tok[:, :],
                    in_offset=bass.IndirectOffsetOnAxis(ap=recs[:, t, 0:1], axis=0), bounds_check=S.breg(e, NTOK - 1),
                    oob_is_err=False), reads=[brecs], writes=[bht], dma=bht)
            hT, bhT = hTr.next()
            for t in range(4 if SPS >= 2 else 0):
                ptr, bptr = ptrr.next()

                def tr(e, ptr=ptr, ht=ht, t=t):
                    for kc in range(8):
                        ins = e.transpose(out=ptr[:, kc, :], in_=ht[:, t, kc * 128:(kc + 1) * 128], identity=C.ident[:])
                    return ins
                S.op('pe', tr, reads=[bht], writes=[bptr], self_ok=True)
                if t % 2 == 0:
                    S.op('dve', lambda e, hT=hT, ptr=ptr, t=t: e.tensor_copy(out=hT[:, :, t * 128:(t + 1) * 128],
                                                                             in_=ptr[:]), reads=[bptr], writes=[bhT])
                else:
                    S.op('act', lambda e, hT=hT, ptr=ptr, t=t: e.activation(out=hT[:, :, t * 128:(t + 1) * 128],
                                                                            in_=ptr[:], func=AF.Copy), reads=[bptr],
                         writes=[bhT])
            for fb in range(NFB if SPS >= 3 else 0):
                wg, bwg = wgr.next()
                wu, bwu = wur.next()
                S.op('sp', lambda e, wg=wg, ex=ex, fb=fb: e.dma_start(
                    out=wg[:], in_=wsrc[0][ex, :, fb * 256:(fb + 1) * 256].rearrange("(kc p) f -> p kc f", p=128)),
                    reads=[C.bmoe], writes=[bwg], dma=bwg)
                S.op('sp', lambda e, wu=wu, ex=ex, fb=fb: e.dma_start(
                    out=wu[:], in_=wsrc[1][ex, :, fb * 256:(fb + 1) * 256].rearrange("(kc p) f -> p kc f", p=128)),
                    reads=[C.bmoe], writes=[bwu], dma=bwu)
                for fi in range(2):
                    fc = fb * 2 + fi
                    pg_, bpg = pgr.next()
                    pu_, bpu = pur.next()

                    def mmg(e, pg_=pg_, wg=wg, fi=fi, hT=hT):
                        for kc in range(8):
                            ins = e.matmul(pg_[:], lhsT=wg[:, kc, fi * 128:(fi + 1) * 128], rhs=hT[:, kc, :],
                                           start=(kc == 0), stop=(kc == 7))
                        return ins

                    def mmu(e, pu_=pu_, wu=wu, fi=fi, hT=hT):
                        for kc in range(8):
                            ins = e.matmul(pu_[:], lhsT=wu[:, kc, fi * 128:(fi + 1) * 128], rhs=hT[:, kc, :],
                                           start=(kc == 0), stop=(kc == 7))
                        return ins
                    S.op('pe', mmg, reads=[bwg, bhT], writes=[bpg], self_ok=True)
                    S.op('pe', mmu, reads=[bwu, bhT], writes=[bpu], self_ok=True)
                    sg, bsg = sgr.next()
                    S.op('act', lambda e, sg=sg, pg_=pg_: e.activation(out=sg[:], in_=pg_[:], func=AF.Silu),
                         reads=[bpg], writes=[bsg])
                    S.op('dve', lambda e, sg=sg, pu_=pu_, fc=fc: e.tensor_tensor(out=hid[:, fc, :], in0=pu_[:],
                                                                                 in1=sg[:], op=ALU.mult),
                         reads=[bsg, bpu], writes=[bhid])
            for t in range(4 if SPS >= 4 else 0):
                ysc, bysc = yscr.next()
                for half in range(2):
                    py_, bpy = pyr.next()
                    hs = slice(half * 512, (half + 1) * 512)

                    def mmd(e, py_=py_, t=t, hs=hs):
                        for fc in range(NFC):
                            ins = e.matmul(py_[:], lhsT=hid[:, fc, t * 128:(t + 1) * 128], rhs=wd[:, fc, hs],
                                           start=(fc == 0), stop=(fc == NFC - 1))
                        return ins
                    S.op('pe', mmd, reads=[bhid, bwd], writes=[bpy], self_ok=True)
                    S.op('act', lambda e, py_=py_, ysc=ysc, hs=hs, recs=recs, t=t: e.activation(
                        out=ysc[:, hs], in_=py_[:], func=AF.Copy, scale=recs[:, t, 2:3].bitcast(F32)),
                        reads=[bpy, brecs], writes=[bysc])
                if SPS >= 5:
                  S.op('pool', lambda e, ysc=ysc, recs=recs, t=t: e.indirect_dma_start(
                    out=C.Ymoe[:, :], out_offset=bass.IndirectOffsetOnAxis(ap=recs[:, t, 1:2], axis=0), in_=ysc[:],
                    in_offset=None, bounds_check=S.breg(e, 2 * NTOK - 1), oob_is_err=False), reads=[bysc, brecs], dma=bysc)
            S.cond_end()
    Ph.close()


def phase_moe_post(C, l, tiles):
    S = C.S
    Ph = Phase(C, f"mpost{l}")
    xr = Ph.rot(2, [128, D], F32, 'x')
    y1r = Ph.rot(2, [128, D], F32, 'y1')
    y2r = Ph.rot(2, [128, D], F32, 'y2')
    ttr = Ph.rot(2, [128, D], F32, 'tt')
    junkr = Ph.rot(1, [128, D], BF16, 'junk')
    str_ = Ph.rot(4, [128, 4], F32, 'fst')
    for k, (b, tile) in enumerate(tiles):
        xt, bx = xr.next()
        y1, by1 = y1r.next()
        y2, by2 = y2r.next()
        S.op('sp', lambda e, xt=xt, b=b, tile=tile: e.dma_start(out=xt[:], in_=res_src(C, l, 1, b, tile)), writes=[bx],
             dma=bx)
        S.op('sp', lambda e, y1=y1, k=k: e.dma_start(out=y1[:], in_=C.Ymoe[k * 128:(k + 1) * 128, :]), writes=[by1],
             dma=by1)
        S.op('sp', lambda e, y2=y2, k=k: e.dma_start(out=y2[:], in_=C.Ymoe[NTOK + k * 128:NTOK + (k + 1) * 128, :]),
             writes=[by2], dma=by2)
        S.op('pool', lambda e, y1=y1, y2=y2: e.tensor_tensor(out=y1[:], in0=y1[:], in1=y2[:], op=ALU.add),
             reads=[by1, by2], writes=[by1])
        dst = C.y_out[b, (tile - 2) * 128:(tile - 1) * 128, :]
        post_norm_res(Ph, y1[:], by1, xt, bx, C.GG2, C.bGG2, b, junkr, str_, ttr, dst)
    Ph.close()


SPARSE_MOE = True


def build_program(debug=False, upto=None, skip=()):
    nc = bass.Bass("TRN2", target_bir_lowering=False)
    C = Ctx()
    C.nc = nc
    C.debug = debug
    L = 2
    C.x_in = _dram_in(nc, "x", [NB, LLAT, D])
    C.ctx_in = _dram_in(nc, "ctx", [NB, LCTX, D])
    C.cT = _dram_in(nc, "cT", [128, 8, 3])
    C.w_ada = _dram_in(nc, "w_ada", [L, D, 6 * D])
    C.badaT3 = _dram_in(nc, "badaT3", [L, 128, 48, 3])
    C.gpre3 = _dram_in(nc, "gpre3", [L, 128, 2, 8, 3])
    C.rowc = _dram_in(nc, "rowc", [L, 128, 7, D])
    C.w_in = _dram_in(nc, "w_in", [L, D, PROJ])
    C.rope_cos = _dram_in(nc, "rope_cos", [128, 32, 64])
    C.rope_sin = _dram_in(nc, "rope_sin", [128, 32, 64])
    C.ident_in = _dram_in(nc, "ident", [128, 128], BF16)
    C.ident32_in = _dram_in(nc, "ident32", [128, 128], F32)
    C.tri_in = _dram_in(nc, "tri", [128, 4, 128], F32)
    C.wgate = _dram_in(nc, "gla_w_gate", [L, 2, 16, 256])
    C.bgate = _dram_in(nc, "gla_b_gate", [L, 2, 1, 256])
    C.gnormB = _dram_in(nc, "gnormB", [L, 128, 512])
    C.natb = _dram_in(nc, "natb", [L, 8, 128, 5, 5, 128])
    C.w_out = _dram_in(nc, "w_out", [L, D, D])
    C.ffn_wg = _dram_in(nc, "ffn_w_gate", [1, D, F_FFN])
    C.ffn_wu = _dram_in(nc, "ffn_w_up", [1, D, F_FFN])
    C.ffn_wd = _dram_in(nc, "ffn_w_down", [1, F_FFN, D])
    C.moe_wr = _dram_in(nc, "moe_w_router", [D, NE])
    C.moe_wg = _dram_in(nc, "moe_w_gate", [NE, D, F_MOE])
    C.moe_wu = _dram_in(nc, "moe_w_up", [NE, D, F_MOE])
    C.moe_wd = _dram_in(nc, "moe_w_down", [NE, F_MOE, D])
    C.lst_init = _dram_in(nc, "lst_init", [NE * CAPE, 4], U32)
    C.nidf = _dram_in(nc, "nidf", [128, 64])
    C.eoff = _dram_in(nc, "eoff", [128, NE])
    C.thr = _dram_in(nc, "thr", [128, NGRP])
    C.lst = _dram_tmp(nc, "lst", [NE * CAPE, 4], U32)
    C.flags = _dram_tmp(nc, "flags", [1, NE * NGRP], I32)
    C.h2tok = _dram_tmp(nc, "h2tok", [NTOK, D], BF16)
    C.Ymoe = _dram_tmp(nc, "Ymoe", [2 * NTOK, D], F32)
    C.y_out = nc.dram_tensor("y", [NB, LLAT, D], F32, kind="ExternalOutput").ap()
    dbg = debug
    C.xs = _dram_tmp(nc, "xs", [NB, LT, D], F32, dbg)
    C.tokmaj = _dram_tmp(nc, "tokmaj", [NB, LT, 2048], BF16, dbg)
    C.nqkT = _dram_tmp(nc, "nqkT", [NB, 1024, LT], BF16, dbg)
    C.lrT = _dram_tmp(nc, "lrT", [NB, 2, 16, LT], F32, dbg)
    C.cat = _dram_tmp(nc, "cat", [NB, LT, D], BF16, dbg)
    C.h2T = _dram_tmp(nc, "h2T", [17, 128, 8, 512], BF16, dbg)
    C.comb = _dram_tmp(nc, "comb", [17, 128, 4, NE], F32, dbg)
    C.ffn_bf = [_dram_tmp(nc, "ffn_wg_bf", [1, D, F_FFN], BF16), _dram_tmp(nc, "ffn_wu_bf", [1, D, F_FFN], BF16),
                _dram_tmp(nc, "ffn_wd_bf", [1, F_FFN, D], BF16)]
    C.moe_bf = [_dram_tmp(nc, "moe_wg_bf", [NE, D, F_MOE], BF16), _dram_tmp(nc, "moe_wu_bf", [NE, D, F_MOE], BF16),
                _dram_tmp(nc, "moe_wd_bf", [NE, F_MOE, D], BF16)]
    if debug:
        C.dbg = nc.dram_tensor("dbg", [128, 8192], F32, kind="ExternalOutput").ap()
    with ExitStack() as gs:
        S = Sched(nc, gs)
        C.S = S
        S.bounds = [NE * CAPE - 1, NTOK - 1, 2 * NTOK - 1]
        GP = Phase(C, "glob")
        C.ident, bid = GP.sbb([128, 128], BF16, 'ident')
        C.ident32, bid32 = GP.sbb([128, 128], F32, 'ident32')
        C.ones, bones = GP.sbb([128, 128], F32, 'ones')
        C.neghalf, bnh = GP.sbb([128, 4], F32, 'neghalf')
        S.op('sp', lambda e: e.dma_start(out=C.ident[:], in_=C.ident_in[:, :]), writes=[bid], dma=bid)
        S.op('sp', lambda e: e.dma_start(out=C.ident32[:], in_=C.ident32_in[:, :]), writes=[bid32], dma=bid32)
        S.op('pool', lambda e: e.memset(C.ones[:], 1.0), writes=[bones])
        S.op('pool', lambda e: e.memset(C.neghalf[:], -0.5), writes=[bnh])
        C.bffn = Buf('ffn_bf')
        C.bmoe = Buf('moe_bf')
        if upto is None or upto >= 5:
            for src, dst in zip([C.ffn_wg, C.ffn_wu, C.ffn_wd], C.ffn_bf):
                S.op('pool', lambda e, src=src, dst=dst: e.dma_start(out=dst[0], in_=src[0]), writes=[C.bffn],
                     dma=C.bffn, track=False)
        if upto is None or upto >= 6:
            for src, dst in zip([C.moe_wg, C.moe_wu, C.moe_wd], C.moe_bf):
                for ex in range(NE):
                    S.op('pool', lambda e, src=src, dst=dst, ex=ex: e.dma_start(out=dst[ex], in_=src[ex]),
                         writes=[C.bmoe], dma=C.bmoe, track=False)
        S.flush()
        for l in range(L):
            LP = Phase(C, f"L{l}")
            C.want_rows = (l == L - 1) and SPARSE_MOE
            phase_mod(C, l, LP)
            if debug and l == debug - 1 and upto == 0:
                dump_mod(C)
            if upto is not None and upto == 0:
                LP.st.close()
                break
            if 1 not in skip:
                phase_proj(C, l)
            if upto is not None and upto <= 1:
                LP.st.close()
                break
            last = (l == L - 1)
            phase_gla(C, l, last)
            if upto is not None and upto <= 2:
                LP.st.close()
                break
            phase_na(C, l, last)
            if upto is not None and upto <= 3:
                LP.st.close()
                break
            phase_outproj(C, l, last)
            if upto is not None and upto <= 4:
                LP.st.close()
                break
            if last:
                tiles = [(b, t) for b in range(NB) for t in range(2, NT)]
            else:
                tiles = [(b, t) for b in range(NB) for t in range(NT)]
            if last and SPARSE_MOE:
                import os
                ms = int(os.environ.get('MOE_STOP', '9'))
                phase_moe_pre(C, l, tiles)
                if ms >= 2:
                    phase_moe_sparse(C, l)
                if ms >= 3:
                    phase_moe_post(C, l, tiles)
            else:
                phase_ffn_pre(C, l, last, tiles)
                phase_ffn(C, l, last, tiles)
            if upto is not None and upto <= 5 + l:
                LP.st.close()
                break
            LP.st.close()
        GP.st.close()
    return nc


def dump_mod(C):
    S = C.S
    Ph = Phase(C, "dump")
    o = 0
    for t, n in [(C.G1, 24), (C.SH1, 24), (C.G2, 24), (C.SH2, 24)]:
        S.op('sp', lambda e, t=t, o=o, n=n: e.dma_start(out=C.dbg[:, o:o + n], in_=t[:].rearrange("p a b -> p (a b)")),
             reads=[C.bG1, C.bG2], dma=S.buf())
        o += n
    for t in [C.GG1, C.GG2]:
        S.op('sp', lambda e, t=t, o=o: e.dma_start(out=C.dbg[:, o:o + 3072], in_=t[:].rearrange("p a b -> p (a b)")),
             reads=[C.bGG1, C.bGG2], dma=S.buf())
        o += 3072
    Ph.close()


def _na_bias_tables(rpb):
    L = rpb.shape[0]
    out = np.full((L, 8, 128, 5, 640), NEG, np.float32)
    reps = [0, 1, 10, 30, 31]
    for pi, j in enumerate(reps):
        ts = min(max(j - 2, 0), 27)
        for rq2 in range(2):
            r = 2 * j + rq2
            rs = min(max(r - 4, 0), 56)
            for cq in range(64):
                cs = min(max(cq - 8, 0), 48)
                p = rq2 * 64 + cq
                ck = np.arange(cs, cs + 16)
                for rk in range(rs, rs + 8):
                    slot = rk - 2 * ts
                    out[:, :, p, pi, slot * 64 + ck] = rpb[:, :, rk - r + 7, ck - cq + 15]
    return out


def _tri():
    i = np.arange(128)
    ut = (i[:, None] <= i[None, :]).astype(np.float32)
    lt = (i[:, None] >= i[None, :]).astype(np.float32)
    sut = (i[:, None] < i[None, :]).astype(np.float32)
    slt = (i[:, None] > i[None, :]).astype(np.float32)
    return np.stack([ut, lt, sut, slt], axis=1).copy()


def make_in_maps(inp, n_cores=8):
    import ml_dtypes
    f = lambda a: np.ascontiguousarray(np.asarray(a, dtype=np.float32))
    L = 2
    w_ada = f(inp['w_ada'])
    b_ada = f(inp['b_ada'])
    badaT3 = np.repeat(b_ada.reshape(L, 48, 128).transpose(0, 2, 1)[:, :, :, None], 3, axis=3).copy()
    gp = np.stack([f(inp['g_pre_mix']), f(inp['g_pre_ffn'])], axis=1)
    gpre3 = np.repeat(gp.reshape(L, 2, 8, 128).transpose(0, 3, 1, 2)[..., None], 3, axis=4).copy()
    rows = np.stack([b_ada[:, 2048:3072], b_ada[:, 5120:6144], f(inp['g_post_mix']), f(inp['g_post_ffn']),
                     b_ada[:, 3072:4096], b_ada[:, 4096:5120], f(inp['g_pre_ffn'])], axis=1)
    rowc = np.repeat(rows[:, None, :, :], 128, axis=1).copy()
    cos, sin = _rope_tables()
    gn = f(inp['gla_g_norm'])
    gnormB = np.repeat(np.tile(gn, (1, 4))[:, None, :], 128, axis=1).copy()
    natb = _na_bias_tables(f(inp['na_rpb']))
    natb = np.ascontiguousarray(natb.reshape(L, 8, 128, 5, 5, 128).transpose(0, 1, 5, 3, 4, 2))
    lst_init = np.zeros((NE * CAPE, 4), np.uint32)
    lst_init[:, 0:2] = 1 << 30
    nidf = (np.arange(64)[None, :] * 128 + np.arange(128)[:, None]).astype(np.float32)
    eoff = np.repeat((np.arange(NE) * CAPE).astype(np.float32)[None, :], 128, axis=0)
    thr = np.repeat((np.arange(NGRP) * GS).astype(np.float32)[None, :], 128, axis=0)
    shared = {
        "lst_init": lst_init, "nidf": nidf, "eoff": eoff, "thr": thr,
        "w_ada": w_ada, "badaT3": badaT3, "gpre3": gpre3, "rowc": rowc, "w_in": f(inp['w_in']),
        "rope_cos": cos, "rope_sin": sin, "ident": np.eye(128).astype(ml_dtypes.bfloat16),
        "ident32": np.eye(128, dtype=np.float32), "tri": _tri(),
        "gla_w_gate": f(inp['gla_w_gate']), "gla_b_gate": f(inp['gla_b_gate']).reshape(L, 2, 1, 256),
        "gnormB": gnormB, "natb": natb, "w_out": f(inp['w_out']),
        "ffn_w_gate": f(inp['ffn_w_gate']), "ffn_w_up": f(inp['ffn_w_up']), "ffn_w_down": f(inp['ffn_w_down']),
        "moe_w_router": f(inp['moe_w_router'])[0], "moe_w_gate": f(inp['moe_w_gate'])[0],
        "moe_w_up": f(inp['moe_w_up'])[0], "moe_w_down": f(inp['moe_w_down'])[0],
    }
    x = f(inp['x'])
    c = f(inp['c'])
    ctx = f(inp['ctx'])
    c_ctx = f(inp['c_ctx'])
    maps = []
    for i in range(n_cores):
        cv = np.stack([c[2 * i], c[2 * i + 1], c_ctx], axis=0)
        cT = cv.reshape(3, 8, 128).transpose(2, 1, 0).copy()
        m = dict(shared)
        m["x"] = x[2 * i:2 * i + 2]
        m["ctx"] = ctx[2 * i:2 * i + 2]
        m["cT"] = cT
        maps.append(m)
    return maps


def kernel(**inputs):
    nc = build_program()
    maps = make_in_maps(inputs, 8)
    res = run_bass_kernel_spmd(nc, maps, core_ids=list(range(8)))
    return np.concatenate([np.asarray(r["y"]) for r in res.results], axis=0).astype(np.float32)


def _rope_tables():
    pos = np.arange(LLAT)
    row, col = pos // 64, pos % 64
    half = 16
    inv = (10000.0 ** (-np.arange(half, dtype=np.float32) / half)).astype(np.float32)
    ang_r = row.astype(np.float32)[:, None] * inv[None, :]
    ang_c = col.astype(np.float32)[:, None] * inv[None, :]
    cr, sr, cc, sc = np.cos(ang_r), np.sin(ang_r), np.cos(ang_c), np.sin(ang_c)
    cos = np.concatenate([cr, cr, cc, cc], axis=1).astype(np.float32)
    sin = np.concatenate([-sr, sr, -sc, sc], axis=1).astype(np.float32)
    cos = cos.reshape(32, 128, 64).transpose(1, 0, 2).copy()
    sin = sin.reshape(32, 128, 64).transpose(1, 0, 2).copy()
    return cos, sin
```

```python
import numpy as np
from contextlib import ExitStack
import concourse.bass as bass
import concourse.mybir as mybir
from concourse.bass_utils import run_bass_kernel_spmd

F32 = mybir.dt.float32
BF16 = mybir.dt.bfloat16
AF = mybir.ActivationFunctionType
ALU = mybir.AluOpType
AX = mybir.AxisListType

D = 1024
NB = 2
LCTX = 256
LLAT = 4096
LT = LCTX + LLAT
NT = LT // 128
PROJ = 3104
EPS = 1e-6
NEG = -30000.0

ENGS = ['pe', 'act', 'dve', 'pool', 'sp']
EPOCH = 30000


class Buf:
    __slots__ = ('name', 'w', 'r', 'dsem')

    def __init__(self, name=''):
        self.name = name
        self.w = None
        self.r = {}
        self.dsem = None


class Sched:
    def __init__(self, nc, stack):
        self.nc = nc
        self.stack = stack
        self.cnt = {e: 0 for e in ENGS}
        self.esems = {e: [] for e in ENGS}
        self.items = {e: [] for e in ENGS}
        self.waited = {e: {} for e in ENGS}
        self.free_dsems = []
        self.phase_bufs = []
        self.outstanding = {}
        self.nsem = 0
        self.ninstr = 0
        self.cregs = {}
        self.bregs = {}
        self.bounds = []
        self._cond = None

    def _newsem(self, name):
        self.nsem += 1
        return self.stack.enter_context(self.nc.semaphore(name))

    def _esem(self, e, seq):
        ep = (seq - 1) // EPOCH
        while len(self.esems[e]) <= ep:
            self.esems[e].append(self._newsem(f"s_{e}_{len(self.esems[e])}"))
        return self.esems[e][ep], (seq - 1) % EPOCH + 1

    def buf(self, name=''):
        b = Buf(name)
        self.phase_bufs.append(b)
        return b

    def bufs(self, n, name=''):
        return [self.buf(f"{name}{i}") for i in range(n)]

    def op(self, eng, fn, reads=(), writes=(), dma=None, self_ok=False, track=True):
        deps = {}

        def add(p):
            if p is None:
                return
            sem, val, peng = p
            if self_ok and peng == eng:
                return
            k = sem.num
            if k not in deps or deps[k][1] < val:
                deps[k] = (sem, val)

        for b in reads:
            add(b.w)
        for b in writes:
            add(b.w)
            for p in b.r.values():
                add(p)
        if dma is None:
            self.cnt[eng] += 1
            sem, val = self._esem(eng, self.cnt[eng])
            inc = 1
            tok = (sem, val, eng)
        else:
            if dma.dsem is None:
                if self.free_dsems:
                    dma.dsem = self.free_dsems.pop()
                else:
                    dma.dsem = [self._newsem(f"d{self.nsem}"), 0]
            dma.dsem[1] += 16
            sem, val = dma.dsem[0], dma.dsem[1]
            inc = 16
            tok = (sem, val, 'dma')
        waits = []
        wd = self.waited[eng]
        for k, (s, v) in deps.items():
            if wd.get(k, 0) >= v:
                continue
            wd[k] = v
            waits.append((s, v))
        self.items[eng].append((fn, waits, sem, inc, val))
        self.ninstr += 1
        for b in writes:
            b.w = tok
            b.r = {}
        for b in reads:
            if b not in writes:
                b.r[sem.num] = tok
        if track:
            self.outstanding[sem.num] = (sem, val)
        return tok

    def breg(self, engine, value):
        if value not in self.bregs:
            r = engine.alloc_register(f"bnd_{value}")
            engine.reg_mov(r, value)
            self.bregs[value] = r
        return self.bregs[value]

    def cond_begin(self, flag_ap):
        self._cond = {'flag': flag_ap, 'start': {e: len(self.items[e]) for e in ENGS},
                      'waited': {e: dict(self.waited[e]) for e in ENGS}}
        for e in ENGS:
            self.items[e].append(('cond_begin', flag_ap))

    def cond_end(self):
        c = self._cond
        for e in ENGS:
            body = self.items[e][c['start'][e] + 1:]
            agg = {}
            for it in body:
                fn, waits, sem, inc = it[0], it[1], it[2], it[3]
                if fn is None or sem is None:
                    continue
                k = sem.num
                if k not in agg:
                    agg[k] = [sem, it[4] - inc, 0]
                agg[k][2] += inc
            self.items[e].append(('cond_end', list(agg.values())))
            self.waited[e] = c['waited'][e]
        self._cond = None

    def barrier(self):
        for e in ENGS:
            waits = []
            for k, (s, v) in self.outstanding.items():
                if self.waited[e].get(k, 0) >= v:
                    continue
                self.waited[e][k] = v
                waits.append((s, v))
            self.items[e].append((None, waits, None, 0, 0))
        self.outstanding = {}

    def flush(self):
        self.barrier()
        nc = self.nc
        with nc.Block() as block:
            regs = {'pe': block.tensor, 'act': block.scalar, 'dve': block.vector,
                    'pool': block.gpsimd, 'sp': block.sync}
            for e in ENGS:
                items = self.items[e]

                def body(engine, items=items, e=e):
                    guard = None
                    if e == 'pool':
                        for v in self.bounds:
                            self.breg(engine, v)
                    for it in items:
                        if it[0] == 'cond_begin':
                            if e not in self.cregs:
                                self.cregs[e] = engine.alloc_register(f"creg_{e}")
                            reg = self.cregs[e]
                            engine.reg_load(reg, it[1])
                            guard = engine.If_ne(reg, 0)
                            guard.__enter__()
                            continue
                        if it[0] == 'cond_end':
                            guard.__exit__(None, None, None)
                            eg = engine.Else()
                            eg.__enter__()
                            for sem, pre, tot in it[1]:
                                if pre > 0:
                                    engine.wait_ge(sem, pre)
                                engine.sem_inc(sem, tot)
                            eg.__exit__(None, None, None)
                            guard = None
                            continue
                        fn, waits, sem, inc, _ = it
                        for s, v in waits:
                            engine.wait_ge(s, v)
                        if fn is not None:
                            ins = fn(engine)
                            ins.then_inc(sem, inc)

                regs[e](body)
        self.items = {e: [] for e in ENGS}
        for b in self.phase_bufs:
            if b.dsem is not None:
                self.free_dsems.append(b.dsem)
                b.dsem = None
        self.phase_bufs = []


class Rot:
    def __init__(self, S, mk, n, name):
        self.t = [mk(f"{name}{i}") for i in range(n)]
        self.b = [S.buf(f"{name}{i}") for i in range(n)]
        self.i = 0

    def next(self):
        k = self.i % len(self.t)
        self.i += 1
        return self.t[k], self.b[k]


class Phase:
    _uid = [0]

    def __init__(self, C, name):
        self.C = C
        self.nc = C.nc
        self.S = C.S
        self.st = ExitStack()
        self.name = name

    def _nm(self, nm):
        Phase._uid[0] += 1
        return f"{self.name}_{nm}_{Phase._uid[0]}"

    def sb(self, shape, dt, nm='t'):
        return self.st.enter_context(self.nc.sbuf_tensor(self._nm(nm), list(shape), dt))

    def ps(self, shape, dt, nm='p'):
        return self.st.enter_context(self.nc.psum_tensor(self._nm(nm), list(shape), dt))

    def sbb(self, shape, dt, nm='t'):
        return self.sb(shape, dt, nm), self.S.buf(nm)

    def rot(self, n, shape, dt, nm, psum=False):
        f = self.ps if psum else self.sb
        return Rot(self.S, lambda s: f(shape, dt, nm), n, nm)

    def close(self):
        self.S.flush()
        self.st.close()


class Ctx:
    pass


def _dram_in(nc, name, shape, dt=F32):
    return nc.dram_tensor(name, list(shape), dt, kind="ExternalInput").ap()


def _dram_tmp(nc, name, shape, dt, dbg=False):
    return nc.dram_tensor(name, list(shape), dt, kind="ExternalOutput" if dbg else "Internal").ap()


import os as _os
USE_TTR = False

O_Q, O_K, O_V, O_R, O_LR, O_NQ, O_NK, O_NV = 0, 256, 512, 1024, 1536, 1568, 2080, 2592
F_FFN = 2816
F_MOE = 3584
NE = 8


def prenorm_tile(Ph, K, xt, bx, hT, bhT, c0, Gt, St, bGS, j, h32=None, bh32=None):
    S = Ph.S
    C = Ph.C
    junk, bj = K['junk'].next()
    st, bst = K['stat'].next()
    xn, bxn = K['xn'].next()
    ptr, bptr = K['ptr'].next()
    S.op('act', lambda e: e.activation(out=junk[:], in_=xt[:], func=AF.Square, accum_out=st[:, 0:1]),
         reads=[bx], writes=[bj, bst])
    S.op('dve', lambda e: e.tensor_scalar(out=st[:, 1:2], in0=st[:, 0:1], scalar1=1.0 / D, scalar2=EPS,
                                          op0=ALU.mult, op1=ALU.add), reads=[bst], writes=[bst])
    S.op('pool', lambda e: e.tensor_tensor(out=st[:, 2:3], in0=st[:, 1:2], in1=C.neghalf[:, 0:1], op=ALU.pow),
         reads=[bst], writes=[bst])
    S.op('act', lambda e: e.activation(out=xn[:], in_=xt[:], func=AF.Copy, scale=st[:, 2:3]),
         reads=[bx, bst], writes=[bxn])

    def tr(e):
        for k in range(8):
            ins = e.transpose(out=ptr[:, k, :], in_=xn[:, k * 128:(k + 1) * 128], identity=C.ident[:])
        return ins
    S.op('pe', tr, reads=[bxn], writes=[bptr], self_ok=True)
    for k in range(8):
        if k % 2 == 0:
            S.op('dve', lambda e, k=k: e.tensor_scalar(out=hT[:, k, c0:c0 + 128], in0=ptr[:, k, :],
                                                       scalar1=Gt[:, k, j:j + 1], scalar2=St[:, k, j:j + 1],
                                                       op0=ALU.mult, op1=ALU.add),
                 reads=[bptr, bGS], writes=[bhT])
        else:
            S.op('act', lambda e, k=k: e.activation(out=hT[:, k, c0:c0 + 128], in_=ptr[:, k, :], func=AF.Identity,
                                                    scale=Gt[:, k, j:j + 1], bias=St[:, k, j:j + 1]),
                 reads=[bptr, bGS], writes=[bhT])
    if h32 is not None:
        xn32, bxn32 = K['xn32'].next()
        p32, bp32 = K['p32'].next()
        S.op('act', lambda e: e.activation(out=xn32[:], in_=xt[:], func=AF.Copy, scale=st[:, 2:3]),
             reads=[bx, bst], writes=[bxn32])

        def tr32(e):
            for k in range(8):
                ins = e.transpose(out=p32[:, k, :], in_=xn32[:, k * 128:(k + 1) * 128], identity=C.ident32[:])
            return ins
        S.op('pe', tr32, reads=[bxn32], writes=[bp32], self_ok=True)
        for k in range(8):
            S.op('dve', lambda e, k=k: e.tensor_scalar(out=h32[:, k, :], in0=p32[:, k, :],
                                                       scalar1=Gt[:, k, j:j + 1], scalar2=St[:, k, j:j + 1],
                                                       op0=ALU.mult, op1=ALU.add),
                 reads=[bp32, bGS], writes=[bh32])


def prenorm_kit(Ph, with32=False):
    K = {
        'junk': Ph.rot(1, [128, D], BF16, 'junk'),
        'stat': Ph.rot(4, [128, 4], F32, 'stat'),
        'xn': Ph.rot(2, [128, D], BF16, 'xn'),
        'ptr': Ph.rot(2, [128, 8, 128], BF16, 'ptr', psum=True),
    }
    if with32:
        K['xn32'] = Ph.rot(2, [128, D], F32, 'xn32')
        K['p32'] = Ph.rot(1, [128, 8, 128], F32, 'p32', psum=True)
    return K


def res_src(C, l, stage, b, tile):
    if l == 0 and stage == 0:
        if tile < 2:
            return C.ctx_in[b, tile * 128:(tile + 1) * 128, :]
        return C.x_in[b, (tile - 2) * 128:(tile - 1) * 128, :]
    return C.xs[b, tile * 128:(tile + 1) * 128, :]


def phase_mod(C, l, LP):
    nc, S = C.nc, C.S
    Ph = Phase(C, f"mod{l}")
    C.G1, C.bG1 = LP.sbb([128, 8, 3], F32, 'G1')
    C.SH1 = LP.sb([128, 8, 3], F32, 'SH1')
    C.G2, C.bG2 = LP.sbb([128, 8, 3], F32, 'G2')
    C.SH2 = LP.sb([128, 8, 3], F32, 'SH2')
    C.GG1, C.bGG1 = LP.sbb([128, 3, D], F32, 'GG1')
    C.GG2, C.bGG2 = LP.sbb([128, 3, D], F32, 'GG2')
    if getattr(C, 'want_rows', False):
        C.G2row, C.bG2row = LP.sbb([128, 2, D], F32, 'G2row')
        C.S2row = LP.sb([128, 2, D], F32, 'S2row')
    scT, bscT = Ph.sbb([128, 8, 3], F32, 'scT')
    scB, bscB = Ph.sbb([128, 3, 8, 128], F32, 'scB')
    bada, bbada = Ph.sbb([128, 48, 3], F32, 'bada')
    gpre, bgpre = Ph.sbb([128, 2, 8, 3], F32, 'gpre')
    rowc, browc = Ph.sbb([128, 7, D], F32, 'rowc')
    sc1, bsc1 = Ph.sbb([128, 8, 3], F32, 'sc1')
    sc2, bsc2 = Ph.sbb([128, 8, 3], F32, 'sc2')
    wblk = Ph.rot(2, [128, 8, 1024], F32, 'wblk')
    pm = Ph.rot(2, [128, 8, 3], F32, 'pm', psum=True)
    pg = Ph.rot(2, [128, 512], F32, 'pg', psum=True)
    S.op('sp', lambda e: e.dma_start(out=scT[:], in_=C.cT[:, :, :]), writes=[bscT], dma=bscT)
    S.op('sp', lambda e: e.dma_start(out=bada[:], in_=C.badaT3[l]), writes=[bbada], dma=bbada)
    S.op('sp', lambda e: e.dma_start(out=gpre[:], in_=C.gpre3[l]), writes=[bgpre], dma=bgpre)
    S.op('sp', lambda e: e.dma_start(out=rowc[:], in_=C.rowc[l]), writes=[browc], dma=browc)
    S.op('act', lambda e: e.activation(out=scT[:], in_=scT[:], func=AF.Silu), reads=[bscT], writes=[bscT])
    for j in range(3):
        for kc in range(8):
            S.op('act', lambda e, j=j, kc=kc: e.activation(out=scB[:, j, kc, :], in_=C.ones[:], func=AF.Copy,
                                                           scale=scT[:, kc, j:j + 1]),
                 reads=[bscT], writes=[bscB])
    fm = [(0, C.SH1, C.bG1), (1, sc1, bsc1), (3, C.SH2, C.bG2), (4, sc2, bsc2)]
    for blk, dst, bdst in fm:
        wt, bw = wblk.next()
        S.op('sp', lambda e, wt=wt, blk=blk: e.dma_start(
            out=wt[:], in_=C.w_ada[l, :, blk * 1024:(blk + 1) * 1024].rearrange("(kc p) n -> p kc n", p=128)),
            writes=[bw], dma=bw)
        pmt, bpm = pm.next()

        def mm(e, wt=wt, pmt=pmt):
            for ch in range(8):
                for kc in range(8):
                    ins = e.matmul(pmt[:, ch, :], lhsT=wt[:, kc, ch * 128:(ch + 1) * 128], rhs=scT[:, kc, :],
                                   start=(kc == 0), stop=(kc == 7))
            return ins
        S.op('pe', mm, reads=[bw, bscT], writes=[bpm], self_ok=True)
        S.op('dve', lambda e, dst=dst, pmt=pmt, blk=blk: e.tensor_tensor(
            out=dst[:], in0=pmt[:], in1=bada[:, blk * 8:(blk + 1) * 8, :], op=ALU.add),
            reads=[bpm, bbada], writes=[bdst])
    S.op('dve', lambda e: e.scalar_tensor_tensor(out=C.G1[:], in0=sc1[:], scalar=1.0, in1=gpre[:, 0], op0=ALU.add,
                                                 op1=ALU.mult), reads=[bsc1, bgpre], writes=[C.bG1])
    S.op('dve', lambda e: e.scalar_tensor_tensor(out=C.G2[:], in0=sc2[:], scalar=1.0, in1=gpre[:, 1], op0=ALU.add,
                                                 op1=ALU.mult), reads=[bsc2, bgpre], writes=[C.bG2])
    for gi, blk, GG, bGG in [(0, 2, C.GG1, C.bGG1), (1, 5, C.GG2, C.bGG2)]:
        wt, bw = wblk.next()
        S.op('sp', lambda e, wt=wt, blk=blk: e.dma_start(
            out=wt[:], in_=C.w_ada[l, :, blk * 1024:(blk + 1) * 1024].rearrange("(kc p) n -> p kc n", p=128)),
            writes=[bw], dma=bw)
        for j in range(3):
            for half in range(2):
                pgt, bpg = pg.next()
                hs = slice(half * 512, (half + 1) * 512)

                def mm(e, wt=wt, pgt=pgt, j=j, hs=hs):
                    for kc in range(8):
                        ins = e.matmul(pgt[:], lhsT=scB[:, j, kc, :], rhs=wt[:, kc, hs], start=(kc == 0),
                                       stop=(kc == 7))
                    return ins
                S.op('pe', mm, reads=[bw, bscB], writes=[bpg], self_ok=True)
                S.op('dve', lambda e, GG=GG, pgt=pgt, j=j, hs=hs, gi=gi: e.tensor_tensor(
                    out=GG[:, j, hs], in0=pgt[:], in1=rowc[:, gi, hs], op=ALU.add), reads=[bpg, browc], writes=[bGG])
                S.op('pool', lambda e, GG=GG, j=j, hs=hs, gi=gi: e.tensor_tensor(
                    out=GG[:, j, hs], in0=GG[:, j, hs], in1=rowc[:, 2 + gi, hs], op=ALU.mult),
                    reads=[bGG, browc], writes=[bGG])
    if getattr(C, 'want_rows', False):
        for blk, dst, ri in [(3, C.S2row, 4), (4, C.G2row, 5)]:
            wt, bw = wblk.next()
            S.op('sp', lambda e, wt=wt, blk=blk: e.dma_start(
                out=wt[:], in_=C.w_ada[l, :, blk * 1024:(blk + 1) * 1024].rearrange("(kc p) n -> p kc n", p=128)),
                writes=[bw], dma=bw)
            for j in range(2):
                for half in range(2):
                    pgt, bpg = pg.next()
                    hs = slice(half * 512, (half + 1) * 512)

                    def mm(e, wt=wt, pgt=pgt, j=j, hs=hs):
                        for kc in range(8):
                            ins = e.matmul(pgt[:], lhsT=scB[:, j, kc, :], rhs=wt[:, kc, hs], start=(kc == 0),
                                           stop=(kc == 7))
                        return ins
                    S.op('pe', mm, reads=[bw, bscB], writes=[bpg], self_ok=True)
                    S.op('dve', lambda e, dst=dst, pgt=pgt, j=j, hs=hs, ri=ri: e.tensor_tensor(
                        out=dst[:, j, hs], in0=pgt[:], in1=rowc[:, ri, hs], op=ALU.add), reads=[bpg, browc],
                        writes=[C.bG2row])
                    if blk == 4:
                        S.op('dve', lambda e, dst=dst, j=j, hs=hs: e.scalar_tensor_tensor(
                            out=dst[:, j, hs], in0=dst[:, j, hs], scalar=1.0, in1=rowc[:, 6, hs], op0=ALU.add,
                            op1=ALU.mult), reads=[C.bG2row, browc], writes=[C.bG2row])
    Ph.close()


def phase_proj(C, l):
    nc, S = C.nc, C.S
    Ph = Phase(C, f"proj{l}")
    K = prenorm_kit(Ph)
    win, bwin = Ph.sbb([128, 8, PROJ], BF16, 'win')
    cos, bcos = Ph.sbb([128, 32, 64], F32, 'cos')
    sin, bsin = Ph.sbb([128, 32, 64], F32, 'sin')
    npc = 4
    pw = PROJ // npc
    for i in range(npc):
        S.op('pool', lambda e, i=i: e.dma_start(
            out=win[:, :, i * pw:(i + 1) * pw],
            in_=C.w_in[l, :, i * pw:(i + 1) * pw].rearrange("(kc p) n -> p kc n", p=128)), writes=[bwin], dma=bwin)
    S.op('sp', lambda e: e.dma_start(out=cos[:], in_=C.rope_cos[:, :, :]), writes=[bcos], dma=bcos)
    S.op('sp', lambda e: e.dma_start(out=sin[:], in_=C.rope_sin[:, :, :]), writes=[bsin], dma=bsin)
    S.op('dve', lambda e: e.tensor_scalar(out=win[:, :, O_Q:O_Q + 256], in0=win[:, :, O_Q:O_Q + 256], scalar1=0.125,
                                          scalar2=None, op0=ALU.mult), reads=[bwin], writes=[bwin])
    S.op('dve', lambda e: e.tensor_scalar(out=win[:, :, O_NQ:O_NQ + 512], in0=win[:, :, O_NQ:O_NQ + 512],
                                          scalar1=0.125, scalar2=None, op0=ALU.mult), reads=[bwin], writes=[bwin])
    xr = Ph.rot(3, [128, D], F32, 'x')
    hTr = Ph.rot(2, [128, 8, 256], BF16, 'hT')
    stg = Ph.rot(2, [128, 2048], BF16, 'stg')
    fstg = Ph.rot(2, [128, 8, 256], BF16, 'fstg')
    lstg = Ph.rot(2, [16, 2, 256], F32, 'lstg')
    t1r = Ph.rot(2, [128, 512], F32, 't1')
    t2r = Ph.rot(2, [128, 512], F32, 't2')
    ptok = Ph.rot(2, [128, 512], F32, 'ptok', psum=True)
    pfe = Ph.rot(2, [128, 512], F32, 'pfe', psum=True)
    tokcols = [(O_Q, O_Q + 512), (O_V, O_V + 512), (O_R, O_R + 512), (O_NV, O_NV + 512)]
    def pre(b, g):
        j = 2 if g == 0 else b
        hT, bhT = hTr.next()
        for t in range(2):
            tile = g * 2 + t
            xt, bx = xr.next()
            S.op('sp', lambda e, xt=xt, tile=tile, b=b: e.dma_start(out=xt[:], in_=res_src(C, l, 0, b, tile)),
                 writes=[bx], dma=bx)
            prenorm_tile(Ph, K, xt, bx, hT, bhT, t * 128, C.G1, C.SH1, C.bG1, j)
        return hT, bhT

    groups = [(b, g) for b in range(NB) for g in range(LT // 256)]
    cur = pre(*groups[0])
    for gi_, (b, g) in enumerate(groups):
        if True:
            hT, bhT = cur
            if gi_ + 1 < len(groups):
                cur = pre(*groups[gi_ + 1])
            for t in range(2):
                tile = g * 2 + t
                st, bst = stg.next()
                for cb in range(4):
                    pt_, bpt = ptok.next()
                    c0, c1 = tokcols[cb]

                    def mm(e, pt_=pt_, t=t, c0=c0, c1=c1, hT=hT):
                        for kc in range(8):
                            ins = e.matmul(pt_[:], lhsT=hT[:, kc, t * 128:(t + 1) * 128], rhs=win[:, kc, c0:c1],
                                           start=(kc == 0), stop=(kc == 7))
                        return ins
                    S.op('pe', mm, reads=[bhT, bwin], writes=[bpt], self_ok=True)
                    so = st[:, cb * 512:(cb + 1) * 512]
                    if cb == 0 and g > 0:
                        lt = tile - 2
                        t1, bt1 = t1r.next()
                        t2, bt2 = t2r.next()
                        cb_ = cos[:, lt, :].unsqueeze(1).to_broadcast([128, 8, 64])
                        p3 = pt_[:].rearrange("p (a d) -> p a d", a=8)
                        S.op('dve', lambda e, t1=t1, p3=p3, cb_=cb_: e.tensor_tensor(
                            out=t1[:].rearrange("p (a d) -> p a d", a=8), in0=p3, in1=cb_, op=ALU.mult),
                            reads=[bpt, bcos], writes=[bt1])
                        p5 = pt_[:].rearrange("p (a b c d) -> p a b c d", a=8, b=2, c=2)
                        s5 = sin[:, lt, :].rearrange("p (b c d) -> p b c d", b=2, c=2)
                        t25 = t2[:].rearrange("p (a b c d) -> p a b c d", a=8, b=2, c=2)
                        for hf in range(2):
                            sb_ = s5[:, :, hf, :].unsqueeze(1).to_broadcast([128, 8, 2, 16])
                            S.op('dve', lambda e, t25=t25, p5=p5, sb_=sb_, hf=hf: e.tensor_tensor(
                                out=t25[:, :, :, hf, :], in0=p5[:, :, :, 1 - hf, :], in1=sb_, op=ALU.mult),
                                reads=[bpt, bsin], writes=[bt2])
                        S.op('pool', lambda e, so=so, t1=t1, t2=t2: e.tensor_tensor(out=so, in0=t1[:], in1=t2[:],
                                                                                   op=ALU.add),
                             reads=[bt1, bt2], writes=[bst])
                    elif cb == 2:
                        S.op('act', lambda e, so=so, pt_=pt_: e.activation(out=so, in_=pt_[:], func=AF.Silu),
                             reads=[bpt], writes=[bst])
                    else:
                        S.op('act', lambda e, so=so, pt_=pt_: e.activation(out=so, in_=pt_[:], func=AF.Copy),
                             reads=[bpt], writes=[bst])
                S.op('sp', lambda e, st=st, tile=tile, b=b: e.dma_start(
                    out=C.tokmaj[b, tile * 128:(tile + 1) * 128, :], in_=st[:]), reads=[bst], dma=bst)
            ft, bft = fstg.next()
            for cc in range(8):
                pf, bpf = pfe.next()
                c0 = O_NQ + cc * 128

                def mmf(e, pf=pf, c0=c0, hT=hT):
                    for kc in range(8):
                        ins = e.matmul(pf[:, 0:256], lhsT=win[:, kc, c0:c0 + 128], rhs=hT[:, kc, :], start=(kc == 0),
                                       stop=(kc == 7))
                    return ins
                S.op('pe', mmf, reads=[bhT, bwin], writes=[bpf], self_ok=True)
                S.op('dve', lambda e, ft=ft, pf=pf, cc=cc: e.tensor_copy(out=ft[:, cc, :], in_=pf[:, 0:256]),
                     reads=[bpf], writes=[bft])
            S.op('sp', lambda e, ft=ft, g=g, b=b: e.dma_start(
                out=C.nqkT[b].rearrange("(cc p) t -> p cc t", p=128)[:, :, g * 256:(g + 1) * 256], in_=ft[:]),
                reads=[bft], dma=bft)
            lt_, blt = lstg.next()
            for d in range(2):
                pf, bpf = pfe.next()
                c0 = O_LR + 16 * d

                def mml(e, pf=pf, c0=c0, hT=hT):
                    for kc in range(8):
                        ins = e.matmul(pf[0:16, 0:256], lhsT=win[:, kc, c0:c0 + 16], rhs=hT[:, kc, :],
                                       start=(kc == 0), stop=(kc == 7))
                    return ins
                S.op('pe', mml, reads=[bhT, bwin], writes=[bpf], self_ok=True)
                S.op('dve', lambda e, lt_=lt_, pf=pf, d=d: e.tensor_copy(out=lt_[:, d, :], in_=pf[0:16, 0:256]),
                     reads=[bpf], writes=[blt])
            S.op('sp', lambda e, lt_=lt_, g=g, b=b: e.dma_start(
                out=C.lrT[b].rearrange("d r t -> r d t")[:, :, g * 256:(g + 1) * 256], in_=lt_[:]),
                reads=[blt], dma=blt)
    Ph.close()


def phase_gla(C, l, last):
    S = C.S
    Ph = Phase(C, f"gla{l}")
    tri, btri = Ph.sbb([128, 4, 128], F32, 'tri')
    wg, bwg = Ph.sbb([16, 2, 256], F32, 'wg')
    bg, bbg = Ph.sbb([1, 2, 256], F32, 'bg')
    gn, bgn = Ph.sbb([128, 512], F32, 'gn')
    S.op('sp', lambda e: e.dma_start(out=tri[:], in_=C.tri_in[:, :, :]), writes=[btri], dma=btri)
    S.op('sp', lambda e: e.dma_start(out=wg[:], in_=C.wgate[l].rearrange("d r n -> r d n")), writes=[bwg], dma=bwg)
    S.op('sp', lambda e: e.dma_start(out=bg[:], in_=C.bgate[l].rearrange("d o n -> o d n")), writes=[bbg], dma=bbg)
    S.op('sp', lambda e: e.dma_start(out=gn[:], in_=C.gnormB[l]), writes=[bgn], dma=bgn)
    lrr = Ph.rot(1, [16, 2, LT], F32, 'lr')
    ost = Ph.sb([128, NT, 512], F32, 'ost')
    bost = [S.buf(f"ost{i}") for i in range(NT)]
    Sst = [Ph.sbb([128, 2, 128], F32, 'Sst') for _ in range(2)]
    Sbf = [Ph.sbb([128, 2, 128], BF16, 'Sbf') for _ in range(2)]
    qkvr = Ph.rot(4, [128, 1024], BF16, 'qkv')
    rgr = Ph.rot(2, [128, 512], BF16, 'rg')
    e1r = Ph.rot(2, [128, 256], F32, 'e1')
    spr = Ph.rot(2, [128, 256], F32, 'sp')
    ebr = Ph.rot(2, [128, 2, 128], F32, 'eb')
    enbr = Ph.rot(2, [128, 2, 128], F32, 'enb')
    eEr = Ph.rot(2, [128, 256], F32, 'eE')
    qdr = Ph.rot(2, [128, 4, 128], BF16, 'qd')
    kdr = Ph.rot(2, [128, 4, 128], BF16, 'kd')
    for rr in (qdr, kdr):
        for t_, b_ in zip(rr.t, rr.b):
            S.op('pool', lambda e, t_=t_: e.memset(t_[:], 0.0), writes=[b_])
    ker = Ph.rot(2, [128, 256], BF16, 'kend')
    Amr = Ph.rot(2, [128, 4, 128], BF16, 'Am')
    osr = Ph.rot(2, [128, 512], F32, 'osum')
    sqr = Ph.rot(2, [128, 512], F32, 'sq')
    ogr = Ph.rot(2, [128, 512], BF16, 'og')
    str_ = Ph.rot(4, [128, 12], F32, 'gst')
    plr = Ph.rot(1, [128, 512], F32, 'pl', psum=True)
    pber = Ph.rot(1, [128, 512], F32, 'pbe', psum=True)
    pTr = Ph.rot(1, [128, 8, 128], BF16, 'pT', psum=True)
    pAr = Ph.rot(2, [128, 4, 128], F32, 'pA', psum=True)
    por = Ph.rot(2, [128, 4, 128], F32, 'po', psum=True)
    pdsr = Ph.rot(1, [128, 2, 256], F32, 'pds', psum=True)

    import os
    STG = int(os.environ.get('GLA_STG', '99'))
    NTL = int(os.environ.get('GLA_NT', str(NT)))

    def block(b, d, tile, first, lr):
        lrt, blr = lr
        rows_of = lambda hp: slice(hp * 64, hp * 64 + 64)
        r0 = tile * 128
        qkv, bqkv = qkvr.next()
        S.op('sp', lambda e: e.dma_start(out=qkv[:], in_=C.tokmaj[b, r0:r0 + 128, 0:1024]), writes=[bqkv], dma=bqkv)
        pl, bpl = plr.next()

        def mml(e):
            e.matmul(pl[:, 0:256], lhsT=lrt[:, d, r0:r0 + 128], rhs=wg[:, d, :], start=True, stop=False)
            return e.matmul(pl[:, 0:256], lhsT=C.ones[0:1, :], rhs=bg[0:1, d, :], start=False, stop=True)
        S.op('pe', mml, reads=[blr, bwg, bbg], writes=[bpl], self_ok=True)
        if STG < 2:
            return
        e1, be1 = e1r.next()
        sp_, bsp = spr.next()
        S.op('act', lambda e: e.activation(out=e1[:], in_=pl[:, 0:256], func=AF.Exp, scale=-1.0), reads=[bpl],
             writes=[be1])
        S.op('act', lambda e: e.activation(out=sp_[:], in_=e1[:], func=AF.Ln, bias=1.0), reads=[be1], writes=[bsp])
        if STG < 3:
            return
        Rm = tri[:, 0 if d == 0 else 1, :]
        Um = tri[:, 3 if d == 0 else 2, :]
        pbe, bpbe = pber.next()

        def mmb(e):
            for g in range(2):
                e.matmul(pbe[:, g * 128:(g + 1) * 128], lhsT=sp_[:, g * 128:(g + 1) * 128], rhs=Rm, start=True,
                         stop=True)
            return e.matmul(pbe[:, 256:512], lhsT=Um, rhs=sp_[:], start=True, stop=True)
        S.op('pe', mmb, reads=[bsp, btri], writes=[bpbe], self_ok=True)
        if STG < 4:
            return
        eb, beb = ebr.next()
        enb, benb = enbr.next()
        eE, beE = eEr.next()
        pb3 = pbe[:, 0:256].rearrange("p (g t) -> p g t", g=2)
        S.op('act', lambda e: e.activation(out=eb[:], in_=pb3, func=AF.Exp, scale=-1.0 / 16), reads=[bpbe],
             writes=[beb])
        S.op('act', lambda e: e.activation(out=enb[:], in_=pb3, func=AF.Exp, scale=1.0 / 16), reads=[bpbe],
             writes=[benb])
        S.op('act', lambda e: e.activation(out=eE[:], in_=pbe[:, 256:512], func=AF.Exp, scale=-1.0 / 16),
             reads=[bpbe], writes=[beE])
        if STG < 5:
            return
        pT, bpT = pTr.next()

        def tr(e):
            for i in range(4):
                ins = e.transpose(out=pT[:, i, :], in_=qkv[:, i * 128:(i + 1) * 128], identity=C.ident[:])
            return ins
        S.op('pe', tr, reads=[bqkv], writes=[bpT], self_ok=True)
        if STG < 6:
            return
        qd, bqd = qdr.next()
        kd, bkd = kdr.next()
        kend, bke = ker.next()
        for h in range(4):
            g, rs = h // 2, rows_of(h % 2)
            S.op('dve', lambda e, h=h, g=g, rs=rs: e.tensor_tensor(out=qd[rs, h, :], in0=pT[rs, g, :], in1=eb[rs, g, :],
                                                                   op=ALU.mult), reads=[bpT, beb], writes=[bqd])
            S.op('dve', lambda e, h=h, g=g, rs=rs: e.tensor_tensor(out=kd[rs, h, :], in0=pT[rs, 2 + g, :],
                                                                   in1=enb[rs, g, :], op=ALU.mult),
                 reads=[bpT, benb], writes=[bkd])
        S.op('pool', lambda e: e.tensor_tensor(out=kend[:], in0=qkv[:, 256:512], in1=eE[:], op=ALU.mult),
             reads=[bqkv, beE], writes=[bke])
        if STG < 7:
            return
        pA, bpA = pAr.next()

        def mmA(e):
            for h in range(4):
                ins = e.matmul(pA[:, h, :], lhsT=kd[:, h, :], rhs=qd[:, h, :], start=True, stop=True)
            return ins
        S.op('pe', mmA, reads=[bkd, bqd], writes=[bpA], self_ok=True)
        if STG < 8:
            return
        Am, bAm = Amr.next()
        mask = tri[:, 0 if d == 0 else 1, :].unsqueeze(1).to_broadcast([128, 4, 128])
        S.op('dve', lambda e: e.tensor_tensor(out=Am[:], in0=pA[:], in1=mask, op=ALU.mult), reads=[bpA, btri],
             writes=[bAm])
        if STG < 9:
            return
        po, bpo = por.next()
        sbf, bsbf = Sbf[d]

        def mmo(e):
            for h in range(4):
                g, rs = h // 2, rows_of(h % 2)
                e.matmul(po[:, h, :], lhsT=Am[:, h, :], rhs=qkv[:, 512 + h * 128:512 + (h + 1) * 128], start=True,
                         stop=False)
                ins = e.matmul(po[:, h, :], lhsT=qd[:, h, :], rhs=sbf[:, g, :], start=False, stop=True)
            return ins
        S.op('pe', mmo, reads=[bAm, bqkv, bqd, bsbf], writes=[bpo], self_ok=True)
        need_out = not (last and tile < 2)
        if STG < 10:
            return
        if need_out:
            if first:
                S.op('act', lambda e: e.activation(out=ost[:, tile, :], in_=po[:].rearrange("p h v -> p (h v)"),
                                                   func=AF.Copy), reads=[bpo], writes=[bost[tile]])
            else:
                osum, bos = osr.next()
                sq, bsq = sqr.next()
                og, bog = ogr.next()
                st, bst = str_.next()
                rg, brg = rgr.next()
                S.op('sp', lambda e: e.dma_start(out=rg[:], in_=C.tokmaj[b, r0:r0 + 128, 1024:1536]), writes=[brg],
                     dma=brg)
                S.op('dve', lambda e: e.tensor_tensor(out=osum[:], in0=po[:].rearrange("p h v -> p (h v)"),
                                                      in1=ost[:, tile, :], op=ALU.add), reads=[bpo, bost[tile]],
                     writes=[bos])
                S.op('pool', lambda e: e.tensor_tensor(out=sq[:], in0=osum[:], in1=osum[:], op=ALU.mult), reads=[bos],
                     writes=[bsq])
                S.op('dve', lambda e: e.tensor_reduce(out=st[:, 0:4], in_=sq[:].rearrange("p (h v) -> p h v", h=4),
                                                      axis=AX.X, op=ALU.add), reads=[bsq], writes=[bst])
                S.op('dve', lambda e: e.tensor_scalar(out=st[:, 4:8], in0=st[:, 0:4], scalar1=1.0 / 128, scalar2=EPS,
                                                      op0=ALU.mult, op1=ALU.add), reads=[bst], writes=[bst])
                S.op('pool', lambda e: e.tensor_tensor(out=st[:, 8:12], in0=st[:, 4:8], in1=C.neghalf[:, 0:4],
                                                       op=ALU.pow), reads=[bst], writes=[bst])
                S.op('dve', lambda e: e.tensor_tensor(
                    out=sq[:].rearrange("p (h v) -> p h v", h=4), in0=osum[:].rearrange("p (h v) -> p h v", h=4),
                    in1=st[:, 8:12].unsqueeze(2).to_broadcast([128, 4, 128]), op=ALU.mult), reads=[bos, bst],
                    writes=[bsq])
                S.op('pool', lambda e: e.tensor_tensor(out=sq[:], in0=sq[:], in1=gn[:], op=ALU.mult), reads=[bsq, bgn],
                     writes=[bsq])
                S.op('pool', lambda e: e.tensor_tensor(out=og[:], in0=sq[:], in1=rg[:], op=ALU.mult),
                     reads=[bsq, brg], writes=[bog])
                S.op('sp', lambda e: e.dma_start(out=C.cat[b, r0:r0 + 128, 0:512], in_=og[:]), reads=[bog], dma=bog)
        if STG < 11:
            return
        pds, bpds = pdsr.next()

        def mmds(e):
            for g in range(2):
                ins = e.matmul(pds[:, g, :], lhsT=kend[:, g * 128:(g + 1) * 128],
                               rhs=qkv[:, 512 + g * 256:512 + (g + 1) * 256], start=True, stop=True)
            return ins
        S.op('pe', mmds, reads=[bke, bqkv], writes=[bpds], self_ok=True)
        if STG < 12:
            return
        sst, bsst = Sst[d]
        dc = 127 if d == 0 else 0
        for g in range(2):
            for hp in range(2):
                rs = rows_of(hp)
                S.op('dve', lambda e, g=g, hp=hp, rs=rs: e.scalar_tensor_tensor(
                    out=sst[rs, g, :], in0=sst[rs, g, :], scalar=eb[rs, g, dc:dc + 1],
                    in1=pds[rs, g, hp * 128:(hp + 1) * 128], op0=ALU.mult, op1=ALU.add),
                    reads=[bsst, beb, bpds], writes=[bsst])
        S.op('act', lambda e: e.activation(out=sbf[:], in_=sst[:], func=AF.Copy), reads=[bsst], writes=[bsbf])

    for b in range(NB):
        lr = lrr.next()
        S.op('sp', lambda e, lr=lr, b=b: e.dma_start(out=lr[0][:], in_=C.lrT[b].rearrange("d r t -> r d t")),
             writes=[lr[1]], dma=lr[1])
        for d in range(2):
            S.op('pool', lambda e, d=d: e.memset(Sst[d][0][:], 0.0), writes=[Sst[d][1]])
            S.op('pool', lambda e, d=d: e.memset(Sbf[d][0][:], 0.0), writes=[Sbf[d][1]])
        orders = [list(range(NT)), [1, 0] + list(range(NT - 1, 1, -1))]
        done = set()
        for i in range(NTL):
            for d in range(2):
                tile = orders[d][i]
                block(b, d, tile, tile not in done, lr)
                done.add(tile)
    Ph.close()


NA_SHIFT = 0.0


def phase_na(C, l, last):
    S = C.S
    Ph = Phase(C, f"na{l}")
    kTr = Ph.rot(1, [128, 4, LT], BF16, 'kT')
    qTr = Ph.rot(2, [128, LT], BF16, 'qT')
    for t_, b_ in zip(qTr.t, qTr.b):
        S.op('pool', lambda e, t_=t_: e.memset(t_[:], 0.0), writes=[b_])
    Vr = Ph.rot(1, [128, NT, 8, 65], BF16, 'V')
    for t_, b_ in zip(Vr.t, Vr.b):
        S.op('pool', lambda e, t_=t_: e.memset(t_[:], 1.0), writes=[b_])
    vstr = Ph.rot(2, [128, 8, 512], BF16, 'vst')
    onar = Ph.rot(1, [128, NT, 512], BF16, 'ona')
    biasr = Ph.rot(1, [128, 5, 5, 128], F32, 'bias')
    ssr = Ph.rot(4, [128, 5, 128], F32, 's')
    pr = Ph.rot(4, [128, 7, 128], BF16, 'p')
    str_ = Ph.rot(8, [128, 4], F32, 'nst')
    negc, bnegc = Ph.sbb([128, 1], F32, 'negc')
    S.op('pool', lambda e: e.memset(negc[:], -NA_SHIFT), writes=[bnegc])
    psr = Ph.rot(3, [128, 8, 128], F32, 'ps', psum=True)
    po_t = Ph.ps([128, 512], F32, 'po')
    po_b = [S.buf(f"po{i}") for i in range(7)]
    po_i = [0]

    def unit(b, h, qt, kT, bkT, qT, bqT, V, bV, ona, bona, bias, bbias):
        g = h // 2
        q0 = qt * 128
        if qt >= 2:
            j = qt - 2
            ts = min(max(j - 2, 0), 27)
            pi = {0: 0, 1: 1, 30: 3, 31: 4}.get(j, 2)
            kcols = [256 + 128 * (ts + k) for k in range(5)] + [0, 128]
            vt = [2 + ts + k for k in range(5)] + [0, 1]
            nl = 5
        else:
            kcols = [0, 128]
            vt = [0, 1]
            nl = 0
        nblk = len(kcols)
        ps_, bps = psr.next()

        def mm(e):
            for kb in range(nblk):
                ins = e.matmul(ps_[:, kb, :], lhsT=kT[:, g, kcols[kb]:kcols[kb] + 128], rhs=qT[:, q0:q0 + 128],
                               start=True, stop=True)
            return ins
        S.op('pe', mm, reads=[bqT, bkT], writes=[bps], self_ok=True)
        p, bp = pr.next()
        if nl:
            s_, bs = ssr.next()
            S.op('dve', lambda e: e.tensor_tensor(out=s_[:], in0=ps_[:, 0:5, :], in1=bias[:, pi, :, :], op=ALU.add),
                 reads=[bps, bbias], writes=[bs])
            S.op('act', lambda e: e.activation(out=p[:, 0:5, :], in_=s_[:], func=AF.Exp, bias=negc[:, 0:1], scale=1.0),
                 reads=[bs, bnegc], writes=[bp])
        S.op('act', lambda e: e.activation(out=p[:, nl:nblk, :], in_=ps_[:, nl:nblk, :], func=AF.Exp,
                                           bias=negc[:, 0:1], scale=1.0), reads=[bps, bnegc], writes=[bp])
        slot = po_i[0] % 7
        po_i[0] += 1
        po = po_t[:, slot * 65:(slot + 1) * 65]
        bpo = po_b[slot]

        def mmpv(e):
            for kb in range(nblk):
                ins = e.matmul(po, lhsT=p[:, kb, :], rhs=V[:, vt[kb], h, :], start=(kb == 0), stop=(kb == nblk - 1))
            return ins
        S.op('pe', mmpv, reads=[bp, bV], writes=[bpo], self_ok=True)
        st, bst = str_.next()
        S.op('dve', lambda e: e.reciprocal(out=st[:, 0:1], in_=po[:, 64:65]), reads=[bpo], writes=[bst])
        S.op('act', lambda e: e.activation(out=ona[:, qt, h * 64:(h + 1) * 64], in_=po[:, 0:64], func=AF.Copy,
                                           scale=st[:, 0:1]), reads=[bpo, bst], writes=[bona])

    for b in range(NB):
        kT, bkT = kTr.next()
        V, bV = Vr.next()
        ona, bona = onar.next()
        for g in range(4):
            S.op('sp', lambda e, kT=kT, b=b, g=g: e.dma_start(
                out=kT[:, g, :], in_=C.nqkT[b, 512 + g * 128:512 + (g + 1) * 128, :]), writes=[bkT], dma=bkT)
        for t0 in range(0, NT, 8):
            t1 = min(NT, t0 + 8)
            vs, bvs = vstr.next()
            S.op('sp', lambda e, vs=vs, b=b, t0=t0, t1=t1: e.dma_start(
                out=vs[:, 0:t1 - t0, :],
                in_=C.tokmaj[b, t0 * 128:t1 * 128, 1536:2048].rearrange("(t p) c -> p t c", p=128)),
                writes=[bvs], dma=bvs)
            S.op('pool', lambda e, vs=vs, V=V, t0=t0, t1=t1: e.tensor_copy(
                out=V[:, t0:t1, :, 0:64], in_=vs[:, 0:t1 - t0, :].rearrange("p t (h d) -> p t h d", h=8)),
                reads=[bvs], writes=[bV])
        if b == NB - 1 and getattr(C, 'moe_cast_pending', False):
            C.moe_cast_pending = False
            for src, dst in zip([C.moe_wg, C.moe_wu, C.moe_wd], C.moe_bf):
                for ex in range(NE):
                    S.op('pool', lambda e, src=src, dst=dst, ex=ex: e.dma_start(out=dst[ex], in_=src[ex]),
                         writes=[C.bmoe], dma=C.bmoe, track=False)
        for h in range(8):
            bias, bbias = biasr.next()
            S.op('sp', lambda e, bias=bias, h=h: e.dma_start(out=bias[:], in_=C.natb[l, h]), writes=[bbias], dma=bbias)
            qT, bqT = qTr.next()
            hr = slice((h % 2) * 64, (h % 2) * 64 + 64)
            S.op('sp', lambda e, qT=qT, b=b, h=h, hr=hr: e.dma_start(
                out=qT[hr, :], in_=C.nqkT[b, h * 64:(h + 1) * 64, :]), writes=[bqT], dma=bqT)
            qts = list(range(2, NT)) + ([] if last else [0, 1])
            for qt in qts:
                unit(b, h, qt, kT, bkT, qT, bqT, V, bV, ona, bona, bias, bbias)
        for t0 in range(2 if last else 0, NT, 8):
            t1 = min(NT, t0 + 8)
            S.op('sp', lambda e, ona=ona, b=b, t0=t0, t1=t1: e.dma_start(
                out=C.cat[b, t0 * 128:t1 * 128, 512:1024].rearrange("(t p) c -> p t c", p=128), in_=ona[:, t0:t1, :]),
                reads=[bona], dma=bona)
    Ph.close()


def phase_outproj(C, l, last):
    S = C.S
    Ph = Phase(C, f"op{l}")
    wo, bwo = Ph.sbb([128, 8, D], BF16, 'wo')
    S.op('pool', lambda e: e.dma_start(out=wo[:], in_=C.w_out[l].rearrange("(kc p) n -> p kc n", p=128)),
         writes=[bwo], dma=bwo)
    ctr = Ph.rot(2, [128, D], BF16, 'ct')
    xr = Ph.rot(2, [128, D], F32, 'x')
    cTsr = Ph.rot(2, [128, 8, 128], BF16, 'cTs')
    ttr = Ph.rot(2, [128, D], F32, 'tt')
    junkr = Ph.rot(1, [128, D], BF16, 'junk')
    str_ = Ph.rot(4, [128, 4], F32, 'ost')
    pTr = Ph.rot(2, [128, 8, 128], BF16, 'pT', psum=True)
    pyr = Ph.rot(2, [128, D], F32, 'py', psum=True)
    for b in range(NB):
        for tile in (range(2, NT) if last else range(NT)):
            j = 2 if tile < 2 else b
            r0 = tile * 128
            ct, bct = ctr.next()
            xt, bx = xr.next()
            S.op('sp', lambda e, ct=ct, b=b, r0=r0: e.dma_start(out=ct[:], in_=C.cat[b, r0:r0 + 128, :]), writes=[bct],
                 dma=bct)
            S.op('sp', lambda e, xt=xt, b=b, tile=tile: e.dma_start(out=xt[:], in_=res_src(C, l, 0, b, tile)),
                 writes=[bx], dma=bx)
            pT, bpT = pTr.next()

            def tr(e, pT=pT, ct=ct):
                for k in range(8):
                    ins = e.transpose(out=pT[:, k, :], in_=ct[:, k * 128:(k + 1) * 128], identity=C.ident[:])
                return ins
            S.op('pe', tr, reads=[bct], writes=[bpT], self_ok=True)
            cTs, bcTs = cTsr.next()
            S.op('dve', lambda e, cTs=cTs, pT=pT: e.tensor_copy(out=cTs[:, 0:4, :], in_=pT[:, 0:4, :]), reads=[bpT],
                 writes=[bcTs])
            S.op('act', lambda e, cTs=cTs, pT=pT: e.activation(out=cTs[:, 4:8, :], in_=pT[:, 4:8, :], func=AF.Copy),
                 reads=[bpT], writes=[bcTs])
            py, bpy = pyr.next()

            def mm(e, py=py, cTs=cTs):
                for half in range(2):
                    for kc in range(8):
                        ins = e.matmul(py[:, half * 512:(half + 1) * 512], lhsT=cTs[:, kc, :],
                                       rhs=wo[:, kc, half * 512:(half + 1) * 512], start=(kc == 0), stop=(kc == 7))
                return ins
            S.op('pe', mm, reads=[bcTs, bwo], writes=[bpy], self_ok=True)
            post_norm_res(Ph, py[:], bpy, xt, bx, C.GG1, C.bGG1, j, junkr, str_, ttr,
                          C.xs[b, r0:r0 + 128, :])
    Ph.close()


def post_norm_res(Ph, y, by, xt, bx, GG, bGG, j, junkr, str_, ttr, dst):
    S = Ph.S
    C = Ph.C
    junk, bj = junkr.next()
    st, bst = str_.next()
    tt, btt = ttr.next()
    S.op('act', lambda e: e.activation(out=junk[:], in_=y, func=AF.Square, accum_out=st[:, 0:1]), reads=[by],
         writes=[bj, bst])
    S.op('dve', lambda e: e.tensor_scalar(out=st[:, 1:2], in0=st[:, 0:1], scalar1=1.0 / D, scalar2=EPS, op0=ALU.mult,
                                          op1=ALU.add), reads=[bst], writes=[bst])
    S.op('pool', lambda e: e.tensor_tensor(out=st[:, 2:3], in0=st[:, 1:2], in1=C.neghalf[:, 0:1], op=ALU.pow),
         reads=[bst], writes=[bst])
    S.op('dve', lambda e: e.scalar_tensor_tensor(out=tt[:], in0=y, scalar=st[:, 2:3], in1=GG[:, j, :], op0=ALU.mult,
                                                 op1=ALU.mult), reads=[by, bst, bGG], writes=[btt])
    S.op('pool', lambda e: e.tensor_tensor(out=tt[:], in0=tt[:], in1=xt[:], op=ALU.add), reads=[btt, bx],
         writes=[btt])
    S.op('sp', lambda e: e.dma_start(out=dst, in_=tt[:]), reads=[btt], dma=btt)


def phase_ffn_pre(C, l, moe, tiles):
    S = C.S
    Ph = Phase(C, f"fpre{l}")
    K = prenorm_kit(Ph, with32=moe)
    xr = Ph.rot(3, [128, D], F32, 'x')
    hTr = Ph.rot(2, [128, 8, 512], BF16, 'hT')
    if moe:
        wr, bwr = Ph.sbb([128, 8, NE], F32, 'wr')
        S.op('sp', lambda e: e.dma_start(out=wr[:], in_=C.moe_wr.rearrange("(kc p) n -> p kc n", p=128)),
             writes=[bwr], dma=bwr)
        h32r = Ph.rot(2, [128, 8, 128], F32, 'h32')
        plg = Ph.rot(1, [128, 512], F32, 'plg', psum=True)
        cmbr = Ph.rot(2, [128, 4, NE], F32, 'cmb')
        rsr = Ph.rot(4, [128, 48], F32, 'rst')
    for gi in range(len(tiles) // 4):
        hT, bhT = hTr.next()
        if moe:
            cmb, bcmb = cmbr.next()
        for t in range(4):
            b, tile = tiles[gi * 4 + t]
            j = 2 if tile < 2 else b
            xt, bx = xr.next()
            S.op('sp', lambda e, xt=xt, b=b, tile=tile: e.dma_start(out=xt[:], in_=res_src(C, l, 1, b, tile)),
                 writes=[bx], dma=bx)
            if not moe:
                prenorm_tile(Ph, K, xt, bx, hT, bhT, t * 128, C.G2, C.SH2, C.bG2, j)
                continue
            h32, bh32 = h32r.next()
            prenorm_tile(Ph, K, xt, bx, hT, bhT, t * 128, C.G2, C.SH2, C.bG2, j, h32, bh32)
            pl, bpl = plg.next()

            def mm(e, pl=pl, h32=h32):
                for kc in range(8):
                    ins = e.matmul(pl[:, 0:NE], lhsT=h32[:, kc, :], rhs=wr[:, kc, :], start=(kc == 0), stop=(kc == 7))
                return ins
            S.op('pe', mm, reads=[bh32, bwr], writes=[bpl], self_ok=True)
            st, bst = rsr.next()
            ops = [
                lambda e, st=st, pl=pl: e.tensor_copy(out=st[:, 0:8], in_=pl[:, 0:NE]),
                lambda e, st=st: e.reduce_max(out=st[:, 8:9], in_=st[:, 0:8], axis=AX.X),
                lambda e, st=st: e.tensor_scalar(out=st[:, 16:24], in0=st[:, 0:8], scalar1=st[:, 8:9], scalar2=-1e30,
                                                 op0=ALU.is_equal, op1=ALU.mult),
                lambda e, st=st: e.tensor_tensor(out=st[:, 16:24], in0=st[:, 16:24], in1=st[:, 0:8], op=ALU.add),
                lambda e, st=st: e.reduce_max(out=st[:, 9:10], in_=st[:, 16:24], axis=AX.X),
                lambda e, st=st: e.tensor_scalar(out=st[:, 24:32], in0=st[:, 0:8], scalar1=st[:, 9:10], scalar2=None,
                                                 op0=ALU.is_ge),
                lambda e, st=st: e.tensor_scalar(out=st[:, 10:11], in0=st[:, 8:9], scalar1=-1.0, scalar2=None,
                                                 op0=ALU.mult),
            ]
            for i, f_ in enumerate(ops):
                S.op('dve', f_, reads=[bst] + ([bpl] if i == 0 else []), writes=[bst])
            S.op('act', lambda e, st=st: e.activation(out=st[:, 32:40], in_=st[:, 0:8], func=AF.Exp, bias=st[:, 10:11],
                                                      scale=1.0), reads=[bst], writes=[bst])
            ops2 = [
                lambda e, st=st: e.tensor_tensor(out=st[:, 32:40], in0=st[:, 32:40], in1=st[:, 24:32], op=ALU.mult),
                lambda e, st=st: e.reduce_sum(out=st[:, 11:12], in_=st[:, 32:40], axis=AX.X),
                lambda e, st=st: e.reciprocal(out=st[:, 12:13], in_=st[:, 11:12]),
            ]
            for f_ in ops2:
                S.op('dve', f_, reads=[bst], writes=[bst])
            S.op('dve', lambda e, st=st, cmb=cmb, t=t: e.tensor_scalar(out=cmb[:, t, :], in0=st[:, 32:40],
                                                                       scalar1=st[:, 12:13], scalar2=None,
                                                                       op0=ALU.mult), reads=[bst], writes=[bcmb])
        S.op('sp', lambda e, hT=hT, gi=gi: e.dma_start(out=C.h2T[gi], in_=hT[:]), reads=[bhT], dma=bhT)
        if moe:
            S.op('sp', lambda e, cmb=cmb, gi=gi: e.dma_start(out=C.comb[gi], in_=cmb[:]), reads=[bcmb], dma=bcmb)
    Ph.close()


def phase_ffn(C, l, moe, tiles):
    S = C.S
    Ph = Phase(C, f"ffn{l}")
    E = NE if moe else 1
    F = F_MOE if moe else F_FFN
    NFC = F // 128
    NFB = F // 256
    wsrc = C.moe_bf if moe else C.ffn_bf
    bwg_ = C.bmoe if moe else C.bffn
    hTr = Ph.rot(2, [128, 8, 512], BF16, 'hT')
    hid, bhid = Ph.sbb([128, NFC, 512], BF16, 'hid')
    wd, bwd = Ph.sbb([128, NFC, D], BF16, 'wd')
    wgr = Ph.rot(3, [128, 8, 256], BF16, 'wg')
    wur = Ph.rot(3, [128, 8, 256], BF16, 'wu')
    yacc, byacc = Ph.sbb([128, 4, D], F32, 'yacc')
    sgr = Ph.rot(2, [128, 512], F32, 'sg')
    xr = Ph.rot(2, [128, D], F32, 'x')
    ttr = Ph.rot(2, [128, D], F32, 'tt')
    junkr = Ph.rot(1, [128, D], BF16, 'junk')
    str_ = Ph.rot(4, [128, 4], F32, 'fst')
    cmbr = Ph.rot(2, [128, 4, NE], F32, 'cmb')
    pgr = Ph.rot(2, [128, 512], F32, 'pg', psum=True)
    pur = Ph.rot(2, [128, 512], F32, 'pu', psum=True)
    pyr = Ph.rot(2, [128, 512], F32, 'py', psum=True)
    for gi in range(len(tiles) // 4):
        hT, bhT = hTr.next()
        S.op('sp', lambda e, hT=hT, gi=gi: e.dma_start(out=hT[:], in_=C.h2T[gi]), writes=[bhT], dma=bhT)
        if moe:
            cmb, bcmb = cmbr.next()
            S.op('sp', lambda e, cmb=cmb, gi=gi: e.dma_start(out=cmb[:], in_=C.comb[gi]), writes=[bcmb], dma=bcmb)
        for ex in range(E):
            hh = NFC // 2
            for (a0, a1) in [(0, hh), (hh, NFC)]:
                S.op('pool', lambda e, ex=ex, a0=a0, a1=a1: e.dma_start(
                    out=wd[:, a0:a1, :],
                    in_=wsrc[2][ex, a0 * 128:a1 * 128, :].rearrange("(fc p) n -> p fc n", p=128)),
                    reads=[bwg_], writes=[bwd], dma=bwd)
            for fb in range(NFB):
                wg, bwg = wgr.next()
                wu, bwu = wur.next()
                S.op('sp', lambda e, wg=wg, ex=ex, fb=fb: e.dma_start(
                    out=wg[:], in_=wsrc[0][ex, :, fb * 256:(fb + 1) * 256].rearrange("(kc p) f -> p kc f", p=128)),
                    reads=[bwg_], writes=[bwg], dma=bwg)
                S.op('sp', lambda e, wu=wu, ex=ex, fb=fb: e.dma_start(
                    out=wu[:], in_=wsrc[1][ex, :, fb * 256:(fb + 1) * 256].rearrange("(kc p) f -> p kc f", p=128)),
                    reads=[bwg_], writes=[bwu], dma=bwu)
                for fi in range(2):
                    fc = fb * 2 + fi
                    pg_, bpg = pgr.next()
                    pu_, bpu = pur.next()

                    def mmg(e, pg_=pg_, wg=wg, fi=fi, hT=hT):
                        for kc in range(8):
                            ins = e.matmul(pg_[:], lhsT=wg[:, kc, fi * 128:(fi + 1) * 128], rhs=hT[:, kc, :],
                                           start=(kc == 0), stop=(kc == 7))
                        return ins

                    def mmu(e, pu_=pu_, wu=wu, fi=fi, hT=hT):
                        for kc in range(8):
                            ins = e.matmul(pu_[:], lhsT=wu[:, kc, fi * 128:(fi + 1) * 128], rhs=hT[:, kc, :],
                                           start=(kc == 0), stop=(kc == 7))
                        return ins
                    S.op('pe', mmg, reads=[bwg, bhT], writes=[bpg], self_ok=True)
                    S.op('pe', mmu, reads=[bwu, bhT], writes=[bpu], self_ok=True)
                    sg, bsg = sgr.next()
                    S.op('act', lambda e, sg=sg, pg_=pg_: e.activation(out=sg[:], in_=pg_[:], func=AF.Silu),
                         reads=[bpg], writes=[bsg])
                    S.op('dve', lambda e, sg=sg, pu_=pu_, fc=fc: e.tensor_tensor(out=hid[:, fc, :], in0=pu_[:],
                                                                                 in1=sg[:], op=ALU.mult),
                         reads=[bsg, bpu], writes=[bhid])
            for t in range(4):
                for half in range(2):
                    py_, bpy = pyr.next()
                    hs = slice(half * 512, (half + 1) * 512)

                    def mmd(e, py_=py_, t=t, hs=hs):
                        for fc in range(NFC):
                            ins = e.matmul(py_[:], lhsT=hid[:, fc, t * 128:(t + 1) * 128], rhs=wd[:, fc, hs],
                                           start=(fc == 0), stop=(fc == NFC - 1))
                        return ins
                    S.op('pe', mmd, reads=[bhid, bwd], writes=[bpy], self_ok=True)
                    if not moe:
                        S.op('act', lambda e, py_=py_, t=t, hs=hs: e.activation(out=yacc[:, t, hs], in_=py_[:],
                                                                                func=AF.Copy),
                             reads=[bpy], writes=[byacc])
                    elif ex == 0:
                        S.op('act', lambda e, py_=py_, t=t, hs=hs, cmb=cmb: e.activation(
                            out=yacc[:, t, hs], in_=py_[:], func=AF.Copy, scale=cmb[:, t, 0:1]),
                            reads=[bpy, bcmb], writes=[byacc])
                    else:
                        S.op('dve', lambda e, py_=py_, t=t, hs=hs, cmb=cmb, ex=ex: e.scalar_tensor_tensor(
                            out=yacc[:, t, hs], in0=py_[:], scalar=cmb[:, t, ex:ex + 1], in1=yacc[:, t, hs],
                            op0=ALU.mult, op1=ALU.add), reads=[bpy, bcmb, byacc], writes=[byacc])
        for t in range(4):
            b, tile = tiles[gi * 4 + t]
            j = 2 if tile < 2 else b
            xt, bx = xr.next()
            S.op('sp', lambda e, xt=xt, b=b, tile=tile: e.dma_start(out=xt[:], in_=res_src(C, l, 1, b, tile)),
                 writes=[bx], dma=bx)
            if moe:
                dst = C.y_out[b, (tile - 2) * 128:(tile - 1) * 128, :]
            else:
                dst = C.xs[b, tile * 128:(tile + 1) * 128, :]

            post_norm_res(Ph, yacc[:, t, :], byacc, xt, bx, C.GG2, C.bGG2, j, junkr, str_, ttr, dst)
    Ph.close()


U32 = mybir.dt.uint32
I32 = mybir.dt.int32
NTOK = NB * LLAT
CAPE = NTOK
GS = 512
NGRP = CAPE // GS


def phase_moe_pre(C, l, tiles):
    S = C.S
    Ph = Phase(C, f"mpre{l}")
    xr = Ph.rot(2, [128, D], F32, 'x')
    junkr = Ph.rot(1, [128, D], BF16, 'junk')
    h32r = Ph.rot(2, [128, D], F32, 'h32')
    hbr = Ph.rot(2, [128, D], BF16, 'hb')
    hTr = Ph.rot(2, [128, 8, 128], F32, 'hT32')
    str_ = Ph.rot(4, [128, 4], F32, 'pst')
    rsr = Ph.rot(4, [128, 80], F32, 'rst')
    selr = Ph.rot(2, [128, NE], BF16, 'selb')
    recr = Ph.rot(8, [128, 4], U32, 'rec')
    slur = Ph.rot(4, [128, 2], U32, 'slu')
    wr, bwr = Ph.sbb([128, 8, NE], F32, 'wr')
    base, bbase = Ph.sbb([128, NE], F32, 'base')
    nid, bnid = Ph.sbb([128, 64], F32, 'nid')
    eoff, beoff = Ph.sbb([128, NE], F32, 'eoff')
    thr, bthr = Ph.sbb([128, NGRP], F32, 'thr')
    trif, btrif = Ph.sbb([128, 128], F32, 'trif')
    trib, btrib = Ph.sbb([128, 128], BF16, 'trib')
    oneb, boneb = Ph.sbb([128, 128], BF16, 'oneb')
    flf, bflf = Ph.sbb([128, NE, NGRP], F32, 'flf')
    fli, bfli = Ph.sbb([128, NE, NGRP], I32, 'fli')
    p32r = Ph.rot(2, [128, 8, 128], F32, 'p32', psum=True)
    plg = Ph.rot(2, [128, 512], F32, 'plg', psum=True)
    blst = S.buf('lst')
    S.op('sp', lambda e: e.dma_start(out=C.lst[:, :], in_=C.lst_init[:, :]), writes=[blst], dma=blst)
    S.op('sp', lambda e: e.dma_start(out=wr[:], in_=C.moe_wr.rearrange("(kc p) n -> p kc n", p=128)), writes=[bwr],
         dma=bwr)
    S.op('sp', lambda e: e.dma_start(out=nid[:], in_=C.nidf[:, :]), writes=[bnid], dma=bnid)
    S.op('sp', lambda e: e.dma_start(out=eoff[:], in_=C.eoff[:, :]), writes=[beoff], dma=beoff)
    S.op('sp', lambda e: e.dma_start(out=thr[:], in_=C.thr[:, :]), writes=[bthr], dma=bthr)
    S.op('sp', lambda e: e.dma_start(out=trif[:], in_=C.tri_in[:, 2, :]), writes=[btrif], dma=btrif)
    S.op('dve', lambda e: e.tensor_copy(out=trib[:], in_=trif[:]), reads=[btrif], writes=[btrib])
    S.op('pool', lambda e: e.memset(oneb[:], 1.0), writes=[boneb])
    S.op('pool', lambda e: e.memset(base[:], 0.0), writes=[bbase])
    for k, (b, tile) in enumerate(tiles):
        xt, bx = xr.next()
        S.op('sp', lambda e, xt=xt, b=b, tile=tile: e.dma_start(out=xt[:], in_=res_src(C, l, 1, b, tile)), writes=[bx],
             dma=bx)
        junk, bj = junkr.next()
        st, bst = str_.next()
        h32, bh32 = h32r.next()
        hb, bhb = hbr.next()
        S.op('act', lambda e, junk=junk, xt=xt, st=st: e.activation(out=junk[:], in_=xt[:], func=AF.Square,
                                                                     accum_out=st[:, 0:1]), reads=[bx], writes=[bj, bst])
        S.op('dve', lambda e, st=st: e.tensor_scalar(out=st[:, 1:2], in0=st[:, 0:1], scalar1=1.0 / D, scalar2=EPS,
                                                     op0=ALU.mult, op1=ALU.add), reads=[bst], writes=[bst])
        S.op('pool', lambda e, st=st: e.tensor_tensor(out=st[:, 2:3], in0=st[:, 1:2], in1=C.neghalf[:, 0:1],
                                                      op=ALU.pow), reads=[bst], writes=[bst])
        S.op('dve', lambda e, h32=h32, xt=xt, st=st, b=b: e.scalar_tensor_tensor(
            out=h32[:], in0=xt[:], scalar=st[:, 2:3], in1=C.G2row[:, b, :], op0=ALU.mult, op1=ALU.mult),
            reads=[bx, bst, C.bG2row], writes=[bh32])
        S.op('pool', lambda e, h32=h32, b=b: e.tensor_tensor(out=h32[:], in0=h32[:], in1=C.S2row[:, b, :], op=ALU.add),
             reads=[bh32, C.bG2row], writes=[bh32])
        S.op('act', lambda e, hb=hb, h32=h32: e.activation(out=hb[:], in_=h32[:], func=AF.Copy), reads=[bh32],
             writes=[bhb])
        S.op('sp', lambda e, hb=hb, k=k: e.dma_start(out=C.h2tok[k * 128:(k + 1) * 128, :], in_=hb[:]), reads=[bhb],
             dma=bhb)
        p32, bp32 = p32r.next()

        def tr32(e, p32=p32, h32=h32):
            for kc in range(8):
                ins = e.transpose(out=p32[:, kc, :], in_=h32[:, kc * 128:(kc + 1) * 128], identity=C.ident32[:])
            return ins
        S.op('pe', tr32, reads=[bh32], writes=[bp32], self_ok=True)
        hT, bhT = hTr.next()
        S.op('dve', lambda e, hT=hT, p32=p32: e.tensor_copy(out=hT[:, 0:4, :], in_=p32[:, 0:4, :]), reads=[bp32],
             writes=[bhT])
        S.op('act', lambda e, hT=hT, p32=p32: e.activation(out=hT[:, 4:8, :], in_=p32[:, 4:8, :], func=AF.Copy),
             reads=[bp32], writes=[bhT])
        pl, bpl = plg.next()

        def mm(e, pl=pl, hT=hT):
            for kc in range(8):
                ins = e.matmul(pl[:, 0:NE], lhsT=hT[:, kc, :], rhs=wr[:, kc, :], start=(kc == 0), stop=(kc == 7))
            return ins
        S.op('pe', mm, reads=[bhT, bwr], writes=[bpl], self_ok=True)
        r, br = rsr.next()
        LG, M1, M2, NM1, DEN, RDEN = r[:, 0:8], r[:, 8:9], r[:, 9:10], r[:, 10:11], r[:, 11:12], r[:, 12:13]
        EQ1, SEL, WN, MSK, OH1, SV, TMP = r[:, 16:24], r[:, 24:32], r[:, 32:40], r[:, 40:48], r[:, 48:56], r[:, 56:64], \
            r[:, 64:72]
        SL0, SL1, W0, W1, D1 = r[:, 72:73], r[:, 73:74], r[:, 74:75], r[:, 75:76], r[:, 76:77]
        selb, bselb = selr.next()
        dv = lambda f_, extra=(): S.op('dve', f_, reads=[br] + list(extra), writes=[br])
        dv(lambda e, LG=LG, pl=pl: e.tensor_copy(out=LG, in_=pl[:, 0:NE]), [bpl])
        dv(lambda e, LG=LG, M1=M1: e.reduce_max(out=M1, in_=LG, axis=AX.X))
        dv(lambda e, EQ1=EQ1, LG=LG, M1=M1: e.tensor_scalar(out=EQ1, in0=LG, scalar1=M1, scalar2=None,
                                                            op0=ALU.is_equal))
        dv(lambda e, MSK=MSK, EQ1=EQ1, LG=LG: e.scalar_tensor_tensor(out=MSK, in0=EQ1, scalar=-1e30, in1=LG,
                                                                     op0=ALU.mult, op1=ALU.add))
        dv(lambda e, MSK=MSK, M2=M2: e.reduce_max(out=M2, in_=MSK, axis=AX.X))
        dv(lambda e, SEL=SEL, LG=LG, M2=M2: e.tensor_scalar(out=SEL, in0=LG, scalar1=M2, scalar2=None, op0=ALU.is_ge))
        dv(lambda e, NM1=NM1, M1=M1: e.tensor_scalar(out=NM1, in0=M1, scalar1=-1.0, scalar2=None, op0=ALU.mult))
        S.op('act', lambda e, WN=WN, LG=LG, NM1=NM1: e.activation(out=WN, in_=LG, func=AF.Exp, bias=NM1, scale=1.0),
             reads=[br], writes=[br])
        dv(lambda e, WN=WN, SEL=SEL: e.tensor_tensor(out=WN, in0=WN, in1=SEL, op=ALU.mult))
        dv(lambda e, WN=WN, DEN=DEN: e.reduce_sum(out=DEN, in_=WN, axis=AX.X))
        dv(lambda e, DEN=DEN, RDEN=RDEN: e.reciprocal(out=RDEN, in_=DEN))
        dv(lambda e, WN=WN, RDEN=RDEN: e.tensor_scalar(out=WN, in0=WN, scalar1=RDEN, scalar2=None, op0=ALU.mult))
        dv(lambda e, OH1=OH1, SEL=SEL, EQ1=EQ1: e.tensor_tensor(out=OH1, in0=SEL, in1=EQ1, op=ALU.subtract))
        S.op('dve', lambda e, selb=selb, SEL=SEL: e.tensor_copy(out=selb[:], in_=SEL), reads=[br], writes=[bselb])
        pc, bpc = plg.next()

        def mmc(e, pc=pc, selb=selb):
            e.matmul(pc[:, 0:NE], lhsT=trib[:], rhs=selb[:], start=True, stop=True)
            return e.matmul(pc[:, 8:8 + NE], lhsT=oneb[:], rhs=selb[:], start=True, stop=True)
        S.op('pe', mmc, reads=[bselb, btrib, boneb], writes=[bpc], self_ok=True)
        dv(lambda e, SV=SV, pc=pc: e.tensor_tensor(out=SV, in0=pc[:, 0:NE], in1=base[:], op=ALU.add), [bpc, bbase])
        dv(lambda e, SV=SV: e.tensor_tensor(out=SV, in0=SV, in1=eoff[:], op=ALU.add), [beoff])
        S.op('dve', lambda e, pc=pc: e.tensor_tensor(out=base[:], in0=pc[:, 8:8 + NE], in1=base[:], op=ALU.add),
             reads=[bpc, br], writes=[bbase])
        for (OH, SL, W) in [(EQ1, SL0, W0), (OH1, SL1, W1)]:
            dv(lambda e, TMP=TMP, OH=OH, SV=SV: e.tensor_tensor(out=TMP, in0=OH, in1=SV, op=ALU.mult))
            dv(lambda e, TMP=TMP, SL=SL: e.reduce_sum(out=SL, in_=TMP, axis=AX.X))
            dv(lambda e, TMP=TMP, OH=OH, WN=WN: e.tensor_tensor(out=TMP, in0=OH, in1=WN, op=ALU.mult))
            dv(lambda e, TMP=TMP, W=W: e.reduce_sum(out=W, in_=TMP, axis=AX.X))
        dv(lambda e, D1=D1, k=k: e.tensor_scalar(out=D1, in0=nid[:, k:k + 1], scalar1=float(NTOK), scalar2=None,
                                                 op0=ALU.add), [bnid])
        slu, bslu = slur.next()
        S.op('dve', lambda e, slu=slu, SL0=SL0: e.tensor_copy(out=slu[:, 0:1], in_=SL0), reads=[br], writes=[bslu])
        S.op('dve', lambda e, slu=slu, SL1=SL1: e.tensor_copy(out=slu[:, 1:2], in_=SL1), reads=[br], writes=[bslu])
        for rk, (DD, WW) in enumerate([(None, W0), (D1, W1)]):
            rec, brec = recr.next()
            S.op('pool', lambda e, rec=rec: e.memset(rec[:], 0), writes=[brec])
            rw = lambda f_: S.op('dve', f_, reads=[br, bnid], writes=[brec])
            rw(lambda e, rec=rec, k=k: e.tensor_copy(out=rec[:, 0:1], in_=nid[:, k:k + 1]))
            if DD is None:
                rw(lambda e, rec=rec, k=k: e.tensor_copy(out=rec[:, 1:2], in_=nid[:, k:k + 1]))
            else:
                rw(lambda e, rec=rec, DD=DD: e.tensor_copy(out=rec[:, 1:2], in_=DD))
            rw(lambda e, rec=rec, WW=WW: e.tensor_copy(out=rec[:, 2:3].bitcast(F32), in_=WW))
            S.op('pool', lambda e, rec=rec, slu=slu, rk=rk: e.indirect_dma_start(
                out=C.lst[:, :], out_offset=bass.IndirectOffsetOnAxis(ap=slu[:, rk:rk + 1], axis=0),
                in_=rec[:], in_offset=None, bounds_check=S.breg(e, NE * CAPE - 1), oob_is_err=False),
                reads=[brec, bslu, blst], dma=brec)
    for ex in range(NE):
        S.op('dve', lambda e, ex=ex: e.tensor_scalar(out=flf[:, ex, :], in0=thr[:], scalar1=base[:, ex:ex + 1],
                                                     scalar2=None, op0=ALU.is_lt), reads=[bbase, bthr], writes=[bflf])
    S.op('dve', lambda e: e.tensor_copy(out=fli[:], in_=flf[:]), reads=[bflf], writes=[bfli])
    S.op('sp', lambda e: e.dma_start(out=C.flags[0:1, :], in_=fli[0:1, :, :].rearrange("p a b -> p (a b)")),
         reads=[bfli], dma=bfli)
    Ph.close()


def phase_moe_sparse(C, l):
    import os
    SPS = int(os.environ.get('SP_STG', '9'))
    S = C.S
    Ph = Phase(C, f"moe{l}")
    NFC = F_MOE // 128
    NFB = F_MOE // 256
    wsrc = C.moe_bf
    hid, bhid = Ph.sbb([128, NFC, 512], BF16, 'hid')
    wd, bwd = Ph.sbb([128, NFC, D], BF16, 'wd')
    wgr = Ph.rot(3, [128, 8, 256], BF16, 'wg')
    wur = Ph.rot(3, [128, 8, 256], BF16, 'wu')
    htr = Ph.rot(2, [128, 4, D], BF16, 'htok')
    hTr = Ph.rot(2, [128, 8, 512], BF16, 'hT')
    recr = Ph.rot(2, [128, 4, 4], U32, 'recs')
    sgr = Ph.rot(2, [128, 512], F32, 'sg')
    yscr = Ph.rot(2, [128, D], F32, 'ysc')
    ptrr = Ph.rot(2, [128, 8, 128], BF16, 'ptr', psum=True)
    pgr = Ph.rot(2, [128, 512], F32, 'pg', psum=True)
    pur = Ph.rot(1, [128, 512], F32, 'pu', psum=True)
    pyr = Ph.rot(2, [128, 512], F32, 'py', psum=True)
    for t_, b_ in zip(htr.t, htr.b):
        S.op('pool', lambda e, t_=t_: e.memset(t_[:], 0.0), writes=[b_])
    for ex in range(NE):
        hh = NFC // 2
        for (a0, a1) in [(0, hh), (hh, NFC)]:
            S.op('sp', lambda e, ex=ex, a0=a0, a1=a1: e.dma_start(
                out=wd[:, a0:a1, :], in_=wsrc[2][ex, a0 * 128:a1 * 128, :].rearrange("(fc p) n -> p fc n", p=128)),
                reads=[C.bmoe], writes=[bwd], dma=bwd)
        for g in range(NGRP):
            S.cond_begin(C.flags[0:1, ex * NGRP + g:ex * NGRP + g + 1])
            recs, brecs = recr.next()
            r0 = ex * CAPE + g * GS
            S.op('sp', lambda e, recs=recs, r0=r0: e.dma_start(
                out=recs[:], in_=C.lst[r0:r0 + GS, :].rearrange("(t p) c -> p t c", p=128)), writes=[brecs], dma=brecs)
            ht, bht = htr.next()
            for t in range(4):
                S.op('pool', lambda e, ht=ht, recs=recs, t=t: e.indirect_dma_start(
                    out=ht[:, t, :], out_offset=None, in_=C.h2tok[:, :],
                    in_offset=bass.IndirectOffsetOnAxis(ap=recs[:, t, 0:1], axis=0), bounds_check=S.breg(e, NTOK - 1),
                    oob_is_err=False), reads=[brecs], writes=[bht], dma=bht)
            hT, bhT = hTr.next()
            for t in range(4 if SPS >= 2 else 0):
                ptr, bptr = ptrr.next()

                def tr(e, ptr=ptr, ht=ht, t=t):
                    for kc in range(8):
                        ins = e.transpose(out=ptr[:, kc, :], in_=ht[:, t, kc * 128:(kc + 1) * 128], identity=C.ident[:])
                    return ins
                S.op('pe', tr, reads=[bht], writes=[bptr], self_ok=True)
                if t % 2 == 0:
                    S.op('dve', lambda e, hT=hT, ptr=ptr, t=t: e.tensor_copy(out=hT[:, :, t * 128:(t + 1) * 128],
                                                                             in_=ptr[:]), reads=[bptr], writes=[bhT])
                else:
                    S.op('act', lambda e, hT=hT, ptr=ptr, t=t: e.activation(out=hT[:, :, t * 128:(t + 1) * 128],
                                                                            in_=ptr[:], func=AF.Copy), reads=[bptr],
                         writes=[bhT])
            for fb in range(NFB if SPS >= 3 else 0):
                wg, bwg = wgr.next()
                wu, bwu = wur.next()
                S.op('sp', lambda e, wg=wg, ex=ex, fb=fb: e.dma_start(
                    out=wg[:], in_=wsrc[0][ex, :, fb * 256:(fb + 1) * 256].rearrange("(kc p) f -> p kc f", p=128)),
                    reads=[C.bmoe], writes=[bwg], dma=bwg)
                S.op('sp', lambda e, wu=wu, ex=ex, fb=fb: e.dma_start(
                    out=wu[:], in_=wsrc[1][ex, :, fb * 256:(fb + 1) * 256].rearrange("(kc p) f -> p kc f", p=128)),
                    reads=[C.bmoe], writes=[bwu], dma=bwu)
                for fi in range(2):
                    fc = fb * 2 + fi
                    pg_, bpg = pgr.next()
                    pu_, bpu = pur.next()

                    def mmg(e, pg_=pg_, wg=wg, fi=fi, hT=hT):
                        for kc in range(8):
                            ins = e.matmul(pg_[:], lhsT=wg[:, kc, fi * 128:(fi + 1) * 128], rhs=hT[:, kc, :],
                                           start=(kc == 0), stop=(kc == 7))
                        return ins

                    def mmu(e, pu_=pu_, wu=wu, fi=fi, hT=hT):
                        for kc in range(8):
                            ins = e.matmul(pu_[:], lhsT=wu[:, kc, fi * 128:(fi + 1) * 128], rhs=hT[:, kc, :],
                                           start=(kc == 0), stop=(kc == 7))
                        return ins
                    S.op('pe', mmg, reads=[bwg, bhT], writes=[bpg], self_ok=True)
                    S.op('pe', mmu, reads=[bwu, bhT], writes=[bpu], self_ok=True)
                    sg, bsg = sgr.next()
                    S.op('act', lambda e, sg=sg, pg_=pg_: e.activation(out=sg[:], in_=pg_[:], func=AF.Silu),
                         reads=[bpg], writes=[bsg])
                    S.op('dve', lambda e, sg=sg, pu_=pu_, fc=fc: e.tensor_tensor(out=hid[:, fc, :], in0=pu_[:],
                                                                                 in1=sg[:], op=ALU.mult),
                         reads=[bsg, bpu], writes=[bhid])
            for t in range(4 if SPS >= 4 else 0):
                ysc, bysc = yscr.next()
                for half in range(2):
                    py_, bpy = pyr.next()
                    hs = slice(half * 512, (half + 1) * 512)

                    def mmd(e, py_=py_, t=t, hs=hs):
                        for fc in range(NFC):
                            ins = e.matmul(py_[:], lhsT=hid[:, fc, t * 128:(t + 1) * 128], rhs=wd[:, fc, hs],
                                           start=(fc == 0), stop=(fc == NFC - 1))
                        return ins
                    S.op('pe', mmd, reads=[bhid, bwd], writes=[bpy], self_ok=True)
                    S.op('act', lambda e, py_=py_, ysc=ysc, hs=hs, recs=recs, t=t: e.activation(
                        out=ysc[:, hs], in_=py_[:], func=AF.Copy, scale=recs[:, t, 2:3].bitcast(F32)),
                        reads=[bpy, brecs], writes=[bysc])
                if SPS >= 5:
                  S.op('pool', lambda e, ysc=ysc, recs=recs, t=t: e.indirect_dma_start(
                    out=C.Ymoe[:, :], out_offset=bass.IndirectOffsetOnAxis(ap=recs[:, t, 1:2], axis=0), in_=ysc[:],
                    in_offset=None, bounds_check=S.breg(e, 2 * NTOK - 1), oob_is_err=False), reads=[bysc, brecs], dma=bysc)
            S.cond_end()
    Ph.close()


def phase_moe_post(C, l, tiles):
    S = C.S
    Ph = Phase(C, f"mpost{l}")
    xr = Ph.rot(4, [128, D], F32, 'x')
    y1r = Ph.rot(4, [128, D], F32, 'y1')
    y2r = Ph.rot(4, [128, D], F32, 'y2')
    ttr = Ph.rot(3, [128, D], F32, 'tt')
    junkr = Ph.rot(2, [128, D], BF16, 'junk')
    str_ = Ph.rot(8, [128, 4], F32, 'fst')
    for k, (b, tile) in enumerate(tiles):
        xt, bx = xr.next()
        y1, by1 = y1r.next()
        y2, by2 = y2r.next()
        S.op('sp', lambda e, xt=xt, b=b, tile=tile: e.dma_start(out=xt[:], in_=res_src(C, l, 1, b, tile)), writes=[bx],
             dma=bx)
        S.op('sp', lambda e, y1=y1, k=k: e.dma_start(out=y1[:], in_=C.Ymoe[k * 128:(k + 1) * 128, :]), writes=[by1],
             dma=by1)
        S.op('sp', lambda e, y2=y2, k=k: e.dma_start(out=y2[:], in_=C.Ymoe[NTOK + k * 128:NTOK + (k + 1) * 128, :]),
             writes=[by2], dma=by2)
        S.op('pool', lambda e, y1=y1, y2=y2: e.tensor_tensor(out=y1[:], in0=y1[:], in1=y2[:], op=ALU.add),
             reads=[by1, by2], writes=[by1])
        dst = C.y_out[b, (tile - 2) * 128:(tile - 1) * 128, :]
        post_norm_res(Ph, y1[:], by1, xt, bx, C.GG2, C.bGG2, b, junkr, str_, ttr, dst)
    Ph.close()


SPARSE_MOE = True


def build_program(debug=False, upto=None, skip=()):
    nc = bass.Bass("TRN2", target_bir_lowering=False)
    C = Ctx()
    C.nc = nc
    C.debug = debug
    L = 2
    C.x_in = _dram_in(nc, "x", [NB, LLAT, D])
    C.ctx_in = _dram_in(nc, "ctx", [NB, LCTX, D])
    C.cT = _dram_in(nc, "cT", [128, 8, 3])
    C.w_ada = _dram_in(nc, "w_ada", [L, D, 6 * D])
    C.badaT3 = _dram_in(nc, "badaT3", [L, 128, 48, 3])
    C.gpre3 = _dram_in(nc, "gpre3", [L, 128, 2, 8, 3])
    C.rowc = _dram_in(nc, "rowc", [L, 128, 7, D])
    C.w_in = _dram_in(nc, "w_in", [L, D, PROJ])
    C.rope_cos = _dram_in(nc, "rope_cos", [128, 32, 64])
    C.rope_sin = _dram_in(nc, "rope_sin", [128, 32, 64])
    C.ident_in = _dram_in(nc, "ident", [128, 128], BF16)
    C.ident32_in = _dram_in(nc, "ident32", [128, 128], F32)
    C.tri_in = _dram_in(nc, "tri", [128, 4, 128], F32)
    C.wgate = _dram_in(nc, "gla_w_gate", [L, 2, 16, 256])
    C.bgate = _dram_in(nc, "gla_b_gate", [L, 2, 1, 256])
    C.gnormB = _dram_in(nc, "gnormB", [L, 128, 512])
    C.natb = _dram_in(nc, "natb", [L, 8, 128, 5, 5, 128])
    C.w_out = _dram_in(nc, "w_out", [L, D, D])
    C.ffn_wg = _dram_in(nc, "ffn_w_gate", [1, D, F_FFN])
    C.ffn_wu = _dram_in(nc, "ffn_w_up", [1, D, F_FFN])
    C.ffn_wd = _dram_in(nc, "ffn_w_down", [1, F_FFN, D])
    C.moe_wr = _dram_in(nc, "moe_w_router", [D, NE])
    C.moe_wg = _dram_in(nc, "moe_w_gate", [NE, D, F_MOE])
    C.moe_wu = _dram_in(nc, "moe_w_up", [NE, D, F_MOE])
    C.moe_wd = _dram_in(nc, "moe_w_down", [NE, F_MOE, D])
    C.lst_init = _dram_in(nc, "lst_init", [NE * CAPE, 4], U32)
    C.nidf = _dram_in(nc, "nidf", [128, 64])
    C.eoff = _dram_in(nc, "eoff", [128, NE])
    C.thr = _dram_in(nc, "thr", [128, NGRP])
    C.lst = _dram_tmp(nc, "lst", [NE * CAPE, 4], U32)
    C.flags = _dram_tmp(nc, "flags", [1, NE * NGRP], I32)
    C.h2tok = _dram_tmp(nc, "h2tok", [NTOK, D], BF16)
    C.Ymoe = _dram_tmp(nc, "Ymoe", [2 * NTOK, D], F32)
    C.y_out = nc.dram_tensor("y", [NB, LLAT, D], F32, kind="ExternalOutput").ap()
    dbg = debug
    C.xs = _dram_tmp(nc, "xs", [NB, LT, D], F32, dbg)
    C.tokmaj = _dram_tmp(nc, "tokmaj", [NB, LT, 2048], BF16, dbg)
    C.nqkT = _dram_tmp(nc, "nqkT", [NB, 1024, LT], BF16, dbg)
    C.lrT = _dram_tmp(nc, "lrT", [NB, 2, 16, LT], F32, dbg)
    C.cat = _dram_tmp(nc, "cat", [NB, LT, D], BF16, dbg)
    C.h2T = _dram_tmp(nc, "h2T", [17, 128, 8, 512], BF16, dbg)
    C.comb = _dram_tmp(nc, "comb", [17, 128, 4, NE], F32, dbg)
    C.ffn_bf = [_dram_tmp(nc, "ffn_wg_bf", [1, D, F_FFN], BF16), _dram_tmp(nc, "ffn_wu_bf", [1, D, F_FFN], BF16),
                _dram_tmp(nc, "ffn_wd_bf", [1, F_FFN, D], BF16)]
    C.moe_bf = [_dram_tmp(nc, "moe_wg_bf", [NE, D, F_MOE], BF16), _dram_tmp(nc, "moe_wu_bf", [NE, D, F_MOE], BF16),
                _dram_tmp(nc, "moe_wd_bf", [NE, F_MOE, D], BF16)]
    if debug:
        C.dbg = nc.dram_tensor("dbg", [128, 8192], F32, kind="ExternalOutput").ap()
    with ExitStack() as gs:
        S = Sched(nc, gs)
        C.S = S
        S.bounds = [NE * CAPE - 1, NTOK - 1, 2 * NTOK - 1]
        GP = Phase(C, "glob")
        C.ident, bid = GP.sbb([128, 128], BF16, 'ident')
        C.ident32, bid32 = GP.sbb([128, 128], F32, 'ident32')
        C.ones, bones = GP.sbb([128, 128], F32, 'ones')
        C.neghalf, bnh = GP.sbb([128, 4], F32, 'neghalf')
        S.op('sp', lambda e: e.dma_start(out=C.ident[:], in_=C.ident_in[:, :]), writes=[bid], dma=bid)
        S.op('sp', lambda e: e.dma_start(out=C.ident32[:], in_=C.ident32_in[:, :]), writes=[bid32], dma=bid32)
        S.op('pool', lambda e: e.memset(C.ones[:], 1.0), writes=[bones])
        S.op('pool', lambda e: e.memset(C.neghalf[:], -0.5), writes=[bnh])
        C.bffn = Buf('ffn_bf')
        C.bmoe = Buf('moe_bf')
        if upto is None or upto >= 5:
            for src, dst in zip([C.ffn_wg, C.ffn_wu, C.ffn_wd], C.ffn_bf):
                S.op('pool', lambda e, src=src, dst=dst: e.dma_start(out=dst[0], in_=src[0]), writes=[C.bffn],
                     dma=C.bffn, track=False)
        C.moe_cast_pending = (upto is None or upto >= 6)
        S.flush()
        for l in range(L):
            LP = Phase(C, f"L{l}")
            C.want_rows = (l == L - 1) and SPARSE_MOE
            phase_mod(C, l, LP)
            if debug and l == debug - 1 and upto == 0:
                dump_mod(C)
            if upto is not None and upto == 0:
                LP.st.close()
                break
            if 1 not in skip:
                phase_proj(C, l)
            if upto is not None and upto <= 1:
                LP.st.close()
                break
            last = (l == L - 1)
            phase_gla(C, l, last)
            if upto is not None and upto <= 2:
                LP.st.close()
                break
            phase_na(C, l, last)
            if upto is not None and upto <= 3:
                LP.st.close()
                break
            phase_outproj(C, l, last)
            if upto is not None and upto <= 4:
                LP.st.close()
                break
            if last:
                tiles = [(b, t) for b in range(NB) for t in range(2, NT)]
            else:
                tiles = [(b, t) for b in range(NB) for t in range(NT)]
            if last and SPARSE_MOE:
                import os
                ms = int(os.environ.get('MOE_STOP', '9'))
                phase_moe_pre(C, l, tiles)
                if ms >= 2:
                    phase_moe_sparse(C, l)
                if ms >= 3:
                    phase_moe_post(C, l, tiles)
            else:
                phase_ffn_pre(C, l, last, tiles)
                phase_ffn(C, l, last, tiles)
            if upto is not None and upto <= 5 + l:
                LP.st.close()
                break
            LP.st.close()
        GP.st.close()
    return nc


def dump_mod(C):
    S = C.S
    Ph = Phase(C, "dump")
    o = 0
    for t, n in [(C.G1, 24), (C.SH1, 24), (C.G2, 24), (C.SH2, 24)]:
        S.op('sp', lambda e, t=t, o=o, n=n: e.dma_start(out=C.dbg[:, o:o + n], in_=t[:].rearrange("p a b -> p (a b)")),
             reads=[C.bG1, C.bG2], dma=S.buf())
        o += n
    for t in [C.GG1, C.GG2]:
        S.op('sp', lambda e, t=t, o=o: e.dma_start(out=C.dbg[:, o:o + 3072], in_=t[:].rearrange("p a b -> p (a b)")),
             reads=[C.bGG1, C.bGG2], dma=S.buf())
        o += 3072
    Ph.close()


def _na_bias_tables(rpb):
    L = rpb.shape[0]
    out = np.full((L, 8, 128, 5, 640), NEG, np.float32)
    reps = [0, 1, 10, 30, 31]
    for pi, j in enumerate(reps):
        ts = min(max(j - 2, 0), 27)
        for rq2 in range(2):
            r = 2 * j + rq2
            rs = min(max(r - 4, 0), 56)
            for cq in range(64):
                cs = min(max(cq - 8, 0), 48)
                p = rq2 * 64 + cq
                ck = np.arange(cs, cs + 16)
                for rk in range(rs, rs + 8):
                    slot = rk - 2 * ts
                    out[:, :, p, pi, slot * 64 + ck] = rpb[:, :, rk - r + 7, ck - cq + 15]
    return out


def _tri():
    i = np.arange(128)
    ut = (i[:, None] <= i[None, :]).astype(np.float32)
    lt = (i[:, None] >= i[None, :]).astype(np.float32)
    sut = (i[:, None] < i[None, :]).astype(np.float32)
    slt = (i[:, None] > i[None, :]).astype(np.float32)
    return np.stack([ut, lt, sut, slt], axis=1).copy()


def make_in_maps(inp, n_cores=8):
    import ml_dtypes
    f = lambda a: np.ascontiguousarray(np.asarray(a, dtype=np.float32))
    L = 2
    w_ada = f(inp['w_ada'])
    b_ada = f(inp['b_ada'])
    badaT3 = np.repeat(b_ada.reshape(L, 48, 128).transpose(0, 2, 1)[:, :, :, None], 3, axis=3).copy()
    gp = np.stack([f(inp['g_pre_mix']), f(inp['g_pre_ffn'])], axis=1)
    gpre3 = np.repeat(gp.reshape(L, 2, 8, 128).transpose(0, 3, 1, 2)[..., None], 3, axis=4).copy()
    rows = np.stack([b_ada[:, 2048:3072], b_ada[:, 5120:6144], f(inp['g_post_mix']), f(inp['g_post_ffn']),
                     b_ada[:, 3072:4096], b_ada[:, 4096:5120], f(inp['g_pre_ffn'])], axis=1)
    rowc = np.repeat(rows[:, None, :, :], 128, axis=1).copy()
    cos, sin = _rope_tables()
    gn = f(inp['gla_g_norm'])
    gnormB = np.repeat(np.tile(gn, (1, 4))[:, None, :], 128, axis=1).copy()
    natb = _na_bias_tables(f(inp['na_rpb']))
    natb = np.ascontiguousarray(natb.reshape(L, 8, 128, 5, 5, 128).transpose(0, 1, 5, 3, 4, 2))
    lst_init = np.zeros((NE * CAPE, 4), np.uint32)
    lst_init[:, 0:2] = 1 << 30
    nidf = (np.arange(64)[None, :] * 128 + np.arange(128)[:, None]).astype(np.float32)
    eoff = np.repeat((np.arange(NE) * CAPE).astype(np.float32)[None, :], 128, axis=0)
    thr = np.repeat((np.arange(NGRP) * GS).astype(np.float32)[None, :], 128, axis=0)
    shared = {
        "lst_init": lst_init, "nidf": nidf, "eoff": eoff, "thr": thr,
        "w_ada": w_ada, "badaT3": badaT3, "gpre3": gpre3, "rowc": rowc, "w_in": f(inp['w_in']),
        "rope_cos": cos, "rope_sin": sin, "ident": np.eye(128).astype(ml_dtypes.bfloat16),
        "ident32": np.eye(128, dtype=np.float32), "tri": _tri(),
        "gla_w_gate": f(inp['gla_w_gate']), "gla_b_gate": f(inp['gla_b_gate']).reshape(L, 2, 1, 256),
        "gnormB": gnormB, "natb": natb, "w_out": f(inp['w_out']),
        "ffn_w_gate": f(inp['ffn_w_gate']), "ffn_w_up": f(inp['ffn_w_up']), "ffn_w_down": f(inp['ffn_w_down']),
        "moe_w_router": f(inp['moe_w_router'])[0], "moe_w_gate": f(inp['moe_w_gate'])[0],
        "moe_w_up": f(inp['moe_w_up'])[0], "moe_w_down": f(inp['moe_w_down'])[0],
    }
    x = f(inp['x'])
    c = f(inp['c'])
    ctx = f(inp['ctx'])
    c_ctx = f(inp['c_ctx'])
    maps = []
    for i in range(n_cores):
        cv = np.stack([c[2 * i], c[2 * i + 1], c_ctx], axis=0)
        cT = cv.reshape(3, 8, 128).transpose(2, 1, 0).copy()
        m = dict(shared)
        m["x"] = x[2 * i:2 * i + 2]
        m["ctx"] = ctx[2 * i:2 * i + 2]
        m["cT"] = cT
        maps.append(m)
    return maps


def kernel(**inputs):
    nc = build_program()
    maps = make_in_maps(inputs, 8)
    res = run_bass_kernel_spmd(nc, maps, core_ids=list(range(8)))
    return np.concatenate([np.asarray(r["y"]) for r in res.results], axis=0).astype(np.float32)


def _rope_tables():
    pos = np.arange(LLAT)
    row, col = pos // 64, pos % 64
    half = 16
    inv = (10000.0 ** (-np.arange(half, dtype=np.float32) / half)).astype(np.float32)
    ang_r = row.astype(np.float32)[:, None] * inv[None, :]
    ang_c = col.astype(np.float32)[:, None] * inv[None, :]
    cr, sr, cc, sc = np.cos(ang_r), np.sin(ang_r), np.cos(ang_c), np.sin(ang_c)
    cos = np.concatenate([cr, cr, cc, cc], axis=1).astype(np.float32)
    sin = np.concatenate([-sr, sr, -sc, sc], axis=1).astype(np.float32)
    cos = cos.reshape(32, 128, 64).transpose(1, 0, 2).copy()
    sin = sin.reshape(32, 128, 64).transpose(1, 0, 2).copy()
    return cos, sin
```

```python
import numpy as np
from contextlib import ExitStack
import concourse.bass as bass
import concourse.mybir as mybir
from concourse.bass_utils import run_bass_kernel_spmd

F32 = mybir.dt.float32
BF16 = mybir.dt.bfloat16
AF = mybir.ActivationFunctionType
ALU = mybir.AluOpType
AX = mybir.AxisListType

D = 1024
NB = 2
LCTX = 256
LLAT = 4096
LT = LCTX + LLAT
NT = LT // 128
PROJ = 3104
EPS = 1e-6
NEG = -30000.0

ENGS = ['pe', 'act', 'dve', 'pool', 'sp']
EPOCH = 30000


class Buf:
    __slots__ = ('name', 'w', 'r', 'dsem')

    def __init__(self, name=''):
        self.name = name
        self.w = None
        self.r = {}
        self.dsem = None


class Sched:
    def __init__(self, nc, stack):
        self.nc = nc
        self.stack = stack
        self.cnt = {e: 0 for e in ENGS}
        self.esems = {e: [] for e in ENGS}
        self.items = {e: [] for e in ENGS}
        self.waited = {e: {} for e in ENGS}
        self.free_dsems = []
        self.phase_bufs = []
        self.outstanding = {}
        self.nsem = 0
        self.ninstr = 0
        self.cregs = {}
        self.bregs = {}
        self.bounds = []
        self._cond = None

    def _newsem(self, name):
        self.nsem += 1
        return self.stack.enter_context(self.nc.semaphore(name))

    def _esem(self, e, seq):
        ep = (seq - 1) // EPOCH
        while len(self.esems[e]) <= ep:
            self.esems[e].append(self._newsem(f"s_{e}_{len(self.esems[e])}"))
        return self.esems[e][ep], (seq - 1) % EPOCH + 1

    def buf(self, name=''):
        b = Buf(name)
        self.phase_bufs.append(b)
        return b

    def bufs(self, n, name=''):
        return [self.buf(f"{name}{i}") for i in range(n)]

    def op(self, eng, fn, reads=(), writes=(), dma=None, self_ok=False, track=True):
        deps = {}

        def add(p):
            if p is None:
                return
            sem, val, peng = p
            if self_ok and peng == eng:
                return
            k = sem.num
            if k not in deps or deps[k][1] < val:
                deps[k] = (sem, val)

        for b in reads:
            add(b.w)
        for b in writes:
            add(b.w)
            for p in b.r.values():
                add(p)
        if dma is None:
            self.cnt[eng] += 1
            sem, val = self._esem(eng, self.cnt[eng])
            inc = 1
            tok = (sem, val, eng)
        else:
            if dma.dsem is None:
                if self.free_dsems:
                    dma.dsem = self.free_dsems.pop()
                else:
                    dma.dsem = [self._newsem(f"d{self.nsem}"), 0]
            dma.dsem[1] += 16
            sem, val = dma.dsem[0], dma.dsem[1]
            inc = 16
            tok = (sem, val, 'dma')
        waits = []
        wd = self.waited[eng]
        for k, (s, v) in deps.items():
            if wd.get(k, 0) >= v:
                continue
            wd[k] = v
            waits.append((s, v))
        self.items[eng].append((fn, waits, sem, inc, val))
        self.ninstr += 1
        for b in writes:
            b.w = tok
            b.r = {}
        for b in reads:
            if b not in writes:
                b.r[sem.num] = tok
        if track:
            self.outstanding[sem.num] = (sem, val)
        return tok

    def breg(self, engine, value):
        if value not in self.bregs:
            r = engine.alloc_register(f"bnd_{value}")
            engine.reg_mov(r, value)
            self.bregs[value] = r
        return self.bregs[value]

    def cond_begin(self, flag_ap):
        self._cond = {'flag': flag_ap, 'start': {e: len(self.items[e]) for e in ENGS},
                      'waited': {e: dict(self.waited[e]) for e in ENGS}}
        for e in ENGS:
            self.items[e].append(('cond_begin', flag_ap))

    def cond_end(self):
        c = self._cond
        for e in ENGS:
            body = self.items[e][c['start'][e] + 1:]
            agg = {}
            for it in body:
                fn, waits, sem, inc = it[0], it[1], it[2], it[3]
                if fn is None or sem is None:
                    continue
                k = sem.num
                if k not in agg:
                    agg[k] = [sem, it[4] - inc, 0]
                agg[k][2] += inc
            self.items[e].append(('cond_end', list(agg.values())))
            self.waited[e] = c['waited'][e]
        self._cond = None

    def barrier(self):
        for e in ENGS:
            waits = []
            for k, (s, v) in self.outstanding.items():
                if self.waited[e].get(k, 0) >= v:
                    continue
                self.waited[e][k] = v
                waits.append((s, v))
            self.items[e].append((None, waits, None, 0, 0))
        self.outstanding = {}

    def flush(self):
        self.barrier()
        nc = self.nc
        with nc.Block() as block:
            regs = {'pe': block.tensor, 'act': block.scalar, 'dve': block.vector,
                    'pool': block.gpsimd, 'sp': block.sync}
            for e in ENGS:
                items = self.items[e]

                def body(engine, items=items, e=e):
                    guard = None
                    if e == 'pool':
                        for v in self.bounds:
                            self.breg(engine, v)
                    for it in items:
                        if it[0] == 'cond_begin':
                            if e not in self.cregs:
                                self.cregs[e] = engine.alloc_register(f"creg_{e}")
                            reg = self.cregs[e]
                            engine.reg_load(reg, it[1])
                            guard = engine.If_ne(reg, 0)
                            guard.__enter__()
                            continue
                        if it[0] == 'cond_end':
                            guard.__exit__(None, None, None)
                            eg = engine.Else()
                            eg.__enter__()
                            for sem, pre, tot in it[1]:
                                if pre > 0:
                                    engine.wait_ge(sem, pre)
                                engine.sem_inc(sem, tot)
                            eg.__exit__(None, None, None)
                            guard = None
                            continue
                        fn, waits, sem, inc, _ = it
                        for s, v in waits:
                            engine.wait_ge(s, v)
                        if fn is not None:
                            ins = fn(engine)
                            ins.then_inc(sem, inc)

                regs[e](body)
        self.items = {e: [] for e in ENGS}
        for b in self.phase_bufs:
            if b.dsem is not None:
                self.free_dsems.append(b.dsem)
                b.dsem = None
        self.phase_bufs = []


class Rot:
    def __init__(self, S, mk, n, name):
        self.t = [mk(f"{name}{i}") for i in range(n)]
        self.b = [S.buf(f"{name}{i}") for i in range(n)]
        self.i = 0

    def next(self):
        k = self.i % len(self.t)
        self.i += 1
        return self.t[k], self.b[k]


class Phase:
    _uid = [0]

    def __init__(self, C, name):
        self.C = C
        self.nc = C.nc
        self.S = C.S
        self.st = ExitStack()
        self.name = name

    def _nm(self, nm):
        Phase._uid[0] += 1
        return f"{self.name}_{nm}_{Phase._uid[0]}"

    def sb(self, shape, dt, nm='t'):
        return self.st.enter_context(self.nc.sbuf_tensor(self._nm(nm), list(shape), dt))

    def ps(self, shape, dt, nm='p'):
        return self.st.enter_context(self.nc.psum_tensor(self._nm(nm), list(shape), dt))

    def sbb(self, shape, dt, nm='t'):
        return self.sb(shape, dt, nm), self.S.buf(nm)

    def rot(self, n, shape, dt, nm, psum=False):
        f = self.ps if psum else self.sb
        return Rot(self.S, lambda s: f(shape, dt, nm), n, nm)

    def close(self):
        self.S.flush()
        self.st.close()


class Ctx:
    pass


def _dram_in(nc, name, shape, dt=F32):
    return nc.dram_tensor(name, list(shape), dt, kind="ExternalInput").ap()


def _dram_tmp(nc, name, shape, dt, dbg=False):
    return nc.dram_tensor(name, list(shape), dt, kind="ExternalOutput" if dbg else "Internal").ap()


import os as _os
USE_TTR = False

O_Q, O_K, O_V, O_R, O_LR, O_NQ, O_NK, O_NV = 0, 256, 512, 1024, 1536, 1568, 2080, 2592
F_FFN = 2816
F_MOE = 3584
NE = 8


def prenorm_tile(Ph, K, xt, bx, hT, bhT, c0, Gt, St, bGS, j, h32=None, bh32=None):
    S = Ph.S
    C = Ph.C
    junk, bj = K['junk'].next()
    st, bst = K['stat'].next()
    xn, bxn = K['xn'].next()
    ptr, bptr = K['ptr'].next()
    S.op('act', lambda e: e.activation(out=junk[:], in_=xt[:], func=AF.Square, accum_out=st[:, 0:1]),
         reads=[bx], writes=[bj, bst])
    S.op('dve', lambda e: e.tensor_scalar(out=st[:, 1:2], in0=st[:, 0:1], scalar1=1.0 / D, scalar2=EPS,
                                          op0=ALU.mult, op1=ALU.add), reads=[bst], writes=[bst])
    S.op('pool', lambda e: e.tensor_tensor(out=st[:, 2:3], in0=st[:, 1:2], in1=C.neghalf[:, 0:1], op=ALU.pow),
         reads=[bst], writes=[bst])
    S.op('act', lambda e: e.activation(out=xn[:], in_=xt[:], func=AF.Copy, scale=st[:, 2:3]),
         reads=[bx, bst], writes=[bxn])

    def tr(e):
        for k in range(8):
            ins = e.transpose(out=ptr[:, k, :], in_=xn[:, k * 128:(k + 1) * 128], identity=C.ident[:])
        return ins
    S.op('pe', tr, reads=[bxn], writes=[bptr], self_ok=True)
    for k in range(8):
        if k % 2 == 0:
            S.op('dve', lambda e, k=k: e.tensor_scalar(out=hT[:, k, c0:c0 + 128], in0=ptr[:, k, :],
                                                       scalar1=Gt[:, k, j:j + 1], scalar2=St[:, k, j:j + 1],
                                                       op0=ALU.mult, op1=ALU.add),
                 reads=[bptr, bGS], writes=[bhT])
        else:
            S.op('act', lambda e, k=k: e.activation(out=hT[:, k, c0:c0 + 128], in_=ptr[:, k, :], func=AF.Identity,
                                                    scale=Gt[:, k, j:j + 1], bias=St[:, k, j:j + 1]),
                 reads=[bptr, bGS], writes=[bhT])
    if h32 is not None:
        xn32, bxn32 = K['xn32'].next()
        p32, bp32 = K['p32'].next()
        S.op('act', lambda e: e.activation(out=xn32[:], in_=xt[:], func=AF.Copy, scale=st[:, 2:3]),
             reads=[bx, bst], writes=[bxn32])

        def tr32(e):
            for k in range(8):
                ins = e.transpose(out=p32[:, k, :], in_=xn32[:, k * 128:(k + 1) * 128], identity=C.ident32[:])
            return ins
        S.op('pe', tr32, reads=[bxn32], writes=[bp32], self_ok=True)
        for k in range(8):
            S.op('dve', lambda e, k=k: e.tensor_scalar(out=h32[:, k, :], in0=p32[:, k, :],
                                                       scalar1=Gt[:, k, j:j + 1], scalar2=St[:, k, j:j + 1],
                                                       op0=ALU.mult, op1=ALU.add),
                 reads=[bp32, bGS], writes=[bh32])


def prenorm_kit(Ph, with32=False):
    K = {
        'junk': Ph.rot(1, [128, D], BF16, 'junk'),
        'stat': Ph.rot(4, [128, 4], F32, 'stat'),
        'xn': Ph.rot(2, [128, D], BF16, 'xn'),
        'ptr': Ph.rot(2, [128, 8, 128], BF16, 'ptr', psum=True),
    }
    if with32:
        K['xn32'] = Ph.rot(2, [128, D], F32, 'xn32')
        K['p32'] = Ph.rot(1, [128, 8, 128], F32, 'p32', psum=True)
    return K


def res_src(C, l, stage, b, tile):
    if l == 0 and stage == 0:
        if tile < 2:
            return C.ctx_in[b, tile * 128:(tile + 1) * 128, :]
        return C.x_in[b, (tile - 2) * 128:(tile - 1) * 128, :]
    return C.xs[b, tile * 128:(tile + 1) * 128, :]


def phase_mod(C, l, LP):
    nc, S = C.nc, C.S
    Ph = Phase(C, f"mod{l}")
    C.G1, C.bG1 = LP.sbb([128, 8, 3], F32, 'G1')
    C.SH1 = LP.sb([128, 8, 3], F32, 'SH1')
    C.G2, C.bG2 = LP.sbb([128, 8, 3], F32, 'G2')
    C.SH2 = LP.sb([128, 8, 3], F32, 'SH2')
    C.GG1, C.bGG1 = LP.sbb([128, 3, D], F32, 'GG1')
    C.GG2, C.bGG2 = LP.sbb([128, 3, D], F32, 'GG2')
    if getattr(C, 'want_rows', False):
        C.G2row, C.bG2row = LP.sbb([128, 2, D], F32, 'G2row')
        C.S2row = LP.sb([128, 2, D], F32, 'S2row')
    scT, bscT = Ph.sbb([128, 8, 3], F32, 'scT')
    scB, bscB = Ph.sbb([128, 3, 8, 128], F32, 'scB')
    bada, bbada = Ph.sbb([128, 48, 3], F32, 'bada')
    gpre, bgpre = Ph.sbb([128, 2, 8, 3], F32, 'gpre')
    rowc, browc = Ph.sbb([128, 7, D], F32, 'rowc')
    sc1, bsc1 = Ph.sbb([128, 8, 3], F32, 'sc1')
    sc2, bsc2 = Ph.sbb([128, 8, 3], F32, 'sc2')
    wblk = Ph.rot(2, [128, 8, 1024], F32, 'wblk')
    pm = Ph.rot(2, [128, 8, 3], F32, 'pm', psum=True)
    pg = Ph.rot(2, [128, 512], F32, 'pg', psum=True)
    S.op('sp', lambda e: e.dma_start(out=scT[:], in_=C.cT[:, :, :]), writes=[bscT], dma=bscT)
    S.op('sp', lambda e: e.dma_start(out=bada[:], in_=C.badaT3[l]), writes=[bbada], dma=bbada)
    S.op('sp', lambda e: e.dma_start(out=gpre[:], in_=C.gpre3[l]), writes=[bgpre], dma=bgpre)
    S.op('sp', lambda e: e.dma_start(out=rowc[:], in_=C.rowc[l]), writes=[browc], dma=browc)
    S.op('act', lambda e: e.activation(out=scT[:], in_=scT[:], func=AF.Silu), reads=[bscT], writes=[bscT])
    for j in range(3):
        for kc in range(8):
            S.op('act', lambda e, j=j, kc=kc: e.activation(out=scB[:, j, kc, :], in_=C.ones[:], func=AF.Copy,
                                                           scale=scT[:, kc, j:j + 1]),
                 reads=[bscT], writes=[bscB])
    fm = [(0, C.SH1, C.bG1), (1, sc1, bsc1), (3, C.SH2, C.bG2), (4, sc2, bsc2)]
    for blk, dst, bdst in fm:
        wt, bw = wblk.next()
        S.op('sp', lambda e, wt=wt, blk=blk: e.dma_start(
            out=wt[:], in_=C.w_ada[l, :, blk * 1024:(blk + 1) * 1024].rearrange("(kc p) n -> p kc n", p=128)),
            writes=[bw], dma=bw)
        pmt, bpm = pm.next()

        def mm(e, wt=wt, pmt=pmt):
            for ch in range(8):
                for kc in range(8):
                    ins = e.matmul(pmt[:, ch, :], lhsT=wt[:, kc, ch * 128:(ch + 1) * 128], rhs=scT[:, kc, :],
                                   start=(kc == 0), stop=(kc == 7))
            return ins
        S.op('pe', mm, reads=[bw, bscT], writes=[bpm], self_ok=True)
        S.op('dve', lambda e, dst=dst, pmt=pmt, blk=blk: e.tensor_tensor(
            out=dst[:], in0=pmt[:], in1=bada[:, blk * 8:(blk + 1) * 8, :], op=ALU.add),
            reads=[bpm, bbada], writes=[bdst])
    S.op('dve', lambda e: e.scalar_tensor_tensor(out=C.G1[:], in0=sc1[:], scalar=1.0, in1=gpre[:, 0], op0=ALU.add,
                                                 op1=ALU.mult), reads=[bsc1, bgpre], writes=[C.bG1])
    S.op('dve', lambda e: e.scalar_tensor_tensor(out=C.G2[:], in0=sc2[:], scalar=1.0, in1=gpre[:, 1], op0=ALU.add,
                                                 op1=ALU.mult), reads=[bsc2, bgpre], writes=[C.bG2])
    for gi, blk, GG, bGG in [(0, 2, C.GG1, C.bGG1), (1, 5, C.GG2, C.bGG2)]:
        wt, bw = wblk.next()
        S.op('sp', lambda e, wt=wt, blk=blk: e.dma_start(
            out=wt[:], in_=C.w_ada[l, :, blk * 1024:(blk + 1) * 1024].rearrange("(kc p) n -> p kc n", p=128)),
            writes=[bw], dma=bw)
        for j in range(3):
            for half in range(2):
                pgt, bpg = pg.next()
                hs = slice(half * 512, (half + 1) * 512)

                def mm(e, wt=wt, pgt=pgt, j=j, hs=hs):
                    for kc in range(8):
                        ins = e.matmul(pgt[:], lhsT=scB[:, j, kc, :], rhs=wt[:, kc, hs], start=(kc == 0),
                                       stop=(kc == 7))
                    return ins
                S.op('pe', mm, reads=[bw, bscB], writes=[bpg], self_ok=True)
                S.op('dve', lambda e, GG=GG, pgt=pgt, j=j, hs=hs, gi=gi: e.tensor_tensor(
                    out=GG[:, j, hs], in0=pgt[:], in1=rowc[:, gi, hs], op=ALU.add), reads=[bpg, browc], writes=[bGG])
                S.op('pool', lambda e, GG=GG, j=j, hs=hs, gi=gi: e.tensor_tensor(
                    out=GG[:, j, hs], in0=GG[:, j, hs], in1=rowc[:, 2 + gi, hs], op=ALU.mult),
                    reads=[bGG, browc], writes=[bGG])
    if getattr(C, 'want_rows', False):
        for blk, dst, ri in [(3, C.S2row, 4), (4, C.G2row, 5)]:
            wt, bw = wblk.next()
            S.op('sp', lambda e, wt=wt, blk=blk: e.dma_start(
                out=wt[:], in_=C.w_ada[l, :, blk * 1024:(blk + 1) * 1024].rearrange("(kc p) n -> p kc n", p=128)),
                writes=[bw], dma=bw)
            for j in range(2):
                for half in range(2):
                    pgt, bpg = pg.next()
                    hs = slice(half * 512, (half + 1) * 512)

                    def mm(e, wt=wt, pgt=pgt, j=j, hs=hs):
                        for kc in range(8):
                            ins = e.matmul(pgt[:], lhsT=scB[:, j, kc, :], rhs=wt[:, kc, hs], start=(kc == 0),
                                           stop=(kc == 7))
                        return ins
                    S.op('pe', mm, reads=[bw, bscB], writes=[bpg], self_ok=True)
                    S.op('dve', lambda e, dst=dst, pgt=pgt, j=j, hs=hs, ri=ri: e.tensor_tensor(
                        out=dst[:, j, hs], in0=pgt[:], in1=rowc[:, ri, hs], op=ALU.add), reads=[bpg, browc],
                        writes=[C.bG2row])
                    if blk == 4:
                        S.op('dve', lambda e, dst=dst, j=j, hs=hs: e.scalar_tensor_tensor(
                            out=dst[:, j, hs], in0=dst[:, j, hs], scalar=1.0, in1=rowc[:, 6, hs], op0=ALU.add,
                            op1=ALU.mult), reads=[C.bG2row, browc], writes=[C.bG2row])
    Ph.close()


def phase_proj(C, l):
    nc, S = C.nc, C.S
    Ph = Phase(C, f"proj{l}")
    K = prenorm_kit(Ph)
    win, bwin = Ph.sbb([128, 8, PROJ], BF16, 'win')
    cos, bcos = Ph.sbb([128, 32, 64], F32, 'cos')
    sin, bsin = Ph.sbb([128, 32, 64], F32, 'sin')
    npc = 4
    pw = PROJ // npc
    for i in range(npc):
        S.op('pool', lambda e, i=i: e.dma_start(
            out=win[:, :, i * pw:(i + 1) * pw],
            in_=C.w_in[l, :, i * pw:(i + 1) * pw].rearrange("(kc p) n -> p kc n", p=128)), writes=[bwin], dma=bwin)
    S.op('sp', lambda e: e.dma_start(out=cos[:], in_=C.rope_cos[:, :, :]), writes=[bcos], dma=bcos)
    S.op('sp', lambda e: e.dma_start(out=sin[:], in_=C.rope_sin[:, :, :]), writes=[bsin], dma=bsin)
    S.op('dve', lambda e: e.tensor_scalar(out=win[:, :, O_Q:O_Q + 256], in0=win[:, :, O_Q:O_Q + 256], scalar1=0.125,
                                          scalar2=None, op0=ALU.mult), reads=[bwin], writes=[bwin])
    S.op('dve', lambda e: e.tensor_scalar(out=win[:, :, O_NQ:O_NQ + 512], in0=win[:, :, O_NQ:O_NQ + 512],
                                          scalar1=0.125, scalar2=None, op0=ALU.mult), reads=[bwin], writes=[bwin])
    xr = Ph.rot(3, [128, D], F32, 'x')
    hTr = Ph.rot(2, [128, 8, 256], BF16, 'hT')
    stg = Ph.rot(2, [128, 2048], BF16, 'stg')
    fstg = Ph.rot(2, [128, 8, 256], BF16, 'fstg')
    lstg = Ph.rot(2, [16, 2, 256], F32, 'lstg')
    t1r = Ph.rot(2, [128, 512], F32, 't1')
    t2r = Ph.rot(2, [128, 512], F32, 't2')
    ptok = Ph.rot(2, [128, 512], F32, 'ptok', psum=True)
    pfe = Ph.rot(2, [128, 512], F32, 'pfe', psum=True)
    tokcols = [(O_Q, O_Q + 512), (O_V, O_V + 512), (O_R, O_R + 512), (O_NV, O_NV + 512)]
    def pre(b, g):
        j = 2 if g == 0 else b
        hT, bhT = hTr.next()
        for t in range(2):
            tile = g * 2 + t
            xt, bx = xr.next()
            S.op('sp', lambda e, xt=xt, tile=tile, b=b: e.dma_start(out=xt[:], in_=res_src(C, l, 0, b, tile)),
                 writes=[bx], dma=bx)
            prenorm_tile(Ph, K, xt, bx, hT, bhT, t * 128, C.G1, C.SH1, C.bG1, j)
        return hT, bhT

    groups = [(b, g) for b in range(NB) for g in range(LT // 256)]
    cur = pre(*groups[0])
    for gi_, (b, g) in enumerate(groups):
        if True:
            hT, bhT = cur
            if gi_ + 1 < len(groups):
                cur = pre(*groups[gi_ + 1])
            for t in range(2):
                tile = g * 2 + t
                st, bst = stg.next()
                for cb in range(4):
                    pt_, bpt = ptok.next()
                    c0, c1 = tokcols[cb]

                    def mm(e, pt_=pt_, t=t, c0=c0, c1=c1, hT=hT):
                        for kc in range(8):
                            ins = e.matmul(pt_[:], lhsT=hT[:, kc, t * 128:(t + 1) * 128], rhs=win[:, kc, c0:c1],
                                           start=(kc == 0), stop=(kc == 7))
                        return ins
                    S.op('pe', mm, reads=[bhT, bwin], writes=[bpt], self_ok=True)
                    so = st[:, cb * 512:(cb + 1) * 512]
                    if cb == 0 and g > 0:
                        lt = tile - 2
                        t1, bt1 = t1r.next()
                        t2, bt2 = t2r.next()
                        cb_ = cos[:, lt, :].unsqueeze(1).to_broadcast([128, 8, 64])
                        p3 = pt_[:].rearrange("p (a d) -> p a d", a=8)
                        S.op('dve', lambda e, t1=t1, p3=p3, cb_=cb_: e.tensor_tensor(
                            out=t1[:].rearrange("p (a d) -> p a d", a=8), in0=p3, in1=cb_, op=ALU.mult),
                            reads=[bpt, bcos], writes=[bt1])
                        p5 = pt_[:].rearrange("p (a b c d) -> p a b c d", a=8, b=2, c=2)
                        s5 = sin[:, lt, :].rearrange("p (b c d) -> p b c d", b=2, c=2)
                        t25 = t2[:].rearrange("p (a b c d) -> p a b c d", a=8, b=2, c=2)
                        for hf in range(2):
                            sb_ = s5[:, :, hf, :].unsqueeze(1).to_broadcast([128, 8, 2, 16])
                            S.op('dve', lambda e, t25=t25, p5=p5, sb_=sb_, hf=hf: e.tensor_tensor(
                                out=t25[:, :, :, hf, :], in0=p5[:, :, :, 1 - hf, :], in1=sb_, op=ALU.mult),
                                reads=[bpt, bsin], writes=[bt2])
                        S.op('pool', lambda e, so=so, t1=t1, t2=t2: e.tensor_tensor(out=so, in0=t1[:], in1=t2[:],
                                                                                   op=ALU.add),
                             reads=[bt1, bt2], writes=[bst])
                    elif cb == 2:
                        S.op('act', lambda e, so=so, pt_=pt_: e.activation(out=so, in_=pt_[:], func=AF.Silu),
                             reads=[bpt], writes=[bst])
                    else:
                        S.op('act', lambda e, so=so, pt_=pt_: e.activation(out=so, in_=pt_[:], func=AF.Copy),
                             reads=[bpt], writes=[bst])
                S.op('sp', lambda e, st=st, tile=tile, b=b: e.dma_start(
                    out=C.tokmaj[b, tile * 128:(tile + 1) * 128, :], in_=st[:]), reads=[bst], dma=bst)
            ft, bft = fstg.next()
            for cc in range(8):
                pf, bpf = pfe.next()
                c0 = O_NQ + cc * 128

                def mmf(e, pf=pf, c0=c0, hT=hT):
                    for kc in range(8):
                        ins = e.matmul(pf[:, 0:256], lhsT=win[:, kc, c0:c0 + 128], rhs=hT[:, kc, :], start=(kc == 0),
                                       stop=(kc == 7))
                    return ins
                S.op('pe', mmf, reads=[bhT, bwin], writes=[bpf], self_ok=True)
                S.op('dve', lambda e, ft=ft, pf=pf, cc=cc: e.tensor_copy(out=ft[:, cc, :], in_=pf[:, 0:256]),
                     reads=[bpf], writes=[bft])
            S.op('sp', lambda e, ft=ft, g=g, b=b: e.dma_start(
                out=C.nqkT[b].rearrange("(cc p) t -> p cc t", p=128)[:, :, g * 256:(g + 1) * 256], in_=ft[:]),
                reads=[bft], dma=bft)
            lt_, blt = lstg.next()
            for d in range(2):
                pf, bpf = pfe.next()
                c0 = O_LR + 16 * d

                def mml(e, pf=pf, c0=c0, hT=hT):
                    for kc in range(8):
                        ins = e.matmul(pf[0:16, 0:256], lhsT=win[:, kc, c0:c0 + 16], rhs=hT[:, kc, :],
                                       start=(kc == 0), stop=(kc == 7))
                    return ins
                S.op('pe', mml, reads=[bhT, bwin], writes=[bpf], self_ok=True)
                S.op('dve', lambda e, lt_=lt_, pf=pf, d=d: e.tensor_copy(out=lt_[:, d, :], in_=pf[0:16, 0:256]),
                     reads=[bpf], writes=[blt])
            S.op('sp', lambda e, lt_=lt_, g=g, b=b: e.dma_start(
                out=C.lrT[b].rearrange("d r t -> r d t")[:, :, g * 256:(g + 1) * 256], in_=lt_[:]),
                reads=[blt], dma=blt)
    Ph.close()


def phase_gla(C, l, last):
    S = C.S
    Ph = Phase(C, f"gla{l}")
    tri, btri = Ph.sbb([128, 4, 128], F32, 'tri')
    wg, bwg = Ph.sbb([16, 2, 256], F32, 'wg')
    bg, bbg = Ph.sbb([1, 2, 256], F32, 'bg')
    gn, bgn = Ph.sbb([128, 512], F32, 'gn')
    S.op('sp', lambda e: e.dma_start(out=tri[:], in_=C.tri_in[:, :, :]), writes=[btri], dma=btri)
    S.op('sp', lambda e: e.dma_start(out=wg[:], in_=C.wgate[l].rearrange("d r n -> r d n")), writes=[bwg], dma=bwg)
    S.op('sp', lambda e: e.dma_start(out=bg[:], in_=C.bgate[l].rearrange("d o n -> o d n")), writes=[bbg], dma=bbg)
    S.op('sp', lambda e: e.dma_start(out=gn[:], in_=C.gnormB[l]), writes=[bgn], dma=bgn)
    lrr = Ph.rot(1, [16, 2, LT], F32, 'lr')
    ost = Ph.sb([128, NT, 512], F32, 'ost')
    bost = [S.buf(f"ost{i}") for i in range(NT)]
    Sst = [Ph.sbb([128, 2, 128], F32, 'Sst') for _ in range(2)]
    Sbf = [Ph.sbb([128, 2, 128], BF16, 'Sbf') for _ in range(2)]
    qkvr = Ph.rot(4, [128, 1024], BF16, 'qkv')
    rgr = Ph.rot(3, [128, 512], BF16, 'rg')
    e1r = Ph.rot(2, [128, 256], F32, 'e1')
    spr = Ph.rot(2, [128, 256], F32, 'sp')
    ebr = Ph.rot(2, [128, 2, 128], F32, 'eb')
    enbr = Ph.rot(2, [128, 2, 128], F32, 'enb')
    eEr = Ph.rot(2, [128, 256], F32, 'eE')
    qdr = Ph.rot(2, [128, 4, 128], BF16, 'qd')
    kdr = Ph.rot(2, [128, 4, 128], BF16, 'kd')
    for rr in (qdr, kdr):
        for t_, b_ in zip(rr.t, rr.b):
            S.op('pool', lambda e, t_=t_: e.memset(t_[:], 0.0), writes=[b_])
    ker = Ph.rot(2, [128, 256], BF16, 'kend')
    Amr = Ph.rot(2, [128, 4, 128], BF16, 'Am')
    osr = Ph.rot(2, [128, 512], F32, 'osum')
    sqr = Ph.rot(2, [128, 512], F32, 'sq')
    ogr = Ph.rot(2, [128, 512], BF16, 'og')
    str_ = Ph.rot(4, [128, 12], F32, 'gst')
    plr = Ph.rot(1, [128, 512], F32, 'pl', psum=True)
    pber = Ph.rot(1, [128, 512], F32, 'pbe', psum=True)
    pTr = Ph.rot(1, [128, 8, 128], BF16, 'pT', psum=True)
    pAr = Ph.rot(2, [128, 4, 128], F32, 'pA', psum=True)
    por = Ph.rot(2, [128, 4, 128], F32, 'po', psum=True)
    pdsr = Ph.rot(1, [128, 2, 256], F32, 'pds', psum=True)

    import os
    STG = int(os.environ.get('GLA_STG', '99'))
    NTL = int(os.environ.get('GLA_NT', str(NT)))

    def block(b, d, tile, first, lr):
        lrt, blr = lr
        rows_of = lambda hp: slice(hp * 64, hp * 64 + 64)
        r0 = tile * 128
        qkv, bqkv = qkvr.next()
        S.op('sp', lambda e: e.dma_start(out=qkv[:], in_=C.tokmaj[b, r0:r0 + 128, 0:1024]), writes=[bqkv], dma=bqkv)
        if (not first) and not (last and tile < 2):
            rg, brg = rgr.next()
            S.op('sp', lambda e: e.dma_start(out=rg[:], in_=C.tokmaj[b, r0:r0 + 128, 1024:1536]), writes=[brg],
                 dma=brg)
        pl, bpl = plr.next()

        def mml(e):
            e.matmul(pl[:, 0:256], lhsT=lrt[:, d, r0:r0 + 128], rhs=wg[:, d, :], start=True, stop=False)
            return e.matmul(pl[:, 0:256], lhsT=C.ones[0:1, :], rhs=bg[0:1, d, :], start=False, stop=True)
        S.op('pe', mml, reads=[blr, bwg, bbg], writes=[bpl], self_ok=True)
        if STG < 2:
            return
        e1, be1 = e1r.next()
        sp_, bsp = spr.next()
        S.op('act', lambda e: e.activation(out=e1[:], in_=pl[:, 0:256], func=AF.Exp, scale=-1.0), reads=[bpl],
             writes=[be1])
        S.op('act', lambda e: e.activation(out=sp_[:], in_=e1[:], func=AF.Ln, bias=1.0), reads=[be1], writes=[bsp])
        if STG < 3:
            return
        Rm = tri[:, 0 if d == 0 else 1, :]
        Um = tri[:, 3 if d == 0 else 2, :]
        pbe, bpbe = pber.next()

        def mmb(e):
            for g in range(2):
                e.matmul(pbe[:, g * 128:(g + 1) * 128], lhsT=sp_[:, g * 128:(g + 1) * 128], rhs=Rm, start=True,
                         stop=True)
            return e.matmul(pbe[:, 256:512], lhsT=Um, rhs=sp_[:], start=True, stop=True)
        S.op('pe', mmb, reads=[bsp, btri], writes=[bpbe], self_ok=True)
        if STG < 4:
            return
        eb, beb = ebr.next()
        enb, benb = enbr.next()
        eE, beE = eEr.next()
        pb3 = pbe[:, 0:256].rearrange("p (g t) -> p g t", g=2)
        S.op('act', lambda e: e.activation(out=eb[:], in_=pb3, func=AF.Exp, scale=-1.0 / 16), reads=[bpbe],
             writes=[beb])
        S.op('act', lambda e: e.activation(out=enb[:], in_=pb3, func=AF.Exp, scale=1.0 / 16), reads=[bpbe],
             writes=[benb])
        S.op('act', lambda e: e.activation(out=eE[:], in_=pbe[:, 256:512], func=AF.Exp, scale=-1.0 / 16),
             reads=[bpbe], writes=[beE])
        if STG < 5:
            return
        pT, bpT = pTr.next()

        def tr(e):
            for i in range(4):
                ins = e.transpose(out=pT[:, i, :], in_=qkv[:, i * 128:(i + 1) * 128], identity=C.ident[:])
            return ins
        S.op('pe', tr, reads=[bqkv], writes=[bpT], self_ok=True)
        if STG < 6:
            return
        qd, bqd = qdr.next()
        kd, bkd = kdr.next()
        kend, bke = ker.next()
        for h in range(4):
            g, rs = h // 2, rows_of(h % 2)
            S.op('dve', lambda e, h=h, g=g, rs=rs: e.tensor_tensor(out=qd[rs, h, :], in0=pT[rs, g, :], in1=eb[rs, g, :],
                                                                   op=ALU.mult), reads=[bpT, beb], writes=[bqd])
            S.op('dve', lambda e, h=h, g=g, rs=rs: e.tensor_tensor(out=kd[rs, h, :], in0=pT[rs, 2 + g, :],
                                                                   in1=enb[rs, g, :], op=ALU.mult),
                 reads=[bpT, benb], writes=[bkd])
        S.op('pool', lambda e: e.tensor_tensor(out=kend[:], in0=qkv[:, 256:512], in1=eE[:], op=ALU.mult),
             reads=[bqkv, beE], writes=[bke])
        if STG < 7:
            return
        pA, bpA = pAr.next()

        def mmA(e):
            for h in range(4):
                ins = e.matmul(pA[:, h, :], lhsT=kd[:, h, :], rhs=qd[:, h, :], start=True, stop=True)
            return ins
        S.op('pe', mmA, reads=[bkd, bqd], writes=[bpA], self_ok=True)
        if STG < 8:
            return
        Am, bAm = Amr.next()
        mask = tri[:, 0 if d == 0 else 1, :].unsqueeze(1).to_broadcast([128, 4, 128])
        S.op('dve', lambda e: e.tensor_tensor(out=Am[:], in0=pA[:], in1=mask, op=ALU.mult), reads=[bpA, btri],
             writes=[bAm])
        if STG < 9:
            return
        po, bpo = por.next()
        sbf, bsbf = Sbf[d]

        def mmo(e):
            for h in range(4):
                g, rs = h // 2, rows_of(h % 2)
                e.matmul(po[:, h, :], lhsT=Am[:, h, :], rhs=qkv[:, 512 + h * 128:512 + (h + 1) * 128], start=True,
                         stop=False)
                ins = e.matmul(po[:, h, :], lhsT=qd[:, h, :], rhs=sbf[:, g, :], start=False, stop=True)
            return ins
        S.op('pe', mmo, reads=[bAm, bqkv, bqd, bsbf], writes=[bpo], self_ok=True)
        need_out = not (last and tile < 2)
        if STG < 10:
            return
        if need_out:
            if first:
                S.op('act', lambda e: e.activation(out=ost[:, tile, :], in_=po[:].rearrange("p h v -> p (h v)"),
                                                   func=AF.Copy), reads=[bpo], writes=[bost[tile]])
            else:
                osum, bos = osr.next()
                sq, bsq = sqr.next()
                og, bog = ogr.next()
                st, bst = str_.next()
                S.op('dve', lambda e: e.tensor_tensor(out=osum[:], in0=po[:].rearrange("p h v -> p (h v)"),
                                                      in1=ost[:, tile, :], op=ALU.add), reads=[bpo, bost[tile]],
                     writes=[bos])
                S.op('pool', lambda e: e.tensor_tensor(out=sq[:], in0=osum[:], in1=osum[:], op=ALU.mult), reads=[bos],
                     writes=[bsq])
                S.op('dve', lambda e: e.tensor_reduce(out=st[:, 0:4], in_=sq[:].rearrange("p (h v) -> p h v", h=4),
                                                      axis=AX.X, op=ALU.add), reads=[bsq], writes=[bst])
                S.op('dve', lambda e: e.tensor_scalar(out=st[:, 4:8], in0=st[:, 0:4], scalar1=1.0 / 128, scalar2=EPS,
                                                      op0=ALU.mult, op1=ALU.add), reads=[bst], writes=[bst])
                S.op('pool', lambda e: e.tensor_tensor(out=st[:, 8:12], in0=st[:, 4:8], in1=C.neghalf[:, 0:4],
                                                       op=ALU.pow), reads=[bst], writes=[bst])
                S.op('dve', lambda e: e.tensor_tensor(
                    out=sq[:].rearrange("p (h v) -> p h v", h=4), in0=osum[:].rearrange("p (h v) -> p h v", h=4),
                    in1=st[:, 8:12].unsqueeze(2).to_broadcast([128, 4, 128]), op=ALU.mult), reads=[bos, bst],
                    writes=[bsq])
                S.op('pool', lambda e: e.tensor_tensor(out=sq[:], in0=sq[:], in1=gn[:], op=ALU.mult), reads=[bsq, bgn],
                     writes=[bsq])
                S.op('pool', lambda e: e.tensor_tensor(out=og[:], in0=sq[:], in1=rg[:], op=ALU.mult),
                     reads=[bsq, brg], writes=[bog])
                S.op('pool', lambda e: e.dma_start(out=C.cat[b, r0:r0 + 128, 0:512], in_=og[:]), reads=[bog], dma=bog)
        if STG < 11:
            return
        pds, bpds = pdsr.next()

        def mmds(e):
            for g in range(2):
                ins = e.matmul(pds[:, g, :], lhsT=kend[:, g * 128:(g + 1) * 128],
                               rhs=qkv[:, 512 + g * 256:512 + (g + 1) * 256], start=True, stop=True)
            return ins
        S.op('pe', mmds, reads=[bke, bqkv], writes=[bpds], self_ok=True)
        if STG < 12:
            return
        sst, bsst = Sst[d]
        dc = 127 if d == 0 else 0
        for g in range(2):
            for hp in range(2):
                rs = rows_of(hp)
                S.op('dve', lambda e, g=g, hp=hp, rs=rs: e.scalar_tensor_tensor(
                    out=sst[rs, g, :], in0=sst[rs, g, :], scalar=eb[rs, g, dc:dc + 1],
                    in1=pds[rs, g, hp * 128:(hp + 1) * 128], op0=ALU.mult, op1=ALU.add),
                    reads=[bsst, beb, bpds], writes=[bsst])
        S.op('act', lambda e: e.activation(out=sbf[:], in_=sst[:], func=AF.Copy), reads=[bsst], writes=[bsbf])

    for b in range(NB):
        lr = lrr.next()
        S.op('sp', lambda e, lr=lr, b=b: e.dma_start(out=lr[0][:], in_=C.lrT[b].rearrange("d r t -> r d t")),
             writes=[lr[1]], dma=lr[1])
        for d in range(2):
            S.op('pool', lambda e, d=d: e.memset(Sst[d][0][:], 0.0), writes=[Sst[d][1]])
            S.op('pool', lambda e, d=d: e.memset(Sbf[d][0][:], 0.0), writes=[Sbf[d][1]])
        orders = [list(range(NT)), [1, 0] + list(range(NT - 1, 1, -1))]
        done = set()
        for i in range(NTL):
            for d in range(2):
                tile = orders[d][i]
                block(b, d, tile, tile not in done, lr)
                done.add(tile)
    Ph.close()


NA_SHIFT = 0.0


def phase_na(C, l, last):
    S = C.S
    Ph = Phase(C, f"na{l}")
    kTr = Ph.rot(1, [128, 4, LT], BF16, 'kT')
    qTr = Ph.rot(2, [128, LT], BF16, 'qT')
    for t_, b_ in zip(qTr.t, qTr.b):
        S.op('pool', lambda e, t_=t_: e.memset(t_[:], 0.0), writes=[b_])
    Vr = Ph.rot(1, [128, NT, 8, 65], BF16, 'V')
    for t_, b_ in zip(Vr.t, Vr.b):
        S.op('pool', lambda e, t_=t_: e.memset(t_[:], 1.0), writes=[b_])
    vstr = Ph.rot(2, [128, 8, 512], BF16, 'vst')
    onar = Ph.rot(1, [128, NT, 512], BF16, 'ona')
    biasr = Ph.rot(1, [128, 5, 5, 128], F32, 'bias')
    ssr = Ph.rot(4, [128, 5, 128], F32, 's')
    pr = Ph.rot(4, [128, 7, 128], BF16, 'p')
    str_ = Ph.rot(8, [128, 4], F32, 'nst')
    negc, bnegc = Ph.sbb([128, 1], F32, 'negc')
    S.op('pool', lambda e: e.memset(negc[:], -NA_SHIFT), writes=[bnegc])
    psr = Ph.rot(3, [128, 8, 128], F32, 'ps', psum=True)
    po_t = Ph.ps([128, 512], F32, 'po')
    po_b = [S.buf(f"po{i}") for i in range(7)]
    po_i = [0]

    def unit(b, h, qt, kT, bkT, qT, bqT, V, bV, ona, bona, bias, bbias):
        g = h // 2
        q0 = qt * 128
        if qt >= 2:
            j = qt - 2
            ts = min(max(j - 2, 0), 27)
            pi = {0: 0, 1: 1, 30: 3, 31: 4}.get(j, 2)
            kcols = [256 + 128 * (ts + k) for k in range(5)] + [0, 128]
            vt = [2 + ts + k for k in range(5)] + [0, 1]
            nl = 5
        else:
            kcols = [0, 128]
            vt = [0, 1]
            nl = 0
        nblk = len(kcols)
        ps_, bps = psr.next()

        def mm(e):
            for kb in range(nblk):
                ins = e.matmul(ps_[:, kb, :], lhsT=kT[:, g, kcols[kb]:kcols[kb] + 128], rhs=qT[:, q0:q0 + 128],
                               start=True, stop=True)
            return ins
        S.op('pe', mm, reads=[bqT, bkT], writes=[bps], self_ok=True)
        p, bp = pr.next()
        if nl:
            s_, bs = ssr.next()
            S.op('dve', lambda e: e.tensor_tensor(out=s_[:], in0=ps_[:, 0:5, :], in1=bias[:, pi, :, :], op=ALU.add),
                 reads=[bps, bbias], writes=[bs])
            S.op('act', lambda e: e.activation(out=p[:, 0:5, :], in_=s_[:], func=AF.Exp, bias=negc[:, 0:1], scale=1.0),
                 reads=[bs, bnegc], writes=[bp])
        S.op('act', lambda e: e.activation(out=p[:, nl:nblk, :], in_=ps_[:, nl:nblk, :], func=AF.Exp,
                                           bias=negc[:, 0:1], scale=1.0), reads=[bps, bnegc], writes=[bp])
        slot = po_i[0] % 7
        po_i[0] += 1
        po = po_t[:, slot * 65:(slot + 1) * 65]
        bpo = po_b[slot]

        def mmpv(e):
            for kb in range(nblk):
                ins = e.matmul(po, lhsT=p[:, kb, :], rhs=V[:, vt[kb], h, :], start=(kb == 0), stop=(kb == nblk - 1))
            return ins
        S.op('pe', mmpv, reads=[bp, bV], writes=[bpo], self_ok=True)
        st, bst = str_.next()
        S.op('dve', lambda e: e.reciprocal(out=st[:, 0:1], in_=po[:, 64:65]), reads=[bpo], writes=[bst])
        S.op('act', lambda e: e.activation(out=ona[:, qt, h * 64:(h + 1) * 64], in_=po[:, 0:64], func=AF.Copy,
                                           scale=st[:, 0:1]), reads=[bpo, bst], writes=[bona])

    for b in range(NB):
        kT, bkT = kTr.next()
        V, bV = Vr.next()
        ona, bona = onar.next()
        for g in range(4):
            S.op('sp', lambda e, kT=kT, b=b, g=g: e.dma_start(
                out=kT[:, g, :], in_=C.nqkT[b, 512 + g * 128:512 + (g + 1) * 128, :]), writes=[bkT], dma=bkT)
        for t0 in range(0, NT, 8):
            t1 = min(NT, t0 + 8)
            vs, bvs = vstr.next()
            S.op('sp', lambda e, vs=vs, b=b, t0=t0, t1=t1: e.dma_start(
                out=vs[:, 0:t1 - t0, :],
                in_=C.tokmaj[b, t0 * 128:t1 * 128, 1536:2048].rearrange("(t p) c -> p t c", p=128)),
                writes=[bvs], dma=bvs)
            S.op('pool', lambda e, vs=vs, V=V, t0=t0, t1=t1: e.tensor_copy(
                out=V[:, t0:t1, :, 0:64], in_=vs[:, 0:t1 - t0, :].rearrange("p t (h d) -> p t h d", h=8)),
                reads=[bvs], writes=[bV])
        if b == NB - 1 and getattr(C, 'moe_cast_pending', False):
            C.moe_cast_pending = False
            for src, dst in zip([C.moe_wg, C.moe_wu, C.moe_wd], C.moe_bf):
                for ex in range(NE):
                    S.op('pool', lambda e, src=src, dst=dst, ex=ex: e.dma_start(out=dst[ex], in_=src[ex]),
                         writes=[C.bmoe], dma=C.bmoe, track=False)
        for h in range(8):
            bias, bbias = biasr.next()
            S.op('sp', lambda e, bias=bias, h=h: e.dma_start(out=bias[:], in_=C.natb[l, h]), writes=[bbias], dma=bbias)
            qT, bqT = qTr.next()
            hr = slice((h % 2) * 64, (h % 2) * 64 + 64)
            S.op('sp', lambda e, qT=qT, b=b, h=h, hr=hr: e.dma_start(
                out=qT[hr, :], in_=C.nqkT[b, h * 64:(h + 1) * 64, :]), writes=[bqT], dma=bqT)
            qts = list(range(2, NT)) + ([] if last else [0, 1])
            for qt in qts:
                unit(b, h, qt, kT, bkT, qT, bqT, V, bV, ona, bona, bias, bbias)
        for t0 in range(2 if last else 0, NT, 8):
            t1 = min(NT, t0 + 8)
            S.op('sp', lambda e, ona=ona, b=b, t0=t0, t1=t1: e.dma_start(
                out=C.cat[b, t0 * 128:t1 * 128, 512:1024].rearrange("(t p) c -> p t c", p=128), in_=ona[:, t0:t1, :]),
                reads=[bona], dma=bona)
    Ph.close()


def phase_outproj(C, l, last):
    S = C.S
    Ph = Phase(C, f"op{l}")
    wo, bwo = Ph.sbb([128, 8, D], BF16, 'wo')
    S.op('pool', lambda e: e.dma_start(out=wo[:], in_=C.w_out[l].rearrange("(kc p) n -> p kc n", p=128)),
         writes=[bwo], dma=bwo)
    ctr = Ph.rot(2, [128, D], BF16, 'ct')
    xr = Ph.rot(2, [128, D], F32, 'x')
    cTsr = Ph.rot(2, [128, 8, 128], BF16, 'cTs')
    ttr = Ph.rot(2, [128, D], F32, 'tt')
    junkr = Ph.rot(1, [128, D], BF16, 'junk')
    str_ = Ph.rot(4, [128, 4], F32, 'ost')
    pTr = Ph.rot(2, [128, 8, 128], BF16, 'pT', psum=True)
    pyr = Ph.rot(2, [128, D], F32, 'py', psum=True)
    for b in range(NB):
        for tile in (range(2, NT) if last else range(NT)):
            j = 2 if tile < 2 else b
            r0 = tile * 128
            ct, bct = ctr.next()
            xt, bx = xr.next()
            S.op('sp', lambda e, ct=ct, b=b, r0=r0: e.dma_start(out=ct[:], in_=C.cat[b, r0:r0 + 128, :]), writes=[bct],
                 dma=bct)
            S.op('sp', lambda e, xt=xt, b=b, tile=tile: e.dma_start(out=xt[:], in_=res_src(C, l, 0, b, tile)),
                 writes=[bx], dma=bx)
            pT, bpT = pTr.next()

            def tr(e, pT=pT, ct=ct):
                for k in range(8):
                    ins = e.transpose(out=pT[:, k, :], in_=ct[:, k * 128:(k + 1) * 128], identity=C.ident[:])
                return ins
            S.op('pe', tr, reads=[bct], writes=[bpT], self_ok=True)
            cTs, bcTs = cTsr.next()
            S.op('dve', lambda e, cTs=cTs, pT=pT: e.tensor_copy(out=cTs[:, 0:4, :], in_=pT[:, 0:4, :]), reads=[bpT],
                 writes=[bcTs])
            S.op('act', lambda e, cTs=cTs, pT=pT: e.activation(out=cTs[:, 4:8, :], in_=pT[:, 4:8, :], func=AF.Copy),
                 reads=[bpT], writes=[bcTs])
            py, bpy = pyr.next()

            def mm(e, py=py, cTs=cTs):
                for half in range(2):
                    for kc in range(8):
                        ins = e.matmul(py[:, half * 512:(half + 1) * 512], lhsT=cTs[:, kc, :],
                                       rhs=wo[:, kc, half * 512:(half + 1) * 512], start=(kc == 0), stop=(kc == 7))
                return ins
            S.op('pe', mm, reads=[bcTs, bwo], writes=[bpy], self_ok=True)
            post_norm_res(Ph, py[:], bpy, xt, bx, C.GG1, C.bGG1, j, junkr, str_, ttr,
                          C.xs[b, r0:r0 + 128, :])
    Ph.close()


def post_norm_res(Ph, y, by, xt, bx, GG, bGG, j, junkr, str_, ttr, dst):
    S = Ph.S
    C = Ph.C
    junk, bj = junkr.next()
    st, bst = str_.next()
    tt, btt = ttr.next()
    S.op('act', lambda e: e.activation(out=junk[:], in_=y, func=AF.Square, accum_out=st[:, 0:1]), reads=[by],
         writes=[bj, bst])
    S.op('dve', lambda e: e.tensor_scalar(out=st[:, 1:2], in0=st[:, 0:1], scalar1=1.0 / D, scalar2=EPS, op0=ALU.mult,
                                          op1=ALU.add), reads=[bst], writes=[bst])
    S.op('pool', lambda e: e.tensor_tensor(out=st[:, 2:3], in0=st[:, 1:2], in1=C.neghalf[:, 0:1], op=ALU.pow),
         reads=[bst], writes=[bst])
    S.op('dve', lambda e: e.scalar_tensor_tensor(out=tt[:], in0=y, scalar=st[:, 2:3], in1=GG[:, j, :], op0=ALU.mult,
                                                 op1=ALU.mult), reads=[by, bst, bGG], writes=[btt])
    S.op('pool', lambda e: e.tensor_tensor(out=tt[:], in0=tt[:], in1=xt[:], op=ALU.add), reads=[btt, bx],
         writes=[btt])
    S.op('pool', lambda e: e.dma_start(out=dst, in_=tt[:]), reads=[btt], dma=btt)


def phase_ffn_pre(C, l, moe, tiles):
    S = C.S
    Ph = Phase(C, f"fpre{l}")
    K = prenorm_kit(Ph, with32=moe)
    xr = Ph.rot(3, [128, D], F32, 'x')
    hTr = Ph.rot(2, [128, 8, 512], BF16, 'hT')
    if moe:
        wr, bwr = Ph.sbb([128, 8, NE], F32, 'wr')
        S.op('sp', lambda e: e.dma_start(out=wr[:], in_=C.moe_wr.rearrange("(kc p) n -> p kc n", p=128)),
             writes=[bwr], dma=bwr)
        h32r = Ph.rot(2, [128, 8, 128], F32, 'h32')
        plg = Ph.rot(1, [128, 512], F32, 'plg', psum=True)
        cmbr = Ph.rot(2, [128, 4, NE], F32, 'cmb')
        rsr = Ph.rot(4, [128, 48], F32, 'rst')
    for gi in range(len(tiles) // 4):
        hT, bhT = hTr.next()
        if moe:
            cmb, bcmb = cmbr.next()
        for t in range(4):
            b, tile = tiles[gi * 4 + t]
            j = 2 if tile < 2 else b
            xt, bx = xr.next()
            S.op('sp', lambda e, xt=xt, b=b, tile=tile: e.dma_start(out=xt[:], in_=res_src(C, l, 1, b, tile)),
                 writes=[bx], dma=bx)
            if not moe:
                prenorm_tile(Ph, K, xt, bx, hT, bhT, t * 128, C.G2, C.SH2, C.bG2, j)
                continue
            h32, bh32 = h32r.next()
            prenorm_tile(Ph, K, xt, bx, hT, bhT, t * 128, C.G2, C.SH2, C.bG2, j, h32, bh32)
            pl, bpl = plg.next()

            def mm(e, pl=pl, h32=h32):
                for kc in range(8):
                    ins = e.matmul(pl[:, 0:NE], lhsT=h32[:, kc, :], rhs=wr[:, kc, :], start=(kc == 0), stop=(kc == 7))
                return ins
            S.op('pe', mm, reads=[bh32, bwr], writes=[bpl], self_ok=True)
            st, bst = rsr.next()
            ops = [
                lambda e, st=st, pl=pl: e.tensor_copy(out=st[:, 0:8], in_=pl[:, 0:NE]),
                lambda e, st=st: e.reduce_max(out=st[:, 8:9], in_=st[:, 0:8], axis=AX.X),
                lambda e, st=st: e.tensor_scalar(out=st[:, 16:24], in0=st[:, 0:8], scalar1=st[:, 8:9], scalar2=-1e30,
                                                 op0=ALU.is_equal, op1=ALU.mult),
                lambda e, st=st: e.tensor_tensor(out=st[:, 16:24], in0=st[:, 16:24], in1=st[:, 0:8], op=ALU.add),
                lambda e, st=st: e.reduce_max(out=st[:, 9:10], in_=st[:, 16:24], axis=AX.X),
                lambda e, st=st: e.tensor_scalar(out=st[:, 24:32], in0=st[:, 0:8], scalar1=st[:, 9:10], scalar2=None,
                                                 op0=ALU.is_ge),
                lambda e, st=st: e.tensor_scalar(out=st[:, 10:11], in0=st[:, 8:9], scalar1=-1.0, scalar2=None,
                                                 op0=ALU.mult),
            ]
            for i, f_ in enumerate(ops):
                S.op('dve', f_, reads=[bst] + ([bpl] if i == 0 else []), writes=[bst])
            S.op('act', lambda e, st=st: e.activation(out=st[:, 32:40], in_=st[:, 0:8], func=AF.Exp, bias=st[:, 10:11],
                                                      scale=1.0), reads=[bst], writes=[bst])
            ops2 = [
                lambda e, st=st: e.tensor_tensor(out=st[:, 32:40], in0=st[:, 32:40], in1=st[:, 24:32], op=ALU.mult),
                lambda e, st=st: e.reduce_sum(out=st[:, 11:12], in_=st[:, 32:40], axis=AX.X),
                lambda e, st=st: e.reciprocal(out=st[:, 12:13], in_=st[:, 11:12]),
            ]
            for f_ in ops2:
                S.op('dve', f_, reads=[bst], writes=[bst])
            S.op('dve', lambda e, st=st, cmb=cmb, t=t: e.tensor_scalar(out=cmb[:, t, :], in0=st[:, 32:40],
                                                                       scalar1=st[:, 12:13], scalar2=None,
                                                                       op0=ALU.mult), reads=[bst], writes=[bcmb])
        S.op('sp', lambda e, hT=hT, gi=gi: e.dma_start(out=C.h2T[gi], in_=hT[:]), reads=[bhT], dma=bhT)
        if moe:
            S.op('sp', lambda e, cmb=cmb, gi=gi: e.dma_start(out=C.comb[gi], in_=cmb[:]), reads=[bcmb], dma=bcmb)
    Ph.close()


def phase_ffn(C, l, moe, tiles):
    S = C.S
    Ph = Phase(C, f"ffn{l}")
    E = NE if moe else 1
    F = F_MOE if moe else F_FFN
    NFC = F // 128
    NFB = F // 256
    wsrc = C.moe_bf if moe else C.ffn_bf
    bwg_ = C.bmoe if moe else C.bffn
    hTr = Ph.rot(2, [128, 8, 512], BF16, 'hT')
    hid, bhid = Ph.sbb([128, NFC, 512], BF16, 'hid')
    wd, bwd = Ph.sbb([128, NFC, D], BF16, 'wd')
    wgr = Ph.rot(3, [128, 8, 256], BF16, 'wg')
    wur = Ph.rot(3, [128, 8, 256], BF16, 'wu')
    yacc, byacc = Ph.sbb([128, 4, D], F32, 'yacc')
    sgr = Ph.rot(2, [128, 512], F32, 'sg')
    xr = Ph.rot(2, [128, D], F32, 'x')
    ttr = Ph.rot(2, [128, D], F32, 'tt')
    junkr = Ph.rot(1, [128, D], BF16, 'junk')
    str_ = Ph.rot(4, [128, 4], F32, 'fst')
    cmbr = Ph.rot(2, [128, 4, NE], F32, 'cmb')
    pgr = Ph.rot(2, [128, 512], F32, 'pg', psum=True)
    pur = Ph.rot(2, [128, 512], F32, 'pu', psum=True)
    pyr = Ph.rot(2, [128, 512], F32, 'py', psum=True)
    for gi in range(len(tiles) // 4):
        hT, bhT = hTr.next()
        S.op('sp', lambda e, hT=hT, gi=gi: e.dma_start(out=hT[:], in_=C.h2T[gi]), writes=[bhT], dma=bhT)
        if moe:
            cmb, bcmb = cmbr.next()
            S.op('sp', lambda e, cmb=cmb, gi=gi: e.dma_start(out=cmb[:], in_=C.comb[gi]), writes=[bcmb], dma=bcmb)
        for ex in range(E):
            hh = NFC // 2
            for (a0, a1) in [(0, hh), (hh, NFC)]:
                S.op('pool', lambda e, ex=ex, a0=a0, a1=a1: e.dma_start(
                    out=wd[:, a0:a1, :],
                    in_=wsrc[2][ex, a0 * 128:a1 * 128, :].rearrange("(fc p) n -> p fc n", p=128)),
                    reads=[bwg_], writes=[bwd], dma=bwd)
            for fb in range(NFB):
                wg, bwg = wgr.next()
                wu, bwu = wur.next()
                S.op('sp', lambda e, wg=wg, ex=ex, fb=fb: e.dma_start(
                    out=wg[:], in_=wsrc[0][ex, :, fb * 256:(fb + 1) * 256].rearrange("(kc p) f -> p kc f", p=128)),
                    reads=[bwg_], writes=[bwg], dma=bwg)
                S.op('sp', lambda e, wu=wu, ex=ex, fb=fb: e.dma_start(
                    out=wu[:], in_=wsrc[1][ex, :, fb * 256:(fb + 1) * 256].rearrange("(kc p) f -> p kc f", p=128)),
                    reads=[bwg_], writes=[bwu], dma=bwu)
                for fi in range(2):
                    fc = fb * 2 + fi
                    pg_, bpg = pgr.next()
                    pu_, bpu = pur.next()

                    def mmg(e, pg_=pg_, wg=wg, fi=fi, hT=hT):
                        for kc in range(8):
                            ins = e.matmul(pg_[:], lhsT=wg[:, kc, fi * 128:(fi + 1) * 128], rhs=hT[:, kc, :],
                                           start=(kc == 0), stop=(kc == 7))
                        return ins

                    def mmu(e, pu_=pu_, wu=wu, fi=fi, hT=hT):
                        for kc in range(8):
                            ins = e.matmul(pu_[:], lhsT=wu[:, kc, fi * 128:(fi + 1) * 128], rhs=hT[:, kc, :],
                                           start=(kc == 0), stop=(kc == 7))
                        return ins
                    S.op('pe', mmg, reads=[bwg, bhT], writes=[bpg], self_ok=True)
                    S.op('pe', mmu, reads=[bwu, bhT], writes=[bpu], self_ok=True)
                    sg, bsg = sgr.next()
                    S.op('act', lambda e, sg=sg, pg_=pg_: e.activation(out=sg[:], in_=pg_[:], func=AF.Silu),
                         reads=[bpg], writes=[bsg])
                    S.op('dve', lambda e, sg=sg, pu_=pu_, fc=fc: e.tensor_tensor(out=hid[:, fc, :], in0=pu_[:],
                                                                                 in1=sg[:], op=ALU.mult),
                         reads=[bsg, bpu], writes=[bhid])
            for t in range(4):
                for half in range(2):
                    py_, bpy = pyr.next()
                    hs = slice(half * 512, (half + 1) * 512)

                    def mmd(e, py_=py_, t=t, hs=hs):
                        for fc in range(NFC):
                            ins = e.matmul(py_[:], lhsT=hid[:, fc, t * 128:(t + 1) * 128], rhs=wd[:, fc, hs],
                                           start=(fc == 0), stop=(fc == NFC - 1))
                        return ins
                    S.op('pe', mmd, reads=[bhid, bwd], writes=[bpy], self_ok=True)
                    if not moe:
                        S.op('act', lambda e, py_=py_, t=t, hs=hs: e.activation(out=yacc[:, t, hs], in_=py_[:],
                                                                                func=AF.Copy),
                             reads=[bpy], writes=[byacc])
                    elif ex == 0:
                        S.op('act', lambda e, py_=py_, t=t, hs=hs, cmb=cmb: e.activation(
                            out=yacc[:, t, hs], in_=py_[:], func=AF.Copy, scale=cmb[:, t, 0:1]),
                            reads=[bpy, bcmb], writes=[byacc])
                    else:
                        S.op('dve', lambda e, py_=py_, t=t, hs=hs, cmb=cmb, ex=ex: e.scalar_tensor_tensor(
                            out=yacc[:, t, hs], in0=py_[:], scalar=cmb[:, t, ex:ex + 1], in1=yacc[:, t, hs],
                            op0=ALU.mult, op1=ALU.add), reads=[bpy, bcmb, byacc], writes=[byacc])
        for t in range(4):
            b, tile = tiles[gi * 4 + t]
            j = 2 if tile < 2 else b
            xt, bx = xr.next()
            S.op('sp', lambda e, xt=xt, b=b, tile=tile: e.dma_start(out=xt[:], in_=res_src(C, l, 1, b, tile)),
                 writes=[bx], dma=bx)
            if moe:
                dst = C.y_out[b, (tile - 2) * 128:(tile - 1) * 128, :]
            else:
                dst = C.xs[b, tile * 128:(tile + 1) * 128, :]

            post_norm_res(Ph, yacc[:, t, :], byacc, xt, bx, C.GG2, C.bGG2, j, junkr, str_, ttr, dst)
    Ph.close()


U32 = mybir.dt.uint32
I32 = mybir.dt.int32
NTOK = NB * LLAT
CAPE = NTOK
GS = 512
NGRP = CAPE // GS


def phase_moe_pre(C, l, tiles):
    S = C.S
    Ph = Phase(C, f"mpre{l}")
    xr = Ph.rot(2, [128, D], F32, 'x')
    junkr = Ph.rot(1, [128, D], BF16, 'junk')
    h32r = Ph.rot(2, [128, D], F32, 'h32')
    hbr = Ph.rot(2, [128, D], BF16, 'hb')
    hTr = Ph.rot(2, [128, 8, 128], F32, 'hT32')
    str_ = Ph.rot(4, [128, 4], F32, 'pst')
    rsr = Ph.rot(4, [128, 80], F32, 'rst')
    selr = Ph.rot(2, [128, NE], BF16, 'selb')
    recr = Ph.rot(8, [128, 4], U32, 'rec')
    slur = Ph.rot(4, [128, 2], U32, 'slu')
    wr, bwr = Ph.sbb([128, 8, NE], F32, 'wr')
    base, bbase = Ph.sbb([128, NE], F32, 'base')
    nid, bnid = Ph.sbb([128, 64], F32, 'nid')
    eoff, beoff = Ph.sbb([128, NE], F32, 'eoff')
    thr, bthr = Ph.sbb([128, NGRP], F32, 'thr')
    trif, btrif = Ph.sbb([128, 128], F32, 'trif')
    trib, btrib = Ph.sbb([128, 128], BF16, 'trib')
    oneb, boneb = Ph.sbb([128, 128], BF16, 'oneb')
    flf, bflf = Ph.sbb([128, NE, NGRP], F32, 'flf')
    fli, bfli = Ph.sbb([128, NE, NGRP], I32, 'fli')
    p32r = Ph.rot(2, [128, 8, 128], F32, 'p32', psum=True)
    plg = Ph.rot(2, [128, 512], F32, 'plg', psum=True)
    blst = S.buf('lst')
    S.op('sp', lambda e: e.dma_start(out=C.lst[:, :], in_=C.lst_init[:, :]), writes=[blst], dma=blst)
    S.op('sp', lambda e: e.dma_start(out=wr[:], in_=C.moe_wr.rearrange("(kc p) n -> p kc n", p=128)), writes=[bwr],
         dma=bwr)
    S.op('sp', lambda e: e.dma_start(out=nid[:], in_=C.nidf[:, :]), writes=[bnid], dma=bnid)
    S.op('sp', lambda e: e.dma_start(out=eoff[:], in_=C.eoff[:, :]), writes=[beoff], dma=beoff)
    S.op('sp', lambda e: e.dma_start(out=thr[:], in_=C.thr[:, :]), writes=[bthr], dma=bthr)
    S.op('sp', lambda e: e.dma_start(out=trif[:], in_=C.tri_in[:, 2, :]), writes=[btrif], dma=btrif)
    S.op('dve', lambda e: e.tensor_copy(out=trib[:], in_=trif[:]), reads=[btrif], writes=[btrib])
    S.op('pool', lambda e: e.memset(oneb[:], 1.0), writes=[boneb])
    S.op('pool', lambda e: e.memset(base[:], 0.0), writes=[bbase])
    for k, (b, tile) in enumerate(tiles):
        xt, bx = xr.next()
        S.op('sp', lambda e, xt=xt, b=b, tile=tile: e.dma_start(out=xt[:], in_=res_src(C, l, 1, b, tile)), writes=[bx],
             dma=bx)
        junk, bj = junkr.next()
        st, bst = str_.next()
        h32, bh32 = h32r.next()
        hb, bhb = hbr.next()
        S.op('act', lambda e, junk=junk, xt=xt, st=st: e.activation(out=junk[:], in_=xt[:], func=AF.Square,
                                                                     accum_out=st[:, 0:1]), reads=[bx], writes=[bj, bst])
        S.op('dve', lambda e, st=st: e.tensor_scalar(out=st[:, 1:2], in0=st[:, 0:1], scalar1=1.0 / D, scalar2=EPS,
                                                     op0=ALU.mult, op1=ALU.add), reads=[bst], writes=[bst])
        S.op('pool', lambda e, st=st: e.tensor_tensor(out=st[:, 2:3], in0=st[:, 1:2], in1=C.neghalf[:, 0:1],
                                                      op=ALU.pow), reads=[bst], writes=[bst])
        S.op('dve', lambda e, h32=h32, xt=xt, st=st, b=b: e.scalar_tensor_tensor(
            out=h32[:], in0=xt[:], scalar=st[:, 2:3], in1=C.G2row[:, b, :], op0=ALU.mult, op1=ALU.mult),
            reads=[bx, bst, C.bG2row], writes=[bh32])
        S.op('pool', lambda e, h32=h32, b=b: e.tensor_tensor(out=h32[:], in0=h32[:], in1=C.S2row[:, b, :], op=ALU.add),
             reads=[bh32, C.bG2row], writes=[bh32])
        S.op('act', lambda e, hb=hb, h32=h32: e.activation(out=hb[:], in_=h32[:], func=AF.Copy), reads=[bh32],
             writes=[bhb])
        S.op('sp', lambda e, hb=hb, k=k: e.dma_start(out=C.h2tok[k * 128:(k + 1) * 128, :], in_=hb[:]), reads=[bhb],
             dma=bhb)
        p32, bp32 = p32r.next()

        def tr32(e, p32=p32, h32=h32):
            for kc in range(8):
                ins = e.transpose(out=p32[:, kc, :], in_=h32[:, kc * 128:(kc + 1) * 128], identity=C.ident32[:])
            return ins
        S.op('pe', tr32, reads=[bh32], writes=[bp32], self_ok=True)
        hT, bhT = hTr.next()
        S.op('dve', lambda e, hT=hT, p32=p32: e.tensor_copy(out=hT[:, 0:4, :], in_=p32[:, 0:4, :]), reads=[bp32],
             writes=[bhT])
        S.op('act', lambda e, hT=hT, p32=p32: e.activation(out=hT[:, 4:8, :], in_=p32[:, 4:8, :], func=AF.Copy),
             reads=[bp32], writes=[bhT])
        pl, bpl = plg.next()

        def mm(e, pl=pl, hT=hT):
            for kc in range(8):
                ins = e.matmul(pl[:, 0:NE], lhsT=hT[:, kc, :], rhs=wr[:, kc, :], start=(kc == 0), stop=(kc == 7))
            return ins
        S.op('pe', mm, reads=[bhT, bwr], writes=[bpl], self_ok=True)
        r, br = rsr.next()
        LG, M1, M2, NM1, DEN, RDEN = r[:, 0:8], r[:, 8:9], r[:, 9:10], r[:, 10:11], r[:, 11:12], r[:, 12:13]
        EQ1, SEL, WN, MSK, OH1, SV, TMP = r[:, 16:24], r[:, 24:32], r[:, 32:40], r[:, 40:48], r[:, 48:56], r[:, 56:64], \
            r[:, 64:72]
        SL0, SL1, W0, W1, D1 = r[:, 72:73], r[:, 73:74], r[:, 74:75], r[:, 75:76], r[:, 76:77]
        selb, bselb = selr.next()
        dv = lambda f_, extra=(): S.op('dve', f_, reads=[br] + list(extra), writes=[br])
        dv(lambda e, LG=LG, pl=pl: e.tensor_copy(out=LG, in_=pl[:, 0:NE]), [bpl])
        dv(lambda e, LG=LG, M1=M1: e.reduce_max(out=M1, in_=LG, axis=AX.X))
        dv(lambda e, EQ1=EQ1, LG=LG, M1=M1: e.tensor_scalar(out=EQ1, in0=LG, scalar1=M1, scalar2=None,
                                                            op0=ALU.is_equal))
        dv(lambda e, MSK=MSK, EQ1=EQ1, LG=LG: e.scalar_tensor_tensor(out=MSK, in0=EQ1, scalar=-1e30, in1=LG,
                                                                     op0=ALU.mult, op1=ALU.add))
        dv(lambda e, MSK=MSK, M2=M2: e.reduce_max(out=M2, in_=MSK, axis=AX.X))
        dv(lambda e, SEL=SEL, LG=LG, M2=M2: e.tensor_scalar(out=SEL, in0=LG, scalar1=M2, scalar2=None, op0=ALU.is_ge))
        dv(lambda e, NM1=NM1, M1=M1: e.tensor_scalar(out=NM1, in0=M1, scalar1=-1.0, scalar2=None, op0=ALU.mult))
        S.op('act', lambda e, WN=WN, LG=LG, NM1=NM1: e.activation(out=WN, in_=LG, func=AF.Exp, bias=NM1, scale=1.0),
             reads=[br], writes=[br])
        dv(lambda e, WN=WN, SEL=SEL: e.tensor_tensor(out=WN, in0=WN, in1=SEL, op=ALU.mult))
        dv(lambda e, WN=WN, DEN=DEN: e.reduce_sum(out=DEN, in_=WN, axis=AX.X))
        dv(lambda e, DEN=DEN, RDEN=RDEN: e.reciprocal(out=RDEN, in_=DEN))
        dv(lambda e, WN=WN, RDEN=RDEN: e.tensor_scalar(out=WN, in0=WN, scalar1=RDEN, scalar2=None, op0=ALU.mult))
        dv(lambda e, OH1=OH1, SEL=SEL, EQ1=EQ1: e.tensor_tensor(out=OH1, in0=SEL, in1=EQ1, op=ALU.subtract))
        S.op('dve', lambda e, selb=selb, SEL=SEL: e.tensor_copy(out=selb[:], in_=SEL), reads=[br], writes=[bselb])
        pc, bpc = plg.next()

        def mmc(e, pc=pc, selb=selb):
            e.matmul(pc[:, 0:NE], lhsT=trib[:], rhs=selb[:], start=True, stop=True)
            return e.matmul(pc[:, 8:8 + NE], lhsT=oneb[:], rhs=selb[:], start=True, stop=True)
        S.op('pe', mmc, reads=[bselb, btrib, boneb], writes=[bpc], self_ok=True)
        dv(lambda e, SV=SV, pc=pc: e.tensor_tensor(out=SV, in0=pc[:, 0:NE], in1=base[:], op=ALU.add), [bpc, bbase])
        dv(lambda e, SV=SV: e.tensor_tensor(out=SV, in0=SV, in1=eoff[:], op=ALU.add), [beoff])
        S.op('dve', lambda e, pc=pc: e.tensor_tensor(out=base[:], in0=pc[:, 8:8 + NE], in1=base[:], op=ALU.add),
             reads=[bpc, br], writes=[bbase])
        for (OH, SL, W) in [(EQ1, SL0, W0), (OH1, SL1, W1)]:
            dv(lambda e, TMP=TMP, OH=OH, SV=SV: e.tensor_tensor(out=TMP, in0=OH, in1=SV, op=ALU.mult))
            dv(lambda e, TMP=TMP, SL=SL: e.reduce_sum(out=SL, in_=TMP, axis=AX.X))
            dv(lambda e, TMP=TMP, OH=OH, WN=WN: e.tensor_tensor(out=TMP, in0=OH, in1=WN, op=ALU.mult))
            dv(lambda e, TMP=TMP, W=W: e.reduce_sum(out=W, in_=TMP, axis=AX.X))
        dv(lambda e, D1=D1, k=k: e.tensor_scalar(out=D1, in0=nid[:, k:k + 1], scalar1=float(NTOK), scalar2=None,
                                                 op0=ALU.add), [bnid])
        slu, bslu = slur.next()
        S.op('dve', lambda e, slu=slu, SL0=SL0: e.tensor_copy(out=slu[:, 0:1], in_=SL0), reads=[br], writes=[bslu])
        S.op('dve', lambda e, slu=slu, SL1=SL1: e.tensor_copy(out=slu[:, 1:2], in_=SL1), reads=[br], writes=[bslu])
        for rk, (DD, WW) in enumerate([(None, W0), (D1, W1)]):
            rec, brec = recr.next()
            S.op('pool', lambda e, rec=rec: e.memset(rec[:], 0), writes=[brec])
            rw = lambda f_: S.op('dve', f_, reads=[br, bnid], writes=[brec])
            rw(lambda e, rec=rec, k=k: e.tensor_copy(out=rec[:, 0:1], in_=nid[:, k:k + 1]))
            if DD is None:
                rw(lambda e, rec=rec, k=k: e.tensor_copy(out=rec[:, 1:2], in_=nid[:, k:k + 1]))
            else:
                rw(lambda e, rec=rec, DD=DD: e.tensor_copy(out=rec[:, 1:2], in_=DD))
            rw(lambda e, rec=rec, WW=WW: e.tensor_copy(out=rec[:, 2:3].bitcast(F32), in_=WW))
            S.op('pool', lambda e, rec=rec, slu=slu, rk=rk: e.indirect_dma_start(
                out=C.lst[:, :], out_offset=bass.IndirectOffsetOnAxis(ap=slu[:, rk:rk + 1], axis=0),
                in_=rec[:], in_offset=None, bounds_check=S.breg(e, NE * CAPE - 1), oob_is_err=False),
                reads=[brec, bslu, blst], dma=brec)
    for ex in range(NE):
        S.op('dve', lambda e, ex=ex: e.tensor_scalar(out=flf[:, ex, :], in0=thr[:], scalar1=base[:, ex:ex + 1],
                                                     scalar2=None, op0=ALU.is_lt), reads=[bbase, bthr], writes=[bflf])
    S.op('dve', lambda e: e.tensor_copy(out=fli[:], in_=flf[:]), reads=[bflf], writes=[bfli])
    S.op('sp', lambda e: e.dma_start(out=C.flags[0:1, :], in_=fli[0:1, :, :].rearrange("p a b -> p (a b)")),
         reads=[bfli], dma=bfli)
    Ph.close()


def phase_moe_sparse(C, l):
    import os
    SPS = int(os.environ.get('SP_STG', '9'))
    S = C.S
    Ph = Phase(C, f"moe{l}")
    NFC = F_MOE // 128
    NFB = F_MOE // 256
    wsrc = C.moe_bf
    hid, bhid = Ph.sbb([128, NFC, 512], BF16, 'hid')
    wd, bwd = Ph.sbb([128, NFC, D], BF16, 'wd')
    wgr = Ph.rot(3, [128, 8, 256], BF16, 'wg')
    wur = Ph.rot(3, [128, 8, 256], BF16, 'wu')
    htr = Ph.rot(2, [128, 4, D], BF16, 'htok')
    hTr = Ph.rot(2, [128, 8, 512], BF16, 'hT')
    recr = Ph.rot(2, [128, 4, 4], U32, 'recs')
    sgr = Ph.rot(2, [128, 512], F32, 'sg')
    yscr = Ph.rot(2, [128, D], F32, 'ysc')
    ptrr = Ph.rot(2, [128, 8, 128], BF16, 'ptr', psum=True)
    pgr = Ph.rot(2, [128, 512], F32, 'pg', psum=True)
    pur = Ph.rot(1, [128, 512], F32, 'pu', psum=True)
    pyr = Ph.rot(2, [128, 512], F32, 'py', psum=True)
    for t_, b_ in zip(htr.t, htr.b):
        S.op('pool', lambda e, t_=t_: e.memset(t_[:], 0.0), writes=[b_])
    for ex in range(NE):
        hh = NFC // 2
        for (a0, a1) in [(0, hh), (hh, NFC)]:
            S.op('sp', lambda e, ex=ex, a0=a0, a1=a1: e.dma_start(
                out=wd[:, a0:a1, :], in_=wsrc[2][ex, a0 * 128:a1 * 128, :].rearrange("(fc p) n -> p fc n", p=128)),
                reads=[C.bmoe], writes=[bwd], dma=bwd)
        for g in range(NGRP):
            S.cond_begin(C.flags[0:1, ex * NGRP + g:ex * NGRP + g + 1])
            recs, brecs = recr.next()
            r0 = ex * CAPE + g * GS
            S.op('sp', lambda e, recs=recs, r0=r0: e.dma_start(
                out=recs[:], in_=C.lst[r0:r0 + GS, :].rearrange("(t p) c -> p t c", p=128)), writes=[brecs], dma=brecs)
            ht, bht = htr.next()
            for t in range(4):
                S.op('pool', lambda e, ht=ht, recs=recs, t=t: e.indirect_dma_start(
                    out=ht[:, t, :], out_offset=None, in_=C.h2tok[:, :],
                    in_offset=bass.IndirectOffsetOnAxis(ap=recs[:, t, 0:1], axis=0), bounds_check=S.breg(e, NTOK - 1),
                    oob_is_err=False), reads=[brecs], writes=[bht], dma=bht)
            hT, bhT = hTr.next()
            for t in range(4 if SPS >= 2 else 0):
                ptr, bptr = ptrr.next()

                def tr(e, ptr=ptr, ht=ht, t=t):
                    for kc in range(8):
                        ins = e.transpose(out=ptr[:, kc, :], in_=ht[:, t, kc * 128:(kc + 1) * 128], identity=C.ident[:])
                    return ins
                S.op('pe', tr, reads=[bht], writes=[bptr], self_ok=True)
                if t % 2 == 0:
                    S.op('dve', lambda e, hT=hT, ptr=ptr, t=t: e.tensor_copy(out=hT[:, :, t * 128:(t + 1) * 128],
                                                                             in_=ptr[:]), reads=[bptr], writes=[bhT])
                else:
                    S.op('act', lambda e, hT=hT, ptr=ptr, t=t: e.activation(out=hT[:, :, t * 128:(t + 1) * 128],
                                                                            in_=ptr[:], func=AF.Copy), reads=[bptr],
                         writes=[bhT])
            for fb in range(NFB if SPS >= 3 else 0):
                wg, bwg = wgr.next()
                wu, bwu = wur.next()
                S.op('sp', lambda e, wg=wg, ex=ex, fb=fb: e.dma_start(
                    out=wg[:], in_=wsrc[0][ex, :, fb * 256:(fb + 1) * 256].rearrange("(kc p) f -> p kc f", p=128)),
                    reads=[C.bmoe], writes=[bwg], dma=bwg)
                S.op('sp', lambda e, wu=wu, ex=ex, fb=fb: e.dma_start(
                    out=wu[:], in_=wsrc[1][ex, :, fb * 256:(fb + 1) * 256].rearrange("(kc p) f -> p kc f", p=128)),
                    reads=[C.bmoe], writes=[bwu], dma=bwu)
                for fi in range(2):
                    fc = fb * 2 + fi
                    pg_, bpg = pgr.next()
                    pu_, bpu = pur.next()

                    def mmg(e, pg_=pg_, wg=wg, fi=fi, hT=hT):
                        for kc in range(8):
                            ins = e.matmul(pg_[:], lhsT=wg[:, kc, fi * 128:(fi + 1) * 128], rhs=hT[:, kc, :],
                                           start=(kc == 0), stop=(kc == 7))
                        return ins

                    def mmu(e, pu_=pu_, wu=wu, fi=fi, hT=hT):
                        for kc in range(8):
                            ins = e.matmul(pu_[:], lhsT=wu[:, kc, fi * 128:(fi + 1) * 128], rhs=hT[:, kc, :],
                                           start=(kc == 0), stop=(kc == 7))
                        return ins
                    S.op('pe', mmg, reads=[bwg, bhT], writes=[bpg], self_ok=True)
                    S.op('pe', mmu, reads=[bwu, bhT], writes=[bpu], self_ok=True)
                    sg, bsg = sgr.next()
                    S.op('act', lambda e, sg=sg, pg_=pg_: e.activation(out=sg[:], in_=pg_[:], func=AF.Silu),
                         reads=[bpg], writes=[bsg])
                    S.op('dve', lambda e, sg=sg, pu_=pu_, fc=fc: e.tensor_tensor(out=hid[:, fc, :], in0=pu_[:],
                                                                                 in1=sg[:], op=ALU.mult),
                         reads=[bsg, bpu], writes=[bhid])
            for t in range(4 if SPS >= 4 else 0):
                ysc, bysc = yscr.next()
                for half in range(2):
                    py_, bpy = pyr.next()
                    hs = slice(half * 512, (half + 1) * 512)

                    def mmd(e, py_=py_, t=t, hs=hs):
                        for fc in range(NFC):
                            ins = e.matmul(py_[:], lhsT=hid[:, fc, t * 128:(t + 1) * 128], rhs=wd[:, fc, hs],
                                           start=(fc == 0), stop=(fc == NFC - 1))
                        return ins
                    S.op('pe', mmd, reads=[bhid, bwd], writes=[bpy], self_ok=True)
                    S.op('act', lambda e, py_=py_, ysc=ysc, hs=hs, recs=recs, t=t: e.activation(
                        out=ysc[:, hs], in_=py_[:], func=AF.Copy, scale=recs[:, t, 2:3].bitcast(F32)),
                        reads=[bpy, brecs], writes=[bysc])
                if SPS >= 5:
                  S.op('pool', lambda e, ysc=ysc, recs=recs, t=t: e.indirect_dma_start(
                    out=C.Ymoe[:, :], out_offset=bass.IndirectOffsetOnAxis(ap=recs[:, t, 1:2], axis=0), in_=ysc[:],
                    in_offset=None, bounds_check=S.breg(e, 2 * NTOK - 1), oob_is_err=False), reads=[bysc, brecs], dma=bysc)
            S.cond_end()
    Ph.close()


def phase_moe_post(C, l, tiles):
    S = C.S
    Ph = Phase(C, f"mpost{l}")
    xr = Ph.rot(4, [128, D], F32, 'x')
    y1r = Ph.rot(4, [128, D], F32, 'y1')
    y2r = Ph.rot(4, [128, D], F32, 'y2')
    ttr = Ph.rot(3, [128, D], F32, 'tt')
    junkr = Ph.rot(2, [128, D], BF16, 'junk')
    str_ = Ph.rot(8, [128, 4], F32, 'fst')
    for k, (b, tile) in enumerate(tiles):
        xt, bx = xr.next()
        y1, by1 = y1r.next()
        y2, by2 = y2r.next()
        S.op('sp', lambda e, xt=xt, b=b, tile=tile: e.dma_start(out=xt[:], in_=res_src(C, l, 1, b, tile)), writes=[bx],
             dma=bx)
        S.op('sp', lambda e, y1=y1, k=k: e.dma_start(out=y1[:], in_=C.Ymoe[k * 128:(k + 1) * 128, :]), writes=[by1],
             dma=by1)
        S.op('sp', lambda e, y2=y2, k=k: e.dma_start(out=y2[:], in_=C.Ymoe[NTOK + k * 128:NTOK + (k + 1) * 128, :]),
             writes=[by2], dma=by2)
        S.op('pool', lambda e, y1=y1, y2=y2: e.tensor_tensor(out=y1[:], in0=y1[:], in1=y2[:], op=ALU.add),
             reads=[by1, by2], writes=[by1])
        dst = C.y_out[b, (tile - 2) * 128:(tile - 1) * 128, :]
        post_norm_res(Ph, y1[:], by1, xt, bx, C.GG2, C.bGG2, b, junkr, str_, ttr, dst)
    Ph.close()


SPARSE_MOE = True


def build_program(debug=False, upto=None, skip=()):
    nc = bass.Bass("TRN2", target_bir_lowering=False)
    C = Ctx()
    C.nc = nc
    C.debug = debug
    L = 2
    C.x_in = _dram_in(nc, "x", [NB, LLAT, D])
    C.ctx_in = _dram_in(nc, "ctx", [NB, LCTX, D])
    C.cT = _dram_in(nc, "cT", [128, 8, 3])
    C.w_ada = _dram_in(nc, "w_ada", [L, D, 6 * D])
    C.badaT3 = _dram_in(nc, "badaT3", [L, 128, 48, 3])
    C.gpre3 = _dram_in(nc, "gpre3", [L, 128, 2, 8, 3])
    C.rowc = _dram_in(nc, "rowc", [L, 128, 7, D])
    C.w_in = _dram_in(nc, "w_in", [L, D, PROJ])
    C.rope_cos = _dram_in(nc, "rope_cos", [128, 32, 64])
    C.rope_sin = _dram_in(nc, "rope_sin", [128, 32, 64])
    C.ident_in = _dram_in(nc, "ident", [128, 128], BF16)
    C.ident32_in = _dram_in(nc, "ident32", [128, 128], F32)
    C.tri_in = _dram_in(nc, "tri", [128, 4, 128], F32)
    C.wgate = _dram_in(nc, "gla_w_gate", [L, 2, 16, 256])
    C.bgate = _dram_in(nc, "gla_b_gate", [L, 2, 1, 256])
    C.gnormB = _dram_in(nc, "gnormB", [L, 128, 512])
    C.natb = _dram_in(nc, "natb", [L, 8, 128, 5, 5, 128])
    C.w_out = _dram_in(nc, "w_out", [L, D, D])
    C.ffn_wg = _dram_in(nc, "ffn_w_gate", [1, D, F_FFN])
    C.ffn_wu = _dram_in(nc, "ffn_w_up", [1, D, F_FFN])
    C.ffn_wd = _dram_in(nc, "ffn_w_down", [1, F_FFN, D])
    C.moe_wr = _dram_in(nc, "moe_w_router", [D, NE])
    C.moe_wg = _dram_in(nc, "moe_w_gate", [NE, D, F_MOE])
    C.moe_wu = _dram_in(nc, "moe_w_up", [NE, D, F_MOE])
    C.moe_wd = _dram_in(nc, "moe_w_down", [NE, F_MOE, D])
    C.lst_init = _dram_in(nc, "lst_init", [NE * CAPE, 4], U32)
    C.nidf = _dram_in(nc, "nidf", [128, 64])
    C.eoff = _dram_in(nc, "eoff", [128, NE])
    C.thr = _dram_in(nc, "thr", [128, NGRP])
    C.lst = _dram_tmp(nc, "lst", [NE * CAPE, 4], U32)
    C.flags = _dram_tmp(nc, "flags", [1, NE * NGRP], I32)
    C.h2tok = _dram_tmp(nc, "h2tok", [NTOK, D], BF16)
    C.Ymoe = _dram_tmp(nc, "Ymoe", [2 * NTOK, D], F32)
    C.y_out = nc.dram_tensor("y", [NB, LLAT, D], F32, kind="ExternalOutput").ap()
    dbg = debug
    C.xs = _dram_tmp(nc, "xs", [NB, LT, D], F32, dbg)
    C.tokmaj = _dram_tmp(nc, "tokmaj", [NB, LT, 2048], BF16, dbg)
    C.nqkT = _dram_tmp(nc, "nqkT", [NB, 1024, LT], BF16, dbg)
    C.lrT = _dram_tmp(nc, "lrT", [NB, 2, 16, LT], F32, dbg)
    C.cat = _dram_tmp(nc, "cat", [NB, LT, D], BF16, dbg)
    C.h2T = _dram_tmp(nc, "h2T", [17, 128, 8, 512], BF16, dbg)
    C.comb = _dram_tmp(nc, "comb", [17, 128, 4, NE], F32, dbg)
    C.ffn_bf = [_dram_tmp(nc, "ffn_wg_bf", [1, D, F_FFN], BF16), _dram_tmp(nc, "ffn_wu_bf", [1, D, F_FFN], BF16),
                _dram_tmp(nc, "ffn_wd_bf", [1, F_FFN, D], BF16)]
    C.moe_bf = [_dram_tmp(nc, "moe_wg_bf", [NE, D, F_MOE], BF16), _dram_tmp(nc, "moe_wu_bf", [NE, D, F_MOE], BF16),
                _dram_tmp(nc, "moe_wd_bf", [NE, F_MOE, D], BF16)]
    if debug:
        C.dbg = nc.dram_tensor("dbg", [128, 8192], F32, kind="ExternalOutput").ap()
    with ExitStack() as gs:
        S = Sched(nc, gs)
        C.S = S
        S.bounds = [NE * CAPE - 1, NTOK - 1, 2 * NTOK - 1]
        GP = Phase(C, "glob")
        C.ident, bid = GP.sbb([128, 128], BF16, 'ident')
        C.ident32, bid32 = GP.sbb([128, 128], F32, 'ident32')
        C.ones, bones = GP.sbb([128, 128], F32, 'ones')
        C.neghalf, bnh = GP.sbb([128, 4], F32, 'neghalf')
        S.op('sp', lambda e: e.dma_start(out=C.ident[:], in_=C.ident_in[:, :]), writes=[bid], dma=bid)
        S.op('sp', lambda e: e.dma_start(out=C.ident32[:], in_=C.ident32_in[:, :]), writes=[bid32], dma=bid32)
        S.op('pool', lambda e: e.memset(C.ones[:], 1.0), writes=[bones])
        S.op('pool', lambda e: e.memset(C.neghalf[:], -0.5), writes=[bnh])
        C.bffn = Buf('ffn_bf')
        C.bmoe = Buf('moe_bf')
        if upto is None or upto >= 5:
            for src, dst in zip([C.ffn_wg, C.ffn_wu, C.ffn_wd], C.ffn_bf):
                S.op('pool', lambda e, src=src, dst=dst: e.dma_start(out=dst[0], in_=src[0]), writes=[C.bffn],
                     dma=C.bffn, track=False)
        C.moe_cast_pending = (upto is None or upto >= 6)
        S.flush()
        for l in range(L):
            LP = Phase(C, f"L{l}")
            C.want_rows = (l == L - 1) and SPARSE_MOE
            phase_mod(C, l, LP)
            if debug and l == debug - 1 and upto == 0:
                dump_mod(C)
            if upto is not None and upto == 0:
                LP.st.close()
                break
            if 1 not in skip:
                phase_proj(C, l)
            if upto is not None and upto <= 1:
                LP.st.close()
                break
            last = (l == L - 1)
            phase_gla(C, l, last)
            if upto is not None and upto <= 2:
                LP.st.close()
                break
            phase_na(C, l, last)
            if upto is not None and upto <= 3:
                LP.st.close()
                break
            phase_outproj(C, l, last)
            if upto is not None and upto <= 4:
                LP.st.close()
                break
            if last:
                tiles = [(b, t) for b in range(NB) for t in range(2, NT)]
            else:
                tiles = [(b, t) for b in range(NB) for t in range(NT)]
            if last and SPARSE_MOE:
                import os
                ms = int(os.environ.get('MOE_STOP', '9'))
                phase_moe_pre(C, l, tiles)
                if ms >= 2:
                    phase_moe_sparse(C, l)
                if ms >= 3:
                    phase_moe_post(C, l, tiles)
            else:
                phase_ffn_pre(C, l, last, tiles)
                phase_ffn(C, l, last, tiles)
            if upto is not None and upto <= 5 + l:
                LP.st.close()
                break
            LP.st.close()
        GP.st.close()
    return nc


def dump_mod(C):
    S = C.S
    Ph = Phase(C, "dump")
    o = 0
    for t, n in [(C.G1, 24), (C.SH1, 24), (C.G2, 24), (C.SH2, 24)]:
        S.op('sp', lambda e, t=t, o=o, n=n: e.dma_start(out=C.dbg[:, o:o + n], in_=t[:].rearrange("p a b -> p (a b)")),
             reads=[C.bG1, C.bG2], dma=S.buf())
        o += n
    for t in [C.GG1, C.GG2]:
        S.op('sp', lambda e, t=t, o=o: e.dma_start(out=C.dbg[:, o:o + 3072], in_=t[:].rearrange("p a b -> p (a b)")),
             reads=[C.bGG1, C.bGG2], dma=S.buf())
        o += 3072
    Ph.close()


def _na_bias_tables(rpb):
    L = rpb.shape[0]
    out = np.full((L, 8, 128, 5, 640), NEG, np.float32)
    reps = [0, 1, 10, 30, 31]
    for pi, j in enumerate(reps):
        ts = min(max(j - 2, 0), 27)
        for rq2 in range(2):
            r = 2 * j + rq2
            rs = min(max(r - 4, 0), 56)
            for cq in range(64):
                cs = min(max(cq - 8, 0), 48)
                p = rq2 * 64 + cq
                ck = np.arange(cs, cs + 16)
                for rk in range(rs, rs + 8):
                    slot = rk - 2 * ts
                    out[:, :, p, pi, slot * 64 + ck] = rpb[:, :, rk - r + 7, ck - cq + 15]
    return out


def _tri():
    i = np.arange(128)
    ut = (i[:, None] <= i[None, :]).astype(np.float32)
    lt = (i[:, None] >= i[None, :]).astype(np.float32)
    sut = (i[:, None] < i[None, :]).astype(np.float32)
    slt = (i[:, None] > i[None, :]).astype(np.float32)
    return np.stack([ut, lt, sut, slt], axis=1).copy()


def make_in_maps(inp, n_cores=8):
    import ml_dtypes
    f = lambda a: np.ascontiguousarray(np.asarray(a, dtype=np.float32))
    L = 2
    w_ada = f(inp['w_ada'])
    b_ada = f(inp['b_ada'])
    badaT3 = np.repeat(b_ada.reshape(L, 48, 128).transpose(0, 2, 1)[:, :, :, None], 3, axis=3).copy()
    gp = np.stack([f(inp['g_pre_mix']), f(inp['g_pre_ffn'])], axis=1)
    gpre3 = np.repeat(gp.reshape(L, 2, 8, 128).transpose(0, 3, 1, 2)[..., None], 3, axis=4).copy()
    rows = np.stack([b_ada[:, 2048:3072], b_ada[:, 5120:6144], f(inp['g_post_mix']), f(inp['g_post_ffn']),
                     b_ada[:, 3072:4096], b_ada[:, 4096:5120], f(inp['g_pre_ffn'])], axis=1)
    rowc = np.repeat(rows[:, None, :, :], 128, axis=1).copy()
    cos, sin = _rope_tables()
    gn = f(inp['gla_g_norm'])
    gnormB = np.repeat(np.tile(gn, (1, 4))[:, None, :], 128, axis=1).copy()
    natb = _na_bias_tables(f(inp['na_rpb']))
    natb = np.ascontiguousarray(natb.reshape(L, 8, 128, 5, 5, 128).transpose(0, 1, 5, 3, 4, 2))
    lst_init = np.zeros((NE * CAPE, 4), np.uint32)
    lst_init[:, 0:2] = 1 << 30
    nidf = (np.arange(64)[None, :] * 128 + np.arange(128)[:, None]).astype(np.float32)
    eoff = np.repeat((np.arange(NE) * CAPE).astype(np.float32)[None, :], 128, axis=0)
    thr = np.repeat((np.arange(NGRP) * GS).astype(np.float32)[None, :], 128, axis=0)
    shared = {
        "lst_init": lst_init, "nidf": nidf, "eoff": eoff, "thr": thr,
        "w_ada": w_ada, "badaT3": badaT3, "gpre3": gpre3, "rowc": rowc, "w_in": f(inp['w_in']),
        "rope_cos": cos, "rope_sin": sin, "ident": np.eye(128).astype(ml_dtypes.bfloat16),
        "ident32": np.eye(128, dtype=np.float32), "tri": _tri(),
        "gla_w_gate": f(inp['gla_w_gate']), "gla_b_gate": f(inp['gla_b_gate']).reshape(L, 2, 1, 256),
        "gnormB": gnormB, "natb": natb, "w_out": f(inp['w_out']),
        "ffn_w_gate": f(inp['ffn_w_gate']), "ffn_w_up": f(inp['ffn_w_up']), "ffn_w_down": f(inp['ffn_w_down']),
        "moe_w_router": f(inp['moe_w_router'])[0], "moe_w_gate": f(inp['moe_w_gate'])[0],
        "moe_w_up": f(inp['moe_w_up'])[0], "moe_w_down": f(inp['moe_w_down'])[0],
    }
    x = f(inp['x'])
    c = f(inp['c'])
    ctx = f(inp['ctx'])
    c_ctx = f(inp['c_ctx'])
    maps = []
    for i in range(n_cores):
        cv = np.stack([c[2 * i], c[2 * i + 1], c_ctx], axis=0)
        cT = cv.reshape(3, 8, 128).transpose(2, 1, 0).copy()
        m = dict(shared)
        m["x"] = x[2 * i:2 * i + 2]
        m["ctx"] = ctx[2 * i:2 * i + 2]
        m["cT"] = cT
        maps.append(m)
    return maps


def kernel(**inputs):
    nc = build_program()
    maps = make_in_maps(inputs, 8)
    res = run_bass_kernel_spmd(nc, maps, core_ids=list(range(8)))
    return np.concatenate([np.asarray(r["y"]) for r in res.results], axis=0).astype(np.float32)


def _rope_tables():
    pos = np.arange(LLAT)
    row, col = pos // 64, pos % 64
    half = 16
    inv = (10000.0 ** (-np.arange(half, dtype=np.float32) / half)).astype(np.float32)
    ang_r = row.astype(np.float32)[:, None] * inv[None, :]
    ang_c = col.astype(np.float32)[:, None] * inv[None, :]
    cr, sr, cc, sc = np.cos(ang_r), np.sin(ang_r), np.cos(ang_c), np.sin(ang_c)
    cos = np.concatenate([cr, cr, cc, cc], axis=1).astype(np.float32)
    sin = np.concatenate([-sr, sr, -sc, sc], axis=1).astype(np.float32)
    cos = cos.reshape(32, 128, 64).transpose(1, 0, 2).copy()
    sin = sin.reshape(32, 128, 64).transpose(1, 0, 2).copy()
    return cos, sin
```

```python
import numpy as np
from contextlib import ExitStack
import concourse.bass as bass
import concourse.mybir as mybir
from concourse.bass_utils import run_bass_kernel_spmd

F32 = mybir.dt.float32
BF16 = mybir.dt.bfloat16
AF = mybir.ActivationFunctionType
ALU = mybir.AluOpType
AX = mybir.AxisListType

D = 1024
NB = 2
LCTX = 256
LLAT = 4096
LT = LCTX + LLAT
NT = LT // 128
PROJ = 3104
EPS = 1e-6
NEG = -30000.0

ENGS = ['pe', 'act', 'dve', 'pool', 'sp']
EPOCH = 30000


class Buf:
    __slots__ = ('name', 'w', 'r', 'dsem')

    def __init__(self, name=''):
        self.name = name
        self.w = None
        self.r = {}
        self.dsem = None


class Sched:
    def __init__(self, nc, stack):
        self.nc = nc
        self.stack = stack
        self.cnt = {e: 0 for e in ENGS}
        self.esems = {e: [] for e in ENGS}
        self.items = {e: [] for e in ENGS}
        self.waited = {e: {} for e in ENGS}
        self.free_dsems = []
        self.phase_bufs = []
        self.outstanding = {}
        self.nsem = 0
        self.ninstr = 0
        self.cregs = {}
        self.bregs = {}
        self.bounds = []
        self._cond = None

    def _newsem(self, name):
        self.nsem += 1
        return self.stack.enter_context(self.nc.semaphore(name))

    def _esem(self, e, seq):
        ep = (seq - 1) // EPOCH
        while len(self.esems[e]) <= ep:
            self.esems[e].append(self._newsem(f"s_{e}_{len(self.esems[e])}"))
        return self.esems[e][ep], (seq - 1) % EPOCH + 1

    def buf(self, name=''):
        b = Buf(name)
        self.phase_bufs.append(b)
        return b

    def bufs(self, n, name=''):
        return [self.buf(f"{name}{i}") for i in range(n)]

    def op(self, eng, fn, reads=(), writes=(), dma=None, self_ok=False, track=True):
        deps = {}

        def add(p):
            if p is None:
                return
            sem, val, peng = p
            if self_ok and peng == eng:
                return
            k = sem.num
            if k not in deps or deps[k][1] < val:
                deps[k] = (sem, val)

        for b in reads:
            add(b.w)
        for b in writes:
            add(b.w)
            for p in b.r.values():
                add(p)
        if dma is None:
            self.cnt[eng] += 1
            sem, val = self._esem(eng, self.cnt[eng])
            inc = 1
            tok = (sem, val, eng)
        else:
            if dma.dsem is None:
                if self.free_dsems:
                    dma.dsem = self.free_dsems.pop()
                else:
                    dma.dsem = [self._newsem(f"d{self.nsem}"), 0]
            dma.dsem[1] += 16
            sem, val = dma.dsem[0], dma.dsem[1]
            inc = 16
            tok = (sem, val, 'dma')
        waits = []
        wd = self.waited[eng]
        for k, (s, v) in deps.items():
            if wd.get(k, 0) >= v:
                continue
            wd[k] = v
            waits.append((s, v))
        self.items[eng].append((fn, waits, sem, inc, val))
        self.ninstr += 1
        for b in writes:
            b.w = tok
            b.r = {}
        for b in reads:
            if b not in writes:
                b.r[sem.num] = tok
        if track:
            self.outstanding[sem.num] = (sem, val)
        return tok

    def breg(self, engine, value):
        if value not in self.bregs:
            r = engine.alloc_register(f"bnd_{value}")
            engine.reg_mov(r, value)
            self.bregs[value] = r
        return self.bregs[value]

    def cond_begin(self, flag_ap):
        self._cond = {'flag': flag_ap, 'start': {e: len(self.items[e]) for e in ENGS},
                      'waited': {e: dict(self.waited[e]) for e in ENGS}}
        for e in ENGS:
            self.items[e].append(('cond_begin', flag_ap))

    def cond_end(self):
        c = self._cond
        for e in ENGS:
            body = self.items[e][c['start'][e] + 1:]
            agg = {}
            for it in body:
                fn, waits, sem, inc = it[0], it[1], it[2], it[3]
                if fn is None or sem is None:
                    continue
                k = sem.num
                if k not in agg:
                    agg[k] = [sem, it[4] - inc, 0]
                agg[k][2] += inc
            self.items[e].append(('cond_end', list(agg.values())))
            self.waited[e] = c['waited'][e]
        self._cond = None

    def barrier(self):
        for e in ENGS:
            waits = []
            for k, (s, v) in self.outstanding.items():
                if self.waited[e].get(k, 0) >= v:
                    continue
                self.waited[e][k] = v
                waits.append((s, v))
            self.items[e].append((None, waits, None, 0, 0))
        self.outstanding = {}

    def flush(self):
        self.barrier()
        nc = self.nc
        with nc.Block() as block:
            regs = {'pe': block.tensor, 'act': block.scalar, 'dve': block.vector,
                    'pool': block.gpsimd, 'sp': block.sync}
            for e in ENGS:
                items = self.items[e]

                def body(engine, items=items, e=e):
                    guard = None
                    if e == 'pool':
                        for v in self.bounds:
                            self.breg(engine, v)
                    for it in items:
                        if it[0] == 'cond_begin':
                            if e not in self.cregs:
                                self.cregs[e] = engine.alloc_register(f"creg_{e}")
                            reg = self.cregs[e]
                            engine.reg_load(reg, it[1])
                            guard = engine.If_ne(reg, 0)
                            guard.__enter__()
                            continue
                        if it[0] == 'cond_end':
                            guard.__exit__(None, None, None)
                            eg = engine.Else()
                            eg.__enter__()
                            for sem, pre, tot in it[1]:
                                if pre > 0:
                                    engine.wait_ge(sem, pre)
                                engine.sem_inc(sem, tot)
                            eg.__exit__(None, None, None)
                            guard = None
                            continue
                        fn, waits, sem, inc, _ = it
                        for s, v in waits:
                            engine.wait_ge(s, v)
                        if fn is not None:
                            ins = fn(engine)
                            ins.then_inc(sem, inc)

                regs[e](body)
        self.items = {e: [] for e in ENGS}
        for b in self.phase_bufs:
            if b.dsem is not None:
                self.free_dsems.append(b.dsem)
                b.dsem = None
        self.phase_bufs = []


class Rot:
    def __init__(self, S, mk, n, name):
        self.t = [mk(f"{name}{i}") for i in range(n)]
        self.b = [S.buf(f"{name}{i}") for i in range(n)]
        self.i = 0

    def next(self):
        k = self.i % len(self.t)
        self.i += 1
        return self.t[k], self.b[k]


class Phase:
    _uid = [0]

    def __init__(self, C, name):
        self.C = C
        self.nc = C.nc
        self.S = C.S
        self.st = ExitStack()
        self.name = name

    def _nm(self, nm):
        Phase._uid[0] += 1
        return f"{self.name}_{nm}_{Phase._uid[0]}"

    def sb(self, shape, dt, nm='t'):
        return self.st.enter_context(self.nc.sbuf_tensor(self._nm(nm), list(shape), dt))

    def ps(self, shape, dt, nm='p'):
        return self.st.enter_context(self.nc.psum_tensor(self._nm(nm), list(shape), dt))

    def sbb(self, shape, dt, nm='t'):
        return self.sb(shape, dt, nm), self.S.buf(nm)

    def rot(self, n, shape, dt, nm, psum=False):
        f = self.ps if psum else self.sb
        return Rot(self.S, lambda s: f(shape, dt, nm), n, nm)

    def close(self):
        self.S.flush()
        self.st.close()


class Ctx:
    pass


def _dram_in(nc, name, shape, dt=F32):
    return nc.dram_tensor(name, list(shape), dt, kind="ExternalInput").ap()


def _dram_tmp(nc, name, shape, dt, dbg=False):
    return nc.dram_tensor(name, list(shape), dt, kind="ExternalOutput" if dbg else "Internal").ap()


import os as _os
USE_TTR = False
GLA_PIPE = True
NA_PIPE = 0

O_Q, O_K, O_V, O_R, O_LR, O_NQ, O_NK, O_NV = 0, 256, 512, 1024, 1536, 1568, 2080, 2592
F_FFN = 2816
F_MOE = 3584
NE = 8


def prenorm_tile(Ph, K, xt, bx, hT, bhT, c0, Gt, St, bGS, j, h32=None, bh32=None):
    S = Ph.S
    C = Ph.C
    junk, bj = K['junk'].next()
    st, bst = K['stat'].next()
    xn, bxn = K['xn'].next()
    ptr, bptr = K['ptr'].next()
    S.op('act', lambda e: e.activation(out=junk[:], in_=xt[:], func=AF.Square, accum_out=st[:, 0:1]),
         reads=[bx], writes=[bj, bst])
    S.op('dve', lambda e: e.tensor_scalar(out=st[:, 1:2], in0=st[:, 0:1], scalar1=1.0 / D, scalar2=EPS,
                                          op0=ALU.mult, op1=ALU.add), reads=[bst], writes=[bst])
    S.op('pool', lambda e: e.tensor_tensor(out=st[:, 2:3], in0=st[:, 1:2], in1=C.neghalf[:, 0:1], op=ALU.pow),
         reads=[bst], writes=[bst])
    S.op('act', lambda e: e.activation(out=xn[:], in_=xt[:], func=AF.Copy, scale=st[:, 2:3]),
         reads=[bx, bst], writes=[bxn])

    def tr(e):
        for k in range(8):
            ins = e.transpose(out=ptr[:, k, :], in_=xn[:, k * 128:(k + 1) * 128], identity=C.ident[:])
        return ins
    S.op('pe', tr, reads=[bxn], writes=[bptr], self_ok=True)
    for k in range(8):
        if k % 2 == 0:
            S.op('dve', lambda e, k=k: e.tensor_scalar(out=hT[:, k, c0:c0 + 128], in0=ptr[:, k, :],
                                                       scalar1=Gt[:, k, j:j + 1], scalar2=St[:, k, j:j + 1],
                                                       op0=ALU.mult, op1=ALU.add),
                 reads=[bptr, bGS], writes=[bhT])
        else:
            S.op('act', lambda e, k=k: e.activation(out=hT[:, k, c0:c0 + 128], in_=ptr[:, k, :], func=AF.Identity,
                                                    scale=Gt[:, k, j:j + 1], bias=St[:, k, j:j + 1]),
                 reads=[bptr, bGS], writes=[bhT])
    if h32 is not None:
        xn32, bxn32 = K['xn32'].next()
        p32, bp32 = K['p32'].next()
        S.op('act', lambda e: e.activation(out=xn32[:], in_=xt[:], func=AF.Copy, scale=st[:, 2:3]),
             reads=[bx, bst], writes=[bxn32])

        def tr32(e):
            for k in range(8):
                ins = e.transpose(out=p32[:, k, :], in_=xn32[:, k * 128:(k + 1) * 128], identity=C.ident32[:])
            return ins
        S.op('pe', tr32, reads=[bxn32], writes=[bp32], self_ok=True)
        for k in range(8):
            S.op('dve', lambda e, k=k: e.tensor_scalar(out=h32[:, k, :], in0=p32[:, k, :],
                                                       scalar1=Gt[:, k, j:j + 1], scalar2=St[:, k, j:j + 1],
                                                       op0=ALU.mult, op1=ALU.add),
                 reads=[bp32, bGS], writes=[bh32])


def prenorm_kit(Ph, with32=False):
    K = {
        'junk': Ph.rot(1, [128, D], BF16, 'junk'),
        'stat': Ph.rot(4, [128, 4], F32, 'stat'),
        'xn': Ph.rot(2, [128, D], BF16, 'xn'),
        'ptr': Ph.rot(2, [128, 8, 128], BF16, 'ptr', psum=True),
    }
    if with32:
        K['xn32'] = Ph.rot(2, [128, D], F32, 'xn32')
        K['p32'] = Ph.rot(1, [128, 8, 128], F32, 'p32', psum=True)
    return K


def res_src(C, l, stage, b, tile):
    if l == 0 and stage == 0:
        if tile < 2:
            return C.ctx_in[b, tile * 128:(tile + 1) * 128, :]
        return C.x_in[b, (tile - 2) * 128:(tile - 1) * 128, :]
    return C.xs[b, tile * 128:(tile + 1) * 128, :]


def phase_mod(C, l, LP):
    nc, S = C.nc, C.S
    Ph = Phase(C, f"mod{l}")
    C.G1, C.bG1 = LP.sbb([128, 8, 3], F32, 'G1')
    C.SH1 = LP.sb([128, 8, 3], F32, 'SH1')
    C.G2, C.bG2 = LP.sbb([128, 8, 3], F32, 'G2')
    C.SH2 = LP.sb([128, 8, 3], F32, 'SH2')
    C.GG1, C.bGG1 = LP.sbb([128, 3, D], F32, 'GG1')
    C.GG2, C.bGG2 = LP.sbb([128, 3, D], F32, 'GG2')
    if getattr(C, 'want_rows', False):
        C.G2row, C.bG2row = LP.sbb([128, 2, D], F32, 'G2row')
        C.S2row = LP.sb([128, 2, D], F32, 'S2row')
    scT, bscT = Ph.sbb([128, 8, 3], F32, 'scT')
    scB, bscB = Ph.sbb([128, 3, 8, 128], F32, 'scB')
    bada, bbada = Ph.sbb([128, 48, 3], F32, 'bada')
    gpre, bgpre = Ph.sbb([128, 2, 8, 3], F32, 'gpre')
    rowc, browc = Ph.sbb([128, 7, D], F32, 'rowc')
    sc1, bsc1 = Ph.sbb([128, 8, 3], F32, 'sc1')
    sc2, bsc2 = Ph.sbb([128, 8, 3], F32, 'sc2')
    wblk = Ph.rot(2, [128, 8, 1024], F32, 'wblk')
    pm = Ph.rot(2, [128, 8, 3], F32, 'pm', psum=True)
    pg = Ph.rot(2, [128, 512], F32, 'pg', psum=True)
    S.op('sp', lambda e: e.dma_start(out=scT[:], in_=C.cT[:, :, :]), writes=[bscT], dma=bscT)
    S.op('sp', lambda e: e.dma_start(out=bada[:], in_=C.badaT3[l]), writes=[bbada], dma=bbada)
    S.op('sp', lambda e: e.dma_start(out=gpre[:], in_=C.gpre3[l]), writes=[bgpre], dma=bgpre)
    S.op('sp', lambda e: e.dma_start(out=rowc[:], in_=C.rowc[l]), writes=[browc], dma=browc)
    S.op('act', lambda e: e.activation(out=scT[:], in_=scT[:], func=AF.Silu), reads=[bscT], writes=[bscT])
    for j in range(3):
        for kc in range(8):
            S.op('act', lambda e, j=j, kc=kc: e.activation(out=scB[:, j, kc, :], in_=C.ones[:], func=AF.Copy,
                                                           scale=scT[:, kc, j:j + 1]),
                 reads=[bscT], writes=[bscB])
    fm = [(0, C.SH1, C.bG1), (1, sc1, bsc1), (3, C.SH2, C.bG2), (4, sc2, bsc2)]
    for blk, dst, bdst in fm:
        wt, bw = wblk.next()
        S.op('sp', lambda e, wt=wt, blk=blk: e.dma_start(
            out=wt[:], in_=C.w_ada[l, :, blk * 1024:(blk + 1) * 1024].rearrange("(kc p) n -> p kc n", p=128)),
            writes=[bw], dma=bw)
        pmt, bpm = pm.next()

        def mm(e, wt=wt, pmt=pmt):
            for ch in range(8):
                for kc in range(8):
                    ins = e.matmul(pmt[:, ch, :], lhsT=wt[:, kc, ch * 128:(ch + 1) * 128], rhs=scT[:, kc, :],
                                   start=(kc == 0), stop=(kc == 7))
            return ins
        S.op('pe', mm, reads=[bw, bscT], writes=[bpm], self_ok=True)
        S.op('dve', lambda e, dst=dst, pmt=pmt, blk=blk: e.tensor_tensor(
            out=dst[:], in0=pmt[:], in1=bada[:, blk * 8:(blk + 1) * 8, :], op=ALU.add),
            reads=[bpm, bbada], writes=[bdst])
    S.op('dve', lambda e: e.scalar_tensor_tensor(out=C.G1[:], in0=sc1[:], scalar=1.0, in1=gpre[:, 0], op0=ALU.add,
                                                 op1=ALU.mult), reads=[bsc1, bgpre], writes=[C.bG1])
    S.op('dve', lambda e: e.scalar_tensor_tensor(out=C.G2[:], in0=sc2[:], scalar=1.0, in1=gpre[:, 1], op0=ALU.add,
                                                 op1=ALU.mult), reads=[bsc2, bgpre], writes=[C.bG2])
    for gi, blk, GG, bGG in [(0, 2, C.GG1, C.bGG1), (1, 5, C.GG2, C.bGG2)]:
        wt, bw = wblk.next()
        S.op('sp', lambda e, wt=wt, blk=blk: e.dma_start(
            out=wt[:], in_=C.w_ada[l, :, blk * 1024:(blk + 1) * 1024].rearrange("(kc p) n -> p kc n", p=128)),
            writes=[bw], dma=bw)
        for j in range(3):
            for half in range(2):
                pgt, bpg = pg.next()
                hs = slice(half * 512, (half + 1) * 512)

                def mm(e, wt=wt, pgt=pgt, j=j, hs=hs):
                    for kc in range(8):
                        ins = e.matmul(pgt[:], lhsT=scB[:, j, kc, :], rhs=wt[:, kc, hs], start=(kc == 0),
                                       stop=(kc == 7))
                    return ins
                S.op('pe', mm, reads=[bw, bscB], writes=[bpg], self_ok=True)
                S.op('dve', lambda e, GG=GG, pgt=pgt, j=j, hs=hs, gi=gi: e.tensor_tensor(
                    out=GG[:, j, hs], in0=pgt[:], in1=rowc[:, gi, hs], op=ALU.add), reads=[bpg, browc], writes=[bGG])
                S.op('pool', lambda e, GG=GG, j=j, hs=hs, gi=gi: e.tensor_tensor(
                    out=GG[:, j, hs], in0=GG[:, j, hs], in1=rowc[:, 2 + gi, hs], op=ALU.mult),
                    reads=[bGG, browc], writes=[bGG])
    if getattr(C, 'want_rows', False):
        for blk, dst, ri in [(3, C.S2row, 4), (4, C.G2row, 5)]:
            wt, bw = wblk.next()
            S.op('sp', lambda e, wt=wt, blk=blk: e.dma_start(
                out=wt[:], in_=C.w_ada[l, :, blk * 1024:(blk + 1) * 1024].rearrange("(kc p) n -> p kc n", p=128)),
                writes=[bw], dma=bw)
            for j in range(2):
                for half in range(2):
                    pgt, bpg = pg.next()
                    hs = slice(half * 512, (half + 1) * 512)

                    def mm(e, wt=wt, pgt=pgt, j=j, hs=hs):
                        for kc in range(8):
                            ins = e.matmul(pgt[:], lhsT=scB[:, j, kc, :], rhs=wt[:, kc, hs], start=(kc == 0),
                                           stop=(kc == 7))
                        return ins
                    S.op('pe', mm, reads=[bw, bscB], writes=[bpg], self_ok=True)
                    S.op('dve', lambda e, dst=dst, pgt=pgt, j=j, hs=hs, ri=ri: e.tensor_tensor(
                        out=dst[:, j, hs], in0=pgt[:], in1=rowc[:, ri, hs], op=ALU.add), reads=[bpg, browc],
                        writes=[C.bG2row])
                    if blk == 4:
                        S.op('dve', lambda e, dst=dst, j=j, hs=hs: e.scalar_tensor_tensor(
                            out=dst[:, j, hs], in0=dst[:, j, hs], scalar=1.0, in1=rowc[:, 6, hs], op0=ALU.add,
                            op1=ALU.mult), reads=[C.bG2row, browc], writes=[C.bG2row])
    Ph.close()


def phase_proj(C, l):
    nc, S = C.nc, C.S
    Ph = Phase(C, f"proj{l}")
    K = prenorm_kit(Ph)
    win, bwin = Ph.sbb([128, 8, PROJ], BF16, 'win')
    cos, bcos = Ph.sbb([128, 32, 64], F32, 'cos')
    sin, bsin = Ph.sbb([128, 32, 64], F32, 'sin')
    npc = 4
    pw = PROJ // npc
    for i in range(npc):
        S.op('pool', lambda e, i=i: e.dma_start(
            out=win[:, :, i * pw:(i + 1) * pw],
            in_=C.w_in[l, :, i * pw:(i + 1) * pw].rearrange("(kc p) n -> p kc n", p=128)), writes=[bwin], dma=bwin)
    S.op('sp', lambda e: e.dma_start(out=cos[:], in_=C.rope_cos[:, :, :]), writes=[bcos], dma=bcos)
    S.op('sp', lambda e: e.dma_start(out=sin[:], in_=C.rope_sin[:, :, :]), writes=[bsin], dma=bsin)
    S.op('dve', lambda e: e.tensor_scalar(out=win[:, :, O_Q:O_Q + 256], in0=win[:, :, O_Q:O_Q + 256], scalar1=0.125,
                                          scalar2=None, op0=ALU.mult), reads=[bwin], writes=[bwin])
    S.op('dve', lambda e: e.tensor_scalar(out=win[:, :, O_NQ:O_NQ + 512], in0=win[:, :, O_NQ:O_NQ + 512],
                                          scalar1=0.125, scalar2=None, op0=ALU.mult), reads=[bwin], writes=[bwin])
    xr = Ph.rot(3, [128, D], F32, 'x')
    hTr = Ph.rot(2, [128, 8, 256], BF16, 'hT')
    stg = Ph.rot(2, [128, 2048], BF16, 'stg')
    fstg = Ph.rot(2, [128, 8, 256], BF16, 'fstg')
    lstg = Ph.rot(2, [16, 2, 256], F32, 'lstg')
    t1r = Ph.rot(2, [128, 512], F32, 't1')
    t2r = Ph.rot(2, [128, 512], F32, 't2')
    ptok = Ph.rot(2, [128, 512], F32, 'ptok', psum=True)
    pfe = Ph.rot(2, [128, 512], F32, 'pfe', psum=True)
    tokcols = [(O_Q, O_Q + 512), (O_V, O_V + 512), (O_R, O_R + 512), (O_NV, O_NV + 512)]
    def pre(b, g):
        j = 2 if g == 0 else b
        hT, bhT = hTr.next()
        for t in range(2):
            tile = g * 2 + t
            xt, bx = xr.next()
            S.op('sp', lambda e, xt=xt, tile=tile, b=b: e.dma_start(out=xt[:], in_=res_src(C, l, 0, b, tile)),
                 writes=[bx], dma=bx)
            prenorm_tile(Ph, K, xt, bx, hT, bhT, t * 128, C.G1, C.SH1, C.bG1, j)
        return hT, bhT

    groups = [(b, g) for b in range(NB) for g in range(LT // 256)]
    cur = pre(*groups[0])
    for gi_, (b, g) in enumerate(groups):
        if True:
            hT, bhT = cur
            if gi_ + 1 < len(groups):
                cur = pre(*groups[gi_ + 1])
            for t in range(2):
                tile = g * 2 + t
                st, bst = stg.next()
                for cb in range(4):
                    pt_, bpt = ptok.next()
                    c0, c1 = tokcols[cb]

                    def mm(e, pt_=pt_, t=t, c0=c0, c1=c1, hT=hT):
                        for kc in range(8):
                            ins = e.matmul(pt_[:], lhsT=hT[:, kc, t * 128:(t + 1) * 128], rhs=win[:, kc, c0:c1],
                                           start=(kc == 0), stop=(kc == 7))
                        return ins
                    S.op('pe', mm, reads=[bhT, bwin], writes=[bpt], self_ok=True)
                    so = st[:, cb * 512:(cb + 1) * 512]
                    if cb == 0 and g > 0:
                        lt = tile - 2
                        t1, bt1 = t1r.next()
                        t2, bt2 = t2r.next()
                        cb_ = cos[:, lt, :].unsqueeze(1).to_broadcast([128, 8, 64])
                        p3 = pt_[:].rearrange("p (a d) -> p a d", a=8)
                        S.op('dve', lambda e, t1=t1, p3=p3, cb_=cb_: e.tensor_tensor(
                            out=t1[:].rearrange("p (a d) -> p a d", a=8), in0=p3, in1=cb_, op=ALU.mult),
                            reads=[bpt, bcos], writes=[bt1])
                        p5 = pt_[:].rearrange("p (a b c d) -> p a b c d", a=8, b=2, c=2)
                        s5 = sin[:, lt, :].rearrange("p (b c d) -> p b c d", b=2, c=2)
                        t25 = t2[:].rearrange("p (a b c d) -> p a b c d", a=8, b=2, c=2)
                        for hf in range(2):
                            sb_ = s5[:, :, hf, :].unsqueeze(1).to_broadcast([128, 8, 2, 16])
                            S.op('dve', lambda e, t25=t25, p5=p5, sb_=sb_, hf=hf: e.tensor_tensor(
                                out=t25[:, :, :, hf, :], in0=p5[:, :, :, 1 - hf, :], in1=sb_, op=ALU.mult),
                                reads=[bpt, bsin], writes=[bt2])
                        S.op('pool', lambda e, so=so, t1=t1, t2=t2: e.tensor_tensor(out=so, in0=t1[:], in1=t2[:],
                                                                                   op=ALU.add),
                             reads=[bt1, bt2], writes=[bst])
                    elif cb == 2:
                        S.op('act', lambda e, so=so, pt_=pt_: e.activation(out=so, in_=pt_[:], func=AF.Silu),
                             reads=[bpt], writes=[bst])
                    else:
                        S.op('act', lambda e, so=so, pt_=pt_: e.activation(out=so, in_=pt_[:], func=AF.Copy),
                             reads=[bpt], writes=[bst])
                S.op('sp', lambda e, st=st, tile=tile, b=b: e.dma_start(
                    out=C.tokmaj[b, tile * 128:(tile + 1) * 128, :], in_=st[:]), reads=[bst], dma=bst)
            ft, bft = fstg.next()
            for cc in range(8):
                pf, bpf = pfe.next()
                c0 = O_NQ + cc * 128

                def mmf(e, pf=pf, c0=c0, hT=hT):
                    for kc in range(8):
                        ins = e.matmul(pf[:, 0:256], lhsT=win[:, kc, c0:c0 + 128], rhs=hT[:, kc, :], start=(kc == 0),
                                       stop=(kc == 7))
                    return ins
                S.op('pe', mmf, reads=[bhT, bwin], writes=[bpf], self_ok=True)
                S.op('dve', lambda e, ft=ft, pf=pf, cc=cc: e.tensor_copy(out=ft[:, cc, :], in_=pf[:, 0:256]),
                     reads=[bpf], writes=[bft])
            S.op('sp', lambda e, ft=ft, g=g, b=b: e.dma_start(
                out=C.nqkT[b].rearrange("(cc p) t -> p cc t", p=128)[:, :, g * 256:(g + 1) * 256], in_=ft[:]),
                reads=[bft], dma=bft)
            lt_, blt = lstg.next()
            for d in range(2):
                pf, bpf = pfe.next()
                c0 = O_LR + 16 * d

                def mml(e, pf=pf, c0=c0, hT=hT):
                    for kc in range(8):
                        ins = e.matmul(pf[0:16, 0:256], lhsT=win[:, kc, c0:c0 + 16], rhs=hT[:, kc, :],
                                       start=(kc == 0), stop=(kc == 7))
                    return ins
                S.op('pe', mml, reads=[bhT, bwin], writes=[bpf], self_ok=True)
                S.op('dve', lambda e, lt_=lt_, pf=pf, d=d: e.tensor_copy(out=lt_[:, d, :], in_=pf[0:16, 0:256]),
                     reads=[bpf], writes=[blt])
            S.op('sp', lambda e, lt_=lt_, g=g, b=b: e.dma_start(
                out=C.lrT[b].rearrange("d r t -> r d t")[:, :, g * 256:(g + 1) * 256], in_=lt_[:]),
                reads=[blt], dma=blt)
    Ph.close()


def phase_gla(C, l, last):
    S = C.S
    Ph = Phase(C, f"gla{l}")
    tri, btri = Ph.sbb([128, 4, 128], F32, 'tri')
    wg, bwg = Ph.sbb([16, 2, 256], F32, 'wg')
    bg, bbg = Ph.sbb([1, 2, 256], F32, 'bg')
    gn, bgn = Ph.sbb([128, 512], F32, 'gn')
    S.op('sp', lambda e: e.dma_start(out=tri[:], in_=C.tri_in[:, :, :]), writes=[btri], dma=btri)
    S.op('sp', lambda e: e.dma_start(out=wg[:], in_=C.wgate[l].rearrange("d r n -> r d n")), writes=[bwg], dma=bwg)
    S.op('sp', lambda e: e.dma_start(out=bg[:], in_=C.bgate[l].rearrange("d o n -> o d n")), writes=[bbg], dma=bbg)
    S.op('sp', lambda e: e.dma_start(out=gn[:], in_=C.gnormB[l]), writes=[bgn], dma=bgn)
    lrr = Ph.rot(1, [16, 2, LT], F32, 'lr')
    ost = Ph.sb([128, NT, 512], F32, 'ost')
    bost = [S.buf(f"ost{i}") for i in range(NT)]
    Sst = [Ph.sbb([128, 2, 128], F32, 'Sst') for _ in range(2)]
    Sbf = [Ph.sbb([128, 2, 128], BF16, 'Sbf') for _ in range(2)]
    qkvr = Ph.rot(4, [128, 1024], BF16, 'qkv')
    rgr = Ph.rot(3, [128, 512], BF16, 'rg')
    e1r = Ph.rot(2, [128, 256], F32, 'e1')
    spr = Ph.rot(2, [128, 256], F32, 'sp')
    ebr = Ph.rot(2, [128, 2, 128], F32, 'eb')
    enbr = Ph.rot(2, [128, 2, 128], F32, 'enb')
    eEr = Ph.rot(2, [128, 256], F32, 'eE')
    qdr = Ph.rot(2, [128, 4, 128], BF16, 'qd')
    kdr = Ph.rot(2, [128, 4, 128], BF16, 'kd')
    for rr in (qdr, kdr):
        for t_, b_ in zip(rr.t, rr.b):
            S.op('pool', lambda e, t_=t_: e.memset(t_[:], 0.0), writes=[b_])
    ker = Ph.rot(2, [128, 256], BF16, 'kend')
    Amr = Ph.rot(2, [128, 4, 128], BF16, 'Am')
    osr = Ph.rot(2, [128, 512], F32, 'osum')
    sqr = Ph.rot(2, [128, 512], F32, 'sq')
    ogr = Ph.rot(2, [128, 512], BF16, 'og')
    str_ = Ph.rot(4, [128, 12], F32, 'gst')
    plr = Ph.rot(1, [128, 512], F32, 'pl', psum=True)
    pber = Ph.rot(1, [128, 512], F32, 'pbe', psum=True)
    pTr = Ph.rot(1, [128, 8, 128], BF16, 'pT', psum=True)
    pAr = Ph.rot(2, [128, 4, 128], F32, 'pA', psum=True)
    por = Ph.rot(2, [128, 4, 128], F32, 'po', psum=True)
    pdsr = Ph.rot(1, [128, 2, 256], F32, 'pds', psum=True)

    import os
    STG = int(os.environ.get('GLA_STG', '99'))
    NTL = int(os.environ.get('GLA_NT', str(NT)))

    def block(b, d, tile, first, lr):
        lrt, blr = lr
        rows_of = lambda hp: slice(hp * 64, hp * 64 + 64)
        r0 = tile * 128
        qkv, bqkv = qkvr.next()
        S.op('sp', lambda e: e.dma_start(out=qkv[:], in_=C.tokmaj[b, r0:r0 + 128, 0:1024]), writes=[bqkv], dma=bqkv)
        if (not first) and not (last and tile < 2):
            rg, brg = rgr.next()
            S.op('sp', lambda e: e.dma_start(out=rg[:], in_=C.tokmaj[b, r0:r0 + 128, 1024:1536]), writes=[brg],
                 dma=brg)
        pl, bpl = plr.next()

        def mml(e):
            e.matmul(pl[:, 0:256], lhsT=lrt[:, d, r0:r0 + 128], rhs=wg[:, d, :], start=True, stop=False)
            return e.matmul(pl[:, 0:256], lhsT=C.ones[0:1, :], rhs=bg[0:1, d, :], start=False, stop=True)
        S.op('pe', mml, reads=[blr, bwg, bbg], writes=[bpl], self_ok=True)
        yield
        e1, be1 = e1r.next()
        sp_, bsp = spr.next()
        S.op('act', lambda e: e.activation(out=e1[:], in_=pl[:, 0:256], func=AF.Exp, scale=-1.0), reads=[bpl],
             writes=[be1])
        S.op('act', lambda e: e.activation(out=sp_[:], in_=e1[:], func=AF.Ln, bias=1.0), reads=[be1], writes=[bsp])
        yield
        Rm = tri[:, 0 if d == 0 else 1, :]
        Um = tri[:, 3 if d == 0 else 2, :]
        pbe, bpbe = pber.next()

        def mmb(e):
            for g in range(2):
                e.matmul(pbe[:, g * 128:(g + 1) * 128], lhsT=sp_[:, g * 128:(g + 1) * 128], rhs=Rm, start=True,
                         stop=True)
            return e.matmul(pbe[:, 256:512], lhsT=Um, rhs=sp_[:], start=True, stop=True)
        S.op('pe', mmb, reads=[bsp, btri], writes=[bpbe], self_ok=True)
        yield
        eb, beb = ebr.next()
        enb, benb = enbr.next()
        eE, beE = eEr.next()
        pb3 = pbe[:, 0:256].rearrange("p (g t) -> p g t", g=2)
        S.op('act', lambda e: e.activation(out=eb[:], in_=pb3, func=AF.Exp, scale=-1.0 / 16), reads=[bpbe],
             writes=[beb])
        S.op('act', lambda e: e.activation(out=enb[:], in_=pb3, func=AF.Exp, scale=1.0 / 16), reads=[bpbe],
             writes=[benb])
        S.op('act', lambda e: e.activation(out=eE[:], in_=pbe[:, 256:512], func=AF.Exp, scale=-1.0 / 16),
             reads=[bpbe], writes=[beE])
        yield
        pT, bpT = pTr.next()

        def tr(e):
            for i in range(4):
                ins = e.transpose(out=pT[:, i, :], in_=qkv[:, i * 128:(i + 1) * 128], identity=C.ident[:])
            return ins
        S.op('pe', tr, reads=[bqkv], writes=[bpT], self_ok=True)
        yield
        qd, bqd = qdr.next()
        kd, bkd = kdr.next()
        kend, bke = ker.next()
        for h in range(4):
            g, rs = h // 2, rows_of(h % 2)
            S.op('dve', lambda e, h=h, g=g, rs=rs: e.tensor_tensor(out=qd[rs, h, :], in0=pT[rs, g, :], in1=eb[rs, g, :],
                                                                   op=ALU.mult), reads=[bpT, beb], writes=[bqd])
            S.op('dve', lambda e, h=h, g=g, rs=rs: e.tensor_tensor(out=kd[rs, h, :], in0=pT[rs, 2 + g, :],
                                                                   in1=enb[rs, g, :], op=ALU.mult),
                 reads=[bpT, benb], writes=[bkd])
        S.op('pool', lambda e: e.tensor_tensor(out=kend[:], in0=qkv[:, 256:512], in1=eE[:], op=ALU.mult),
             reads=[bqkv, beE], writes=[bke])
        yield
        pA, bpA = pAr.next()

        def mmA(e):
            for h in range(4):
                ins = e.matmul(pA[:, h, :], lhsT=kd[:, h, :], rhs=qd[:, h, :], start=True, stop=True)
            return ins
        S.op('pe', mmA, reads=[bkd, bqd], writes=[bpA], self_ok=True)
        yield
        Am, bAm = Amr.next()
        mask = tri[:, 0 if d == 0 else 1, :].unsqueeze(1).to_broadcast([128, 4, 128])
        S.op('dve', lambda e: e.tensor_tensor(out=Am[:], in0=pA[:], in1=mask, op=ALU.mult), reads=[bpA, btri],
             writes=[bAm])
        yield
        po, bpo = por.next()
        sbf, bsbf = Sbf[d]

        def mmo(e):
            for h in range(4):
                g, rs = h // 2, rows_of(h % 2)
                e.matmul(po[:, h, :], lhsT=Am[:, h, :], rhs=qkv[:, 512 + h * 128:512 + (h + 1) * 128], start=True,
                         stop=False)
                ins = e.matmul(po[:, h, :], lhsT=qd[:, h, :], rhs=sbf[:, g, :], start=False, stop=True)
            return ins
        S.op('pe', mmo, reads=[bAm, bqkv, bqd, bsbf], writes=[bpo], self_ok=True)
        need_out = not (last and tile < 2)
        yield
        if need_out:
            if first:
                S.op('act', lambda e: e.activation(out=ost[:, tile, :], in_=po[:].rearrange("p h v -> p (h v)"),
                                                   func=AF.Copy), reads=[bpo], writes=[bost[tile]])
            else:
                osum, bos = osr.next()
                sq, bsq = sqr.next()
                og, bog = ogr.next()
                st, bst = str_.next()
                S.op('dve', lambda e: e.tensor_tensor(out=osum[:], in0=po[:].rearrange("p h v -> p (h v)"),
                                                      in1=ost[:, tile, :], op=ALU.add), reads=[bpo, bost[tile]],
                     writes=[bos])
                S.op('pool', lambda e: e.tensor_tensor(out=sq[:], in0=osum[:], in1=osum[:], op=ALU.mult), reads=[bos],
                     writes=[bsq])
                S.op('dve', lambda e: e.tensor_reduce(out=st[:, 0:4], in_=sq[:].rearrange("p (h v) -> p h v", h=4),
                                                      axis=AX.X, op=ALU.add), reads=[bsq], writes=[bst])
                S.op('dve', lambda e: e.tensor_scalar(out=st[:, 4:8], in0=st[:, 0:4], scalar1=1.0 / 128, scalar2=EPS,
                                                      op0=ALU.mult, op1=ALU.add), reads=[bst], writes=[bst])
                S.op('pool', lambda e: e.tensor_tensor(out=st[:, 8:12], in0=st[:, 4:8], in1=C.neghalf[:, 0:4],
                                                       op=ALU.pow), reads=[bst], writes=[bst])
                S.op('dve', lambda e: e.tensor_tensor(
                    out=sq[:].rearrange("p (h v) -> p h v", h=4), in0=osum[:].rearrange("p (h v) -> p h v", h=4),
                    in1=st[:, 8:12].unsqueeze(2).to_broadcast([128, 4, 128]), op=ALU.mult), reads=[bos, bst],
                    writes=[bsq])
                S.op('pool', lambda e: e.tensor_tensor(out=sq[:], in0=sq[:], in1=gn[:], op=ALU.mult), reads=[bsq, bgn],
                     writes=[bsq])
                S.op('pool', lambda e: e.tensor_tensor(out=og[:], in0=sq[:], in1=rg[:], op=ALU.mult),
                     reads=[bsq, brg], writes=[bog])
                S.op('pool', lambda e: e.dma_start(out=C.cat[b, r0:r0 + 128, 0:512], in_=og[:]), reads=[bog], dma=bog)
        yield
        pds, bpds = pdsr.next()

        def mmds(e):
            for g in range(2):
                ins = e.matmul(pds[:, g, :], lhsT=kend[:, g * 128:(g + 1) * 128],
                               rhs=qkv[:, 512 + g * 256:512 + (g + 1) * 256], start=True, stop=True)
            return ins
        S.op('pe', mmds, reads=[bke, bqkv], writes=[bpds], self_ok=True)
        yield
        sst, bsst = Sst[d]
        dc = 127 if d == 0 else 0
        for g in range(2):
            for hp in range(2):
                rs = rows_of(hp)
                S.op('dve', lambda e, g=g, hp=hp, rs=rs: e.scalar_tensor_tensor(
                    out=sst[rs, g, :], in0=sst[rs, g, :], scalar=eb[rs, g, dc:dc + 1],
                    in1=pds[rs, g, hp * 128:(hp + 1) * 128], op0=ALU.mult, op1=ALU.add),
                    reads=[bsst, beb, bpds], writes=[bsst])
        S.op('act', lambda e: e.activation(out=sbf[:], in_=sst[:], func=AF.Copy), reads=[bsst], writes=[bsbf])

    for b in range(NB):
        lr = lrr.next()
        S.op('sp', lambda e, lr=lr, b=b: e.dma_start(out=lr[0][:], in_=C.lrT[b].rearrange("d r t -> r d t")),
             writes=[lr[1]], dma=lr[1])
        for d in range(2):
            S.op('pool', lambda e, d=d: e.memset(Sst[d][0][:], 0.0), writes=[Sst[d][1]])
            S.op('pool', lambda e, d=d: e.memset(Sbf[d][0][:], 0.0), writes=[Sbf[d][1]])
        orders = [list(range(NT)), [1, 0] + list(range(NT - 1, 1, -1))]
        done = set()
        older = None
        for i in range(NTL):
            for d in range(2):
                tile = orders[d][i]
                newer = block(b, d, tile, tile not in done, lr)
                done.add(tile)
                if not GLA_PIPE:
                    for _ in newer:
                        pass
                    continue
                ne = 0
                while ne < 6 or older is not None:
                    if ne < 6:
                        next(newer)
                        ne += 1
                    if older is not None:
                        try:
                            next(older)
                        except StopIteration:
                            older = None
                older = newer
        if older is not None:
            for _ in older:
                pass
    Ph.close()


NA_SHIFT = 0.0


def phase_na(C, l, last):
    S = C.S
    Ph = Phase(C, f"na{l}")
    kTr = Ph.rot(1, [128, 4, LT], BF16, 'kT')
    qTr = Ph.rot(2, [128, LT], BF16, 'qT')
    for t_, b_ in zip(qTr.t, qTr.b):
        S.op('pool', lambda e, t_=t_: e.memset(t_[:], 0.0), writes=[b_])
    Vr = Ph.rot(1, [128, NT, 8, 65], BF16, 'V')
    for t_, b_ in zip(Vr.t, Vr.b):
        S.op('pool', lambda e, t_=t_: e.memset(t_[:], 1.0), writes=[b_])
    vstr = Ph.rot(2, [128, 8, 512], BF16, 'vst')
    onar = Ph.rot(1, [128, NT, 512], BF16, 'ona')
    biasr = Ph.rot(1, [128, 5, 5, 128], F32, 'bias')
    ssr = Ph.rot(4, [128, 5, 128], F32, 's')
    pr = Ph.rot(4, [128, 7, 128], BF16, 'p')
    str_ = Ph.rot(8, [128, 4], F32, 'nst')
    negc, bnegc = Ph.sbb([128, 1], F32, 'negc')
    S.op('pool', lambda e: e.memset(negc[:], -NA_SHIFT), writes=[bnegc])
    psr = Ph.rot(3, [128, 8, 128], F32, 'ps', psum=True)
    po_t = Ph.ps([128, 512], F32, 'po')
    po_b = [S.buf(f"po{i}") for i in range(7)]
    po_i = [0]

    def unit(b, h, qt, kT, bkT, qT, bqT, V, bV, ona, bona, bias, bbias):
        g = h // 2
        q0 = qt * 128
        if qt >= 2:
            j = qt - 2
            ts = min(max(j - 2, 0), 27)
            pi = {0: 0, 1: 1, 30: 3, 31: 4}.get(j, 2)
            kcols = [256 + 128 * (ts + k) for k in range(5)] + [0, 128]
            vt = [2 + ts + k for k in range(5)] + [0, 1]
            nl = 5
        else:
            kcols = [0, 128]
            vt = [0, 1]
            nl = 0
        nblk = len(kcols)
        ps_, bps = psr.next()

        def mm(e):
            for kb in range(nblk):
                ins = e.matmul(ps_[:, kb, :], lhsT=kT[:, g, kcols[kb]:kcols[kb] + 128], rhs=qT[:, q0:q0 + 128],
                               start=True, stop=True)
            return ins
        S.op('pe', mm, reads=[bqT, bkT], writes=[bps], self_ok=True)
        p, bp = pr.next()
        if nl:
            s_, bs = ssr.next()
            S.op('dve', lambda e: e.tensor_tensor(out=s_[:], in0=ps_[:, 0:5, :], in1=bias[:, pi, :, :], op=ALU.add),
                 reads=[bps, bbias], writes=[bs])
            S.op('act', lambda e: e.activation(out=p[:, 0:5, :], in_=s_[:], func=AF.Exp, bias=negc[:, 0:1], scale=1.0),
                 reads=[bs, bnegc], writes=[bp])
        S.op('act', lambda e: e.activation(out=p[:, nl:nblk, :], in_=ps_[:, nl:nblk, :], func=AF.Exp,
                                           bias=negc[:, 0:1], scale=1.0), reads=[bps, bnegc], writes=[bp])
        yield
        slot = po_i[0] % 7
        po_i[0] += 1
        po = po_t[:, slot * 65:(slot + 1) * 65]
        bpo = po_b[slot]

        def mmpv(e):
            for kb in range(nblk):
                ins = e.matmul(po, lhsT=p[:, kb, :], rhs=V[:, vt[kb], h, :], start=(kb == 0), stop=(kb == nblk - 1))
            return ins
        S.op('pe', mmpv, reads=[bp, bV], writes=[bpo], self_ok=True)
        st, bst = str_.next()
        S.op('dve', lambda e: e.reciprocal(out=st[:, 0:1], in_=po[:, 64:65]), reads=[bpo], writes=[bst])
        S.op('act', lambda e: e.activation(out=ona[:, qt, h * 64:(h + 1) * 64], in_=po[:, 0:64], func=AF.Copy,
                                           scale=st[:, 0:1]), reads=[bpo, bst], writes=[bona])

    inflight = []
    for b in range(NB):
        kT, bkT = kTr.next()
        V, bV = Vr.next()
        ona, bona = onar.next()
        for g in range(4):
            S.op('sp', lambda e, kT=kT, b=b, g=g: e.dma_start(
                out=kT[:, g, :], in_=C.nqkT[b, 512 + g * 128:512 + (g + 1) * 128, :]), writes=[bkT], dma=bkT)
        for t0 in range(0, NT, 8):
            t1 = min(NT, t0 + 8)
            vs, bvs = vstr.next()
            S.op('sp', lambda e, vs=vs, b=b, t0=t0, t1=t1: e.dma_start(
                out=vs[:, 0:t1 - t0, :],
                in_=C.tokmaj[b, t0 * 128:t1 * 128, 1536:2048].rearrange("(t p) c -> p t c", p=128)),
                writes=[bvs], dma=bvs)
            S.op('pool', lambda e, vs=vs, V=V, t0=t0, t1=t1: e.tensor_copy(
                out=V[:, t0:t1, :, 0:64], in_=vs[:, 0:t1 - t0, :].rearrange("p t (h d) -> p t h d", h=8)),
                reads=[bvs], writes=[bV])
        if b == NB - 1 and getattr(C, 'moe_cast_pending', False):
            C.moe_cast_pending = False
            for src, dst in zip([C.moe_wg, C.moe_wu, C.moe_wd], C.moe_bf):
                for ex in range(NE):
                    S.op('pool', lambda e, src=src, dst=dst, ex=ex: e.dma_start(out=dst[ex], in_=src[ex]),
                         writes=[C.bmoe], dma=C.bmoe, track=False)
        for h in range(8):
            bias, bbias = biasr.next()
            S.op('sp', lambda e, bias=bias, h=h: e.dma_start(out=bias[:], in_=C.natb[l, h]), writes=[bbias], dma=bbias)
            qT, bqT = qTr.next()
            hr = slice((h % 2) * 64, (h % 2) * 64 + 64)
            S.op('sp', lambda e, qT=qT, b=b, h=h, hr=hr: e.dma_start(
                out=qT[hr, :], in_=C.nqkT[b, h * 64:(h + 1) * 64, :]), writes=[bqT], dma=bqT)
            qts = list(range(2, NT)) + ([] if last else [0, 1])
            for qt in qts:
                gen = unit(b, h, qt, kT, bkT, qT, bqT, V, bV, ona, bona, bias, bbias)
                next(gen)
                inflight.append(gen)
                if len(inflight) > NA_PIPE:
                    for _ in inflight.pop(0):
                        pass
        while inflight:
            for _ in inflight.pop(0):
                pass
        for t0 in range(2 if last else 0, NT, 8):
            t1 = min(NT, t0 + 8)
            S.op('sp', lambda e, ona=ona, b=b, t0=t0, t1=t1: e.dma_start(
                out=C.cat[b, t0 * 128:t1 * 128, 512:1024].rearrange("(t p) c -> p t c", p=128), in_=ona[:, t0:t1, :]),
                reads=[bona], dma=bona)
    Ph.close()


def phase_outproj(C, l, last):
    S = C.S
    Ph = Phase(C, f"op{l}")
    wo, bwo = Ph.sbb([128, 8, D], BF16, 'wo')
    S.op('pool', lambda e: e.dma_start(out=wo[:], in_=C.w_out[l].rearrange("(kc p) n -> p kc n", p=128)),
         writes=[bwo], dma=bwo)
    ctr = Ph.rot(2, [128, D], BF16, 'ct')
    xr = Ph.rot(2, [128, D], F32, 'x')
    cTsr = Ph.rot(2, [128, 8, 128], BF16, 'cTs')
    ttr = Ph.rot(2, [128, D], F32, 'tt')
    junkr = Ph.rot(1, [128, D], BF16, 'junk')
    str_ = Ph.rot(4, [128, 4], F32, 'ost')
    pTr = Ph.rot(2, [128, 8, 128], BF16, 'pT', psum=True)
    pyr = Ph.rot(2, [128, D], F32, 'py', psum=True)
    for b in range(NB):
        for tile in (range(2, NT) if last else range(NT)):
            j = 2 if tile < 2 else b
            r0 = tile * 128
            ct, bct = ctr.next()
            xt, bx = xr.next()
            S.op('sp', lambda e, ct=ct, b=b, r0=r0: e.dma_start(out=ct[:], in_=C.cat[b, r0:r0 + 128, :]), writes=[bct],
                 dma=bct)
            S.op('sp', lambda e, xt=xt, b=b, tile=tile: e.dma_start(out=xt[:], in_=res_src(C, l, 0, b, tile)),
                 writes=[bx], dma=bx)
            pT, bpT = pTr.next()

            def tr(e, pT=pT, ct=ct):
                for k in range(8):
                    ins = e.transpose(out=pT[:, k, :], in_=ct[:, k * 128:(k + 1) * 128], identity=C.ident[:])
                return ins
            S.op('pe', tr, reads=[bct], writes=[bpT], self_ok=True)
            cTs, bcTs = cTsr.next()
            S.op('dve', lambda e, cTs=cTs, pT=pT: e.tensor_copy(out=cTs[:, 0:4, :], in_=pT[:, 0:4, :]), reads=[bpT],
                 writes=[bcTs])
            S.op('act', lambda e, cTs=cTs, pT=pT: e.activation(out=cTs[:, 4:8, :], in_=pT[:, 4:8, :], func=AF.Copy),
                 reads=[bpT], writes=[bcTs])
            py, bpy = pyr.next()

            def mm(e, py=py, cTs=cTs):
                for half in range(2):
                    for kc in range(8):
                        ins = e.matmul(py[:, half * 512:(half + 1) * 512], lhsT=cTs[:, kc, :],
                                       rhs=wo[:, kc, half * 512:(half + 1) * 512], start=(kc == 0), stop=(kc == 7))
                return ins
            S.op('pe', mm, reads=[bcTs, bwo], writes=[bpy], self_ok=True)
            post_norm_res(Ph, py[:], bpy, xt, bx, C.GG1, C.bGG1, j, junkr, str_, ttr,
                          C.xs[b, r0:r0 + 128, :])
    Ph.close()


def post_norm_res(Ph, y, by, xt, bx, GG, bGG, j, junkr, str_, ttr, dst):
    S = Ph.S
    C = Ph.C
    junk, bj = junkr.next()
    st, bst = str_.next()
    tt, btt = ttr.next()
    S.op('act', lambda e: e.activation(out=junk[:], in_=y, func=AF.Square, accum_out=st[:, 0:1]), reads=[by],
         writes=[bj, bst])
    S.op('dve', lambda e: e.tensor_scalar(out=st[:, 1:2], in0=st[:, 0:1], scalar1=1.0 / D, scalar2=EPS, op0=ALU.mult,
                                          op1=ALU.add), reads=[bst], writes=[bst])
    S.op('pool', lambda e: e.tensor_tensor(out=st[:, 2:3], in0=st[:, 1:2], in1=C.neghalf[:, 0:1], op=ALU.pow),
         reads=[bst], writes=[bst])
    S.op('dve', lambda e: e.scalar_tensor_tensor(out=tt[:], in0=y, scalar=st[:, 2:3], in1=GG[:, j, :], op0=ALU.mult,
                                                 op1=ALU.mult), reads=[by, bst, bGG], writes=[btt])
    S.op('pool', lambda e: e.tensor_tensor(out=tt[:], in0=tt[:], in1=xt[:], op=ALU.add), reads=[btt, bx],
         writes=[btt])
    S.op('pool', lambda e: e.dma_start(out=dst, in_=tt[:]), reads=[btt], dma=btt)


def phase_ffn_pre(C, l, moe, tiles):
    S = C.S
    Ph = Phase(C, f"fpre{l}")
    K = prenorm_kit(Ph, with32=moe)
    xr = Ph.rot(3, [128, D], F32, 'x')
    hTr = Ph.rot(2, [128, 8, 512], BF16, 'hT')
    if moe:
        wr, bwr = Ph.sbb([128, 8, NE], F32, 'wr')
        S.op('sp', lambda e: e.dma_start(out=wr[:], in_=C.moe_wr.rearrange("(kc p) n -> p kc n", p=128)),
             writes=[bwr], dma=bwr)
        h32r = Ph.rot(2, [128, 8, 128], F32, 'h32')
        plg = Ph.rot(1, [128, 512], F32, 'plg', psum=True)
        cmbr = Ph.rot(2, [128, 4, NE], F32, 'cmb')
        rsr = Ph.rot(4, [128, 48], F32, 'rst')
    for gi in range(len(tiles) // 4):
        hT, bhT = hTr.next()
        if moe:
            cmb, bcmb = cmbr.next()
        for t in range(4):
            b, tile = tiles[gi * 4 + t]
            j = 2 if tile < 2 else b
            xt, bx = xr.next()
            S.op('sp', lambda e, xt=xt, b=b, tile=tile: e.dma_start(out=xt[:], in_=res_src(C, l, 1, b, tile)),
                 writes=[bx], dma=bx)
            if not moe:
                prenorm_tile(Ph, K, xt, bx, hT, bhT, t * 128, C.G2, C.SH2, C.bG2, j)
                continue
            h32, bh32 = h32r.next()
            prenorm_tile(Ph, K, xt, bx, hT, bhT, t * 128, C.G2, C.SH2, C.bG2, j, h32, bh32)
            pl, bpl = plg.next()

            def mm(e, pl=pl, h32=h32):
                for kc in range(8):
                    ins = e.matmul(pl[:, 0:NE], lhsT=h32[:, kc, :], rhs=wr[:, kc, :], start=(kc == 0), stop=(kc == 7))
                return ins
            S.op('pe', mm, reads=[bh32, bwr], writes=[bpl], self_ok=True)
            st, bst = rsr.next()
            ops = [
                lambda e, st=st, pl=pl: e.tensor_copy(out=st[:, 0:8], in_=pl[:, 0:NE]),
                lambda e, st=st: e.reduce_max(out=st[:, 8:9], in_=st[:, 0:8], axis=AX.X),
                lambda e, st=st: e.tensor_scalar(out=st[:, 16:24], in0=st[:, 0:8], scalar1=st[:, 8:9], scalar2=-1e30,
                                                 op0=ALU.is_equal, op1=ALU.mult),
                lambda e, st=st: e.tensor_tensor(out=st[:, 16:24], in0=st[:, 16:24], in1=st[:, 0:8], op=ALU.add),
                lambda e, st=st: e.reduce_max(out=st[:, 9:10], in_=st[:, 16:24], axis=AX.X),
                lambda e, st=st: e.tensor_scalar(out=st[:, 24:32], in0=st[:, 0:8], scalar1=st[:, 9:10], scalar2=None,
                                                 op0=ALU.is_ge),
                lambda e, st=st: e.tensor_scalar(out=st[:, 10:11], in0=st[:, 8:9], scalar1=-1.0, scalar2=None,
                                                 op0=ALU.mult),
            ]
            for i, f_ in enumerate(ops):
                S.op('dve', f_, reads=[bst] + ([bpl] if i == 0 else []), writes=[bst])
            S.op('act', lambda e, st=st: e.activation(out=st[:, 32:40], in_=st[:, 0:8], func=AF.Exp, bias=st[:, 10:11],
                                                      scale=1.0), reads=[bst], writes=[bst])
            ops2 = [
                lambda e, st=st: e.tensor_tensor(out=st[:, 32:40], in0=st[:, 32:40], in1=st[:, 24:32], op=ALU.mult),
                lambda e, st=st: e.reduce_sum(out=st[:, 11:12], in_=st[:, 32:40], axis=AX.X),
                lambda e, st=st: e.reciprocal(out=st[:, 12:13], in_=st[:, 11:12]),
            ]
            for f_ in ops2:
                S.op('dve', f_, reads=[bst], writes=[bst])
            S.op('dve', lambda e, st=st, cmb=cmb, t=t: e.tensor_scalar(out=cmb[:, t, :], in0=st[:, 32:40],
                                                                       scalar1=st[:, 12:13], scalar2=None,
                                                                       op0=ALU.mult), reads=[bst], writes=[bcmb])
        S.op('sp', lambda e, hT=hT, gi=gi: e.dma_start(out=C.h2T[gi], in_=hT[:]), reads=[bhT], dma=bhT)
        if moe:
            S.op('sp', lambda e, cmb=cmb, gi=gi: e.dma_start(out=C.comb[gi], in_=cmb[:]), reads=[bcmb], dma=bcmb)
    Ph.close()


def phase_ffn(C, l, moe, tiles):
    S = C.S
    Ph = Phase(C, f"ffn{l}")
    E = NE if moe else 1
    F = F_MOE if moe else F_FFN
    NFC = F // 128
    NFB = F // 256
    wsrc = C.moe_bf if moe else C.ffn_bf
    bwg_ = C.bmoe if moe else C.bffn
    hTr = Ph.rot(2, [128, 8, 512], BF16, 'hT')
    hid, bhid = Ph.sbb([128, NFC, 512], BF16, 'hid')
    wd, bwd = Ph.sbb([128, NFC, D], BF16, 'wd')
    wgr = Ph.rot(3, [128, 8, 256], BF16, 'wg')
    wur = Ph.rot(3, [128, 8, 256], BF16, 'wu')
    yacc, byacc = Ph.sbb([128, 4, D], F32, 'yacc')
    sgr = Ph.rot(2, [128, 512], F32, 'sg')
    xr = Ph.rot(2, [128, D], F32, 'x')
    ttr = Ph.rot(2, [128, D], F32, 'tt')
    junkr = Ph.rot(1, [128, D], BF16, 'junk')
    str_ = Ph.rot(4, [128, 4], F32, 'fst')
    cmbr = Ph.rot(2, [128, 4, NE], F32, 'cmb')
    pgr = Ph.rot(2, [128, 512], F32, 'pg', psum=True)
    pur = Ph.rot(2, [128, 512], F32, 'pu', psum=True)
    pyr = Ph.rot(2, [128, 512], F32, 'py', psum=True)
    for gi in range(len(tiles) // 4):
        hT, bhT = hTr.next()
        S.op('sp', lambda e, hT=hT, gi=gi: e.dma_start(out=hT[:], in_=C.h2T[gi]), writes=[bhT], dma=bhT)
        if moe:
            cmb, bcmb = cmbr.next()
            S.op('sp', lambda e, cmb=cmb, gi=gi: e.dma_start(out=cmb[:], in_=C.comb[gi]), writes=[bcmb], dma=bcmb)
        for ex in range(E):
            hh = NFC // 2
            for (a0, a1) in [(0, hh), (hh, NFC)]:
                S.op('pool', lambda e, ex=ex, a0=a0, a1=a1: e.dma_start(
                    out=wd[:, a0:a1, :],
                    in_=wsrc[2][ex, a0 * 128:a1 * 128, :].rearrange("(fc p) n -> p fc n", p=128)),
                    reads=[bwg_], writes=[bwd], dma=bwd)
            for fb in range(NFB):
                wg, bwg = wgr.next()
                wu, bwu = wur.next()
                S.op('sp', lambda e, wg=wg, ex=ex, fb=fb: e.dma_start(
                    out=wg[:], in_=wsrc[0][ex, :, fb * 256:(fb + 1) * 256].rearrange("(kc p) f -> p kc f", p=128)),
                    reads=[bwg_], writes=[bwg], dma=bwg)
                S.op('sp', lambda e, wu=wu, ex=ex, fb=fb: e.dma_start(
                    out=wu[:], in_=wsrc[1][ex, :, fb * 256:(fb + 1) * 256].rearrange("(kc p) f -> p kc f", p=128)),
                    reads=[bwg_], writes=[bwu], dma=bwu)
                for fi in range(2):
                    fc = fb * 2 + fi
                    pg_, bpg = pgr.next()
                    pu_, bpu = pur.next()

                    def mmg(e, pg_=pg_, wg=wg, fi=fi, hT=hT):
                        for kc in range(8):
                            ins = e.matmul(pg_[:], lhsT=wg[:, kc, fi * 128:(fi + 1) * 128], rhs=hT[:, kc, :],
                                           start=(kc == 0), stop=(kc == 7))
                        return ins

                    def mmu(e, pu_=pu_, wu=wu, fi=fi, hT=hT):
                        for kc in range(8):
                            ins = e.matmul(pu_[:], lhsT=wu[:, kc, fi * 128:(fi + 1) * 128], rhs=hT[:, kc, :],
                                           start=(kc == 0), stop=(kc == 7))
                        return ins
                    S.op('pe', mmg, reads=[bwg, bhT], writes=[bpg], self_ok=True)
                    S.op('pe', mmu, reads=[bwu, bhT], writes=[bpu], self_ok=True)
                    sg, bsg = sgr.next()
                    S.op('act', lambda e, sg=sg, pg_=pg_: e.activation(out=sg[:], in_=pg_[:], func=AF.Silu),
                         reads=[bpg], writes=[bsg])
                    S.op('dve', lambda e, sg=sg, pu_=pu_, fc=fc: e.tensor_tensor(out=hid[:, fc, :], in0=pu_[:],
                                                                                 in1=sg[:], op=ALU.mult),
                         reads=[bsg, bpu], writes=[bhid])
            for t in range(4):
                for half in range(2):
                    py_, bpy = pyr.next()
                    hs = slice(half * 512, (half + 1) * 512)

                    def mmd(e, py_=py_, t=t, hs=hs):
                        for fc in range(NFC):
                            ins = e.matmul(py_[:], lhsT=hid[:, fc, t * 128:(t + 1) * 128], rhs=wd[:, fc, hs],
                                           start=(fc == 0), stop=(fc == NFC - 1))
                        return ins
                    S.op('pe', mmd, reads=[bhid, bwd], writes=[bpy], self_ok=True)
                    if not moe:
                        S.op('act', lambda e, py_=py_, t=t, hs=hs: e.activation(out=yacc[:, t, hs], in_=py_[:],
                                                                                func=AF.Copy),
                             reads=[bpy], writes=[byacc])
                    elif ex == 0:
                        S.op('act', lambda e, py_=py_, t=t, hs=hs, cmb=cmb: e.activation(
                            out=yacc[:, t, hs], in_=py_[:], func=AF.Copy, scale=cmb[:, t, 0:1]),
                            reads=[bpy, bcmb], writes=[byacc])
                    else:
                        S.op('dve', lambda e, py_=py_, t=t, hs=hs, cmb=cmb, ex=ex: e.scalar_tensor_tensor(
                            out=yacc[:, t, hs], in0=py_[:], scalar=cmb[:, t, ex:ex + 1], in1=yacc[:, t, hs],
                            op0=ALU.mult, op1=ALU.add), reads=[bpy, bcmb, byacc], writes=[byacc])
        for t in range(4):
            b, tile = tiles[gi * 4 + t]
            j = 2 if tile < 2 else b
            xt, bx = xr.next()
            S.op('sp', lambda e, xt=xt, b=b, tile=tile: e.dma_start(out=xt[:], in_=res_src(C, l, 1, b, tile)),
                 writes=[bx], dma=bx)
            if moe:
                dst = C.y_out[b, (tile - 2) * 128:(tile - 1) * 128, :]
            else:
                dst = C.xs[b, tile * 128:(tile + 1) * 128, :]

            post_norm_res(Ph, yacc[:, t, :], byacc, xt, bx, C.GG2, C.bGG2, j, junkr, str_, ttr, dst)
    Ph.close()


U32 = mybir.dt.uint32
I32 = mybir.dt.int32
NTOK = NB * LLAT
CAPE = NTOK
GS = 512
NGRP = CAPE // GS


def phase_moe_pre(C, l, tiles):
    S = C.S
    Ph = Phase(C, f"mpre{l}")
    xr = Ph.rot(2, [128, D], F32, 'x')
    junkr = Ph.rot(1, [128, D], BF16, 'junk')
    h32r = Ph.rot(2, [128, D], F32, 'h32')
    hbr = Ph.rot(2, [128, D], BF16, 'hb')
    hTr = Ph.rot(2, [128, 8, 128], F32, 'hT32')
    str_ = Ph.rot(4, [128, 4], F32, 'pst')
    rsr = Ph.rot(4, [128, 80], F32, 'rst')
    selr = Ph.rot(2, [128, NE], BF16, 'selb')
    recr = Ph.rot(8, [128, 4], U32, 'rec')
    slur = Ph.rot(4, [128, 2], U32, 'slu')
    wr, bwr = Ph.sbb([128, 8, NE], F32, 'wr')
    base, bbase = Ph.sbb([128, NE], F32, 'base')
    nid, bnid = Ph.sbb([128, 64], F32, 'nid')
    eoff, beoff = Ph.sbb([128, NE], F32, 'eoff')
    thr, bthr = Ph.sbb([128, NGRP], F32, 'thr')
    trif, btrif = Ph.sbb([128, 128], F32, 'trif')
    trib, btrib = Ph.sbb([128, 128], BF16, 'trib')
    oneb, boneb = Ph.sbb([128, 128], BF16, 'oneb')
    flf, bflf = Ph.sbb([128, NE, NGRP], F32, 'flf')
    fli, bfli = Ph.sbb([128, NE, NGRP], I32, 'fli')
    p32r = Ph.rot(2, [128, 8, 128], F32, 'p32', psum=True)
    plg = Ph.rot(2, [128, 512], F32, 'plg', psum=True)
    blst = S.buf('lst')
    S.op('sp', lambda e: e.dma_start(out=C.lst[:, :], in_=C.lst_init[:, :]), writes=[blst], dma=blst)
    S.op('sp', lambda e: e.dma_start(out=wr[:], in_=C.moe_wr.rearrange("(kc p) n -> p kc n", p=128)), writes=[bwr],
         dma=bwr)
    S.op('sp', lambda e: e.dma_start(out=nid[:], in_=C.nidf[:, :]), writes=[bnid], dma=bnid)
    S.op('sp', lambda e: e.dma_start(out=eoff[:], in_=C.eoff[:, :]), writes=[beoff], dma=beoff)
    S.op('sp', lambda e: e.dma_start(out=thr[:], in_=C.thr[:, :]), writes=[bthr], dma=bthr)
    S.op('sp', lambda e: e.dma_start(out=trif[:], in_=C.tri_in[:, 2, :]), writes=[btrif], dma=btrif)
    S.op('dve', lambda e: e.tensor_copy(out=trib[:], in_=trif[:]), reads=[btrif], writes=[btrib])
    S.op('pool', lambda e: e.memset(oneb[:], 1.0), writes=[boneb])
    S.op('pool', lambda e: e.memset(base[:], 0.0), writes=[bbase])
    for k, (b, tile) in enumerate(tiles):
        xt, bx = xr.next()
        S.op('sp', lambda e, xt=xt, b=b, tile=tile: e.dma_start(out=xt[:], in_=res_src(C, l, 1, b, tile)), writes=[bx],
             dma=bx)
        junk, bj = junkr.next()
        st, bst = str_.next()
        h32, bh32 = h32r.next()
        hb, bhb = hbr.next()
        S.op('act', lambda e, junk=junk, xt=xt, st=st: e.activation(out=junk[:], in_=xt[:], func=AF.Square,
                                                                     accum_out=st[:, 0:1]), reads=[bx], writes=[bj, bst])
        S.op('dve', lambda e, st=st: e.tensor_scalar(out=st[:, 1:2], in0=st[:, 0:1], scalar1=1.0 / D, scalar2=EPS,
                                                     op0=ALU.mult, op1=ALU.add), reads=[bst], writes=[bst])
        S.op('pool', lambda e, st=st: e.tensor_tensor(out=st[:, 2:3], in0=st[:, 1:2], in1=C.neghalf[:, 0:1],
                                                      op=ALU.pow), reads=[bst], writes=[bst])
        S.op('dve', lambda e, h32=h32, xt=xt, st=st, b=b: e.scalar_tensor_tensor(
            out=h32[:], in0=xt[:], scalar=st[:, 2:3], in1=C.G2row[:, b, :], op0=ALU.mult, op1=ALU.mult),
            reads=[bx, bst, C.bG2row], writes=[bh32])
        S.op('pool', lambda e, h32=h32, b=b: e.tensor_tensor(out=h32[:], in0=h32[:], in1=C.S2row[:, b, :], op=ALU.add),
             reads=[bh32, C.bG2row], writes=[bh32])
        S.op('act', lambda e, hb=hb, h32=h32: e.activation(out=hb[:], in_=h32[:], func=AF.Copy), reads=[bh32],
             writes=[bhb])
        S.op('sp', lambda e, hb=hb, k=k: e.dma_start(out=C.h2tok[k * 128:(k + 1) * 128, :], in_=hb[:]), reads=[bhb],
             dma=bhb)
        p32, bp32 = p32r.next()

        def tr32(e, p32=p32, h32=h32):
            for kc in range(8):
                ins = e.transpose(out=p32[:, kc, :], in_=h32[:, kc * 128:(kc + 1) * 128], identity=C.ident32[:])
            return ins
        S.op('pe', tr32, reads=[bh32], writes=[bp32], self_ok=True)
        hT, bhT = hTr.next()
        S.op('dve', lambda e, hT=hT, p32=p32: e.tensor_copy(out=hT[:, 0:4, :], in_=p32[:, 0:4, :]), reads=[bp32],
             writes=[bhT])
        S.op('act', lambda e, hT=hT, p32=p32: e.activation(out=hT[:, 4:8, :], in_=p32[:, 4:8, :], func=AF.Copy),
             reads=[bp32], writes=[bhT])
        pl, bpl = plg.next()

        def mm(e, pl=pl, hT=hT):
            for kc in range(8):
                ins = e.matmul(pl[:, 0:NE], lhsT=hT[:, kc, :], rhs=wr[:, kc, :], start=(kc == 0), stop=(kc == 7))
            return ins
        S.op('pe', mm, reads=[bhT, bwr], writes=[bpl], self_ok=True)
        r, br = rsr.next()
        LG, M1, M2, NM1, DEN, RDEN = r[:, 0:8], r[:, 8:9], r[:, 9:10], r[:, 10:11], r[:, 11:12], r[:, 12:13]
        EQ1, SEL, WN, MSK, OH1, SV, TMP = r[:, 16:24], r[:, 24:32], r[:, 32:40], r[:, 40:48], r[:, 48:56], r[:, 56:64], \
            r[:, 64:72]
        SL0, SL1, W0, W1, D1 = r[:, 72:73], r[:, 73:74], r[:, 74:75], r[:, 75:76], r[:, 76:77]
        selb, bselb = selr.next()
        dv = lambda f_, extra=(): S.op('dve', f_, reads=[br] + list(extra), writes=[br])
        dv(lambda e, LG=LG, pl=pl: e.tensor_copy(out=LG, in_=pl[:, 0:NE]), [bpl])
        dv(lambda e, LG=LG, M1=M1: e.reduce_max(out=M1, in_=LG, axis=AX.X))
        dv(lambda e, EQ1=EQ1, LG=LG, M1=M1: e.tensor_scalar(out=EQ1, in0=LG, scalar1=M1, scalar2=None,
                                                            op0=ALU.is_equal))
        dv(lambda e, MSK=MSK, EQ1=EQ1, LG=LG: e.scalar_tensor_tensor(out=MSK, in0=EQ1, scalar=-1e30, in1=LG,
                                                                     op0=ALU.mult, op1=ALU.add))
        dv(lambda e, MSK=MSK, M2=M2: e.reduce_max(out=M2, in_=MSK, axis=AX.X))
        dv(lambda e, SEL=SEL, LG=LG, M2=M2: e.tensor_scalar(out=SEL, in0=LG, scalar1=M2, scalar2=None, op0=ALU.is_ge))
        dv(lambda e, NM1=NM1, M1=M1: e.tensor_scalar(out=NM1, in0=M1, scalar1=-1.0, scalar2=None, op0=ALU.mult))
        S.op('act', lambda e, WN=WN, LG=LG, NM1=NM1: e.activation(out=WN, in_=LG, func=AF.Exp, bias=NM1, scale=1.0),
             reads=[br], writes=[br])
        dv(lambda e, WN=WN, SEL=SEL: e.tensor_tensor(out=WN, in0=WN, in1=SEL, op=ALU.mult))
        dv(lambda e, WN=WN, DEN=DEN: e.reduce_sum(out=DEN, in_=WN, axis=AX.X))
        dv(lambda e, DEN=DEN, RDEN=RDEN: e.reciprocal(out=RDEN, in_=DEN))
        dv(lambda e, WN=WN, RDEN=RDEN: e.tensor_scalar(out=WN, in0=WN, scalar1=RDEN, scalar2=None, op0=ALU.mult))
        dv(lambda e, OH1=OH1, SEL=SEL, EQ1=EQ1: e.tensor_tensor(out=OH1, in0=SEL, in1=EQ1, op=ALU.subtract))
        S.op('dve', lambda e, selb=selb, SEL=SEL: e.tensor_copy(out=selb[:], in_=SEL), reads=[br], writes=[bselb])
        pc, bpc = plg.next()

        def mmc(e, pc=pc, selb=selb):
            e.matmul(pc[:, 0:NE], lhsT=trib[:], rhs=selb[:], start=True, stop=True)
            return e.matmul(pc[:, 8:8 + NE], lhsT=oneb[:], rhs=selb[:], start=True, stop=True)
        S.op('pe', mmc, reads=[bselb, btrib, boneb], writes=[bpc], self_ok=True)
        dv(lambda e, SV=SV, pc=pc: e.tensor_tensor(out=SV, in0=pc[:, 0:NE], in1=base[:], op=ALU.add), [bpc, bbase])
        dv(lambda e, SV=SV: e.tensor_tensor(out=SV, in0=SV, in1=eoff[:], op=ALU.add), [beoff])
        S.op('dve', lambda e, pc=pc: e.tensor_tensor(out=base[:], in0=pc[:, 8:8 + NE], in1=base[:], op=ALU.add),
             reads=[bpc, br], writes=[bbase])
        for (OH, SL, W) in [(EQ1, SL0, W0), (OH1, SL1, W1)]:
            dv(lambda e, TMP=TMP, OH=OH, SV=SV: e.tensor_tensor(out=TMP, in0=OH, in1=SV, op=ALU.mult))
            dv(lambda e, TMP=TMP, SL=SL: e.reduce_sum(out=SL, in_=TMP, axis=AX.X))
            dv(lambda e, TMP=TMP, OH=OH, WN=WN: e.tensor_tensor(out=TMP, in0=OH, in1=WN, op=ALU.mult))
            dv(lambda e, TMP=TMP, W=W: e.reduce_sum(out=W, in_=TMP, axis=AX.X))
        dv(lambda e, D1=D1, k=k: e.tensor_scalar(out=D1, in0=nid[:, k:k + 1], scalar1=float(NTOK), scalar2=None,
                                                 op0=ALU.add), [bnid])
        slu, bslu = slur.next()
        S.op('dve', lambda e, slu=slu, SL0=SL0: e.tensor_copy(out=slu[:, 0:1], in_=SL0), reads=[br], writes=[bslu])
        S.op('dve', lambda e, slu=slu, SL1=SL1: e.tensor_copy(out=slu[:, 1:2], in_=SL1), reads=[br], writes=[bslu])
        for rk, (DD, WW) in enumerate([(None, W0), (D1, W1)]):
            rec, brec = recr.next()
            S.op('pool', lambda e, rec=rec: e.memset(rec[:], 0), writes=[brec])
            rw = lambda f_: S.op('dve', f_, reads=[br, bnid], writes=[brec])
            rw(lambda e, rec=rec, k=k: e.tensor_copy(out=rec[:, 0:1], in_=nid[:, k:k + 1]))
            if DD is None:
                rw(lambda e, rec=rec, k=k: e.tensor_copy(out=rec[:, 1:2], in_=nid[:, k:k + 1]))
            else:
                rw(lambda e, rec=rec, DD=DD: e.tensor_copy(out=rec[:, 1:2], in_=DD))
            rw(lambda e, rec=rec, WW=WW: e.tensor_copy(out=rec[:, 2:3].bitcast(F32), in_=WW))
            S.op('pool', lambda e, rec=rec, slu=slu, rk=rk: e.indirect_dma_start(
                out=C.lst[:, :], out_offset=bass.IndirectOffsetOnAxis(ap=slu[:, rk:rk + 1], axis=0),
                in_=rec[:], in_offset=None, bounds_check=S.breg(e, NE * CAPE - 1), oob_is_err=False),
                reads=[brec, bslu, blst], dma=brec)
    for ex in range(NE):
        S.op('dve', lambda e, ex=ex: e.tensor_scalar(out=flf[:, ex, :], in0=thr[:], scalar1=base[:, ex:ex + 1],
                                                     scalar2=None, op0=ALU.is_lt), reads=[bbase, bthr], writes=[bflf])
    S.op('dve', lambda e: e.tensor_copy(out=fli[:], in_=flf[:]), reads=[bflf], writes=[bfli])
    S.op('sp', lambda e: e.dma_start(out=C.flags[0:1, :], in_=fli[0:1, :, :].rearrange("p a b -> p (a b)")),
         reads=[bfli], dma=bfli)
    Ph.close()


def phase_moe_sparse(C, l):
    import os
    SPS = int(os.environ.get('SP_STG', '9'))
    S = C.S
    Ph = Phase(C, f"moe{l}")
    NFC = F_MOE // 128
    NFB = F_MOE // 256
    wsrc = C.moe_bf
    hid, bhid = Ph.sbb([128, NFC, 512], BF16, 'hid')
    wd, bwd = Ph.sbb([128, NFC, D], BF16, 'wd')
    wgr = Ph.rot(3, [128, 8, 256], BF16, 'wg')
    wur = Ph.rot(3, [128, 8, 256], BF16, 'wu')
    htr = Ph.rot(2, [128, 4, D], BF16, 'htok')
    hTr = Ph.rot(2, [128, 8, 512], BF16, 'hT')
    recr = Ph.rot(2, [128, 4, 4], U32, 'recs')
    sgr = Ph.rot(2, [128, 512], F32, 'sg')
    yscr = Ph.rot(2, [128, D], F32, 'ysc')
    ptrr = Ph.rot(2, [128, 8, 128], BF16, 'ptr', psum=True)
    pgr = Ph.rot(2, [128, 512], F32, 'pg', psum=True)
    pur = Ph.rot(1, [128, 512], F32, 'pu', psum=True)
    pyr = Ph.rot(2, [128, 512], F32, 'py', psum=True)
    for t_, b_ in zip(htr.t, htr.b):
        S.op('pool', lambda e, t_=t_: e.memset(t_[:], 0.0), writes=[b_])
    for ex in range(NE):
        hh = NFC // 2
        for (a0, a1) in [(0, hh), (hh, NFC)]:
            S.op('sp', lambda e, ex=ex, a0=a0, a1=a1: e.dma_start(
                out=wd[:, a0:a1, :], in_=wsrc[2][ex, a0 * 128:a1 * 128, :].rearrange("(fc p) n -> p fc n", p=128)),
                reads=[C.bmoe], writes=[bwd], dma=bwd)
        for g in range(NGRP):
            S.cond_begin(C.flags[0:1, ex * NGRP + g:ex * NGRP + g + 1])
            recs, brecs = recr.next()
            r0 = ex * CAPE + g * GS
            S.op('sp', lambda e, recs=recs, r0=r0: e.dma_start(
                out=recs[:], in_=C.lst[r0:r0 + GS, :].rearrange("(t p) c -> p t c", p=128)), writes=[brecs], dma=brecs)
            ht, bht = htr.next()
            for t in range(4):
                S.op('pool', lambda e, ht=ht, recs=recs, t=t: e.indirect_dma_start(
                    out=ht[:, t, :], out_offset=None, in_=C.h2tok[:, :],
                    in_offset=bass.IndirectOffsetOnAxis(ap=recs[:, t, 0:1], axis=0), bounds_check=S.breg(e, NTOK - 1),
                    oob_is_err=False), reads=[brecs], writes=[bht], dma=bht)
            hT, bhT = hTr.next()
            for t in range(4 if SPS >= 2 else 0):
                ptr, bptr = ptrr.next()

                def tr(e, ptr=ptr, ht=ht, t=t):
                    for kc in range(8):
                        ins = e.transpose(out=ptr[:, kc, :], in_=ht[:, t, kc * 128:(kc + 1) * 128], identity=C.ident[:])
                    return ins
                S.op('pe', tr, reads=[bht], writes=[bptr], self_ok=True)
                if t % 2 == 0:
                    S.op('dve', lambda e, hT=hT, ptr=ptr, t=t: e.tensor_copy(out=hT[:, :, t * 128:(t + 1) * 128],
                                                                             in_=ptr[:]), reads=[bptr], writes=[bhT])
                else:
                    S.op('act', lambda e, hT=hT, ptr=ptr, t=t: e.activation(out=hT[:, :, t * 128:(t + 1) * 128],
                                                                            in_=ptr[:], func=AF.Copy), reads=[bptr],
                         writes=[bhT])
            for fb in range(NFB if SPS >= 3 else 0):
                wg, bwg = wgr.next()
                wu, bwu = wur.next()
                S.op('sp', lambda e, wg=wg, ex=ex, fb=fb: e.dma_start(
                    out=wg[:], in_=wsrc[0][ex, :, fb * 256:(fb + 1) * 256].rearrange("(kc p) f -> p kc f", p=128)),
                    reads=[C.bmoe], writes=[bwg], dma=bwg)
                S.op('sp', lambda e, wu=wu, ex=ex, fb=fb: e.dma_start(
                    out=wu[:], in_=wsrc[1][ex, :, fb * 256:(fb + 1) * 256].rearrange("(kc p) f -> p kc f", p=128)),
                    reads=[C.bmoe], writes=[bwu], dma=bwu)
                for fi in range(2):
                    fc = fb * 2 + fi
                    pg_, bpg = pgr.next()
                    pu_, bpu = pur.next()

                    def mmg(e, pg_=pg_, wg=wg, fi=fi, hT=hT):
                        for kc in range(8):
                            ins = e.matmul(pg_[:], lhsT=wg[:, kc, fi * 128:(fi + 1) * 128], rhs=hT[:, kc, :],
                                           start=(kc == 0), stop=(kc == 7))
                        return ins

                    def mmu(e, pu_=pu_, wu=wu, fi=fi, hT=hT):
                        for kc in range(8):
                            ins = e.matmul(pu_[:], lhsT=wu[:, kc, fi * 128:(fi + 1) * 128], rhs=hT[:, kc, :],
                                           start=(kc == 0), stop=(kc == 7))
                        return ins
                    S.op('pe', mmg, reads=[bwg, bhT], writes=[bpg], self_ok=True)
                    S.op('pe', mmu, reads=[bwu, bhT], writes=[bpu], self_ok=True)
                    sg, bsg = sgr.next()
                    S.op('act', lambda e, sg=sg, pg_=pg_: e.activation(out=sg[:], in_=pg_[:], func=AF.Silu),
                         reads=[bpg], writes=[bsg])
                    S.op('dve', lambda e, sg=sg, pu_=pu_, fc=fc: e.tensor_tensor(out=hid[:, fc, :], in0=pu_[:],
                                                                                 in1=sg[:], op=ALU.mult),
                         reads=[bsg, bpu], writes=[bhid])
            for t in range(4 if SPS >= 4 else 0):
                ysc, bysc = yscr.next()
                for half in range(2):
                    py_, bpy = pyr.next()
                    hs = slice(half * 512, (half + 1) * 512)

                    def mmd(e, py_=py_, t=t, hs=hs):
                        for fc in range(NFC):
                            ins = e.matmul(py_[:], lhsT=hid[:, fc, t * 128:(t + 1) * 128], rhs=wd[:, fc, hs],
                                           start=(fc == 0), stop=(fc == NFC - 1))
                        return ins
                    S.op('pe', mmd, reads=[bhid, bwd], writes=[bpy], self_ok=True)
                    S.op('act', lambda e, py_=py_, ysc=ysc, hs=hs, recs=recs, t=t: e.activation(
                        out=ysc[:, hs], in_=py_[:], func=AF.Copy, scale=recs[:, t, 2:3].bitcast(F32)),
                        reads=[bpy, brecs], writes=[bysc])
                if SPS >= 5:
                  S.op('pool', lambda e, ysc=ysc, recs=recs, t=t: e.indirect_dma_start(
                    out=C.Ymoe[:, :], out_offset=bass.IndirectOffsetOnAxis(ap=recs[:, t, 1:2], axis=0), in_=ysc[:],
                    in_offset=None, bounds_check=S.breg(e, 2 * NTOK - 1), oob_is_err=False), reads=[bysc, brecs], dma=bysc)
            S.cond_end()
    Ph.close()


def phase_moe_post(C, l, tiles):
    S = C.S
    Ph = Phase(C, f"mpost{l}")
    xr = Ph.rot(4, [128, D], F32, 'x')
    y1r = Ph.rot(4, [128, D], F32, 'y1')
    y2r = Ph.rot(4, [128, D], F32, 'y2')
    ttr = Ph.rot(3, [128, D], F32, 'tt')
    junkr = Ph.rot(2, [128, D], BF16, 'junk')
    str_ = Ph.rot(8, [128, 4], F32, 'fst')
    for k, (b, tile) in enumerate(tiles):
        xt, bx = xr.next()
        y1, by1 = y1r.next()
        y2, by2 = y2r.next()
        S.op('sp', lambda e, xt=xt, b=b, tile=tile: e.dma_start(out=xt[:], in_=res_src(C, l, 1, b, tile)), writes=[bx],
             dma=bx)
        S.op('sp', lambda e, y1=y1, k=k: e.dma_start(out=y1[:], in_=C.Ymoe[k * 128:(k + 1) * 128, :]), writes=[by1],
             dma=by1)
        S.op('sp', lambda e, y2=y2, k=k: e.dma_start(out=y2[:], in_=C.Ymoe[NTOK + k * 128:NTOK + (k + 1) * 128, :]),
             writes=[by2], dma=by2)
        S.op('pool', lambda e, y1=y1, y2=y2: e.tensor_tensor(out=y1[:], in0=y1[:], in1=y2[:], op=ALU.add),
             reads=[by1, by2], writes=[by1])
        dst = C.y_out[b, (tile - 2) * 128:(tile - 1) * 128, :]
        post_norm_res(Ph, y1[:], by1, xt, bx, C.GG2, C.bGG2, b, junkr, str_, ttr, dst)
    Ph.close()


SPARSE_MOE = True


def build_program(debug=False, upto=None, skip=()):
    nc = bass.Bass("TRN2", target_bir_lowering=False)
    C = Ctx()
    C.nc = nc
    C.debug = debug
    L = 2
    C.x_in = _dram_in(nc, "x", [NB, LLAT, D])
    C.ctx_in = _dram_in(nc, "ctx", [NB, LCTX, D])
    C.cT = _dram_in(nc, "cT", [128, 8, 3])
    C.w_ada = _dram_in(nc, "w_ada", [L, D, 6 * D])
    C.badaT3 = _dram_in(nc, "badaT3", [L, 128, 48, 3])
    C.gpre3 = _dram_in(nc, "gpre3", [L, 128, 2, 8, 3])
    C.rowc = _dram_in(nc, "rowc", [L, 128, 7, D])
    C.w_in = _dram_in(nc, "w_in", [L, D, PROJ])
    C.rope_cos = _dram_in(nc, "rope_cos", [128, 32, 64])
    C.rope_sin = _dram_in(nc, "rope_sin", [128, 32, 64])
    C.ident_in = _dram_in(nc, "ident", [128, 128], BF16)
    C.ident32_in = _dram_in(nc, "ident32", [128, 128], F32)
    C.tri_in = _dram_in(nc, "tri", [128, 4, 128], F32)
    C.wgate = _dram_in(nc, "gla_w_gate", [L, 2, 16, 256])
    C.bgate = _dram_in(nc, "gla_b_gate", [L, 2, 1, 256])
    C.gnormB = _dram_in(nc, "gnormB", [L, 128, 512])
    C.natb = _dram_in(nc, "natb", [L, 8, 128, 5, 5, 128])
    C.w_out = _dram_in(nc, "w_out", [L, D, D])
    C.ffn_wg = _dram_in(nc, "ffn_w_gate", [1, D, F_FFN])
    C.ffn_wu = _dram_in(nc, "ffn_w_up", [1, D, F_FFN])
    C.ffn_wd = _dram_in(nc, "ffn_w_down", [1, F_FFN, D])
    C.moe_wr = _dram_in(nc, "moe_w_router", [D, NE])
    C.moe_wg = _dram_in(nc, "moe_w_gate", [NE, D, F_MOE])
    C.moe_wu = _dram_in(nc, "moe_w_up", [NE, D, F_MOE])
    C.moe_wd = _dram_in(nc, "moe_w_down", [NE, F_MOE, D])
    C.lst_init = _dram_in(nc, "lst_init", [NE * CAPE, 4], U32)
    C.nidf = _dram_in(nc, "nidf", [128, 64])
    C.eoff = _dram_in(nc, "eoff", [128, NE])
    C.thr = _dram_in(nc, "thr", [128, NGRP])
    C.lst = _dram_tmp(nc, "lst", [NE * CAPE, 4], U32)
    C.flags = _dram_tmp(nc, "flags", [1, NE * NGRP], I32)
    C.h2tok = _dram_tmp(nc, "h2tok", [NTOK, D], BF16)
    C.Ymoe = _dram_tmp(nc, "Ymoe", [2 * NTOK, D], F32)
    C.y_out = nc.dram_tensor("y", [NB, LLAT, D], F32, kind="ExternalOutput").ap()
    dbg = debug
    C.xs = _dram_tmp(nc, "xs", [NB, LT, D], F32, dbg)
    C.tokmaj = _dram_tmp(nc, "tokmaj", [NB, LT, 2048], BF16, dbg)
    C.nqkT = _dram_tmp(nc, "nqkT", [NB, 1024, LT], BF16, dbg)
    C.lrT = _dram_tmp(nc, "lrT", [NB, 2, 16, LT], F32, dbg)
    C.cat = _dram_tmp(nc, "cat", [NB, LT, D], BF16, dbg)
    C.h2T = _dram_tmp(nc, "h2T", [17, 128, 8, 512], BF16, dbg)
    C.comb = _dram_tmp(nc, "comb", [17, 128, 4, NE], F32, dbg)
    C.ffn_bf = [_dram_tmp(nc, "ffn_wg_bf", [1, D, F_FFN], BF16), _dram_tmp(nc, "ffn_wu_bf", [1, D, F_FFN], BF16),
                _dram_tmp(nc, "ffn_wd_bf", [1, F_FFN, D], BF16)]
    C.moe_bf = [_dram_tmp(nc, "moe_wg_bf", [NE, D, F_MOE], BF16), _dram_tmp(nc, "moe_wu_bf", [NE, D, F_MOE], BF16),
                _dram_tmp(nc, "moe_wd_bf", [NE, F_MOE, D], BF16)]
    if debug:
        C.dbg = nc.dram_tensor("dbg", [128, 8192], F32, kind="ExternalOutput").ap()
    with ExitStack() as gs:
        S = Sched(nc, gs)
        C.S = S
        S.bounds = [NE * CAPE - 1, NTOK - 1, 2 * NTOK - 1]
        GP = Phase(C, "glob")
        C.ident, bid = GP.sbb([128, 128], BF16, 'ident')
        C.ident32, bid32 = GP.sbb([128, 128], F32, 'ident32')
        C.ones, bones = GP.sbb([128, 128], F32, 'ones')
        C.neghalf, bnh = GP.sbb([128, 4], F32, 'neghalf')
        S.op('sp', lambda e: e.dma_start(out=C.ident[:], in_=C.ident_in[:, :]), writes=[bid], dma=bid)
        S.op('sp', lambda e: e.dma_start(out=C.ident32[:], in_=C.ident32_in[:, :]), writes=[bid32], dma=bid32)
        S.op('pool', lambda e: e.memset(C.ones[:], 1.0), writes=[bones])
        S.op('pool', lambda e: e.memset(C.neghalf[:], -0.5), writes=[bnh])
        C.bffn = Buf('ffn_bf')
        C.bmoe = Buf('moe_bf')
        if upto is None or upto >= 5:
            for src, dst in zip([C.ffn_wg, C.ffn_wu, C.ffn_wd], C.ffn_bf):
                S.op('pool', lambda e, src=src, dst=dst: e.dma_start(out=dst[0], in_=src[0]), writes=[C.bffn],
                     dma=C.bffn, track=False)
        C.moe_cast_pending = (upto is None or upto >= 6)
        S.flush()
        for l in range(L):
            LP = Phase(C, f"L{l}")
            C.want_rows = (l == L - 1) and SPARSE_MOE
            phase_mod(C, l, LP)
            if debug and l == debug - 1 and upto == 0:
                dump_mod(C)
            if upto is not None and upto == 0:
                LP.st.close()
                break
            if 1 not in skip:
                phase_proj(C, l)
            if upto is not None and upto <= 1:
                LP.st.close()
                break
            last = (l == L - 1)
            phase_gla(C, l, last)
            if upto is not None and upto <= 2:
                LP.st.close()
                break
            phase_na(C, l, last)
            if upto is not None and upto <= 3:
                LP.st.close()
                break
            phase_outproj(C, l, last)
            if upto is not None and upto <= 4:
                LP.st.close()
                break
            if last:
                tiles = [(b, t) for b in range(NB) for t in range(2, NT)]
            else:
                tiles = [(b, t) for b in range(NB) for t in range(NT)]
            if last and SPARSE_MOE:
                import os
                ms = int(os.environ.get('MOE_STOP', '9'))
                phase_moe_pre(C, l, tiles)
                if ms >= 2:
                    phase_moe_sparse(C, l)
                if ms >= 3:
                    phase_moe_post(C, l, tiles)
            else:
                phase_ffn_pre(C, l, last, tiles)
                phase_ffn(C, l, last, tiles)
            if upto is not None and upto <= 5 + l:
                LP.st.close()
                break
            LP.st.close()
        GP.st.close()
    return nc


def dump_mod(C):
    S = C.S
    Ph = Phase(C, "dump")
    o = 0
    for t, n in [(C.G1, 24), (C.SH1, 24), (C.G2, 24), (C.SH2, 24)]:
        S.op('sp', lambda e, t=t, o=o, n=n: e.dma_start(out=C.dbg[:, o:o + n], in_=t[:].rearrange("p a b -> p (a b)")),
             reads=[C.bG1, C.bG2], dma=S.buf())
        o += n
    for t in [C.GG1, C.GG2]:
        S.op('sp', lambda e, t=t, o=o: e.dma_start(out=C.dbg[:, o:o + 3072], in_=t[:].rearrange("p a b -> p (a b)")),
             reads=[C.bGG1, C.bGG2], dma=S.buf())
        o += 3072
    Ph.close()


def _na_bias_tables(rpb):
    L = rpb.shape[0]
    out = np.full((L, 8, 128, 5, 640), NEG, np.float32)
    reps = [0, 1, 10, 30, 31]
    for pi, j in enumerate(reps):
        ts = min(max(j - 2, 0), 27)
        for rq2 in range(2):
            r = 2 * j + rq2
            rs = min(max(r - 4, 0), 56)
            for cq in range(64):
                cs = min(max(cq - 8, 0), 48)
                p = rq2 * 64 + cq
                ck = np.arange(cs, cs + 16)
                for rk in range(rs, rs + 8):
                    slot = rk - 2 * ts
                    out[:, :, p, pi, slot * 64 + ck] = rpb[:, :, rk - r + 7, ck - cq + 15]
    return out


def _tri():
    i = np.arange(128)
    ut = (i[:, None] <= i[None, :]).astype(np.float32)
    lt = (i[:, None] >= i[None, :]).astype(np.float32)
    sut = (i[:, None] < i[None, :]).astype(np.float32)
    slt = (i[:, None] > i[None, :]).astype(np.float32)
    return np.stack([ut, lt, sut, slt], axis=1).copy()


def make_in_maps(inp, n_cores=8):
    import ml_dtypes
    f = lambda a: np.ascontiguousarray(np.asarray(a, dtype=np.float32))
    L = 2
    w_ada = f(inp['w_ada'])
    b_ada = f(inp['b_ada'])
    badaT3 = np.repeat(b_ada.reshape(L, 48, 128).transpose(0, 2, 1)[:, :, :, None], 3, axis=3).copy()
    gp = np.stack([f(inp['g_pre_mix']), f(inp['g_pre_ffn'])], axis=1)
    gpre3 = np.repeat(gp.reshape(L, 2, 8, 128).transpose(0, 3, 1, 2)[..., None], 3, axis=4).copy()
    rows = np.stack([b_ada[:, 2048:3072], b_ada[:, 5120:6144], f(inp['g_post_mix']), f(inp['g_post_ffn']),
                     b_ada[:, 3072:4096], b_ada[:, 4096:5120], f(inp['g_pre_ffn'])], axis=1)
    rowc = np.repeat(rows[:, None, :, :], 128, axis=1).copy()
    cos, sin = _rope_tables()
    gn = f(inp['gla_g_norm'])
    gnormB = np.repeat(np.tile(gn, (1, 4))[:, None, :], 128, axis=1).copy()
    natb = _na_bias_tables(f(inp['na_rpb']))
    natb = np.ascontiguousarray(natb.reshape(L, 8, 128, 5, 5, 128).transpose(0, 1, 5, 3, 4, 2))
    lst_init = np.zeros((NE * CAPE, 4), np.uint32)
    lst_init[:, 0:2] = 1 << 30
    nidf = (np.arange(64)[None, :] * 128 + np.arange(128)[:, None]).astype(np.float32)
    eoff = np.repeat((np.arange(NE) * CAPE).astype(np.float32)[None, :], 128, axis=0)
    thr = np.repeat((np.arange(NGRP) * GS).astype(np.float32)[None, :], 128, axis=0)
    shared = {
        "lst_init": lst_init, "nidf": nidf, "eoff": eoff, "thr": thr,
        "w_ada": w_ada, "badaT3": badaT3, "gpre3": gpre3, "rowc": rowc, "w_in": f(inp['w_in']),
        "rope_cos": cos, "rope_sin": sin, "ident": np.eye(128).astype(ml_dtypes.bfloat16),
        "ident32": np.eye(128, dtype=np.float32), "tri": _tri(),
        "gla_w_gate": f(inp['gla_w_gate']), "gla_b_gate": f(inp['gla_b_gate']).reshape(L, 2, 1, 256),
        "gnormB": gnormB, "natb": natb, "w_out": f(inp['w_out']),
        "ffn_w_gate": f(inp['ffn_w_gate']), "ffn_w_up": f(inp['ffn_w_up']), "ffn_w_down": f(inp['ffn_w_down']),
        "moe_w_router": f(inp['moe_w_router'])[0], "moe_w_gate": f(inp['moe_w_gate'])[0],
        "moe_w_up": f(inp['moe_w_up'])[0], "moe_w_down": f(inp['moe_w_down'])[0],
    }
    x = f(inp['x'])
    c = f(inp['c'])
    ctx = f(inp['ctx'])
    c_ctx = f(inp['c_ctx'])
    maps = []
    for i in range(n_cores):
        cv = np.stack([c[2 * i], c[2 * i + 1], c_ctx], axis=0)
        cT = cv.reshape(3, 8, 128).transpose(2, 1, 0).copy()
        m = dict(shared)
        m["x"] = x[2 * i:2 * i + 2]
        m["ctx"] = ctx[2 * i:2 * i + 2]
        m["cT"] = cT
        maps.append(m)
    return maps


def kernel(**inputs):
    nc = build_program()
    maps = make_in_maps(inputs, 8)
    res = run_bass_kernel_spmd(nc, maps, core_ids=list(range(8)))
    return np.concatenate([np.asarray(r["y"]) for r in res.results], axis=0).astype(np.float32)


def _rope_tables():
    pos = np.arange(LLAT)
    row, col = pos // 64, pos % 64
    half = 16
    inv = (10000.0 ** (-np.arange(half, dtype=np.float32) / half)).astype(np.float32)
    ang_r = row.astype(np.float32)[:, None] * inv[None, :]
    ang_c = col.astype(np.float32)[:, None] * inv[None, :]
    cr, sr, cc, sc = np.cos(ang_r), np.sin(ang_r), np.cos(ang_c), np.sin(ang_c)
    cos = np.concatenate([cr, cr, cc, cc], axis=1).astype(np.float32)
    sin = np.concatenate([-sr, sr, -sc, sc], axis=1).astype(np.float32)
    cos = cos.reshape(32, 128, 64).transpose(1, 0, 2).copy()
    sin = sin.reshape(32, 128, 64).transpose(1, 0, 2).copy()
    return cos, sin
```

```python
import numpy as np
from contextlib import ExitStack
import concourse.bass as bass
import concourse.mybir as mybir
from concourse.bass_utils import run_bass_kernel_spmd

F32 = mybir.dt.float32
BF16 = mybir.dt.bfloat16
AF = mybir.ActivationFunctionType
ALU = mybir.AluOpType
AX = mybir.AxisListType

D = 1024
NB = 2
LCTX = 256
LLAT = 4096
LT = LCTX + LLAT
NT = LT // 128
PROJ = 3104
EPS = 1e-6
NEG = -30000.0

ENGS = ['pe', 'act', 'dve', 'pool', 'sp']
EPOCH = 30000


class Buf:
    __slots__ = ('name', 'w', 'r', 'dsem')

    def __init__(self, name=''):
        self.name = name
        self.w = None
        self.r = {}
        self.dsem = None


class Sched:
    def __init__(self, nc, stack):
        self.nc = nc
        self.stack = stack
        self.cnt = {e: 0 for e in ENGS}
        self.esems = {e: [] for e in ENGS}
        self.items = {e: [] for e in ENGS}
        self.waited = {e: {} for e in ENGS}
        self.free_dsems = []
        self.phase_bufs = []
        self.outstanding = {}
        self.nsem = 0
        self.ninstr = 0
        self.cregs = {}
        self.bregs = {}
        self.bounds = []
        self._cond = None

    def _newsem(self, name):
        self.nsem += 1
        return self.stack.enter_context(self.nc.semaphore(name))

    def _esem(self, e, seq):
        ep = (seq - 1) // EPOCH
        while len(self.esems[e]) <= ep:
            self.esems[e].append(self._newsem(f"s_{e}_{len(self.esems[e])}"))
        return self.esems[e][ep], (seq - 1) % EPOCH + 1

    def buf(self, name=''):
        b = Buf(name)
        self.phase_bufs.append(b)
        return b

    def bufs(self, n, name=''):
        return [self.buf(f"{name}{i}") for i in range(n)]

    def op(self, eng, fn, reads=(), writes=(), dma=None, self_ok=False, track=True):
        deps = {}

        def add(p):
            if p is None:
                return
            sem, val, peng = p
            if self_ok and peng == eng:
                return
            k = sem.num
            if k not in deps or deps[k][1] < val:
                deps[k] = (sem, val)

        for b in reads:
            add(b.w)
        for b in writes:
            add(b.w)
            for p in b.r.values():
                add(p)
        if dma is None:
            self.cnt[eng] += 1
            sem, val = self._esem(eng, self.cnt[eng])
            inc = 1
            tok = (sem, val, eng)
        else:
            if dma.dsem is None:
                if self.free_dsems:
                    dma.dsem = self.free_dsems.pop()
                else:
                    dma.dsem = [self._newsem(f"d{self.nsem}"), 0]
            dma.dsem[1] += 16
            sem, val = dma.dsem[0], dma.dsem[1]
            inc = 16
            tok = (sem, val, 'dma')
        waits = []
        wd = self.waited[eng]
        for k, (s, v) in deps.items():
            if wd.get(k, 0) >= v:
                continue
            wd[k] = v
            waits.append((s, v))
        self.items[eng].append((fn, waits, sem, inc, val))
        self.ninstr += 1
        for b in writes:
            b.w = tok
            b.r = {}
        for b in reads:
            if b not in writes:
                b.r[sem.num] = tok
        if track:
            self.outstanding[sem.num] = (sem, val)
        return tok

    def breg(self, engine, value):
        if value not in self.bregs:
            r = engine.alloc_register(f"bnd_{value}")
            engine.reg_mov(r, value)
            self.bregs[value] = r
        return self.bregs[value]

    def cond_begin(self, flag_ap):
        self._cond = {'flag': flag_ap, 'start': {e: len(self.items[e]) for e in ENGS},
                      'waited': {e: dict(self.waited[e]) for e in ENGS}}
        for e in ENGS:
            self.items[e].append(('cond_begin', flag_ap))

    def cond_end(self):
        c = self._cond
        for e in ENGS:
            body = self.items[e][c['start'][e] + 1:]
            agg = {}
            for it in body:
                fn, waits, sem, inc = it[0], it[1], it[2], it[3]
                if fn is None or sem is None:
                    continue
                k = sem.num
                if k not in agg:
                    agg[k] = [sem, it[4] - inc, 0]
                agg[k][2] += inc
            self.items[e].append(('cond_end', list(agg.values())))
            self.waited[e] = c['waited'][e]
        self._cond = None

    def barrier(self):
        for e in ENGS:
            waits = []
            for k, (s, v) in self.outstanding.items():
                if self.waited[e].get(k, 0) >= v:
                    continue
                self.waited[e][k] = v
                waits.append((s, v))
            self.items[e].append((None, waits, None, 0, 0))
        self.outstanding = {}

    def flush(self):
        self.barrier()
        nc = self.nc
        with nc.Block() as block:
            regs = {'pe': block.tensor, 'act': block.scalar, 'dve': block.vector,
                    'pool': block.gpsimd, 'sp': block.sync}
            for e in ENGS:
                items = self.items[e]

                def body(engine, items=items, e=e):
                    guard = None
                    if e == 'pool':
                        for v in self.bounds:
                            self.breg(engine, v)
                    for it in items:
                        if it[0] == 'cond_begin':
                            if e not in self.cregs:
                                self.cregs[e] = engine.alloc_register(f"creg_{e}")
                            reg = self.cregs[e]
                            engine.reg_load(reg, it[1])
                            guard = engine.If_ne(reg, 0)
                            guard.__enter__()
                            continue
                        if it[0] == 'cond_end':
                            guard.__exit__(None, None, None)
                            eg = engine.Else()
                            eg.__enter__()
                            for sem, pre, tot in it[1]:
                                if pre > 0:
                                    engine.wait_ge(sem, pre)
                                engine.sem_inc(sem, tot)
                            eg.__exit__(None, None, None)
                            guard = None
                            continue
                        fn, waits, sem, inc, _ = it
                        for s, v in waits:
                            engine.wait_ge(s, v)
                        if fn is not None:
                            ins = fn(engine)
                            ins.then_inc(sem, inc)

                regs[e](body)
        self.items = {e: [] for e in ENGS}
        for b in self.phase_bufs:
            if b.dsem is not None:
                self.free_dsems.append(b.dsem)
                b.dsem = None
        self.phase_bufs = []


class Rot:
    def __init__(self, S, mk, n, name):
        self.t = [mk(f"{name}{i}") for i in range(n)]
        self.b = [S.buf(f"{name}{i}") for i in range(n)]
        self.i = 0

    def next(self):
        k = self.i % len(self.t)
        self.i += 1
        return self.t[k], self.b[k]


class Phase:
    _uid = [0]

    def __init__(self, C, name):
        self.C = C
        self.nc = C.nc
        self.S = C.S
        self.st = ExitStack()
        self.name = name

    def _nm(self, nm):
        Phase._uid[0] += 1
        return f"{self.name}_{nm}_{Phase._uid[0]}"

    def sb(self, shape, dt, nm='t'):
        return self.st.enter_context(self.nc.sbuf_tensor(self._nm(nm), list(shape), dt))

    def ps(self, shape, dt, nm='p'):
        return self.st.enter_context(self.nc.psum_tensor(self._nm(nm), list(shape), dt))

    def sbb(self, shape, dt, nm='t'):
        return self.sb(shape, dt, nm), self.S.buf(nm)

    def rot(self, n, shape, dt, nm, psum=False):
        f = self.ps if psum else self.sb
        return Rot(self.S, lambda s: f(shape, dt, nm), n, nm)

    def close(self):
        self.S.flush()
        self.st.close()


class Ctx:
    pass


def _dram_in(nc, name, shape, dt=F32):
    return nc.dram_tensor(name, list(shape), dt, kind="ExternalInput").ap()


def _dram_tmp(nc, name, shape, dt, dbg=False):
    return nc.dram_tensor(name, list(shape), dt, kind="ExternalOutput" if dbg else "Internal").ap()


import os as _os
USE_TTR = False
GLA_PIPE = True
NA_PIPE = 0

O_Q, O_K, O_V, O_R, O_LR, O_NQ, O_NK, O_NV = 0, 256, 512, 1024, 1536, 1568, 2080, 2592
F_FFN = 2816
F_MOE = 3584
NE = 8


def prenorm_tile(Ph, K, xt, bx, hT, bhT, c0, Gt, St, bGS, j, h32=None, bh32=None):
    S = Ph.S
    C = Ph.C
    junk, bj = K['junk'].next()
    st, bst = K['stat'].next()
    xn, bxn = K['xn'].next()
    ptr, bptr = K['ptr'].next()
    S.op('act', lambda e: e.activation(out=junk[:], in_=xt[:], func=AF.Square, accum_out=st[:, 0:1]),
         reads=[bx], writes=[bj, bst])
    S.op('dve', lambda e: e.tensor_scalar(out=st[:, 1:2], in0=st[:, 0:1], scalar1=1.0 / D, scalar2=EPS,
                                          op0=ALU.mult, op1=ALU.add), reads=[bst], writes=[bst])
    S.op('pool', lambda e: e.tensor_tensor(out=st[:, 2:3], in0=st[:, 1:2], in1=C.neghalf[:, 0:1], op=ALU.pow),
         reads=[bst], writes=[bst])
    S.op('act', lambda e: e.activation(out=xn[:], in_=xt[:], func=AF.Copy, scale=st[:, 2:3]),
         reads=[bx, bst], writes=[bxn])

    def tr(e):
        for k in range(8):
            ins = e.transpose(out=ptr[:, k, :], in_=xn[:, k * 128:(k + 1) * 128], identity=C.ident[:])
        return ins
    S.op('pe', tr, reads=[bxn], writes=[bptr], self_ok=True)
    for k in range(8):
        if k % 2 == 0:
            S.op('dve', lambda e, k=k: e.tensor_scalar(out=hT[:, k, c0:c0 + 128], in0=ptr[:, k, :],
                                                       scalar1=Gt[:, k, j:j + 1], scalar2=St[:, k, j:j + 1],
                                                       op0=ALU.mult, op1=ALU.add),
                 reads=[bptr, bGS], writes=[bhT])
        else:
            S.op('act', lambda e, k=k: e.activation(out=hT[:, k, c0:c0 + 128], in_=ptr[:, k, :], func=AF.Identity,
                                                    scale=Gt[:, k, j:j + 1], bias=St[:, k, j:j + 1]),
                 reads=[bptr, bGS], writes=[bhT])
    if h32 is not None:
        xn32, bxn32 = K['xn32'].next()
        p32, bp32 = K['p32'].next()
        S.op('act', lambda e: e.activation(out=xn32[:], in_=xt[:], func=AF.Copy, scale=st[:, 2:3]),
             reads=[bx, bst], writes=[bxn32])

        def tr32(e):
            for k in range(8):
                ins = e.transpose(out=p32[:, k, :], in_=xn32[:, k * 128:(k + 1) * 128], identity=C.ident32[:])
            return ins
        S.op('pe', tr32, reads=[bxn32], writes=[bp32], self_ok=True)
        for k in range(8):
            S.op('dve', lambda e, k=k: e.tensor_scalar(out=h32[:, k, :], in0=p32[:, k, :],
                                                       scalar1=Gt[:, k, j:j + 1], scalar2=St[:, k, j:j + 1],
                                                       op0=ALU.mult, op1=ALU.add),
                 reads=[bp32, bGS], writes=[bh32])


def prenorm_kit(Ph, with32=False):
    K = {
        'junk': Ph.rot(1, [128, D], BF16, 'junk'),
        'stat': Ph.rot(4, [128, 4], F32, 'stat'),
        'xn': Ph.rot(2, [128, D], BF16, 'xn'),
        'ptr': Ph.rot(2, [128, 8, 128], BF16, 'ptr', psum=True),
    }
    if with32:
        K['xn32'] = Ph.rot(2, [128, D], F32, 'xn32')
        K['p32'] = Ph.rot(1, [128, 8, 128], F32, 'p32', psum=True)
    return K


def res_src(C, l, stage, b, tile):
    if l == 0 and stage == 0:
        if tile < 2:
            return C.ctx_in[b, tile * 128:(tile + 1) * 128, :]
        return C.x_in[b, (tile - 2) * 128:(tile - 1) * 128, :]
    return C.xs[b, tile * 128:(tile + 1) * 128, :]


def phase_mod(C, l, LP):
    nc, S = C.nc, C.S
    Ph = Phase(C, f"mod{l}")
    C.G1, C.bG1 = LP.sbb([128, 8, 3], F32, 'G1')
    C.SH1 = LP.sb([128, 8, 3], F32, 'SH1')
    C.G2, C.bG2 = LP.sbb([128, 8, 3], F32, 'G2')
    C.SH2 = LP.sb([128, 8, 3], F32, 'SH2')
    C.GG1, C.bGG1 = LP.sbb([128, 3, D], F32, 'GG1')
    C.GG2, C.bGG2 = LP.sbb([128, 3, D], F32, 'GG2')
    if getattr(C, 'want_rows', False):
        C.G2row, C.bG2row = LP.sbb([128, 2, D], F32, 'G2row')
        C.S2row = LP.sb([128, 2, D], F32, 'S2row')
    scT, bscT = Ph.sbb([128, 8, 3], F32, 'scT')
    scB, bscB = Ph.sbb([128, 3, 8, 128], F32, 'scB')
    bada, bbada = Ph.sbb([128, 48, 3], F32, 'bada')
    gpre, bgpre = Ph.sbb([128, 2, 8, 3], F32, 'gpre')
    rowc, browc = Ph.sbb([128, 7, D], F32, 'rowc')
    sc1, bsc1 = Ph.sbb([128, 8, 3], F32, 'sc1')
    sc2, bsc2 = Ph.sbb([128, 8, 3], F32, 'sc2')
    wblk = Ph.rot(2, [128, 8, 1024], F32, 'wblk')
    pm = Ph.rot(2, [128, 8, 3], F32, 'pm', psum=True)
    pg = Ph.rot(2, [128, 512], F32, 'pg', psum=True)
    S.op('sp', lambda e: e.dma_start(out=scT[:], in_=C.cT[:, :, :]), writes=[bscT], dma=bscT)
    S.op('sp', lambda e: e.dma_start(out=bada[:], in_=C.badaT3[l]), writes=[bbada], dma=bbada)
    S.op('sp', lambda e: e.dma_start(out=gpre[:], in_=C.gpre3[l]), writes=[bgpre], dma=bgpre)
    S.op('sp', lambda e: e.dma_start(out=rowc[:], in_=C.rowc[l]), writes=[browc], dma=browc)
    S.op('act', lambda e: e.activation(out=scT[:], in_=scT[:], func=AF.Silu), reads=[bscT], writes=[bscT])
    for j in range(3):
        for kc in range(8):
            S.op('act', lambda e, j=j, kc=kc: e.activation(out=scB[:, j, kc, :], in_=C.ones[:], func=AF.Copy,
                                                           scale=scT[:, kc, j:j + 1]),
                 reads=[bscT], writes=[bscB])
    fm = [(0, C.SH1, C.bG1), (1, sc1, bsc1), (3, C.SH2, C.bG2), (4, sc2, bsc2)]
    for blk, dst, bdst in fm:
        wt, bw = wblk.next()
        S.op('sp', lambda e, wt=wt, blk=blk: e.dma_start(
            out=wt[:], in_=C.w_ada[l, :, blk * 1024:(blk + 1) * 1024].rearrange("(kc p) n -> p kc n", p=128)),
            writes=[bw], dma=bw)
        pmt, bpm = pm.next()

        def mm(e, wt=wt, pmt=pmt):
            for ch in range(8):
                for kc in range(8):
                    ins = e.matmul(pmt[:, ch, :], lhsT=wt[:, kc, ch * 128:(ch + 1) * 128], rhs=scT[:, kc, :],
                                   start=(kc == 0), stop=(kc == 7))
            return ins
        S.op('pe', mm, reads=[bw, bscT], writes=[bpm], self_ok=True)
        S.op('dve', lambda e, dst=dst, pmt=pmt, blk=blk: e.tensor_tensor(
            out=dst[:], in0=pmt[:], in1=bada[:, blk * 8:(blk + 1) * 8, :], op=ALU.add),
            reads=[bpm, bbada], writes=[bdst])
    S.op('dve', lambda e: e.scalar_tensor_tensor(out=C.G1[:], in0=sc1[:], scalar=1.0, in1=gpre[:, 0], op0=ALU.add,
                                                 op1=ALU.mult), reads=[bsc1, bgpre], writes=[C.bG1])
    S.op('dve', lambda e: e.scalar_tensor_tensor(out=C.G2[:], in0=sc2[:], scalar=1.0, in1=gpre[:, 1], op0=ALU.add,
                                                 op1=ALU.mult), reads=[bsc2, bgpre], writes=[C.bG2])
    for gi, blk, GG, bGG in [(0, 2, C.GG1, C.bGG1), (1, 5, C.GG2, C.bGG2)]:
        wt, bw = wblk.next()
        S.op('sp', lambda e, wt=wt, blk=blk: e.dma_start(
            out=wt[:], in_=C.w_ada[l, :, blk * 1024:(blk + 1) * 1024].rearrange("(kc p) n -> p kc n", p=128)),
            writes=[bw], dma=bw)
        for j in range(3):
            for half in range(2):
                pgt, bpg = pg.next()
                hs = slice(half * 512, (half + 1) * 512)

                def mm(e, wt=wt, pgt=pgt, j=j, hs=hs):
                    for kc in range(8):
                        ins = e.matmul(pgt[:], lhsT=scB[:, j, kc, :], rhs=wt[:, kc, hs], start=(kc == 0),
                                       stop=(kc == 7))
                    return ins
                S.op('pe', mm, reads=[bw, bscB], writes=[bpg], self_ok=True)
                S.op('dve', lambda e, GG=GG, pgt=pgt, j=j, hs=hs, gi=gi: e.tensor_tensor(
                    out=GG[:, j, hs], in0=pgt[:], in1=rowc[:, gi, hs], op=ALU.add), reads=[bpg, browc], writes=[bGG])
                S.op('pool', lambda e, GG=GG, j=j, hs=hs, gi=gi: e.tensor_tensor(
                    out=GG[:, j, hs], in0=GG[:, j, hs], in1=rowc[:, 2 + gi, hs], op=ALU.mult),
                    reads=[bGG, browc], writes=[bGG])
    if getattr(C, 'want_rows', False):
        for blk, dst, ri in [(3, C.S2row, 4), (4, C.G2row, 5)]:
            wt, bw = wblk.next()
            S.op('sp', lambda e, wt=wt, blk=blk: e.dma_start(
                out=wt[:], in_=C.w_ada[l, :, blk * 1024:(blk + 1) * 1024].rearrange("(kc p) n -> p kc n", p=128)),
                writes=[bw], dma=bw)
            for j in range(2):
                for half in range(2):
                    pgt, bpg = pg.next()
                    hs = slice(half * 512, (half + 1) * 512)

                    def mm(e, wt=wt, pgt=pgt, j=j, hs=hs):
                        for kc in range(8):
                            ins = e.matmul(pgt[:], lhsT=scB[:, j, kc, :], rhs=wt[:, kc, hs], start=(kc == 0),
                                           stop=(kc == 7))
                        return ins
                    S.op('pe', mm, reads=[bw, bscB], writes=[bpg], self_ok=True)
                    S.op('dve', lambda e, dst=dst, pgt=pgt, j=j, hs=hs, ri=ri: e.tensor_tensor(
                        out=dst[:, j, hs], in0=pgt[:], in1=rowc[:, ri, hs], op=ALU.add), reads=[bpg, browc],
                        writes=[C.bG2row])
                    if blk == 4:
                        S.op('dve', lambda e, dst=dst, j=j, hs=hs: e.scalar_tensor_tensor(
                            out=dst[:, j, hs], in0=dst[:, j, hs], scalar=1.0, in1=rowc[:, 6, hs], op0=ALU.add,
                            op1=ALU.mult), reads=[C.bG2row, browc], writes=[C.bG2row])
    Ph.close()


def phase_proj(C, l):
    nc, S = C.nc, C.S
    Ph = Phase(C, f"proj{l}")
    K = prenorm_kit(Ph)
    win, bwin = Ph.sbb([128, 8, PROJ], BF16, 'win')
    cos, bcos = Ph.sbb([128, 32, 64], F32, 'cos')
    sin, bsin = Ph.sbb([128, 32, 64], F32, 'sin')
    npc = 4
    pw = PROJ // npc
    for i in range(npc):
        S.op('pool', lambda e, i=i: e.dma_start(
            out=win[:, :, i * pw:(i + 1) * pw],
            in_=C.w_in[l, :, i * pw:(i + 1) * pw].rearrange("(kc p) n -> p kc n", p=128)), writes=[bwin], dma=bwin)
    S.op('sp', lambda e: e.dma_start(out=cos[:], in_=C.rope_cos[:, :, :]), writes=[bcos], dma=bcos)
    S.op('sp', lambda e: e.dma_start(out=sin[:], in_=C.rope_sin[:, :, :]), writes=[bsin], dma=bsin)
    S.op('dve', lambda e: e.tensor_scalar(out=win[:, :, O_Q:O_Q + 256], in0=win[:, :, O_Q:O_Q + 256], scalar1=0.125,
                                          scalar2=None, op0=ALU.mult), reads=[bwin], writes=[bwin])
    S.op('dve', lambda e: e.tensor_scalar(out=win[:, :, O_NQ:O_NQ + 512], in0=win[:, :, O_NQ:O_NQ + 512],
                                          scalar1=0.125, scalar2=None, op0=ALU.mult), reads=[bwin], writes=[bwin])
    xr = Ph.rot(3, [128, D], F32, 'x')
    hTr = Ph.rot(2, [128, 8, 256], BF16, 'hT')
    stg = Ph.rot(2, [128, 2048], BF16, 'stg')
    fstg = Ph.rot(2, [128, 8, 256], BF16, 'fstg')
    lstg = Ph.rot(2, [16, 2, 256], F32, 'lstg')
    t1r = Ph.rot(2, [128, 512], F32, 't1')
    t2r = Ph.rot(2, [128, 512], F32, 't2')
    ptok = Ph.rot(2, [128, 512], F32, 'ptok', psum=True)
    pfe = Ph.rot(2, [128, 512], F32, 'pfe', psum=True)
    tokcols = [(O_Q, O_Q + 512), (O_V, O_V + 512), (O_R, O_R + 512), (O_NV, O_NV + 512)]
    def pre(b, g):
        j = 2 if g == 0 else b
        hT, bhT = hTr.next()
        for t in range(2):
            tile = g * 2 + t
            xt, bx = xr.next()
            S.op('sp', lambda e, xt=xt, tile=tile, b=b: e.dma_start(out=xt[:], in_=res_src(C, l, 0, b, tile)),
                 writes=[bx], dma=bx)
            prenorm_tile(Ph, K, xt, bx, hT, bhT, t * 128, C.G1, C.SH1, C.bG1, j)
        return hT, bhT

    groups = [(b, g) for b in range(NB) for g in range(LT // 256)]
    cur = pre(*groups[0])
    for gi_, (b, g) in enumerate(groups):
        if True:
            hT, bhT = cur
            if gi_ + 1 < len(groups):
                cur = pre(*groups[gi_ + 1])
            for t in range(2):
                tile = g * 2 + t
                st, bst = stg.next()
                for cb in range(4):
                    pt_, bpt = ptok.next()
                    c0, c1 = tokcols[cb]

                    def mm(e, pt_=pt_, t=t, c0=c0, c1=c1, hT=hT):
                        for kc in range(8):
                            ins = e.matmul(pt_[:], lhsT=hT[:, kc, t * 128:(t + 1) * 128], rhs=win[:, kc, c0:c1],
                                           start=(kc == 0), stop=(kc == 7))
                        return ins
                    S.op('pe', mm, reads=[bhT, bwin], writes=[bpt], self_ok=True)
                    so = st[:, cb * 512:(cb + 1) * 512]
                    if cb == 0 and g > 0:
                        lt = tile - 2
                        t1, bt1 = t1r.next()
                        t2, bt2 = t2r.next()
                        cb_ = cos[:, lt, :].unsqueeze(1).to_broadcast([128, 8, 64])
                        p3 = pt_[:].rearrange("p (a d) -> p a d", a=8)
                        S.op('dve', lambda e, t1=t1, p3=p3, cb_=cb_: e.tensor_tensor(
                            out=t1[:].rearrange("p (a d) -> p a d", a=8), in0=p3, in1=cb_, op=ALU.mult),
                            reads=[bpt, bcos], writes=[bt1])
                        p5 = pt_[:].rearrange("p (a b c d) -> p a b c d", a=8, b=2, c=2)
                        s5 = sin[:, lt, :].rearrange("p (b c d) -> p b c d", b=2, c=2)
                        t25 = t2[:].rearrange("p (a b c d) -> p a b c d", a=8, b=2, c=2)
                        for hf in range(2):
                            sb_ = s5[:, :, hf, :].unsqueeze(1).to_broadcast([128, 8, 2, 16])
                            S.op('dve', lambda e, t25=t25, p5=p5, sb_=sb_, hf=hf: e.tensor_tensor(
                                out=t25[:, :, :, hf, :], in0=p5[:, :, :, 1 - hf, :], in1=sb_, op=ALU.mult),
                                reads=[bpt, bsin], writes=[bt2])
                        S.op('pool', lambda e, so=so, t1=t1, t2=t2: e.tensor_tensor(out=so, in0=t1[:], in1=t2[:],
                                                                                   op=ALU.add),
                             reads=[bt1, bt2], writes=[bst])
                    elif cb == 2:
                        S.op('act', lambda e, so=so, pt_=pt_: e.activation(out=so, in_=pt_[:], func=AF.Silu),
                             reads=[bpt], writes=[bst])
                    else:
                        S.op('act', lambda e, so=so, pt_=pt_: e.activation(out=so, in_=pt_[:], func=AF.Copy),
                             reads=[bpt], writes=[bst])
                S.op('sp', lambda e, st=st, tile=tile, b=b: e.dma_start(
                    out=C.tokmaj[b, tile * 128:(tile + 1) * 128, :], in_=st[:]), reads=[bst], dma=bst)
            ft, bft = fstg.next()
            for cc in range(8):
                pf, bpf = pfe.next()
                c0 = O_NQ + cc * 128

                def mmf(e, pf=pf, c0=c0, hT=hT):
                    for kc in range(8):
                        ins = e.matmul(pf[:, 0:256], lhsT=win[:, kc, c0:c0 + 128], rhs=hT[:, kc, :], start=(kc == 0),
                                       stop=(kc == 7))
                    return ins
                S.op('pe', mmf, reads=[bhT, bwin], writes=[bpf], self_ok=True)
                S.op('dve', lambda e, ft=ft, pf=pf, cc=cc: e.tensor_copy(out=ft[:, cc, :], in_=pf[:, 0:256]),
                     reads=[bpf], writes=[bft])
            S.op('sp', lambda e, ft=ft, g=g, b=b: e.dma_start(
                out=C.nqkT[b].rearrange("(cc p) t -> p cc t", p=128)[:, :, g * 256:(g + 1) * 256], in_=ft[:]),
                reads=[bft], dma=bft)
            lt_, blt = lstg.next()
            for d in range(2):
                pf, bpf = pfe.next()
                c0 = O_LR + 16 * d

                def mml(e, pf=pf, c0=c0, hT=hT):
                    for kc in range(8):
                        ins = e.matmul(pf[0:16, 0:256], lhsT=win[:, kc, c0:c0 + 16], rhs=hT[:, kc, :],
                                       start=(kc == 0), stop=(kc == 7))
                    return ins
                S.op('pe', mml, reads=[bhT, bwin], writes=[bpf], self_ok=True)
                S.op('dve', lambda e, lt_=lt_, pf=pf, d=d: e.tensor_copy(out=lt_[:, d, :], in_=pf[0:16, 0:256]),
                     reads=[bpf], writes=[blt])
            S.op('sp', lambda e, lt_=lt_, g=g, b=b: e.dma_start(
                out=C.lrT[b].rearrange("d r t -> r d t")[:, :, g * 256:(g + 1) * 256], in_=lt_[:]),
                reads=[blt], dma=blt)
    Ph.close()


def phase_gla(C, l, last):
    S = C.S
    Ph = Phase(C, f"gla{l}")
    tri, btri = Ph.sbb([128, 4, 128], F32, 'tri')
    wg, bwg = Ph.sbb([16, 2, 256], F32, 'wg')
    bg, bbg = Ph.sbb([1, 2, 256], F32, 'bg')
    gn, bgn = Ph.sbb([128, 512], F32, 'gn')
    S.op('sp', lambda e: e.dma_start(out=tri[:], in_=C.tri_in[:, :, :]), writes=[btri], dma=btri)
    S.op('sp', lambda e: e.dma_start(out=wg[:], in_=C.wgate[l].rearrange("d r n -> r d n")), writes=[bwg], dma=bwg)
    S.op('sp', lambda e: e.dma_start(out=bg[:], in_=C.bgate[l].rearrange("d o n -> o d n")), writes=[bbg], dma=bbg)
    S.op('sp', lambda e: e.dma_start(out=gn[:], in_=C.gnormB[l]), writes=[bgn], dma=bgn)
    lrr = Ph.rot(1, [16, 2, LT], F32, 'lr')
    ost = Ph.sb([128, NT, 512], F32, 'ost')
    bost = [S.buf(f"ost{i}") for i in range(NT)]
    Sst = [Ph.sbb([128, 2, 128], F32, 'Sst') for _ in range(2)]
    Sbf = [Ph.sbb([128, 2, 128], BF16, 'Sbf') for _ in range(2)]
    qkvr = Ph.rot(4, [128, 1024], BF16, 'qkv')
    rgr = Ph.rot(3, [128, 512], BF16, 'rg')
    e1r = Ph.rot(2, [128, 256], F32, 'e1')
    spr = Ph.rot(2, [128, 256], F32, 'sp')
    ebr = Ph.rot(2, [128, 2, 128], F32, 'eb')
    enbr = Ph.rot(2, [128, 2, 128], F32, 'enb')
    eEr = Ph.rot(2, [128, 256], F32, 'eE')
    qdr = Ph.rot(2, [128, 4, 128], BF16, 'qd')
    kdr = Ph.rot(2, [128, 4, 128], BF16, 'kd')
    for rr in (qdr, kdr):
        for t_, b_ in zip(rr.t, rr.b):
            S.op('pool', lambda e, t_=t_: e.memset(t_[:], 0.0), writes=[b_])
    ker = Ph.rot(2, [128, 256], BF16, 'kend')
    Amr = Ph.rot(2, [128, 4, 128], BF16, 'Am')
    osr = Ph.rot(2, [128, 512], F32, 'osum')
    sqr = Ph.rot(2, [128, 512], F32, 'sq')
    ogr = Ph.rot(2, [128, 512], BF16, 'og')
    str_ = Ph.rot(4, [128, 12], F32, 'gst')
    plr = Ph.rot(1, [128, 512], F32, 'pl', psum=True)
    pber = Ph.rot(1, [128, 512], F32, 'pbe', psum=True)
    pTr = Ph.rot(1, [128, 8, 128], BF16, 'pT', psum=True)
    pAr = Ph.rot(2, [128, 4, 128], F32, 'pA', psum=True)
    por = Ph.rot(2, [128, 4, 128], F32, 'po', psum=True)
    pdsr = Ph.rot(1, [128, 2, 256], F32, 'pds', psum=True)

    import os
    STG = int(os.environ.get('GLA_STG', '99'))
    NTL = int(os.environ.get('GLA_NT', str(NT)))

    def block(b, d, tile, first, lr):
        lrt, blr = lr
        rows_of = lambda hp: slice(hp * 64, hp * 64 + 64)
        r0 = tile * 128
        qkv, bqkv = qkvr.next()
        S.op('sp', lambda e: e.dma_start(out=qkv[:], in_=C.tokmaj[b, r0:r0 + 128, 0:1024]), writes=[bqkv], dma=bqkv)
        if (not first) and not (last and tile < 2):
            rg, brg = rgr.next()
            S.op('sp', lambda e: e.dma_start(out=rg[:], in_=C.tokmaj[b, r0:r0 + 128, 1024:1536]), writes=[brg],
                 dma=brg)
        pl, bpl = plr.next()

        def mml(e):
            e.matmul(pl[:, 0:256], lhsT=lrt[:, d, r0:r0 + 128], rhs=wg[:, d, :], start=True, stop=False)
            return e.matmul(pl[:, 0:256], lhsT=C.ones[0:1, :], rhs=bg[0:1, d, :], start=False, stop=True)
        S.op('pe', mml, reads=[blr, bwg, bbg], writes=[bpl], self_ok=True)
        yield
        e1, be1 = e1r.next()
        sp_, bsp = spr.next()
        S.op('act', lambda e: e.activation(out=e1[:], in_=pl[:, 0:256], func=AF.Exp, scale=-1.0), reads=[bpl],
             writes=[be1])
        S.op('act', lambda e: e.activation(out=sp_[:], in_=e1[:], func=AF.Ln, bias=1.0), reads=[be1], writes=[bsp])
        yield
        Rm = tri[:, 0 if d == 0 else 1, :]
        Um = tri[:, 3 if d == 0 else 2, :]
        pbe, bpbe = pber.next()

        def mmb(e):
            for g in range(2):
                e.matmul(pbe[:, g * 128:(g + 1) * 128], lhsT=sp_[:, g * 128:(g + 1) * 128], rhs=Rm, start=True,
                         stop=True)
            return e.matmul(pbe[:, 256:512], lhsT=Um, rhs=sp_[:], start=True, stop=True)
        S.op('pe', mmb, reads=[bsp, btri], writes=[bpbe], self_ok=True)
        yield
        eb, beb = ebr.next()
        enb, benb = enbr.next()
        eE, beE = eEr.next()
        pb3 = pbe[:, 0:256].rearrange("p (g t) -> p g t", g=2)
        S.op('act', lambda e: e.activation(out=eb[:], in_=pb3, func=AF.Exp, scale=-1.0 / 16), reads=[bpbe],
             writes=[beb])
        S.op('act', lambda e: e.activation(out=enb[:], in_=pb3, func=AF.Exp, scale=1.0 / 16), reads=[bpbe],
             writes=[benb])
        S.op('act', lambda e: e.activation(out=eE[:], in_=pbe[:, 256:512], func=AF.Exp, scale=-1.0 / 16),
             reads=[bpbe], writes=[beE])
        yield
        pT, bpT = pTr.next()

        def tr(e):
            for i in range(4):
                ins = e.transpose(out=pT[:, i, :], in_=qkv[:, i * 128:(i + 1) * 128], identity=C.ident[:])
            return ins
        S.op('pe', tr, reads=[bqkv], writes=[bpT], self_ok=True)
        yield
        qd, bqd = qdr.next()
        kd, bkd = kdr.next()
        kend, bke = ker.next()
        for h in range(4):
            g, rs = h // 2, rows_of(h % 2)
            S.op('dve', lambda e, h=h, g=g, rs=rs: e.tensor_tensor(out=qd[rs, h, :], in0=pT[rs, g, :], in1=eb[rs, g, :],
                                                                   op=ALU.mult), reads=[bpT, beb], writes=[bqd])
            S.op('dve', lambda e, h=h, g=g, rs=rs: e.tensor_tensor(out=kd[rs, h, :], in0=pT[rs, 2 + g, :],
                                                                   in1=enb[rs, g, :], op=ALU.mult),
                 reads=[bpT, benb], writes=[bkd])
        S.op('pool', lambda e: e.tensor_tensor(out=kend[:], in0=qkv[:, 256:512], in1=eE[:], op=ALU.mult),
             reads=[bqkv, beE], writes=[bke])
        yield
        pA, bpA = pAr.next()

        def mmA(e):
            for h in range(4):
                ins = e.matmul(pA[:, h, :], lhsT=kd[:, h, :], rhs=qd[:, h, :], start=True, stop=True)
            return ins
        S.op('pe', mmA, reads=[bkd, bqd], writes=[bpA], self_ok=True)
        yield
        Am, bAm = Amr.next()
        mask = tri[:, 0 if d == 0 else 1, :].unsqueeze(1).to_broadcast([128, 4, 128])
        S.op('dve', lambda e: e.tensor_tensor(out=Am[:], in0=pA[:], in1=mask, op=ALU.mult), reads=[bpA, btri],
             writes=[bAm])
        yield
        po, bpo = por.next()
        sbf, bsbf = Sbf[d]

        def mmo(e):
            for h in range(4):
                g, rs = h // 2, rows_of(h % 2)
                e.matmul(po[:, h, :], lhsT=Am[:, h, :], rhs=qkv[:, 512 + h * 128:512 + (h + 1) * 128], start=True,
                         stop=False)
                ins = e.matmul(po[:, h, :], lhsT=qd[:, h, :], rhs=sbf[:, g, :], start=False, stop=True)
            return ins
        S.op('pe', mmo, reads=[bAm, bqkv, bqd, bsbf], writes=[bpo], self_ok=True)
        need_out = not (last and tile < 2)
        yield
        if need_out:
            if first:
                S.op('act', lambda e: e.activation(out=ost[:, tile, :], in_=po[:].rearrange("p h v -> p (h v)"),
                                                   func=AF.Copy), reads=[bpo], writes=[bost[tile]])
            else:
                osum, bos = osr.next()
                sq, bsq = sqr.next()
                og, bog = ogr.next()
                st, bst = str_.next()
                S.op('dve', lambda e: e.tensor_tensor(out=osum[:], in0=po[:].rearrange("p h v -> p (h v)"),
                                                      in1=ost[:, tile, :], op=ALU.add), reads=[bpo, bost[tile]],
                     writes=[bos])
                S.op('pool', lambda e: e.tensor_tensor(out=sq[:], in0=osum[:], in1=osum[:], op=ALU.mult), reads=[bos],
                     writes=[bsq])
                S.op('dve', lambda e: e.tensor_reduce(out=st[:, 0:4], in_=sq[:].rearrange("p (h v) -> p h v", h=4),
                                                      axis=AX.X, op=ALU.add), reads=[bsq], writes=[bst])
                S.op('dve', lambda e: e.tensor_scalar(out=st[:, 4:8], in0=st[:, 0:4], scalar1=1.0 / 128, scalar2=EPS,
                                                      op0=ALU.mult, op1=ALU.add), reads=[bst], writes=[bst])
                S.op('pool', lambda e: e.tensor_tensor(out=st[:, 8:12], in0=st[:, 4:8], in1=C.neghalf[:, 0:4],
                                                       op=ALU.pow), reads=[bst], writes=[bst])
                S.op('dve', lambda e: e.tensor_tensor(
                    out=sq[:].rearrange("p (h v) -> p h v", h=4), in0=osum[:].rearrange("p (h v) -> p h v", h=4),
                    in1=st[:, 8:12].unsqueeze(2).to_broadcast([128, 4, 128]), op=ALU.mult), reads=[bos, bst],
                    writes=[bsq])
                S.op('pool', lambda e: e.tensor_tensor(out=sq[:], in0=sq[:], in1=gn[:], op=ALU.mult), reads=[bsq, bgn],
                     writes=[bsq])
                S.op('pool', lambda e: e.tensor_tensor(out=og[:], in0=sq[:], in1=rg[:], op=ALU.mult),
                     reads=[bsq, brg], writes=[bog])
                S.op('pool', lambda e: e.dma_start(out=C.cat[b, r0:r0 + 128, 0:512], in_=og[:]), reads=[bog], dma=bog)
        yield
        pds, bpds = pdsr.next()

        def mmds(e):
            for g in range(2):
                ins = e.matmul(pds[:, g, :], lhsT=kend[:, g * 128:(g + 1) * 128],
                               rhs=qkv[:, 512 + g * 256:512 + (g + 1) * 256], start=True, stop=True)
            return ins
        S.op('pe', mmds, reads=[bke, bqkv], writes=[bpds], self_ok=True)
        yield
        sst, bsst = Sst[d]
        dc = 127 if d == 0 else 0
        for g in range(2):
            for hp in range(2):
                rs = rows_of(hp)
                S.op('dve', lambda e, g=g, hp=hp, rs=rs: e.scalar_tensor_tensor(
                    out=sst[rs, g, :], in0=sst[rs, g, :], scalar=eb[rs, g, dc:dc + 1],
                    in1=pds[rs, g, hp * 128:(hp + 1) * 128], op0=ALU.mult, op1=ALU.add),
                    reads=[bsst, beb, bpds], writes=[bsst])
        S.op('act', lambda e: e.activation(out=sbf[:], in_=sst[:], func=AF.Copy), reads=[bsst], writes=[bsbf])

    for b in range(NB):
        lr = lrr.next()
        S.op('sp', lambda e, lr=lr, b=b: e.dma_start(out=lr[0][:], in_=C.lrT[b].rearrange("d r t -> r d t")),
             writes=[lr[1]], dma=lr[1])
        for d in range(2):
            S.op('pool', lambda e, d=d: e.memset(Sst[d][0][:], 0.0), writes=[Sst[d][1]])
            S.op('pool', lambda e, d=d: e.memset(Sbf[d][0][:], 0.0), writes=[Sbf[d][1]])
        orders = [list(range(NT)), [1, 0] + list(range(NT - 1, 1, -1))]
        done = set()
        older = None
        for i in range(NTL):
            for d in range(2):
                tile = orders[d][i]
                newer = block(b, d, tile, tile not in done, lr)
                done.add(tile)
                if not GLA_PIPE:
                    for _ in newer:
                        pass
                    continue
                ne = 0
                while ne < 6 or older is not None:
                    if ne < 6:
                        next(newer)
                        ne += 1
                    if older is not None:
                        try:
                            next(older)
                        except StopIteration:
                            older = None
                older = newer
        if older is not None:
            for _ in older:
                pass
    Ph.close()


NA_SHIFT = 0.0


def phase_na(C, l, last):
    S = C.S
    Ph = Phase(C, f"na{l}")
    kTr = Ph.rot(1, [128, 4, LT], BF16, 'kT')
    qTr = Ph.rot(2, [128, LT], BF16, 'qT')
    for t_, b_ in zip(qTr.t, qTr.b):
        S.op('pool', lambda e, t_=t_: e.memset(t_[:], 0.0), writes=[b_])
    Vr = Ph.rot(1, [128, NT, 8, 65], BF16, 'V')
    for t_, b_ in zip(Vr.t, Vr.b):
        S.op('pool', lambda e, t_=t_: e.memset(t_[:], 1.0), writes=[b_])
    vstr = Ph.rot(2, [128, 8, 512], BF16, 'vst')
    onar = Ph.rot(1, [128, NT, 512], BF16, 'ona')
    biasr = Ph.rot(1, [128, 5, 5, 128], F32, 'bias')
    ssr = Ph.rot(4, [128, 5, 128], F32, 's')
    pr = Ph.rot(4, [128, 7, 128], BF16, 'p')
    str_ = Ph.rot(8, [128, 4], F32, 'nst')
    negc, bnegc = Ph.sbb([128, 1], F32, 'negc')
    S.op('pool', lambda e: e.memset(negc[:], -NA_SHIFT), writes=[bnegc])
    psr = Ph.rot(3, [128, 8, 128], F32, 'ps', psum=True)
    po_t = Ph.ps([128, 512], F32, 'po')
    po_b = [S.buf(f"po{i}") for i in range(7)]
    po_i = [0]

    def unit(b, h, qt, kT, bkT, qT, bqT, V, bV, ona, bona, bias, bbias):
        g = h // 2
        q0 = qt * 128
        if qt >= 2:
            j = qt - 2
            ts = min(max(j - 2, 0), 27)
            pi = {0: 0, 1: 1, 30: 3, 31: 4}.get(j, 2)
            kcols = [256 + 128 * (ts + k) for k in range(5)] + [0, 128]
            vt = [2 + ts + k for k in range(5)] + [0, 1]
            nl = 5
        else:
            kcols = [0, 128]
            vt = [0, 1]
            nl = 0
        nblk = len(kcols)
        ps_, bps = psr.next()

        def mm(e):
            for kb in range(nblk):
                ins = e.matmul(ps_[:, kb, :], lhsT=kT[:, g, kcols[kb]:kcols[kb] + 128], rhs=qT[:, q0:q0 + 128],
                               start=True, stop=True)
            return ins
        S.op('pe', mm, reads=[bqT, bkT], writes=[bps], self_ok=True)
        p, bp = pr.next()
        if nl:
            s_, bs = ssr.next()
            S.op('dve', lambda e: e.tensor_tensor(out=s_[:], in0=ps_[:, 0:5, :], in1=bias[:, pi, :, :], op=ALU.add),
                 reads=[bps, bbias], writes=[bs])
            S.op('act', lambda e: e.activation(out=p[:, 0:5, :], in_=s_[:], func=AF.Exp, bias=negc[:, 0:1], scale=1.0),
                 reads=[bs, bnegc], writes=[bp])
        S.op('act', lambda e: e.activation(out=p[:, nl:nblk, :], in_=ps_[:, nl:nblk, :], func=AF.Exp,
                                           bias=negc[:, 0:1], scale=1.0), reads=[bps, bnegc], writes=[bp])
        yield
        slot = po_i[0] % 7
        po_i[0] += 1
        po = po_t[:, slot * 65:(slot + 1) * 65]
        bpo = po_b[slot]

        def mmpv(e):
            for kb in range(nblk):
                ins = e.matmul(po, lhsT=p[:, kb, :], rhs=V[:, vt[kb], h, :], start=(kb == 0), stop=(kb == nblk - 1))
            return ins
        S.op('pe', mmpv, reads=[bp, bV], writes=[bpo], self_ok=True)
        st, bst = str_.next()
        S.op('dve', lambda e: e.reciprocal(out=st[:, 0:1], in_=po[:, 64:65]), reads=[bpo], writes=[bst])
        S.op('act', lambda e: e.activation(out=ona[:, qt, h * 64:(h + 1) * 64], in_=po[:, 0:64], func=AF.Copy,
                                           scale=st[:, 0:1]), reads=[bpo, bst], writes=[bona])

    inflight = []
    for b in range(NB):
        kT, bkT = kTr.next()
        V, bV = Vr.next()
        ona, bona = onar.next()
        for g in range(4):
            S.op('sp', lambda e, kT=kT, b=b, g=g: e.dma_start(
                out=kT[:, g, :], in_=C.nqkT[b, 512 + g * 128:512 + (g + 1) * 128, :]), writes=[bkT], dma=bkT)
        for t0 in range(0, NT, 8):
            t1 = min(NT, t0 + 8)
            vs, bvs = vstr.next()
            S.op('sp', lambda e, vs=vs, b=b, t0=t0, t1=t1: e.dma_start(
                out=vs[:, 0:t1 - t0, :],
                in_=C.tokmaj[b, t0 * 128:t1 * 128, 1536:2048].rearrange("(t p) c -> p t c", p=128)),
                writes=[bvs], dma=bvs)
            S.op('pool', lambda e, vs=vs, V=V, t0=t0, t1=t1: e.tensor_copy(
                out=V[:, t0:t1, :, 0:64], in_=vs[:, 0:t1 - t0, :].rearrange("p t (h d) -> p t h d", h=8)),
                reads=[bvs], writes=[bV])
        if b == NB - 1 and getattr(C, 'moe_cast_pending', False):
            C.moe_cast_pending = False
            for src, dst in zip([C.moe_wg, C.moe_wu, C.moe_wd], C.moe_bf):
                for ex in range(NE):
                    S.op('pool', lambda e, src=src, dst=dst, ex=ex: e.dma_start(out=dst[ex], in_=src[ex]),
                         writes=[C.bmoe], dma=C.bmoe, track=False)
        for h in range(8):
            bias, bbias = biasr.next()
            S.op('sp', lambda e, bias=bias, h=h: e.dma_start(out=bias[:], in_=C.natb[l, h]), writes=[bbias], dma=bbias)
            qT, bqT = qTr.next()
            hr = slice((h % 2) * 64, (h % 2) * 64 + 64)
            S.op('sp', lambda e, qT=qT, b=b, h=h, hr=hr: e.dma_start(
                out=qT[hr, :], in_=C.nqkT[b, h * 64:(h + 1) * 64, :]), writes=[bqT], dma=bqT)
            qts = list(range(2, NT)) + ([] if last else [0, 1])
            for qt in qts:
                gen = unit(b, h, qt, kT, bkT, qT, bqT, V, bV, ona, bona, bias, bbias)
                next(gen)
                inflight.append(gen)
                if len(inflight) > NA_PIPE:
                    for _ in inflight.pop(0):
                        pass
        while inflight:
            for _ in inflight.pop(0):
                pass
        for t0 in range(2 if last else 0, NT, 8):
            t1 = min(NT, t0 + 8)
            S.op('sp', lambda e, ona=ona, b=b, t0=t0, t1=t1: e.dma_start(
                out=C.cat[b, t0 * 128:t1 * 128, 512:1024].rearrange("(t p) c -> p t c", p=128), in_=ona[:, t0:t1, :]),
                reads=[bona], dma=bona)
    Ph.close()


def phase_outproj(C, l, last):
    S = C.S
    Ph = Phase(C, f"op{l}")
    wo, bwo = Ph.sbb([128, 8, D], BF16, 'wo')
    S.op('pool', lambda e: e.dma_start(out=wo[:], in_=C.w_out[l].rearrange("(kc p) n -> p kc n", p=128)),
         writes=[bwo], dma=bwo)
    ctr = Ph.rot(2, [128, D], BF16, 'ct')
    xr = Ph.rot(2, [128, D], F32, 'x')
    cTsr = Ph.rot(2, [128, 8, 128], BF16, 'cTs')
    ttr = Ph.rot(2, [128, D], F32, 'tt')
    junkr = Ph.rot(1, [128, D], BF16, 'junk')
    str_ = Ph.rot(4, [128, 4], F32, 'ost')
    pTr = Ph.rot(2, [128, 8, 128], BF16, 'pT', psum=True)
    pyr = Ph.rot(2, [128, D], F32, 'py', psum=True)
    for b in range(NB):
        for tile in (range(2, NT) if last else range(NT)):
            j = 2 if tile < 2 else b
            r0 = tile * 128
            ct, bct = ctr.next()
            xt, bx = xr.next()
            S.op('sp', lambda e, ct=ct, b=b, r0=r0: e.dma_start(out=ct[:], in_=C.cat[b, r0:r0 + 128, :]), writes=[bct],
                 dma=bct)
            S.op('sp', lambda e, xt=xt, b=b, tile=tile: e.dma_start(out=xt[:], in_=res_src(C, l, 0, b, tile)),
                 writes=[bx], dma=bx)
            pT, bpT = pTr.next()

            def tr(e, pT=pT, ct=ct):
                for k in range(8):
                    ins = e.transpose(out=pT[:, k, :], in_=ct[:, k * 128:(k + 1) * 128], identity=C.ident[:])
                return ins
            S.op('pe', tr, reads=[bct], writes=[bpT], self_ok=True)
            cTs, bcTs = cTsr.next()
            S.op('dve', lambda e, cTs=cTs, pT=pT: e.tensor_copy(out=cTs[:, 0:4, :], in_=pT[:, 0:4, :]), reads=[bpT],
                 writes=[bcTs])
            S.op('act', lambda e, cTs=cTs, pT=pT: e.activation(out=cTs[:, 4:8, :], in_=pT[:, 4:8, :], func=AF.Copy),
                 reads=[bpT], writes=[bcTs])
            py, bpy = pyr.next()

            def mm(e, py=py, cTs=cTs):
                for half in range(2):
                    for kc in range(8):
                        ins = e.matmul(py[:, half * 512:(half + 1) * 512], lhsT=cTs[:, kc, :],
                                       rhs=wo[:, kc, half * 512:(half + 1) * 512], start=(kc == 0), stop=(kc == 7))
                return ins
            S.op('pe', mm, reads=[bcTs, bwo], writes=[bpy], self_ok=True)
            post_norm_res(Ph, py[:], bpy, xt, bx, C.GG1, C.bGG1, j, junkr, str_, ttr,
                          C.xs[b, r0:r0 + 128, :])
    Ph.close()


def post_norm_res(Ph, y, by, xt, bx, GG, bGG, j, junkr, str_, ttr, dst):
    S = Ph.S
    C = Ph.C
    junk, bj = junkr.next()
    st, bst = str_.next()
    tt, btt = ttr.next()
    S.op('act', lambda e: e.activation(out=junk[:], in_=y, func=AF.Square, accum_out=st[:, 0:1]), reads=[by],
         writes=[bj, bst])
    S.op('dve', lambda e: e.tensor_scalar(out=st[:, 1:2], in0=st[:, 0:1], scalar1=1.0 / D, scalar2=EPS, op0=ALU.mult,
                                          op1=ALU.add), reads=[bst], writes=[bst])
    S.op('pool', lambda e: e.tensor_tensor(out=st[:, 2:3], in0=st[:, 1:2], in1=C.neghalf[:, 0:1], op=ALU.pow),
         reads=[bst], writes=[bst])
    S.op('dve', lambda e: e.scalar_tensor_tensor(out=tt[:], in0=y, scalar=st[:, 2:3], in1=GG[:, j, :], op0=ALU.mult,
                                                 op1=ALU.mult), reads=[by, bst, bGG], writes=[btt])
    S.op('pool', lambda e: e.tensor_tensor(out=tt[:], in0=tt[:], in1=xt[:], op=ALU.add), reads=[btt, bx],
         writes=[btt])
    S.op('pool', lambda e: e.dma_start(out=dst, in_=tt[:]), reads=[btt], dma=btt)


def phase_ffn_pre(C, l, moe, tiles):
    S = C.S
    Ph = Phase(C, f"fpre{l}")
    K = prenorm_kit(Ph, with32=moe)
    xr = Ph.rot(3, [128, D], F32, 'x')
    hTr = Ph.rot(2, [128, 8, 512], BF16, 'hT')
    if moe:
        wr, bwr = Ph.sbb([128, 8, NE], F32, 'wr')
        S.op('sp', lambda e: e.dma_start(out=wr[:], in_=C.moe_wr.rearrange("(kc p) n -> p kc n", p=128)),
             writes=[bwr], dma=bwr)
        h32r = Ph.rot(2, [128, 8, 128], F32, 'h32')
        plg = Ph.rot(1, [128, 512], F32, 'plg', psum=True)
        cmbr = Ph.rot(2, [128, 4, NE], F32, 'cmb')
        rsr = Ph.rot(4, [128, 48], F32, 'rst')
    for gi in range(len(tiles) // 4):
        hT, bhT = hTr.next()
        if moe:
            cmb, bcmb = cmbr.next()
        for t in range(4):
            b, tile = tiles[gi * 4 + t]
            j = 2 if tile < 2 else b
            xt, bx = xr.next()
            S.op('sp', lambda e, xt=xt, b=b, tile=tile: e.dma_start(out=xt[:], in_=res_src(C, l, 1, b, tile)),
                 writes=[bx], dma=bx)
            if not moe:
                prenorm_tile(Ph, K, xt, bx, hT, bhT, t * 128, C.G2, C.SH2, C.bG2, j)
                continue
            h32, bh32 = h32r.next()
            prenorm_tile(Ph, K, xt, bx, hT, bhT, t * 128, C.G2, C.SH2, C.bG2, j, h32, bh32)
            pl, bpl = plg.next()

            def mm(e, pl=pl, h32=h32):
                for kc in range(8):
                    ins = e.matmul(pl[:, 0:NE], lhsT=h32[:, kc, :], rhs=wr[:, kc, :], start=(kc == 0), stop=(kc == 7))
                return ins
            S.op('pe', mm, reads=[bh32, bwr], writes=[bpl], self_ok=True)
            st, bst = rsr.next()
            ops = [
                lambda e, st=st, pl=pl: e.tensor_copy(out=st[:, 0:8], in_=pl[:, 0:NE]),
                lambda e, st=st: e.reduce_max(out=st[:, 8:9], in_=st[:, 0:8], axis=AX.X),
                lambda e, st=st: e.tensor_scalar(out=st[:, 16:24], in0=st[:, 0:8], scalar1=st[:, 8:9], scalar2=-1e30,
                                                 op0=ALU.is_equal, op1=ALU.mult),
                lambda e, st=st: e.tensor_tensor(out=st[:, 16:24], in0=st[:, 16:24], in1=st[:, 0:8], op=ALU.add),
                lambda e, st=st: e.reduce_max(out=st[:, 9:10], in_=st[:, 16:24], axis=AX.X),
                lambda e, st=st: e.tensor_scalar(out=st[:, 24:32], in0=st[:, 0:8], scalar1=st[:, 9:10], scalar2=None,
                                                 op0=ALU.is_ge),
                lambda e, st=st: e.tensor_scalar(out=st[:, 10:11], in0=st[:, 8:9], scalar1=-1.0, scalar2=None,
                                                 op0=ALU.mult),
            ]
            for i, f_ in enumerate(ops):
                S.op('dve', f_, reads=[bst] + ([bpl] if i == 0 else []), writes=[bst])
            S.op('act', lambda e, st=st: e.activation(out=st[:, 32:40], in_=st[:, 0:8], func=AF.Exp, bias=st[:, 10:11],
                                                      scale=1.0), reads=[bst], writes=[bst])
            ops2 = [
                lambda e, st=st: e.tensor_tensor(out=st[:, 32:40], in0=st[:, 32:40], in1=st[:, 24:32], op=ALU.mult),
                lambda e, st=st: e.reduce_sum(out=st[:, 11:12], in_=st[:, 32:40], axis=AX.X),
                lambda e, st=st: e.reciprocal(out=st[:, 12:13], in_=st[:, 11:12]),
            ]
            for f_ in ops2:
                S.op('dve', f_, reads=[bst], writes=[bst])
            S.op('dve', lambda e, st=st, cmb=cmb, t=t: e.tensor_scalar(out=cmb[:, t, :], in0=st[:, 32:40],
                                                                       scalar1=st[:, 12:13], scalar2=None,
                                                                       op0=ALU.mult), reads=[bst], writes=[bcmb])
        S.op('sp', lambda e, hT=hT, gi=gi: e.dma_start(out=C.h2T[gi], in_=hT[:]), reads=[bhT], dma=bhT)
        if moe:
            S.op('sp', lambda e, cmb=cmb, gi=gi: e.dma_start(out=C.comb[gi], in_=cmb[:]), reads=[bcmb], dma=bcmb)
    Ph.close()


def phase_ffn(C, l, moe, tiles):
    S = C.S
    Ph = Phase(C, f"ffn{l}")
    E = NE if moe else 1
    F = F_MOE if moe else F_FFN
    NFC = F // 128
    NFB = F // 256
    wsrc = C.moe_bf if moe else C.ffn_bf
    bwg_ = C.bmoe if moe else C.bffn
    hTr = Ph.rot(2, [128, 8, 512], BF16, 'hT')
    hid, bhid = Ph.sbb([128, NFC, 512], BF16, 'hid')
    wd, bwd = Ph.sbb([128, NFC, D], BF16, 'wd')
    wgr = Ph.rot(3, [128, 8, 256], BF16, 'wg')
    wur = Ph.rot(3, [128, 8, 256], BF16, 'wu')
    yacc, byacc = Ph.sbb([128, 4, D], F32, 'yacc')
    sgr = Ph.rot(2, [128, 512], F32, 'sg')
    xr = Ph.rot(2, [128, D], F32, 'x')
    ttr = Ph.rot(2, [128, D], F32, 'tt')
    junkr = Ph.rot(1, [128, D], BF16, 'junk')
    str_ = Ph.rot(4, [128, 4], F32, 'fst')
    cmbr = Ph.rot(2, [128, 4, NE], F32, 'cmb')
    pgr = Ph.rot(2, [128, 512], F32, 'pg', psum=True)
    pur = Ph.rot(2, [128, 512], F32, 'pu', psum=True)
    pyr = Ph.rot(2, [128, 512], F32, 'py', psum=True)
    for gi in range(len(tiles) // 4):
        hT, bhT = hTr.next()
        S.op('sp', lambda e, hT=hT, gi=gi: e.dma_start(out=hT[:], in_=C.h2T[gi]), writes=[bhT], dma=bhT)
        if moe:
            cmb, bcmb = cmbr.next()
            S.op('sp', lambda e, cmb=cmb, gi=gi: e.dma_start(out=cmb[:], in_=C.comb[gi]), writes=[bcmb], dma=bcmb)
        for ex in range(E):
            hh = NFC // 2
            for (a0, a1) in ([(0, hh), (hh, NFC)] if (moe or gi == 0) else []):
                S.op('pool', lambda e, ex=ex, a0=a0, a1=a1: e.dma_start(
                    out=wd[:, a0:a1, :],
                    in_=wsrc[2][ex, a0 * 128:a1 * 128, :].rearrange("(fc p) n -> p fc n", p=128)),
                    reads=[bwg_], writes=[bwd], dma=bwd)
            for fb in range(NFB):
                wg, bwg = wgr.next()
                wu, bwu = wur.next()
                S.op('sp', lambda e, wg=wg, ex=ex, fb=fb: e.dma_start(
                    out=wg[:], in_=wsrc[0][ex, :, fb * 256:(fb + 1) * 256].rearrange("(kc p) f -> p kc f", p=128)),
                    reads=[bwg_], writes=[bwg], dma=bwg)
                S.op('sp', lambda e, wu=wu, ex=ex, fb=fb: e.dma_start(
                    out=wu[:], in_=wsrc[1][ex, :, fb * 256:(fb + 1) * 256].rearrange("(kc p) f -> p kc f", p=128)),
                    reads=[bwg_], writes=[bwu], dma=bwu)
                for fi in range(2):
                    fc = fb * 2 + fi
                    pg_, bpg = pgr.next()
                    pu_, bpu = pur.next()

                    def mmg(e, pg_=pg_, wg=wg, fi=fi, hT=hT):
                        for kc in range(8):
                            ins = e.matmul(pg_[:], lhsT=wg[:, kc, fi * 128:(fi + 1) * 128], rhs=hT[:, kc, :],
                                           start=(kc == 0), stop=(kc == 7))
                        return ins

                    def mmu(e, pu_=pu_, wu=wu, fi=fi, hT=hT):
                        for kc in range(8):
                            ins = e.matmul(pu_[:], lhsT=wu[:, kc, fi * 128:(fi + 1) * 128], rhs=hT[:, kc, :],
                                           start=(kc == 0), stop=(kc == 7))
                        return ins
                    S.op('pe', mmg, reads=[bwg, bhT], writes=[bpg], self_ok=True)
                    S.op('pe', mmu, reads=[bwu, bhT], writes=[bpu], self_ok=True)
                    sg, bsg = sgr.next()
                    S.op('act', lambda e, sg=sg, pg_=pg_: e.activation(out=sg[:], in_=pg_[:], func=AF.Silu),
                         reads=[bpg], writes=[bsg])
                    S.op('dve', lambda e, sg=sg, pu_=pu_, fc=fc: e.tensor_tensor(out=hid[:, fc, :], in0=pu_[:],
                                                                                 in1=sg[:], op=ALU.mult),
                         reads=[bsg, bpu], writes=[bhid])
            for t in range(4):
                for half in range(2):
                    py_, bpy = pyr.next()
                    hs = slice(half * 512, (half + 1) * 512)

                    def mmd(e, py_=py_, t=t, hs=hs):
                        for fc in range(NFC):
                            ins = e.matmul(py_[:], lhsT=hid[:, fc, t * 128:(t + 1) * 128], rhs=wd[:, fc, hs],
                                           start=(fc == 0), stop=(fc == NFC - 1))
                        return ins
                    S.op('pe', mmd, reads=[bhid, bwd], writes=[bpy], self_ok=True)
                    if not moe:
                        S.op('act', lambda e, py_=py_, t=t, hs=hs: e.activation(out=yacc[:, t, hs], in_=py_[:],
                                                                                func=AF.Copy),
                             reads=[bpy], writes=[byacc])
                    elif ex == 0:
                        S.op('act', lambda e, py_=py_, t=t, hs=hs, cmb=cmb: e.activation(
                            out=yacc[:, t, hs], in_=py_[:], func=AF.Copy, scale=cmb[:, t, 0:1]),
                            reads=[bpy, bcmb], writes=[byacc])
                    else:
                        S.op('dve', lambda e, py_=py_, t=t, hs=hs, cmb=cmb, ex=ex: e.scalar_tensor_tensor(
                            out=yacc[:, t, hs], in0=py_[:], scalar=cmb[:, t, ex:ex + 1], in1=yacc[:, t, hs],
                            op0=ALU.mult, op1=ALU.add), reads=[bpy, bcmb, byacc], writes=[byacc])
        for t in range(4):
            b, tile = tiles[gi * 4 + t]
            j = 2 if tile < 2 else b
            xt, bx = xr.next()
            S.op('sp', lambda e, xt=xt, b=b, tile=tile: e.dma_start(out=xt[:], in_=res_src(C, l, 1, b, tile)),
                 writes=[bx], dma=bx)
            if moe:
                dst = C.y_out[b, (tile - 2) * 128:(tile - 1) * 128, :]
            else:
                dst = C.xs[b, tile * 128:(tile + 1) * 128, :]

            post_norm_res(Ph, yacc[:, t, :], byacc, xt, bx, C.GG2, C.bGG2, j, junkr, str_, ttr, dst)
    Ph.close()


U32 = mybir.dt.uint32
I32 = mybir.dt.int32
NTOK = NB * LLAT
CAPE = NTOK
GS = 512
NGRP = CAPE // GS


def phase_moe_pre(C, l, tiles):
    S = C.S
    Ph = Phase(C, f"mpre{l}")
    xr = Ph.rot(3, [128, D], F32, 'x')
    junkr = Ph.rot(1, [128, D], BF16, 'junk')
    h32r = Ph.rot(2, [128, D], F32, 'h32')
    hbr = Ph.rot(3, [128, D], BF16, 'hb')
    hTr = Ph.rot(2, [128, 8, 128], F32, 'hT32')
    str_ = Ph.rot(4, [128, 4], F32, 'pst')
    rsr = Ph.rot(4, [128, 80], F32, 'rst')
    selr = Ph.rot(2, [128, NE], BF16, 'selb')
    recr = Ph.rot(8, [128, 4], U32, 'rec')
    slur = Ph.rot(4, [128, 2], U32, 'slu')
    wr, bwr = Ph.sbb([128, 8, NE], F32, 'wr')
    base, bbase = Ph.sbb([128, NE], F32, 'base')
    nid, bnid = Ph.sbb([128, 64], F32, 'nid')
    eoff, beoff = Ph.sbb([128, NE], F32, 'eoff')
    thr, bthr = Ph.sbb([128, NGRP], F32, 'thr')
    trif, btrif = Ph.sbb([128, 128], F32, 'trif')
    trib, btrib = Ph.sbb([128, 128], BF16, 'trib')
    oneb, boneb = Ph.sbb([128, 128], BF16, 'oneb')
    flf, bflf = Ph.sbb([128, NE, NGRP], F32, 'flf')
    fli, bfli = Ph.sbb([128, NE, NGRP], I32, 'fli')
    p32r = Ph.rot(2, [128, 8, 128], F32, 'p32', psum=True)
    plg = Ph.rot(2, [128, 512], F32, 'plg', psum=True)
    blst = S.buf('lst')
    S.op('sp', lambda e: e.dma_start(out=C.lst[:, :], in_=C.lst_init[:, :]), writes=[blst], dma=blst)
    S.op('sp', lambda e: e.dma_start(out=wr[:], in_=C.moe_wr.rearrange("(kc p) n -> p kc n", p=128)), writes=[bwr],
         dma=bwr)
    S.op('sp', lambda e: e.dma_start(out=nid[:], in_=C.nidf[:, :]), writes=[bnid], dma=bnid)
    S.op('sp', lambda e: e.dma_start(out=eoff[:], in_=C.eoff[:, :]), writes=[beoff], dma=beoff)
    S.op('sp', lambda e: e.dma_start(out=thr[:], in_=C.thr[:, :]), writes=[bthr], dma=bthr)
    S.op('sp', lambda e: e.dma_start(out=trif[:], in_=C.tri_in[:, 2, :]), writes=[btrif], dma=btrif)
    S.op('dve', lambda e: e.tensor_copy(out=trib[:], in_=trif[:]), reads=[btrif], writes=[btrib])
    S.op('pool', lambda e: e.memset(oneb[:], 1.0), writes=[boneb])
    S.op('pool', lambda e: e.memset(base[:], 0.0), writes=[bbase])
    for k, (b, tile) in enumerate(tiles):
        xt, bx = xr.next()
        S.op('sp', lambda e, xt=xt, b=b, tile=tile: e.dma_start(out=xt[:], in_=res_src(C, l, 1, b, tile)), writes=[bx],
             dma=bx)
        junk, bj = junkr.next()
        st, bst = str_.next()
        h32, bh32 = h32r.next()
        hb, bhb = hbr.next()
        S.op('act', lambda e, junk=junk, xt=xt, st=st: e.activation(out=junk[:], in_=xt[:], func=AF.Square,
                                                                     accum_out=st[:, 0:1]), reads=[bx], writes=[bj, bst])
        S.op('dve', lambda e, st=st: e.tensor_scalar(out=st[:, 1:2], in0=st[:, 0:1], scalar1=1.0 / D, scalar2=EPS,
                                                     op0=ALU.mult, op1=ALU.add), reads=[bst], writes=[bst])
        S.op('pool', lambda e, st=st: e.tensor_tensor(out=st[:, 2:3], in0=st[:, 1:2], in1=C.neghalf[:, 0:1],
                                                      op=ALU.pow), reads=[bst], writes=[bst])
        S.op('dve', lambda e, h32=h32, xt=xt, st=st, b=b: e.scalar_tensor_tensor(
            out=h32[:], in0=xt[:], scalar=st[:, 2:3], in1=C.G2row[:, b, :], op0=ALU.mult, op1=ALU.mult),
            reads=[bx, bst, C.bG2row], writes=[bh32])
        S.op('pool', lambda e, h32=h32, b=b: e.tensor_tensor(out=h32[:], in0=h32[:], in1=C.S2row[:, b, :], op=ALU.add),
             reads=[bh32, C.bG2row], writes=[bh32])
        S.op('act', lambda e, hb=hb, h32=h32: e.activation(out=hb[:], in_=h32[:], func=AF.Copy), reads=[bh32],
             writes=[bhb])
        S.op('act', lambda e, hb=hb, k=k: e.dma_start(out=C.h2tok[k * 128:(k + 1) * 128, :], in_=hb[:]), reads=[bhb],
             dma=bhb)
        p32, bp32 = p32r.next()

        def tr32(e, p32=p32, h32=h32):
            for kc in range(8):
                ins = e.transpose(out=p32[:, kc, :], in_=h32[:, kc * 128:(kc + 1) * 128], identity=C.ident32[:])
            return ins
        S.op('pe', tr32, reads=[bh32], writes=[bp32], self_ok=True)
        hT, bhT = hTr.next()
        S.op('dve', lambda e, hT=hT, p32=p32: e.tensor_copy(out=hT[:, 0:4, :], in_=p32[:, 0:4, :]), reads=[bp32],
             writes=[bhT])
        S.op('act', lambda e, hT=hT, p32=p32: e.activation(out=hT[:, 4:8, :], in_=p32[:, 4:8, :], func=AF.Copy),
             reads=[bp32], writes=[bhT])
        pl, bpl = plg.next()

        def mm(e, pl=pl, hT=hT):
            for kc in range(8):
                ins = e.matmul(pl[:, 0:NE], lhsT=hT[:, kc, :], rhs=wr[:, kc, :], start=(kc == 0), stop=(kc == 7))
            return ins
        S.op('pe', mm, reads=[bhT, bwr], writes=[bpl], self_ok=True)
        r, br = rsr.next()
        LG, M1, M2, NM1, DEN, RDEN = r[:, 0:8], r[:, 8:9], r[:, 9:10], r[:, 10:11], r[:, 11:12], r[:, 12:13]
        EQ1, SEL, WN, MSK, OH1, SV, TMP = r[:, 16:24], r[:, 24:32], r[:, 32:40], r[:, 40:48], r[:, 48:56], r[:, 56:64], \
            r[:, 64:72]
        SL0, SL1, W0, W1, D1 = r[:, 72:73], r[:, 73:74], r[:, 74:75], r[:, 75:76], r[:, 76:77]
        selb, bselb = selr.next()
        dv = lambda f_, extra=(): S.op('dve', f_, reads=[br] + list(extra), writes=[br])
        dv(lambda e, LG=LG, pl=pl: e.tensor_copy(out=LG, in_=pl[:, 0:NE]), [bpl])
        dv(lambda e, LG=LG, M1=M1: e.reduce_max(out=M1, in_=LG, axis=AX.X))
        dv(lambda e, EQ1=EQ1, LG=LG, M1=M1: e.tensor_scalar(out=EQ1, in0=LG, scalar1=M1, scalar2=None,
                                                            op0=ALU.is_equal))
        dv(lambda e, MSK=MSK, EQ1=EQ1, LG=LG: e.scalar_tensor_tensor(out=MSK, in0=EQ1, scalar=-1e30, in1=LG,
                                                                     op0=ALU.mult, op1=ALU.add))
        dv(lambda e, MSK=MSK, M2=M2: e.reduce_max(out=M2, in_=MSK, axis=AX.X))
        dv(lambda e, SEL=SEL, LG=LG, M2=M2: e.tensor_scalar(out=SEL, in0=LG, scalar1=M2, scalar2=None, op0=ALU.is_ge))
        dv(lambda e, NM1=NM1, M1=M1: e.tensor_scalar(out=NM1, in0=M1, scalar1=-1.0, scalar2=None, op0=ALU.mult))
        S.op('act', lambda e, WN=WN, LG=LG, NM1=NM1: e.activation(out=WN, in_=LG, func=AF.Exp, bias=NM1, scale=1.0),
             reads=[br], writes=[br])
        dv(lambda e, WN=WN, SEL=SEL: e.tensor_tensor(out=WN, in0=WN, in1=SEL, op=ALU.mult))
        dv(lambda e, WN=WN, DEN=DEN: e.reduce_sum(out=DEN, in_=WN, axis=AX.X))
        dv(lambda e, DEN=DEN, RDEN=RDEN: e.reciprocal(out=RDEN, in_=DEN))
        dv(lambda e, WN=WN, RDEN=RDEN: e.tensor_scalar(out=WN, in0=WN, scalar1=RDEN, scalar2=None, op0=ALU.mult))
        dv(lambda e, OH1=OH1, SEL=SEL, EQ1=EQ1: e.tensor_tensor(out=OH1, in0=SEL, in1=EQ1, op=ALU.subtract))
        S.op('dve', lambda e, selb=selb, SEL=SEL: e.tensor_copy(out=selb[:], in_=SEL), reads=[br], writes=[bselb])
        pc, bpc = plg.next()

        def mmc(e, pc=pc, selb=selb):
            e.matmul(pc[:, 0:NE], lhsT=trib[:], rhs=selb[:], start=True, stop=True)
            return e.matmul(pc[:, 8:8 + NE], lhsT=oneb[:], rhs=selb[:], start=True, stop=True)
        S.op('pe', mmc, reads=[bselb, btrib, boneb], writes=[bpc], self_ok=True)
        dv(lambda e, SV=SV, pc=pc: e.tensor_tensor(out=SV, in0=pc[:, 0:NE], in1=base[:], op=ALU.add), [bpc, bbase])
        dv(lambda e, SV=SV: e.tensor_tensor(out=SV, in0=SV, in1=eoff[:], op=ALU.add), [beoff])
        S.op('dve', lambda e, pc=pc: e.tensor_tensor(out=base[:], in0=pc[:, 8:8 + NE], in1=base[:], op=ALU.add),
             reads=[bpc, br], writes=[bbase])
        for (OH, SL, W) in [(EQ1, SL0, W0), (OH1, SL1, W1)]:
            dv(lambda e, TMP=TMP, OH=OH, SV=SV: e.tensor_tensor(out=TMP, in0=OH, in1=SV, op=ALU.mult))
            dv(lambda e, TMP=TMP, SL=SL: e.reduce_sum(out=SL, in_=TMP, axis=AX.X))
            dv(lambda e, TMP=TMP, OH=OH, WN=WN: e.tensor_tensor(out=TMP, in0=OH, in1=WN, op=ALU.mult))
            dv(lambda e, TMP=TMP, W=W: e.reduce_sum(out=W, in_=TMP, axis=AX.X))
        dv(lambda e, D1=D1, k=k: e.tensor_scalar(out=D1, in0=nid[:, k:k + 1], scalar1=float(NTOK), scalar2=None,
                                                 op0=ALU.add), [bnid])
        slu, bslu = slur.next()
        S.op('dve', lambda e, slu=slu, SL0=SL0: e.tensor_copy(out=slu[:, 0:1], in_=SL0), reads=[br], writes=[bslu])
        S.op('dve', lambda e, slu=slu, SL1=SL1: e.tensor_copy(out=slu[:, 1:2], in_=SL1), reads=[br], writes=[bslu])
        for rk, (DD, WW) in enumerate([(None, W0), (D1, W1)]):
            rec, brec = recr.next()
            S.op('pool', lambda e, rec=rec: e.memset(rec[:], 0), writes=[brec])
            rw = lambda f_: S.op('dve', f_, reads=[br, bnid], writes=[brec])
            rw(lambda e, rec=rec, k=k: e.tensor_copy(out=rec[:, 0:1], in_=nid[:, k:k + 1]))
            if DD is None:
                rw(lambda e, rec=rec, k=k: e.tensor_copy(out=rec[:, 1:2], in_=nid[:, k:k + 1]))
            else:
                rw(lambda e, rec=rec, DD=DD: e.tensor_copy(out=rec[:, 1:2], in_=DD))
            rw(lambda e, rec=rec, WW=WW: e.tensor_copy(out=rec[:, 2:3].bitcast(F32), in_=WW))
            S.op('pool', lambda e, rec=rec, slu=slu, rk=rk: e.indirect_dma_start(
                out=C.lst[:, :], out_offset=bass.IndirectOffsetOnAxis(ap=slu[:, rk:rk + 1], axis=0),
                in_=rec[:], in_offset=None, bounds_check=S.breg(e, NE * CAPE - 1), oob_is_err=False),
                reads=[brec, bslu, blst], dma=brec)
    for ex in range(NE):
        S.op('dve', lambda e, ex=ex: e.tensor_scalar(out=flf[:, ex, :], in0=thr[:], scalar1=base[:, ex:ex + 1],
                                                     scalar2=None, op0=ALU.is_lt), reads=[bbase, bthr], writes=[bflf])
    S.op('dve', lambda e: e.tensor_copy(out=fli[:], in_=flf[:]), reads=[bflf], writes=[bfli])
    S.op('sp', lambda e: e.dma_start(out=C.flags[0:1, :], in_=fli[0:1, :, :].rearrange("p a b -> p (a b)")),
         reads=[bfli], dma=bfli)
    Ph.close()


def phase_moe_sparse(C, l):
    import os
    SPS = int(os.environ.get('SP_STG', '9'))
    S = C.S
    Ph = Phase(C, f"moe{l}")
    NFC = F_MOE // 128
    NFB = F_MOE // 256
    wsrc = C.moe_bf
    hid, bhid = Ph.sbb([128, NFC, 512], BF16, 'hid')
    wd, bwd = Ph.sbb([128, NFC, D], BF16, 'wd')
    wgr = Ph.rot(3, [128, 8, 256], BF16, 'wg')
    wur = Ph.rot(3, [128, 8, 256], BF16, 'wu')
    htr = Ph.rot(2, [128, 4, D], BF16, 'htok')
    hTr = Ph.rot(2, [128, 8, 512], BF16, 'hT')
    recr = Ph.rot(2, [128, 4, 4], U32, 'recs')
    sgr = Ph.rot(2, [128, 512], F32, 'sg')
    yscr = Ph.rot(2, [128, D], F32, 'ysc')
    ptrr = Ph.rot(2, [128, 8, 128], BF16, 'ptr', psum=True)
    pgr = Ph.rot(2, [128, 512], F32, 'pg', psum=True)
    pur = Ph.rot(1, [128, 512], F32, 'pu', psum=True)
    pyr = Ph.rot(2, [128, 512], F32, 'py', psum=True)
    for t_, b_ in zip(htr.t, htr.b):
        S.op('pool', lambda e, t_=t_: e.memset(t_[:], 0.0), writes=[b_])
    for ex in range(NE):
        hh = NFC // 2
        for (a0, a1) in [(0, hh), (hh, NFC)]:
            S.op('sp', lambda e, ex=ex, a0=a0, a1=a1: e.dma_start(
                out=wd[:, a0:a1, :], in_=wsrc[2][ex, a0 * 128:a1 * 128, :].rearrange("(fc p) n -> p fc n", p=128)),
                reads=[C.bmoe], writes=[bwd], dma=bwd)
        for g in range(NGRP):
            S.cond_begin(C.flags[0:1, ex * NGRP + g:ex * NGRP + g + 1])
            recs, brecs = recr.next()
            r0 = ex * CAPE + g * GS
            S.op('sp', lambda e, recs=recs, r0=r0: e.dma_start(
                out=recs[:], in_=C.lst[r0:r0 + GS, :].rearrange("(t p) c -> p t c", p=128)), writes=[brecs], dma=brecs)
            ht, bht = htr.next()
            for t in range(4):
                S.op('pool', lambda e, ht=ht, recs=recs, t=t: e.indirect_dma_start(
                    out=ht[:, t, :], out_offset=None, in_=C.h2tok[:, :],
                    in_offset=bass.IndirectOffsetOnAxis(ap=recs[:, t, 0:1], axis=0), bounds_check=S.breg(e, NTOK - 1),
                    oob_is_err=False), reads=[brecs], writes=[bht], dma=bht)
            hT, bhT = hTr.next()
            for t in range(4 if SPS >= 2 else 0):
                ptr, bptr = ptrr.next()

                def tr(e, ptr=ptr, ht=ht, t=t):
                    for kc in range(8):
                        ins = e.transpose(out=ptr[:, kc, :], in_=ht[:, t, kc * 128:(kc + 1) * 128], identity=C.ident[:])
                    return ins
                S.op('pe', tr, reads=[bht], writes=[bptr], self_ok=True)
                if t % 2 == 0:
                    S.op('dve', lambda e, hT=hT, ptr=ptr, t=t: e.tensor_copy(out=hT[:, :, t * 128:(t + 1) * 128],
                                                                             in_=ptr[:]), reads=[bptr], writes=[bhT])
                else:
                    S.op('act', lambda e, hT=hT, ptr=ptr, t=t: e.activation(out=hT[:, :, t * 128:(t + 1) * 128],
                                                                            in_=ptr[:], func=AF.Copy), reads=[bptr],
                         writes=[bhT])
            for fb in range(NFB if SPS >= 3 else 0):
                wg, bwg = wgr.next()
                wu, bwu = wur.next()
                S.op('sp', lambda e, wg=wg, ex=ex, fb=fb: e.dma_start(
                    out=wg[:], in_=wsrc[0][ex, :, fb * 256:(fb + 1) * 256].rearrange("(kc p) f -> p kc f", p=128)),
                    reads=[C.bmoe], writes=[bwg], dma=bwg)
                S.op('sp', lambda e, wu=wu, ex=ex, fb=fb: e.dma_start(
                    out=wu[:], in_=wsrc[1][ex, :, fb * 256:(fb + 1) * 256].rearrange("(kc p) f -> p kc f", p=128)),
                    reads=[C.bmoe], writes=[bwu], dma=bwu)
                for fi in range(2):
                    fc = fb * 2 + fi
                    pg_, bpg = pgr.next()
                    pu_, bpu = pur.next()

                    def mmg(e, pg_=pg_, wg=wg, fi=fi, hT=hT):
                        for kc in range(8):
                            ins = e.matmul(pg_[:], lhsT=wg[:, kc, fi * 128:(fi + 1) * 128], rhs=hT[:, kc, :],
                                           start=(kc == 0), stop=(kc == 7))
                        return ins

                    def mmu(e, pu_=pu_, wu=wu, fi=fi, hT=hT):
                        for kc in range(8):
                            ins = e.matmul(pu_[:], lhsT=wu[:, kc, fi * 128:(fi + 1) * 128], rhs=hT[:, kc, :],
                                           start=(kc == 0), stop=(kc == 7))
                        return ins
                    S.op('pe', mmg, reads=[bwg, bhT], writes=[bpg], self_ok=True)
                    S.op('pe', mmu, reads=[bwu, bhT], writes=[bpu], self_ok=True)
                    sg, bsg = sgr.next()
                    S.op('act', lambda e, sg=sg, pg_=pg_: e.activation(out=sg[:], in_=pg_[:], func=AF.Silu),
                         reads=[bpg], writes=[bsg])
                    S.op('dve', lambda e, sg=sg, pu_=pu_, fc=fc: e.tensor_tensor(out=hid[:, fc, :], in0=pu_[:],
                                                                                 in1=sg[:], op=ALU.mult),
                         reads=[bsg, bpu], writes=[bhid])
            for t in range(4 if SPS >= 4 else 0):
                ysc, bysc = yscr.next()
                for half in range(2):
                    py_, bpy = pyr.next()
                    hs = slice(half * 512, (half + 1) * 512)

                    def mmd(e, py_=py_, t=t, hs=hs):
                        for fc in range(NFC):
                            ins = e.matmul(py_[:], lhsT=hid[:, fc, t * 128:(t + 1) * 128], rhs=wd[:, fc, hs],
                                           start=(fc == 0), stop=(fc == NFC - 1))
                        return ins
                    S.op('pe', mmd, reads=[bhid, bwd], writes=[bpy], self_ok=True)
                    S.op('act', lambda e, py_=py_, ysc=ysc, hs=hs, recs=recs, t=t: e.activation(
                        out=ysc[:, hs], in_=py_[:], func=AF.Copy, scale=recs[:, t, 2:3].bitcast(F32)),
                        reads=[bpy, brecs], writes=[bysc])
                if SPS >= 5:
                  S.op('pool', lambda e, ysc=ysc, recs=recs, t=t: e.indirect_dma_start(
                    out=C.Ymoe[:, :], out_offset=bass.IndirectOffsetOnAxis(ap=recs[:, t, 1:2], axis=0), in_=ysc[:],
                    in_offset=None, bounds_check=S.breg(e, 2 * NTOK - 1), oob_is_err=False), reads=[bysc, brecs], dma=bysc)
            S.cond_end()
    Ph.close()


def phase_moe_post(C, l, tiles):
    S = C.S
    Ph = Phase(C, f"mpost{l}")
    xr = Ph.rot(4, [128, D], F32, 'x')
    y1r = Ph.rot(4, [128, D], F32, 'y1')
    y2r = Ph.rot(4, [128, D], F32, 'y2')
    ttr = Ph.rot(3, [128, D], F32, 'tt')
    junkr = Ph.rot(2, [128, D], BF16, 'junk')
    str_ = Ph.rot(8, [128, 4], F32, 'fst')
    for k, (b, tile) in enumerate(tiles):
        xt, bx = xr.next()
        y1, by1 = y1r.next()
        y2, by2 = y2r.next()
        S.op('sp', lambda e, xt=xt, b=b, tile=tile: e.dma_start(out=xt[:], in_=res_src(C, l, 1, b, tile)), writes=[bx],
             dma=bx)
        S.op('sp', lambda e, y1=y1, k=k: e.dma_start(out=y1[:], in_=C.Ymoe[k * 128:(k + 1) * 128, :]), writes=[by1],
             dma=by1)
        S.op('sp', lambda e, y2=y2, k=k: e.dma_start(out=y2[:], in_=C.Ymoe[NTOK + k * 128:NTOK + (k + 1) * 128, :]),
             writes=[by2], dma=by2)
        S.op('pool', lambda e, y1=y1, y2=y2: e.tensor_tensor(out=y1[:], in0=y1[:], in1=y2[:], op=ALU.add),
             reads=[by1, by2], writes=[by1])
        dst = C.y_out[b, (tile - 2) * 128:(tile - 1) * 128, :]
        post_norm_res(Ph, y1[:], by1, xt, bx, C.GG2, C.bGG2, b, junkr, str_, ttr, dst)
    Ph.close()


SPARSE_MOE = True


def build_program(debug=False, upto=None, skip=()):
    nc = bass.Bass("TRN2", target_bir_lowering=False)
    C = Ctx()
    C.nc = nc
    C.debug = debug
    L = 2
    C.x_in = _dram_in(nc, "x", [NB, LLAT, D])
    C.ctx_in = _dram_in(nc, "ctx", [NB, LCTX, D])
    C.cT = _dram_in(nc, "cT", [128, 8, 3])
    C.w_ada = _dram_in(nc, "w_ada", [L, D, 6 * D])
    C.badaT3 = _dram_in(nc, "badaT3", [L, 128, 48, 3])
    C.gpre3 = _dram_in(nc, "gpre3", [L, 128, 2, 8, 3])
    C.rowc = _dram_in(nc, "rowc", [L, 128, 7, D])
    C.w_in = _dram_in(nc, "w_in", [L, D, PROJ])
    C.rope_cos = _dram_in(nc, "rope_cos", [128, 32, 64])
    C.rope_sin = _dram_in(nc, "rope_sin", [128, 32, 64])
    C.ident_in = _dram_in(nc, "ident", [128, 128], BF16)
    C.ident32_in = _dram_in(nc, "ident32", [128, 128], F32)
    C.tri_in = _dram_in(nc, "tri", [128, 4, 128], F32)
    C.wgate = _dram_in(nc, "gla_w_gate", [L, 2, 16, 256])
    C.bgate = _dram_in(nc, "gla_b_gate", [L, 2, 1, 256])
    C.gnormB = _dram_in(nc, "gnormB", [L, 128, 512])
    C.natb = _dram_in(nc, "natb", [L, 8, 128, 5, 5, 128])
    C.w_out = _dram_in(nc, "w_out", [L, D, D])
    C.ffn_wg = _dram_in(nc, "ffn_w_gate", [1, D, F_FFN])
    C.ffn_wu = _dram_in(nc, "ffn_w_up", [1, D, F_FFN])
    C.ffn_wd = _dram_in(nc, "ffn_w_down", [1, F_FFN, D])
    C.moe_wr = _dram_in(nc, "moe_w_router", [D, NE])
    C.moe_wg = _dram_in(nc, "moe_w_gate", [NE, D, F_MOE])
    C.moe_wu = _dram_in(nc, "moe_w_up", [NE, D, F_MOE])
    C.moe_wd = _dram_in(nc, "moe_w_down", [NE, F_MOE, D])
    C.lst_init = _dram_in(nc, "lst_init", [NE * CAPE, 4], U32)
    C.nidf = _dram_in(nc, "nidf", [128, 64])
    C.eoff = _dram_in(nc, "eoff", [128, NE])
    C.thr = _dram_in(nc, "thr", [128, NGRP])
    C.lst = _dram_tmp(nc, "lst", [NE * CAPE, 4], U32)
    C.flags = _dram_tmp(nc, "flags", [1, NE * NGRP], I32)
    C.h2tok = _dram_tmp(nc, "h2tok", [NTOK, D], BF16)
    C.Ymoe = _dram_tmp(nc, "Ymoe", [2 * NTOK, D], F32)
    C.y_out = nc.dram_tensor("y", [NB, LLAT, D], F32, kind="ExternalOutput").ap()
    dbg = debug
    C.xs = _dram_tmp(nc, "xs", [NB, LT, D], F32, dbg)
    C.tokmaj = _dram_tmp(nc, "tokmaj", [NB, LT, 2048], BF16, dbg)
    C.nqkT = _dram_tmp(nc, "nqkT", [NB, 1024, LT], BF16, dbg)
    C.lrT = _dram_tmp(nc, "lrT", [NB, 2, 16, LT], F32, dbg)
    C.cat = _dram_tmp(nc, "cat", [NB, LT, D], BF16, dbg)
    C.h2T = _dram_tmp(nc, "h2T", [17, 128, 8, 512], BF16, dbg)
    C.comb = _dram_tmp(nc, "comb", [17, 128, 4, NE], F32, dbg)
    C.ffn_bf = [_dram_tmp(nc, "ffn_wg_bf", [1, D, F_FFN], BF16), _dram_tmp(nc, "ffn_wu_bf", [1, D, F_FFN], BF16),
                _dram_tmp(nc, "ffn_wd_bf", [1, F_FFN, D], BF16)]
    C.moe_bf = [_dram_tmp(nc, "moe_wg_bf", [NE, D, F_MOE], BF16), _dram_tmp(nc, "moe_wu_bf", [NE, D, F_MOE], BF16),
                _dram_tmp(nc, "moe_wd_bf", [NE, F_MOE, D], BF16)]
    if debug:
        C.dbg = nc.dram_tensor("dbg", [128, 8192], F32, kind="ExternalOutput").ap()
    with ExitStack() as gs:
        S = Sched(nc, gs)
        C.S = S
        S.bounds = [NE * CAPE - 1, NTOK - 1, 2 * NTOK - 1]
        GP = Phase(C, "glob")
        C.ident, bid = GP.sbb([128, 128], BF16, 'ident')
        C.ident32, bid32 = GP.sbb([128, 128], F32, 'ident32')
        C.ones, bones = GP.sbb([128, 128], F32, 'ones')
        C.neghalf, bnh = GP.sbb([128, 4], F32, 'neghalf')
        S.op('sp', lambda e: e.dma_start(out=C.ident[:], in_=C.ident_in[:, :]), writes=[bid], dma=bid)
        S.op('sp', lambda e: e.dma_start(out=C.ident32[:], in_=C.ident32_in[:, :]), writes=[bid32], dma=bid32)
        S.op('pool', lambda e: e.memset(C.ones[:], 1.0), writes=[bones])
        S.op('pool', lambda e: e.memset(C.neghalf[:], -0.5), writes=[bnh])
        C.bffn = Buf('ffn_bf')
        C.bmoe = Buf('moe_bf')
        if upto is None or upto >= 5:
            for src, dst in zip([C.ffn_wg, C.ffn_wu, C.ffn_wd], C.ffn_bf):
                S.op('pool', lambda e, src=src, dst=dst: e.dma_start(out=dst[0], in_=src[0]), writes=[C.bffn],
                     dma=C.bffn, track=False)
        C.moe_cast_pending = (upto is None or upto >= 6)
        S.flush()
        for l in range(L):
            LP = Phase(C, f"L{l}")
            C.want_rows = (l == L - 1) and SPARSE_MOE
            phase_mod(C, l, LP)
            if debug and l == debug - 1 and upto == 0:
                dump_mod(C)
            if upto is not None and upto == 0:
                LP.st.close()
                break
            if 1 not in skip:
                phase_proj(C, l)
            if upto is not None and upto <= 1:
                LP.st.close()
                break
            last = (l == L - 1)
            phase_gla(C, l, last)
            if upto is not None and upto <= 2:
                LP.st.close()
                break
            phase_na(C, l, last)
            if upto is not None and upto <= 3:
                LP.st.close()
                break
            phase_outproj(C, l, last)
            if upto is not None and upto <= 4:
                LP.st.close()
                break
            if last:
                tiles = [(b, t) for b in range(NB) for t in range(2, NT)]
            else:
                tiles = [(b, t) for b in range(NB) for t in range(NT)]
            if last and SPARSE_MOE:
                import os
                ms = int(os.environ.get('MOE_STOP', '9'))
                phase_moe_pre(C, l, tiles)
                if ms >= 2:
                    phase_moe_sparse(C, l)
                if ms >= 3:
                    phase_moe_post(C, l, tiles)
            else:
                phase_ffn_pre(C, l, last, tiles)
                phase_ffn(C, l, last, tiles)
            if upto is not None and upto <= 5 + l:
                LP.st.close()
                break
            LP.st.close()
        GP.st.close()
    return nc


def dump_mod(C):
    S = C.S
    Ph = Phase(C, "dump")
    o = 0
    for t, n in [(C.G1, 24), (C.SH1, 24), (C.G2, 24), (C.SH2, 24)]:
        S.op('sp', lambda e, t=t, o=o, n=n: e.dma_start(out=C.dbg[:, o:o + n], in_=t[:].rearrange("p a b -> p (a b)")),
             reads=[C.bG1, C.bG2], dma=S.buf())
        o += n
    for t in [C.GG1, C.GG2]:
        S.op('sp', lambda e, t=t, o=o: e.dma_start(out=C.dbg[:, o:o + 3072], in_=t[:].rearrange("p a b -> p (a b)")),
             reads=[C.bGG1, C.bGG2], dma=S.buf())
        o += 3072
    Ph.close()


def _na_bias_tables(rpb):
    L = rpb.shape[0]
    out = np.full((L, 8, 128, 5, 640), NEG, np.float32)
    reps = [0, 1, 10, 30, 31]
    for pi, j in enumerate(reps):
        ts = min(max(j - 2, 0), 27)
        for rq2 in range(2):
            r = 2 * j + rq2
            rs = min(max(r - 4, 0), 56)
            for cq in range(64):
                cs = min(max(cq - 8, 0), 48)
                p = rq2 * 64 + cq
                ck = np.arange(cs, cs + 16)
                for rk in range(rs, rs + 8):
                    slot = rk - 2 * ts
                    out[:, :, p, pi, slot * 64 + ck] = rpb[:, :, rk - r + 7, ck - cq + 15]
    return out


def _tri():
    i = np.arange(128)
    ut = (i[:, None] <= i[None, :]).astype(np.float32)
    lt = (i[:, None] >= i[None, :]).astype(np.float32)
    sut = (i[:, None] < i[None, :]).astype(np.float32)
    slt = (i[:, None] > i[None, :]).astype(np.float32)
    return np.stack([ut, lt, sut, slt], axis=1).copy()


def make_in_maps(inp, n_cores=8):
    import ml_dtypes
    f = lambda a: np.ascontiguousarray(np.asarray(a, dtype=np.float32))
    L = 2
    w_ada = f(inp['w_ada'])
    b_ada = f(inp['b_ada'])
    badaT3 = np.repeat(b_ada.reshape(L, 48, 128).transpose(0, 2, 1)[:, :, :, None], 3, axis=3).copy()
    gp = np.stack([f(inp['g_pre_mix']), f(inp['g_pre_ffn'])], axis=1)
    gpre3 = np.repeat(gp.reshape(L, 2, 8, 128).transpose(0, 3, 1, 2)[..., None], 3, axis=4).copy()
    rows = np.stack([b_ada[:, 2048:3072], b_ada[:, 5120:6144], f(inp['g_post_mix']), f(inp['g_post_ffn']),
                     b_ada[:, 3072:4096], b_ada[:, 4096:5120], f(inp['g_pre_ffn'])], axis=1)
    rowc = np.repeat(rows[:, None, :, :], 128, axis=1).copy()
    cos, sin = _rope_tables()
    gn = f(inp['gla_g_norm'])
    gnormB = np.repeat(np.tile(gn, (1, 4))[:, None, :], 128, axis=1).copy()
    natb = _na_bias_tables(f(inp['na_rpb']))
    natb = np.ascontiguousarray(natb.reshape(L, 8, 128, 5, 5, 128).transpose(0, 1, 5, 3, 4, 2))
    lst_init = np.zeros((NE * CAPE, 4), np.uint32)
    lst_init[:, 0:2] = 1 << 30
    nidf = (np.arange(64)[None, :] * 128 + np.arange(128)[:, None]).astype(np.float32)
    eoff = np.repeat((np.arange(NE) * CAPE).astype(np.float32)[None, :], 128, axis=0)
    thr = np.repeat((np.arange(NGRP) * GS).astype(np.float32)[None, :], 128, axis=0)
    shared = {
        "lst_init": lst_init, "nidf": nidf, "eoff": eoff, "thr": thr,
        "w_ada": w_ada, "badaT3": badaT3, "gpre3": gpre3, "rowc": rowc, "w_in": f(inp['w_in']),
        "rope_cos": cos, "rope_sin": sin, "ident": np.eye(128).astype(ml_dtypes.bfloat16),
        "ident32": np.eye(128, dtype=np.float32), "tri": _tri(),
        "gla_w_gate": f(inp['gla_w_gate']), "gla_b_gate": f(inp['gla_b_gate']).reshape(L, 2, 1, 256),
        "gnormB": gnormB, "natb": natb, "w_out": f(inp['w_out']),
        "ffn_w_gate": f(inp['ffn_w_gate']), "ffn_w_up": f(inp['ffn_w_up']), "ffn_w_down": f(inp['ffn_w_down']),
        "moe_w_router": f(inp['moe_w_router'])[0], "moe_w_gate": f(inp['moe_w_gate'])[0],
        "moe_w_up": f(inp['moe_w_up'])[0], "moe_w_down": f(inp['moe_w_down'])[0],
    }
    x = f(inp['x'])
    c = f(inp['c'])
    ctx = f(inp['ctx'])
    c_ctx = f(inp['c_ctx'])
    maps = []
    for i in range(n_cores):
        cv = np.stack([c[2 * i], c[2 * i + 1], c_ctx], axis=0)
        cT = cv.reshape(3, 8, 128).transpose(2, 1, 0).copy()
        m = dict(shared)
        m["x"] = x[2 * i:2 * i + 2]
        m["ctx"] = ctx[2 * i:2 * i + 2]
        m["cT"] = cT
        maps.append(m)
    return maps


def kernel(**inputs):
    nc = build_program()
    maps = make_in_maps(inputs, 8)
    res = run_bass_kernel_spmd(nc, maps, core_ids=list(range(8)))
    return np.concatenate([np.asarray(r["y"]) for r in res.results], axis=0).astype(np.float32)


def _rope_tables():
    pos = np.arange(LLAT)
    row, col = pos // 64, pos % 64
    half = 16
    inv = (10000.0 ** (-np.arange(half, dtype=np.float32) / half)).astype(np.float32)
    ang_r = row.astype(np.float32)[:, None] * inv[None, :]
    ang_c = col.astype(np.float32)[:, None] * inv[None, :]
    cr, sr, cc, sc = np.cos(ang_r), np.sin(ang_r), np.cos(ang_c), np.sin(ang_c)
    cos = np.concatenate([cr, cr, cc, cc], axis=1).astype(np.float32)
    sin = np.concatenate([-sr, sr, -sc, sc], axis=1).astype(np.float32)
    cos = cos.reshape(32, 128, 64).transpose(1, 0, 2).copy()
    sin = sin.reshape(32, 128, 64).transpose(1, 0, 2).copy()
    return cos, sin
```

```python
import numpy as np
from contextlib import ExitStack
import concourse.bass as bass
import concourse.mybir as mybir
from concourse.bass_utils import run_bass_kernel_spmd

F32 = mybir.dt.float32
BF16 = mybir.dt.bfloat16
AF = mybir.ActivationFunctionType
ALU = mybir.AluOpType
AX = mybir.AxisListType

D = 1024
NB = 2
LCTX = 256
LLAT = 4096
LT = LCTX + LLAT
NT = LT // 128
PROJ = 3104
EPS = 1e-6
NEG = -30000.0

ENGS = ['pe', 'act', 'dve', 'pool', 'sp']
EPOCH = 30000


class Buf:
    __slots__ = ('name', 'w', 'r', 'dsem')

    def __init__(self, name=''):
        self.name = name
        self.w = None
        self.r = {}
        self.dsem = None


class Sched:
    def __init__(self, nc, stack):
        self.nc = nc
        self.stack = stack
        self.cnt = {e: 0 for e in ENGS}
        self.esems = {e: [] for e in ENGS}
        self.items = {e: [] for e in ENGS}
        self.waited = {e: {} for e in ENGS}
        self.free_dsems = []
        self.phase_bufs = []
        self.outstanding = {}
        self.nsem = 0
        self.ninstr = 0
        self.cregs = {}
        self.bregs = {}
        self.bounds = []
        self._cond = None

    def _newsem(self, name):
        self.nsem += 1
        return self.stack.enter_context(self.nc.semaphore(name))

    def _esem(self, e, seq):
        ep = (seq - 1) // EPOCH
        while len(self.esems[e]) <= ep:
            self.esems[e].append(self._newsem(f"s_{e}_{len(self.esems[e])}"))
        return self.esems[e][ep], (seq - 1) % EPOCH + 1

    def buf(self, name=''):
        b = Buf(name)
        self.phase_bufs.append(b)
        return b

    def bufs(self, n, name=''):
        return [self.buf(f"{name}{i}") for i in range(n)]

    def op(self, eng, fn, reads=(), writes=(), dma=None, self_ok=False, track=True):
        deps = {}

        def add(p):
            if p is None:
                return
            sem, val, peng = p
            if self_ok and peng == eng:
                return
            k = sem.num
            if k not in deps or deps[k][1] < val:
                deps[k] = (sem, val)

        for b in reads:
            add(b.w)
        for b in writes:
            add(b.w)
            for p in b.r.values():
                add(p)
        if dma is None:
            self.cnt[eng] += 1
            sem, val = self._esem(eng, self.cnt[eng])
            inc = 1
            tok = (sem, val, eng)
        else:
            if dma.dsem is None:
                if eng == 'pool':
                    dma.dsem = [self._newsem(f"w{self.nsem}"), 0, True]
                elif self.free_dsems:
                    dma.dsem = self.free_dsems.pop()
                else:
                    dma.dsem = [self._newsem(f"d{self.nsem}"), 0, False]
            assert dma.dsem[2] == (eng == 'pool'), "a buffer's DMAs must stay on one kind of queue"
            dma.dsem[1] += 16
            sem, val = dma.dsem[0], dma.dsem[1]
            inc = 16
            tok = (sem, val, 'dma')
        waits = []
        wd = self.waited[eng]
        for k, (s, v) in deps.items():
            if wd.get(k, 0) >= v:
                continue
            wd[k] = v
            waits.append((s, v))
        self.items[eng].append((fn, waits, sem, inc, val))
        self.ninstr += 1
        for b in writes:
            b.w = tok
            b.r = {}
        for b in reads:
            if b not in writes:
                b.r[sem.num] = tok
        if track:
            self.outstanding[sem.num] = (sem, val)
        return tok

    def breg(self, engine, value):
        if value not in self.bregs:
            r = engine.alloc_register(f"bnd_{value}")
            engine.reg_mov(r, value)
            self.bregs[value] = r
        return self.bregs[value]

    def cond_begin(self, flag_ap):
        self._cond = {'flag': flag_ap, 'start': {e: len(self.items[e]) for e in ENGS},
                      'waited': {e: dict(self.waited[e]) for e in ENGS}}
        for e in ENGS:
            self.items[e].append(('cond_begin', flag_ap))

    def cond_end(self):
        c = self._cond
        for e in ENGS:
            body = self.items[e][c['start'][e] + 1:]
            agg = {}
            for it in body:
                fn, waits, sem, inc = it[0], it[1], it[2], it[3]
                if fn is None or sem is None:
                    continue
                k = sem.num
                if k not in agg:
                    agg[k] = [sem, it[4] - inc, 0]
                agg[k][2] += inc
            self.items[e].append(('cond_end', list(agg.values())))
            self.waited[e] = c['waited'][e]
        self._cond = None

    def barrier(self):
        for e in ENGS:
            waits = []
            for k, (s, v) in self.outstanding.items():
                if self.waited[e].get(k, 0) >= v:
                    continue
                self.waited[e][k] = v
                waits.append((s, v))
            self.items[e].append((None, waits, None, 0, 0))
        self.outstanding = {}

    def flush(self):
        self.barrier()
        nc = self.nc
        with nc.Block() as block:
            regs = {'pe': block.tensor, 'act': block.scalar, 'dve': block.vector,
                    'pool': block.gpsimd, 'sp': block.sync}
            for e in ENGS:
                items = self.items[e]

                def body(engine, items=items, e=e):
                    guard = None
                    if e == 'pool':
                        for v in self.bounds:
                            self.breg(engine, v)
                    for it in items:
                        if it[0] == 'cond_begin':
                            if e not in self.cregs:
                                self.cregs[e] = engine.alloc_register(f"creg_{e}")
                            reg = self.cregs[e]
                            engine.reg_load(reg, it[1])
                            guard = engine.If_ne(reg, 0)
                            guard.__enter__()
                            continue
                        if it[0] == 'cond_end':
                            guard.__exit__(None, None, None)
                            eg = engine.Else()
                            eg.__enter__()
                            for sem, pre, tot in it[1]:
                                if pre > 0:
                                    engine.wait_ge(sem, pre)
                                engine.sem_inc(sem, tot)
                            eg.__exit__(None, None, None)
                            guard = None
                            continue
                        fn, waits, sem, inc, _ = it
                        for s, v in waits:
                            engine.wait_ge(s, v)
                        if fn is not None:
                            ins = fn(engine)
                            ins.then_inc(sem, inc)

                regs[e](body)
        self.items = {e: [] for e in ENGS}
        for b in self.phase_bufs:
            if b.dsem is not None and not b.dsem[2]:
                self.free_dsems.append(b.dsem)
            b.dsem = None
        self.phase_bufs = []


class Rot:
    def __init__(self, S, mk, n, name):
        self.t = [mk(f"{name}{i}") for i in range(n)]
        self.b = [S.buf(f"{name}{i}") for i in range(n)]
        self.i = 0

    def next(self):
        k = self.i % len(self.t)
        self.i += 1
        return self.t[k], self.b[k]


class Phase:
    _uid = [0]

    def __init__(self, C, name):
        self.C = C
        self.nc = C.nc
        self.S = C.S
        self.st = ExitStack()
        self.name = name

    def _nm(self, nm):
        Phase._uid[0] += 1
        return f"{self.name}_{nm}_{Phase._uid[0]}"

    def sb(self, shape, dt, nm='t'):
        return self.st.enter_context(self.nc.sbuf_tensor(self._nm(nm), list(shape), dt))

    def ps(self, shape, dt, nm='p'):
        return self.st.enter_context(self.nc.psum_tensor(self._nm(nm), list(shape), dt))

    def sbb(self, shape, dt, nm='t'):
        return self.sb(shape, dt, nm), self.S.buf(nm)

    def rot(self, n, shape, dt, nm, psum=False):
        f = self.ps if psum else self.sb
        return Rot(self.S, lambda s: f(shape, dt, nm), n, nm)

    def close(self):
        self.S.flush()
        self.st.close()


class Ctx:
    pass


def _dram_in(nc, name, shape, dt=F32):
    return nc.dram_tensor(name, list(shape), dt, kind="ExternalInput").ap()


def _dram_tmp(nc, name, shape, dt, dbg=False):
    return nc.dram_tensor(name, list(shape), dt, kind="ExternalOutput" if dbg else "Internal").ap()


import os as _os
USE_TTR = False
GLA_PIPE = True
NA_PIPE = 0

O_Q, O_K, O_V, O_R, O_LR, O_NQ, O_NK, O_NV = 0, 256, 512, 1024, 1536, 1568, 2080, 2592
F_FFN = 2816
F_MOE = 3584
NE = 8


def prenorm_tile(Ph, K, xt, bx, hT, bhT, c0, Gt, St, bGS, j, h32=None, bh32=None):
    S = Ph.S
    C = Ph.C
    junk, bj = K['junk'].next()
    st, bst = K['stat'].next()
    xn, bxn = K['xn'].next()
    ptr, bptr = K['ptr'].next()
    S.op('act', lambda e: e.activation(out=junk[:], in_=xt[:], func=AF.Square, accum_out=st[:, 0:1]),
         reads=[bx], writes=[bj, bst])
    S.op('dve', lambda e: e.tensor_scalar(out=st[:, 1:2], in0=st[:, 0:1], scalar1=1.0 / D, scalar2=EPS,
                                          op0=ALU.mult, op1=ALU.add), reads=[bst], writes=[bst])
    S.op('pool', lambda e: e.tensor_tensor(out=st[:, 2:3], in0=st[:, 1:2], in1=C.neghalf[:, 0:1], op=ALU.pow),
         reads=[bst], writes=[bst])
    S.op('act', lambda e: e.activation(out=xn[:], in_=xt[:], func=AF.Copy, scale=st[:, 2:3]),
         reads=[bx, bst], writes=[bxn])

    def tr(e):
        for k in range(8):
            ins = e.transpose(out=ptr[:, k, :], in_=xn[:, k * 128:(k + 1) * 128], identity=C.ident[:])
        return ins
    S.op('pe', tr, reads=[bxn], writes=[bptr], self_ok=True)
    for k in range(8):
        if k % 2 == 0:
            S.op('dve', lambda e, k=k: e.tensor_scalar(out=hT[:, k, c0:c0 + 128], in0=ptr[:, k, :],
                                                       scalar1=Gt[:, k, j:j + 1], scalar2=St[:, k, j:j + 1],
                                                       op0=ALU.mult, op1=ALU.add),
                 reads=[bptr, bGS], writes=[bhT])
        else:
            S.op('act', lambda e, k=k: e.activation(out=hT[:, k, c0:c0 + 128], in_=ptr[:, k, :], func=AF.Identity,
                                                    scale=Gt[:, k, j:j + 1], bias=St[:, k, j:j + 1]),
                 reads=[bptr, bGS], writes=[bhT])
    if h32 is not None:
        xn32, bxn32 = K['xn32'].next()
        p32, bp32 = K['p32'].next()
        S.op('act', lambda e: e.activation(out=xn32[:], in_=xt[:], func=AF.Copy, scale=st[:, 2:3]),
             reads=[bx, bst], writes=[bxn32])

        def tr32(e):
            for k in range(8):
                ins = e.transpose(out=p32[:, k, :], in_=xn32[:, k * 128:(k + 1) * 128], identity=C.ident32[:])
            return ins
        S.op('pe', tr32, reads=[bxn32], writes=[bp32], self_ok=True)
        for k in range(8):
            S.op('dve', lambda e, k=k: e.tensor_scalar(out=h32[:, k, :], in0=p32[:, k, :],
                                                       scalar1=Gt[:, k, j:j + 1], scalar2=St[:, k, j:j + 1],
                                                       op0=ALU.mult, op1=ALU.add),
                 reads=[bp32, bGS], writes=[bh32])


def prenorm_kit(Ph, with32=False):
    K = {
        'junk': Ph.rot(1, [128, D], BF16, 'junk'),
        'stat': Ph.rot(4, [128, 4], F32, 'stat'),
        'xn': Ph.rot(2, [128, D], BF16, 'xn'),
        'ptr': Ph.rot(2, [128, 8, 128], BF16, 'ptr', psum=True),
    }
    if with32:
        K['xn32'] = Ph.rot(2, [128, D], F32, 'xn32')
        K['p32'] = Ph.rot(1, [128, 8, 128], F32, 'p32', psum=True)
    return K


def res_src(C, l, stage, b, tile):
    if l == 0 and stage == 0:
        if tile < 2:
            return C.ctx_in[b, tile * 128:(tile + 1) * 128, :]
        return C.x_in[b, (tile - 2) * 128:(tile - 1) * 128, :]
    return C.xs[b, tile * 128:(tile + 1) * 128, :]


def phase_mod(C, l, LP):
    nc, S = C.nc, C.S
    Ph = Phase(C, f"mod{l}")
    C.G1, C.bG1 = LP.sbb([128, 8, 3], F32, 'G1')
    C.SH1 = LP.sb([128, 8, 3], F32, 'SH1')
    C.G2, C.bG2 = LP.sbb([128, 8, 3], F32, 'G2')
    C.SH2 = LP.sb([128, 8, 3], F32, 'SH2')
    C.GG1, C.bGG1 = LP.sbb([128, 3, D], F32, 'GG1')
    C.GG2, C.bGG2 = LP.sbb([128, 3, D], F32, 'GG2')
    if getattr(C, 'want_rows', False):
        C.G2row, C.bG2row = LP.sbb([128, 2, D], F32, 'G2row')
        C.S2row = LP.sb([128, 2, D], F32, 'S2row')
    scT, bscT = Ph.sbb([128, 8, 3], F32, 'scT')
    scB, bscB = Ph.sbb([128, 3, 8, 128], F32, 'scB')
    bada, bbada = Ph.sbb([128, 48, 3], F32, 'bada')
    gpre, bgpre = Ph.sbb([128, 2, 8, 3], F32, 'gpre')
    rowc, browc = Ph.sbb([128, 7, D], F32, 'rowc')
    sc1, bsc1 = Ph.sbb([128, 8, 3], F32, 'sc1')
    sc2, bsc2 = Ph.sbb([128, 8, 3], F32, 'sc2')
    wblk = Ph.rot(2, [128, 8, 1024], F32, 'wblk')
    pm = Ph.rot(2, [128, 8, 3], F32, 'pm', psum=True)
    pg = Ph.rot(2, [128, 512], F32, 'pg', psum=True)
    S.op('sp', lambda e: e.dma_start(out=scT[:], in_=C.cT[:, :, :]), writes=[bscT], dma=bscT)
    S.op('sp', lambda e: e.dma_start(out=bada[:], in_=C.badaT3[l]), writes=[bbada], dma=bbada)
    S.op('sp', lambda e: e.dma_start(out=gpre[:], in_=C.gpre3[l]), writes=[bgpre], dma=bgpre)
    S.op('sp', lambda e: e.dma_start(out=rowc[:], in_=C.rowc[l]), writes=[browc], dma=browc)
    S.op('act', lambda e: e.activation(out=scT[:], in_=scT[:], func=AF.Silu), reads=[bscT], writes=[bscT])
    for j in range(3):
        for kc in range(8):
            S.op('act', lambda e, j=j, kc=kc: e.activation(out=scB[:, j, kc, :], in_=C.ones[:], func=AF.Copy,
                                                           scale=scT[:, kc, j:j + 1]),
                 reads=[bscT], writes=[bscB])
    fm = [(0, C.SH1, C.bG1), (1, sc1, bsc1), (3, C.SH2, C.bG2), (4, sc2, bsc2)]
    for blk, dst, bdst in fm:
        wt, bw = wblk.next()
        S.op('sp', lambda e, wt=wt, blk=blk: e.dma_start(
            out=wt[:], in_=C.w_ada[l, :, blk * 1024:(blk + 1) * 1024].rearrange("(kc p) n -> p kc n", p=128)),
            writes=[bw], dma=bw)
        pmt, bpm = pm.next()

        def mm(e, wt=wt, pmt=pmt):
            for ch in range(8):
                for kc in range(8):
                    ins = e.matmul(pmt[:, ch, :], lhsT=wt[:, kc, ch * 128:(ch + 1) * 128], rhs=scT[:, kc, :],
                                   start=(kc == 0), stop=(kc == 7))
            return ins
        S.op('pe', mm, reads=[bw, bscT], writes=[bpm], self_ok=True)
        S.op('dve', lambda e, dst=dst, pmt=pmt, blk=blk: e.tensor_tensor(
            out=dst[:], in0=pmt[:], in1=bada[:, blk * 8:(blk + 1) * 8, :], op=ALU.add),
            reads=[bpm, bbada], writes=[bdst])
    S.op('dve', lambda e: e.scalar_tensor_tensor(out=C.G1[:], in0=sc1[:], scalar=1.0, in1=gpre[:, 0], op0=ALU.add,
                                                 op1=ALU.mult), reads=[bsc1, bgpre], writes=[C.bG1])
    S.op('dve', lambda e: e.scalar_tensor_tensor(out=C.G2[:], in0=sc2[:], scalar=1.0, in1=gpre[:, 1], op0=ALU.add,
                                                 op1=ALU.mult), reads=[bsc2, bgpre], writes=[C.bG2])
    for gi, blk, GG, bGG in [(0, 2, C.GG1, C.bGG1), (1, 5, C.GG2, C.bGG2)]:
        wt, bw = wblk.next()
        S.op('sp', lambda e, wt=wt, blk=blk: e.dma_start(
            out=wt[:], in_=C.w_ada[l, :, blk * 1024:(blk + 1) * 1024].rearrange("(kc p) n -> p kc n", p=128)),
            writes=[bw], dma=bw)
        for j in range(3):
            for half in range(2):
                pgt, bpg = pg.next()
                hs = slice(half * 512, (half + 1) * 512)

                def mm(e, wt=wt, pgt=pgt, j=j, hs=hs):
                    for kc in range(8):
                        ins = e.matmul(pgt[:], lhsT=scB[:, j, kc, :], rhs=wt[:, kc, hs], start=(kc == 0),
                                       stop=(kc == 7))
                    return ins
                S.op('pe', mm, reads=[bw, bscB], writes=[bpg], self_ok=True)
                S.op('dve', lambda e, GG=GG, pgt=pgt, j=j, hs=hs, gi=gi: e.tensor_tensor(
                    out=GG[:, j, hs], in0=pgt[:], in1=rowc[:, gi, hs], op=ALU.add), reads=[bpg, browc], writes=[bGG])
                S.op('pool', lambda e, GG=GG, j=j, hs=hs, gi=gi: e.tensor_tensor(
                    out=GG[:, j, hs], in0=GG[:, j, hs], in1=rowc[:, 2 + gi, hs], op=ALU.mult),
                    reads=[bGG, browc], writes=[bGG])
    if getattr(C, 'want_rows', False):
        for blk, dst, ri in [(3, C.S2row, 4), (4, C.G2row, 5)]:
            wt, bw = wblk.next()
            S.op('sp', lambda e, wt=wt, blk=blk: e.dma_start(
                out=wt[:], in_=C.w_ada[l, :, blk * 1024:(blk + 1) * 1024].rearrange("(kc p) n -> p kc n", p=128)),
                writes=[bw], dma=bw)
            for j in range(2):
                for half in range(2):
                    pgt, bpg = pg.next()
                    hs = slice(half * 512, (half + 1) * 512)

                    def mm(e, wt=wt, pgt=pgt, j=j, hs=hs):
                        for kc in range(8):
                            ins = e.matmul(pgt[:], lhsT=scB[:, j, kc, :], rhs=wt[:, kc, hs], start=(kc == 0),
                                           stop=(kc == 7))
                        return ins
                    S.op('pe', mm, reads=[bw, bscB], writes=[bpg], self_ok=True)
                    S.op('dve', lambda e, dst=dst, pgt=pgt, j=j, hs=hs, ri=ri: e.tensor_tensor(
                        out=dst[:, j, hs], in0=pgt[:], in1=rowc[:, ri, hs], op=ALU.add), reads=[bpg, browc],
                        writes=[C.bG2row])
                    if blk == 4:
                        S.op('dve', lambda e, dst=dst, j=j, hs=hs: e.scalar_tensor_tensor(
                            out=dst[:, j, hs], in0=dst[:, j, hs], scalar=1.0, in1=rowc[:, 6, hs], op0=ALU.add,
                            op1=ALU.mult), reads=[C.bG2row, browc], writes=[C.bG2row])
    Ph.close()


def phase_proj(C, l):
    nc, S = C.nc, C.S
    Ph = Phase(C, f"proj{l}")
    K = prenorm_kit(Ph)
    win, bwin = Ph.sbb([128, 8, PROJ], BF16, 'win')
    cos, bcos = Ph.sbb([128, 32, 64], F32, 'cos')
    sin, bsin = Ph.sbb([128, 32, 64], F32, 'sin')
    npc = 4
    pw = PROJ // npc
    for i in range(npc):
        S.op('pool', lambda e, i=i: e.dma_start(
            out=win[:, :, i * pw:(i + 1) * pw],
            in_=C.w_in[l, :, i * pw:(i + 1) * pw].rearrange("(kc p) n -> p kc n", p=128)), writes=[bwin], dma=bwin)
    S.op('sp', lambda e: e.dma_start(out=cos[:], in_=C.rope_cos[:, :, :]), writes=[bcos], dma=bcos)
    S.op('sp', lambda e: e.dma_start(out=sin[:], in_=C.rope_sin[:, :, :]), writes=[bsin], dma=bsin)
    S.op('dve', lambda e: e.tensor_scalar(out=win[:, :, O_Q:O_Q + 256], in0=win[:, :, O_Q:O_Q + 256], scalar1=0.125,
                                          scalar2=None, op0=ALU.mult), reads=[bwin], writes=[bwin])
    S.op('dve', lambda e: e.tensor_scalar(out=win[:, :, O_NQ:O_NQ + 512], in0=win[:, :, O_NQ:O_NQ + 512],
                                          scalar1=0.125, scalar2=None, op0=ALU.mult), reads=[bwin], writes=[bwin])
    xr = Ph.rot(3, [128, D], F32, 'x')
    hTr = Ph.rot(2, [128, 8, 256], BF16, 'hT')
    stg = Ph.rot(2, [128, 2048], BF16, 'stg')
    fstg = Ph.rot(2, [128, 8, 256], BF16, 'fstg')
    lstg = Ph.rot(2, [16, 2, 256], F32, 'lstg')
    t1r = Ph.rot(2, [128, 512], F32, 't1')
    t2r = Ph.rot(2, [128, 512], F32, 't2')
    ptok = Ph.rot(2, [128, 512], F32, 'ptok', psum=True)
    pfe = Ph.rot(2, [128, 512], F32, 'pfe', psum=True)
    tokcols = [(O_Q, O_Q + 512), (O_V, O_V + 512), (O_R, O_R + 512), (O_NV, O_NV + 512)]
    def pre(b, g):
        j = 2 if g == 0 else b
        hT, bhT = hTr.next()
        for t in range(2):
            tile = g * 2 + t
            xt, bx = xr.next()
            S.op('sp', lambda e, xt=xt, tile=tile, b=b: e.dma_start(out=xt[:], in_=res_src(C, l, 0, b, tile)),
                 writes=[bx], dma=bx)
            prenorm_tile(Ph, K, xt, bx, hT, bhT, t * 128, C.G1, C.SH1, C.bG1, j)
        return hT, bhT

    groups = [(b, g) for b in range(NB) for g in range(LT // 256)]
    cur = pre(*groups[0])
    for gi_, (b, g) in enumerate(groups):
        if True:
            hT, bhT = cur
            if gi_ + 1 < len(groups):
                cur = pre(*groups[gi_ + 1])
            for t in range(2):
                tile = g * 2 + t
                st, bst = stg.next()
                for cb in range(4):
                    pt_, bpt = ptok.next()
                    c0, c1 = tokcols[cb]

                    def mm(e, pt_=pt_, t=t, c0=c0, c1=c1, hT=hT):
                        for kc in range(8):
                            ins = e.matmul(pt_[:], lhsT=hT[:, kc, t * 128:(t + 1) * 128], rhs=win[:, kc, c0:c1],
                                           start=(kc == 0), stop=(kc == 7))
                        return ins
                    S.op('pe', mm, reads=[bhT, bwin], writes=[bpt], self_ok=True)
                    so = st[:, cb * 512:(cb + 1) * 512]
                    if cb == 0 and g > 0:
                        lt = tile - 2
                        t1, bt1 = t1r.next()
                        t2, bt2 = t2r.next()
                        cb_ = cos[:, lt, :].unsqueeze(1).to_broadcast([128, 8, 64])
                        p3 = pt_[:].rearrange("p (a d) -> p a d", a=8)
                        S.op('dve', lambda e, t1=t1, p3=p3, cb_=cb_: e.tensor_tensor(
                            out=t1[:].rearrange("p (a d) -> p a d", a=8), in0=p3, in1=cb_, op=ALU.mult),
                            reads=[bpt, bcos], writes=[bt1])
                        p5 = pt_[:].rearrange("p (a b c d) -> p a b c d", a=8, b=2, c=2)
                        s5 = sin[:, lt, :].rearrange("p (b c d) -> p b c d", b=2, c=2)
                        t25 = t2[:].rearrange("p (a b c d) -> p a b c d", a=8, b=2, c=2)
                        for hf in range(2):
                            sb_ = s5[:, :, hf, :].unsqueeze(1).to_broadcast([128, 8, 2, 16])
                            S.op('dve', lambda e, t25=t25, p5=p5, sb_=sb_, hf=hf: e.tensor_tensor(
                                out=t25[:, :, :, hf, :], in0=p5[:, :, :, 1 - hf, :], in1=sb_, op=ALU.mult),
                                reads=[bpt, bsin], writes=[bt2])
                        S.op('pool', lambda e, so=so, t1=t1, t2=t2: e.tensor_tensor(out=so, in0=t1[:], in1=t2[:],
                                                                                   op=ALU.add),
                             reads=[bt1, bt2], writes=[bst])
                    elif cb == 2:
                        S.op('act', lambda e, so=so, pt_=pt_: e.activation(out=so, in_=pt_[:], func=AF.Silu),
                             reads=[bpt], writes=[bst])
                    else:
                        S.op('act', lambda e, so=so, pt_=pt_: e.activation(out=so, in_=pt_[:], func=AF.Copy),
                             reads=[bpt], writes=[bst])
                S.op('sp', lambda e, st=st, tile=tile, b=b: e.dma_start(
                    out=C.tokmaj[b, tile * 128:(tile + 1) * 128, :], in_=st[:]), reads=[bst], dma=bst)
            ft, bft = fstg.next()
            for cc in range(8):
                pf, bpf = pfe.next()
                c0 = O_NQ + cc * 128

                def mmf(e, pf=pf, c0=c0, hT=hT):
                    for kc in range(8):
                        ins = e.matmul(pf[:, 0:256], lhsT=win[:, kc, c0:c0 + 128], rhs=hT[:, kc, :], start=(kc == 0),
                                       stop=(kc == 7))
                    return ins
                S.op('pe', mmf, reads=[bhT, bwin], writes=[bpf], self_ok=True)
                S.op('dve', lambda e, ft=ft, pf=pf, cc=cc: e.tensor_copy(out=ft[:, cc, :], in_=pf[:, 0:256]),
                     reads=[bpf], writes=[bft])
            S.op('sp', lambda e, ft=ft, g=g, b=b: e.dma_start(
                out=C.nqkT[b].rearrange("(cc p) t -> p cc t", p=128)[:, :, g * 256:(g + 1) * 256], in_=ft[:]),
                reads=[bft], dma=bft)
            lt_, blt = lstg.next()
            for d in range(2):
                pf, bpf = pfe.next()
                c0 = O_LR + 16 * d

                def mml(e, pf=pf, c0=c0, hT=hT):
                    for kc in range(8):
                        ins = e.matmul(pf[0:16, 0:256], lhsT=win[:, kc, c0:c0 + 16], rhs=hT[:, kc, :],
                                       start=(kc == 0), stop=(kc == 7))
                    return ins
                S.op('pe', mml, reads=[bhT, bwin], writes=[bpf], self_ok=True)
                S.op('dve', lambda e, lt_=lt_, pf=pf, d=d: e.tensor_copy(out=lt_[:, d, :], in_=pf[0:16, 0:256]),
                     reads=[bpf], writes=[blt])
            S.op('sp', lambda e, lt_=lt_, g=g, b=b: e.dma_start(
                out=C.lrT[b].rearrange("d r t -> r d t")[:, :, g * 256:(g + 1) * 256], in_=lt_[:]),
                reads=[blt], dma=blt)
    Ph.close()


def phase_gla(C, l, last):
    S = C.S
    Ph = Phase(C, f"gla{l}")
    tri, btri = Ph.sbb([128, 4, 128], F32, 'tri')
    wg, bwg = Ph.sbb([16, 2, 256], F32, 'wg')
    bg, bbg = Ph.sbb([1, 2, 256], F32, 'bg')
    gn, bgn = Ph.sbb([128, 512], F32, 'gn')
    S.op('sp', lambda e: e.dma_start(out=tri[:], in_=C.tri_in[:, :, :]), writes=[btri], dma=btri)
    S.op('sp', lambda e: e.dma_start(out=wg[:], in_=C.wgate[l].rearrange("d r n -> r d n")), writes=[bwg], dma=bwg)
    S.op('sp', lambda e: e.dma_start(out=bg[:], in_=C.bgate[l].rearrange("d o n -> o d n")), writes=[bbg], dma=bbg)
    S.op('sp', lambda e: e.dma_start(out=gn[:], in_=C.gnormB[l]), writes=[bgn], dma=bgn)
    lrr = Ph.rot(1, [16, 2, LT], F32, 'lr')
    ost = Ph.sb([128, NT, 512], F32, 'ost')
    bost = [S.buf(f"ost{i}") for i in range(NT)]
    Sst = [Ph.sbb([128, 2, 128], F32, 'Sst') for _ in range(2)]
    Sbf = [Ph.sbb([128, 2, 128], BF16, 'Sbf') for _ in range(2)]
    qkvr = Ph.rot(4, [128, 1024], BF16, 'qkv')
    rgr = Ph.rot(3, [128, 512], BF16, 'rg')
    e1r = Ph.rot(2, [128, 256], F32, 'e1')
    spr = Ph.rot(2, [128, 256], F32, 'sp')
    ebr = Ph.rot(2, [128, 2, 128], F32, 'eb')
    enbr = Ph.rot(2, [128, 2, 128], F32, 'enb')
    eEr = Ph.rot(2, [128, 256], F32, 'eE')
    qdr = Ph.rot(2, [128, 4, 128], BF16, 'qd')
    kdr = Ph.rot(2, [128, 4, 128], BF16, 'kd')
    for rr in (qdr, kdr):
        for t_, b_ in zip(rr.t, rr.b):
            S.op('pool', lambda e, t_=t_: e.memset(t_[:], 0.0), writes=[b_])
    ker = Ph.rot(2, [128, 256], BF16, 'kend')
    Amr = Ph.rot(2, [128, 4, 128], BF16, 'Am')
    osr = Ph.rot(2, [128, 512], F32, 'osum')
    sqr = Ph.rot(2, [128, 512], F32, 'sq')
    ogr = Ph.rot(2, [128, 512], BF16, 'og')
    str_ = Ph.rot(4, [128, 12], F32, 'gst')
    plr = Ph.rot(1, [128, 512], F32, 'pl', psum=True)
    pber = Ph.rot(1, [128, 512], F32, 'pbe', psum=True)
    pTr = Ph.rot(1, [128, 8, 128], BF16, 'pT', psum=True)
    pAr = Ph.rot(2, [128, 4, 128], F32, 'pA', psum=True)
    por = Ph.rot(2, [128, 4, 128], F32, 'po', psum=True)
    pdsr = Ph.rot(1, [128, 2, 256], F32, 'pds', psum=True)

    import os
    STG = int(os.environ.get('GLA_STG', '99'))
    NTL = int(os.environ.get('GLA_NT', str(NT)))

    def block(b, d, tile, first, lr):
        lrt, blr = lr
        rows_of = lambda hp: slice(hp * 64, hp * 64 + 64)
        r0 = tile * 128
        qkv, bqkv = qkvr.next()
        S.op('sp', lambda e: e.dma_start(out=qkv[:], in_=C.tokmaj[b, r0:r0 + 128, 0:1024]), writes=[bqkv], dma=bqkv)
        if (not first) and not (last and tile < 2):
            rg, brg = rgr.next()
            S.op('sp', lambda e: e.dma_start(out=rg[:], in_=C.tokmaj[b, r0:r0 + 128, 1024:1536]), writes=[brg],
                 dma=brg)
        pl, bpl = plr.next()

        def mml(e):
            e.matmul(pl[:, 0:256], lhsT=lrt[:, d, r0:r0 + 128], rhs=wg[:, d, :], start=True, stop=False)
            return e.matmul(pl[:, 0:256], lhsT=C.ones[0:1, :], rhs=bg[0:1, d, :], start=False, stop=True)
        S.op('pe', mml, reads=[blr, bwg, bbg], writes=[bpl], self_ok=True)
        yield
        e1, be1 = e1r.next()
        sp_, bsp = spr.next()
        S.op('act', lambda e: e.activation(out=e1[:], in_=pl[:, 0:256], func=AF.Exp, scale=-1.0), reads=[bpl],
             writes=[be1])
        S.op('act', lambda e: e.activation(out=sp_[:], in_=e1[:], func=AF.Ln, bias=1.0), reads=[be1], writes=[bsp])
        yield
        Rm = tri[:, 0 if d == 0 else 1, :]
        Um = tri[:, 3 if d == 0 else 2, :]
        pbe, bpbe = pber.next()

        def mmb(e):
            for g in range(2):
                e.matmul(pbe[:, g * 128:(g + 1) * 128], lhsT=sp_[:, g * 128:(g + 1) * 128], rhs=Rm, start=True,
                         stop=True)
            return e.matmul(pbe[:, 256:512], lhsT=Um, rhs=sp_[:], start=True, stop=True)
        S.op('pe', mmb, reads=[bsp, btri], writes=[bpbe], self_ok=True)
        yield
        eb, beb = ebr.next()
        enb, benb = enbr.next()
        eE, beE = eEr.next()
        pb3 = pbe[:, 0:256].rearrange("p (g t) -> p g t", g=2)
        S.op('act', lambda e: e.activation(out=eb[:], in_=pb3, func=AF.Exp, scale=-1.0 / 16), reads=[bpbe],
             writes=[beb])
        S.op('act', lambda e: e.activation(out=enb[:], in_=pb3, func=AF.Exp, scale=1.0 / 16), reads=[bpbe],
             writes=[benb])
        S.op('act', lambda e: e.activation(out=eE[:], in_=pbe[:, 256:512], func=AF.Exp, scale=-1.0 / 16),
             reads=[bpbe], writes=[beE])
        yield
        pT, bpT = pTr.next()

        def tr(e):
            for i in range(4):
                ins = e.transpose(out=pT[:, i, :], in_=qkv[:, i * 128:(i + 1) * 128], identity=C.ident[:])
            return ins
        S.op('pe', tr, reads=[bqkv], writes=[bpT], self_ok=True)
        yield
        qd, bqd = qdr.next()
        kd, bkd = kdr.next()
        kend, bke = ker.next()
        for h in range(4):
            g, rs = h // 2, rows_of(h % 2)
            S.op('dve', lambda e, h=h, g=g, rs=rs: e.tensor_tensor(out=qd[rs, h, :], in0=pT[rs, g, :], in1=eb[rs, g, :],
                                                                   op=ALU.mult), reads=[bpT, beb], writes=[bqd])
            S.op('dve', lambda e, h=h, g=g, rs=rs: e.tensor_tensor(out=kd[rs, h, :], in0=pT[rs, 2 + g, :],
                                                                   in1=enb[rs, g, :], op=ALU.mult),
                 reads=[bpT, benb], writes=[bkd])
        S.op('pool', lambda e: e.tensor_tensor(out=kend[:], in0=qkv[:, 256:512], in1=eE[:], op=ALU.mult),
             reads=[bqkv, beE], writes=[bke])
        yield
        pA, bpA = pAr.next()

        def mmA(e):
            for h in range(4):
                ins = e.matmul(pA[:, h, :], lhsT=kd[:, h, :], rhs=qd[:, h, :], start=True, stop=True)
            return ins
        S.op('pe', mmA, reads=[bkd, bqd], writes=[bpA], self_ok=True)
        yield
        Am, bAm = Amr.next()
        mask = tri[:, 0 if d == 0 else 1, :].unsqueeze(1).to_broadcast([128, 4, 128])
        S.op('dve', lambda e: e.tensor_tensor(out=Am[:], in0=pA[:], in1=mask, op=ALU.mult), reads=[bpA, btri],
             writes=[bAm])
        yield
        po, bpo = por.next()
        sbf, bsbf = Sbf[d]

        def mmo(e):
            for h in range(4):
                g, rs = h // 2, rows_of(h % 2)
                e.matmul(po[:, h, :], lhsT=Am[:, h, :], rhs=qkv[:, 512 + h * 128:512 + (h + 1) * 128], start=True,
                         stop=False)
                ins = e.matmul(po[:, h, :], lhsT=qd[:, h, :], rhs=sbf[:, g, :], start=False, stop=True)
            return ins
        S.op('pe', mmo, reads=[bAm, bqkv, bqd, bsbf], writes=[bpo], self_ok=True)
        need_out = not (last and tile < 2)
        yield
        if need_out:
            if first:
                S.op('act', lambda e: e.activation(out=ost[:, tile, :], in_=po[:].rearrange("p h v -> p (h v)"),
                                                   func=AF.Copy), reads=[bpo], writes=[bost[tile]])
            else:
                osum, bos = osr.next()
                sq, bsq = sqr.next()
                og, bog = ogr.next()
                st, bst = str_.next()
                S.op('dve', lambda e: e.tensor_tensor(out=osum[:], in0=po[:].rearrange("p h v -> p (h v)"),
                                                      in1=ost[:, tile, :], op=ALU.add), reads=[bpo, bost[tile]],
                     writes=[bos])
                S.op('pool', lambda e: e.tensor_tensor(out=sq[:], in0=osum[:], in1=osum[:], op=ALU.mult), reads=[bos],
                     writes=[bsq])
                S.op('dve', lambda e: e.tensor_reduce(out=st[:, 0:4], in_=sq[:].rearrange("p (h v) -> p h v", h=4),
                                                      axis=AX.X, op=ALU.add), reads=[bsq], writes=[bst])
                S.op('dve', lambda e: e.tensor_scalar(out=st[:, 4:8], in0=st[:, 0:4], scalar1=1.0 / 128, scalar2=EPS,
                                                      op0=ALU.mult, op1=ALU.add), reads=[bst], writes=[bst])
                S.op('pool', lambda e: e.tensor_tensor(out=st[:, 8:12], in0=st[:, 4:8], in1=C.neghalf[:, 0:4],
                                                       op=ALU.pow), reads=[bst], writes=[bst])
                S.op('dve', lambda e: e.tensor_tensor(
                    out=sq[:].rearrange("p (h v) -> p h v", h=4), in0=osum[:].rearrange("p (h v) -> p h v", h=4),
                    in1=st[:, 8:12].unsqueeze(2).to_broadcast([128, 4, 128]), op=ALU.mult), reads=[bos, bst],
                    writes=[bsq])
                S.op('pool', lambda e: e.tensor_tensor(out=sq[:], in0=sq[:], in1=gn[:], op=ALU.mult), reads=[bsq, bgn],
                     writes=[bsq])
                S.op('pool', lambda e: e.tensor_tensor(out=og[:], in0=sq[:], in1=rg[:], op=ALU.mult),
                     reads=[bsq, brg], writes=[bog])
                S.op('pool', lambda e: e.dma_start(out=C.cat[b, r0:r0 + 128, 0:512], in_=og[:]), reads=[bog], dma=bog)
        yield
        pds, bpds = pdsr.next()

        def mmds(e):
            for g in range(2):
                ins = e.matmul(pds[:, g, :], lhsT=kend[:, g * 128:(g + 1) * 128],
                               rhs=qkv[:, 512 + g * 256:512 + (g + 1) * 256], start=True, stop=True)
            return ins
        S.op('pe', mmds, reads=[bke, bqkv], writes=[bpds], self_ok=True)
        yield
        sst, bsst = Sst[d]
        dc = 127 if d == 0 else 0
        for g in range(2):
            for hp in range(2):
                rs = rows_of(hp)
                S.op('dve', lambda e, g=g, hp=hp, rs=rs: e.scalar_tensor_tensor(
                    out=sst[rs, g, :], in0=sst[rs, g, :], scalar=eb[rs, g, dc:dc + 1],
                    in1=pds[rs, g, hp * 128:(hp + 1) * 128], op0=ALU.mult, op1=ALU.add),
                    reads=[bsst, beb, bpds], writes=[bsst])
        S.op('act', lambda e: e.activation(out=sbf[:], in_=sst[:], func=AF.Copy), reads=[bsst], writes=[bsbf])

    for b in range(NB):
        lr = lrr.next()
        S.op('sp', lambda e, lr=lr, b=b: e.dma_start(out=lr[0][:], in_=C.lrT[b].rearrange("d r t -> r d t")),
             writes=[lr[1]], dma=lr[1])
        for d in range(2):
            S.op('pool', lambda e, d=d: e.memset(Sst[d][0][:], 0.0), writes=[Sst[d][1]])
            S.op('pool', lambda e, d=d: e.memset(Sbf[d][0][:], 0.0), writes=[Sbf[d][1]])
        orders = [list(range(NT)), [1, 0] + list(range(NT - 1, 1, -1))]
        done = set()
        older = None
        for i in range(NTL):
            for d in range(2):
                tile = orders[d][i]
                newer = block(b, d, tile, tile not in done, lr)
                done.add(tile)
                if not GLA_PIPE:
                    for _ in newer:
                        pass
                    continue
                ne = 0
                while ne < 6 or older is not None:
                    if ne < 6:
                        next(newer)
                        ne += 1
                    if older is not None:
                        try:
                            next(older)
                        except StopIteration:
                            older = None
                older = newer
        if older is not None:
            for _ in older:
                pass
    Ph.close()


NA_SHIFT = 0.0


def phase_na(C, l, last):
    S = C.S
    Ph = Phase(C, f"na{l}")
    kTr = Ph.rot(1, [128, 4, LT], BF16, 'kT')
    qTr = Ph.rot(2, [128, LT], BF16, 'qT')
    for t_, b_ in zip(qTr.t, qTr.b):
        S.op('pool', lambda e, t_=t_: e.memset(t_[:], 0.0), writes=[b_])
    Vr = Ph.rot(1, [128, NT, 8, 65], BF16, 'V')
    for t_, b_ in zip(Vr.t, Vr.b):
        S.op('pool', lambda e, t_=t_: e.memset(t_[:], 1.0), writes=[b_])
    vstr = Ph.rot(2, [128, 8, 512], BF16, 'vst')
    onar = Ph.rot(1, [128, NT, 512], BF16, 'ona')
    biasr = Ph.rot(1, [128, 5, 5, 128], F32, 'bias')
    ssr = Ph.rot(4, [128, 5, 128], F32, 's')
    pr = Ph.rot(4, [128, 7, 128], BF16, 'p')
    str_ = Ph.rot(8, [128, 4], F32, 'nst')
    negc, bnegc = Ph.sbb([128, 1], F32, 'negc')
    S.op('pool', lambda e: e.memset(negc[:], -NA_SHIFT), writes=[bnegc])
    psr = Ph.rot(3, [128, 8, 128], F32, 'ps', psum=True)
    po_t = Ph.ps([128, 512], F32, 'po')
    po_b = [S.buf(f"po{i}") for i in range(7)]
    po_i = [0]

    def unit(b, h, qt, kT, bkT, qT, bqT, V, bV, ona, bona, bias, bbias):
        g = h // 2
        q0 = qt * 128
        if qt >= 2:
            j = qt - 2
            ts = min(max(j - 2, 0), 27)
            pi = {0: 0, 1: 1, 30: 3, 31: 4}.get(j, 2)
            kcols = [256 + 128 * (ts + k) for k in range(5)] + [0, 128]
            vt = [2 + ts + k for k in range(5)] + [0, 1]
            nl = 5
        else:
            kcols = [0, 128]
            vt = [0, 1]
            nl = 0
        nblk = len(kcols)
        ps_, bps = psr.next()

        def mm(e):
            for kb in range(nblk):
                ins = e.matmul(ps_[:, kb, :], lhsT=kT[:, g, kcols[kb]:kcols[kb] + 128], rhs=qT[:, q0:q0 + 128],
                               start=True, stop=True)
            return ins
        S.op('pe', mm, reads=[bqT, bkT], writes=[bps], self_ok=True)
        p, bp = pr.next()
        if nl:
            s_, bs = ssr.next()
            S.op('dve', lambda e: e.tensor_tensor(out=s_[:], in0=ps_[:, 0:5, :], in1=bias[:, pi, :, :], op=ALU.add),
                 reads=[bps, bbias], writes=[bs])
            S.op('act', lambda e: e.activation(out=p[:, 0:5, :], in_=s_[:], func=AF.Exp, bias=negc[:, 0:1], scale=1.0),
                 reads=[bs, bnegc], writes=[bp])
        S.op('act', lambda e: e.activation(out=p[:, nl:nblk, :], in_=ps_[:, nl:nblk, :], func=AF.Exp,
                                           bias=negc[:, 0:1], scale=1.0), reads=[bps, bnegc], writes=[bp])
        yield
        slot = po_i[0] % 7
        po_i[0] += 1
        po = po_t[:, slot * 65:(slot + 1) * 65]
        bpo = po_b[slot]

        def mmpv(e):
            for kb in range(nblk):
                ins = e.matmul(po, lhsT=p[:, kb, :], rhs=V[:, vt[kb], h, :], start=(kb == 0), stop=(kb == nblk - 1))
            return ins
        S.op('pe', mmpv, reads=[bp, bV], writes=[bpo], self_ok=True)
        st, bst = str_.next()
        S.op('dve', lambda e: e.reciprocal(out=st[:, 0:1], in_=po[:, 64:65]), reads=[bpo], writes=[bst])
        S.op('act', lambda e: e.activation(out=ona[:, qt, h * 64:(h + 1) * 64], in_=po[:, 0:64], func=AF.Copy,
                                           scale=st[:, 0:1]), reads=[bpo, bst], writes=[bona])

    inflight = []
    for b in range(NB):
        kT, bkT = kTr.next()
        V, bV = Vr.next()
        ona, bona = onar.next()
        for g in range(4):
            S.op('sp', lambda e, kT=kT, b=b, g=g: e.dma_start(
                out=kT[:, g, :], in_=C.nqkT[b, 512 + g * 128:512 + (g + 1) * 128, :]), writes=[bkT], dma=bkT)
        for t0 in range(0, NT, 8):
            t1 = min(NT, t0 + 8)
            vs, bvs = vstr.next()
            S.op('sp', lambda e, vs=vs, b=b, t0=t0, t1=t1: e.dma_start(
                out=vs[:, 0:t1 - t0, :],
                in_=C.tokmaj[b, t0 * 128:t1 * 128, 1536:2048].rearrange("(t p) c -> p t c", p=128)),
                writes=[bvs], dma=bvs)
            S.op('pool', lambda e, vs=vs, V=V, t0=t0, t1=t1: e.tensor_copy(
                out=V[:, t0:t1, :, 0:64], in_=vs[:, 0:t1 - t0, :].rearrange("p t (h d) -> p t h d", h=8)),
                reads=[bvs], writes=[bV])
        if b == NB - 1 and getattr(C, 'moe_cast_pending', False):
            C.moe_cast_pending = False
            for src, dst in zip([C.moe_wg, C.moe_wu, C.moe_wd], C.moe_bf):
                for ex in range(NE):
                    S.op('pool', lambda e, src=src, dst=dst, ex=ex: e.dma_start(out=dst[ex], in_=src[ex]),
                         writes=[C.bmoe], dma=C.bmoe, track=False)
        for h in range(8):
            bias, bbias = biasr.next()
            S.op('sp', lambda e, bias=bias, h=h: e.dma_start(out=bias[:], in_=C.natb[l, h]), writes=[bbias], dma=bbias)
            qT, bqT = qTr.next()
            hr = slice((h % 2) * 64, (h % 2) * 64 + 64)
            S.op('sp', lambda e, qT=qT, b=b, h=h, hr=hr: e.dma_start(
                out=qT[hr, :], in_=C.nqkT[b, h * 64:(h + 1) * 64, :]), writes=[bqT], dma=bqT)
            qts = list(range(2, NT)) + ([] if last else [0, 1])
            for qt in qts:
                gen = unit(b, h, qt, kT, bkT, qT, bqT, V, bV, ona, bona, bias, bbias)
                next(gen)
                inflight.append(gen)
                if len(inflight) > NA_PIPE:
                    for _ in inflight.pop(0):
                        pass
        while inflight:
            for _ in inflight.pop(0):
                pass
        for t0 in range(2 if last else 0, NT, 8):
            t1 = min(NT, t0 + 8)
            S.op('sp', lambda e, ona=ona, b=b, t0=t0, t1=t1: e.dma_start(
                out=C.cat[b, t0 * 128:t1 * 128, 512:1024].rearrange("(t p) c -> p t c", p=128), in_=ona[:, t0:t1, :]),
                reads=[bona], dma=bona)
    Ph.close()


def phase_outproj(C, l, last):
    S = C.S
    Ph = Phase(C, f"op{l}")
    wo, bwo = Ph.sbb([128, 8, D], BF16, 'wo')
    S.op('pool', lambda e: e.dma_start(out=wo[:], in_=C.w_out[l].rearrange("(kc p) n -> p kc n", p=128)),
         writes=[bwo], dma=bwo)
    ctr = Ph.rot(2, [128, D], BF16, 'ct')
    xr = Ph.rot(2, [128, D], F32, 'x')
    cTsr = Ph.rot(2, [128, 8, 128], BF16, 'cTs')
    ttr = Ph.rot(2, [128, D], F32, 'tt')
    junkr = Ph.rot(1, [128, D], BF16, 'junk')
    str_ = Ph.rot(4, [128, 4], F32, 'ost')
    pTr = Ph.rot(2, [128, 8, 128], BF16, 'pT', psum=True)
    pyr = Ph.rot(2, [128, D], F32, 'py', psum=True)
    for b in range(NB):
        for tile in (range(2, NT) if last else range(NT)):
            j = 2 if tile < 2 else b
            r0 = tile * 128
            ct, bct = ctr.next()
            xt, bx = xr.next()
            S.op('sp', lambda e, ct=ct, b=b, r0=r0: e.dma_start(out=ct[:], in_=C.cat[b, r0:r0 + 128, :]), writes=[bct],
                 dma=bct)
            S.op('sp', lambda e, xt=xt, b=b, tile=tile: e.dma_start(out=xt[:], in_=res_src(C, l, 0, b, tile)),
                 writes=[bx], dma=bx)
            pT, bpT = pTr.next()

            def tr(e, pT=pT, ct=ct):
                for k in range(8):
                    ins = e.transpose(out=pT[:, k, :], in_=ct[:, k * 128:(k + 1) * 128], identity=C.ident[:])
                return ins
            S.op('pe', tr, reads=[bct], writes=[bpT], self_ok=True)
            cTs, bcTs = cTsr.next()
            S.op('dve', lambda e, cTs=cTs, pT=pT: e.tensor_copy(out=cTs[:, 0:4, :], in_=pT[:, 0:4, :]), reads=[bpT],
                 writes=[bcTs])
            S.op('act', lambda e, cTs=cTs, pT=pT: e.activation(out=cTs[:, 4:8, :], in_=pT[:, 4:8, :], func=AF.Copy),
                 reads=[bpT], writes=[bcTs])
            py, bpy = pyr.next()

            def mm(e, py=py, cTs=cTs):
                for half in range(2):
                    for kc in range(8):
                        ins = e.matmul(py[:, half * 512:(half + 1) * 512], lhsT=cTs[:, kc, :],
                                       rhs=wo[:, kc, half * 512:(half + 1) * 512], start=(kc == 0), stop=(kc == 7))
                return ins
            S.op('pe', mm, reads=[bcTs, bwo], writes=[bpy], self_ok=True)
            post_norm_res(Ph, py[:], bpy, xt, bx, C.GG1, C.bGG1, j, junkr, str_, ttr,
                          C.xs[b, r0:r0 + 128, :])
    Ph.close()


def post_norm_res(Ph, y, by, xt, bx, GG, bGG, j, junkr, str_, ttr, dst):
    S = Ph.S
    C = Ph.C
    junk, bj = junkr.next()
    st, bst = str_.next()
    tt, btt = ttr.next()
    S.op('act', lambda e: e.activation(out=junk[:], in_=y, func=AF.Square, accum_out=st[:, 0:1]), reads=[by],
         writes=[bj, bst])
    S.op('dve', lambda e: e.tensor_scalar(out=st[:, 1:2], in0=st[:, 0:1], scalar1=1.0 / D, scalar2=EPS, op0=ALU.mult,
                                          op1=ALU.add), reads=[bst], writes=[bst])
    S.op('pool', lambda e: e.tensor_tensor(out=st[:, 2:3], in0=st[:, 1:2], in1=C.neghalf[:, 0:1], op=ALU.pow),
         reads=[bst], writes=[bst])
    S.op('dve', lambda e: e.scalar_tensor_tensor(out=tt[:], in0=y, scalar=st[:, 2:3], in1=GG[:, j, :], op0=ALU.mult,
                                                 op1=ALU.mult), reads=[by, bst, bGG], writes=[btt])
    S.op('pool', lambda e: e.tensor_tensor(out=tt[:], in0=tt[:], in1=xt[:], op=ALU.add), reads=[btt, bx],
         writes=[btt])
    S.op('pool', lambda e: e.dma_start(out=dst, in_=tt[:]), reads=[btt], dma=btt)


def phase_ffn_pre(C, l, moe, tiles):
    S = C.S
    Ph = Phase(C, f"fpre{l}")
    K = prenorm_kit(Ph, with32=moe)
    xr = Ph.rot(3, [128, D], F32, 'x')
    hTr = Ph.rot(2, [128, 8, 512], BF16, 'hT')
    if moe:
        wr, bwr = Ph.sbb([128, 8, NE], F32, 'wr')
        S.op('sp', lambda e: e.dma_start(out=wr[:], in_=C.moe_wr.rearrange("(kc p) n -> p kc n", p=128)),
             writes=[bwr], dma=bwr)
        h32r = Ph.rot(2, [128, 8, 128], F32, 'h32')
        plg = Ph.rot(1, [128, 512], F32, 'plg', psum=True)
        cmbr = Ph.rot(2, [128, 4, NE], F32, 'cmb')
        rsr = Ph.rot(4, [128, 48], F32, 'rst')
    for gi in range(len(tiles) // 4):
        hT, bhT = hTr.next()
        if moe:
            cmb, bcmb = cmbr.next()
        for t in range(4):
            b, tile = tiles[gi * 4 + t]
            j = 2 if tile < 2 else b
            xt, bx = xr.next()
            S.op('sp', lambda e, xt=xt, b=b, tile=tile: e.dma_start(out=xt[:], in_=res_src(C, l, 1, b, tile)),
                 writes=[bx], dma=bx)
            if not moe:
                prenorm_tile(Ph, K, xt, bx, hT, bhT, t * 128, C.G2, C.SH2, C.bG2, j)
                continue
            h32, bh32 = h32r.next()
            prenorm_tile(Ph, K, xt, bx, hT, bhT, t * 128, C.G2, C.SH2, C.bG2, j, h32, bh32)
            pl, bpl = plg.next()

            def mm(e, pl=pl, h32=h32):
                for kc in range(8):
                    ins = e.matmul(pl[:, 0:NE], lhsT=h32[:, kc, :], rhs=wr[:, kc, :], start=(kc == 0), stop=(kc == 7))
                return ins
            S.op('pe', mm, reads=[bh32, bwr], writes=[bpl], self_ok=True)
            st, bst = rsr.next()
            ops = [
                lambda e, st=st, pl=pl: e.tensor_copy(out=st[:, 0:8], in_=pl[:, 0:NE]),
                lambda e, st=st: e.reduce_max(out=st[:, 8:9], in_=st[:, 0:8], axis=AX.X),
                lambda e, st=st: e.tensor_scalar(out=st[:, 16:24], in0=st[:, 0:8], scalar1=st[:, 8:9], scalar2=-1e30,
                                                 op0=ALU.is_equal, op1=ALU.mult),
                lambda e, st=st: e.tensor_tensor(out=st[:, 16:24], in0=st[:, 16:24], in1=st[:, 0:8], op=ALU.add),
                lambda e, st=st: e.reduce_max(out=st[:, 9:10], in_=st[:, 16:24], axis=AX.X),
                lambda e, st=st: e.tensor_scalar(out=st[:, 24:32], in0=st[:, 0:8], scalar1=st[:, 9:10], scalar2=None,
                                                 op0=ALU.is_ge),
                lambda e, st=st: e.tensor_scalar(out=st[:, 10:11], in0=st[:, 8:9], scalar1=-1.0, scalar2=None,
                                                 op0=ALU.mult),
            ]
            for i, f_ in enumerate(ops):
                S.op('dve', f_, reads=[bst] + ([bpl] if i == 0 else []), writes=[bst])
            S.op('act', lambda e, st=st: e.activation(out=st[:, 32:40], in_=st[:, 0:8], func=AF.Exp, bias=st[:, 10:11],
                                                      scale=1.0), reads=[bst], writes=[bst])
            ops2 = [
                lambda e, st=st: e.tensor_tensor(out=st[:, 32:40], in0=st[:, 32:40], in1=st[:, 24:32], op=ALU.mult),
                lambda e, st=st: e.reduce_sum(out=st[:, 11:12], in_=st[:, 32:40], axis=AX.X),
                lambda e, st=st: e.reciprocal(out=st[:, 12:13], in_=st[:, 11:12]),
            ]
            for f_ in ops2:
                S.op('dve', f_, reads=[bst], writes=[bst])
            S.op('dve', lambda e, st=st, cmb=cmb, t=t: e.tensor_scalar(out=cmb[:, t, :], in0=st[:, 32:40],
                                                                       scalar1=st[:, 12:13], scalar2=None,
                                                                       op0=ALU.mult), reads=[bst], writes=[bcmb])
        S.op('sp', lambda e, hT=hT, gi=gi: e.dma_start(out=C.h2T[gi], in_=hT[:]), reads=[bhT], dma=bhT)
        if moe:
            S.op('sp', lambda e, cmb=cmb, gi=gi: e.dma_start(out=C.comb[gi], in_=cmb[:]), reads=[bcmb], dma=bcmb)
    Ph.close()


def phase_ffn(C, l, moe, tiles):
    S = C.S
    Ph = Phase(C, f"ffn{l}")
    E = NE if moe else 1
    F = F_MOE if moe else F_FFN
    NFC = F // 128
    NFB = F // 256
    wsrc = C.moe_bf if moe else C.ffn_bf
    bwg_ = C.bmoe if moe else C.bffn
    hTr = Ph.rot(2, [128, 8, 512], BF16, 'hT')
    hid, bhid = Ph.sbb([128, NFC, 512], BF16, 'hid')
    wd, bwd = Ph.sbb([128, NFC, D], BF16, 'wd')
    wgr = Ph.rot(3, [128, 8, 256], BF16, 'wg')
    wur = Ph.rot(3, [128, 8, 256], BF16, 'wu')
    yacc, byacc = Ph.sbb([128, 4, D], F32, 'yacc')
    sgr = Ph.rot(2, [128, 512], F32, 'sg')
    xr = Ph.rot(2, [128, D], F32, 'x')
    ttr = Ph.rot(2, [128, D], F32, 'tt')
    junkr = Ph.rot(1, [128, D], BF16, 'junk')
    str_ = Ph.rot(4, [128, 4], F32, 'fst')
    cmbr = Ph.rot(2, [128, 4, NE], F32, 'cmb')
    pgr = Ph.rot(2, [128, 512], F32, 'pg', psum=True)
    pur = Ph.rot(2, [128, 512], F32, 'pu', psum=True)
    pyr = Ph.rot(2, [128, 512], F32, 'py', psum=True)
    for gi in range(len(tiles) // 4):
        hT, bhT = hTr.next()
        S.op('sp', lambda e, hT=hT, gi=gi: e.dma_start(out=hT[:], in_=C.h2T[gi]), writes=[bhT], dma=bhT)
        if moe:
            cmb, bcmb = cmbr.next()
            S.op('sp', lambda e, cmb=cmb, gi=gi: e.dma_start(out=cmb[:], in_=C.comb[gi]), writes=[bcmb], dma=bcmb)
        for ex in range(E):
            hh = NFC // 2
            for (a0, a1) in ([(0, hh), (hh, NFC)] if (moe or gi == 0) else []):
                S.op('pool', lambda e, ex=ex, a0=a0, a1=a1: e.dma_start(
                    out=wd[:, a0:a1, :],
                    in_=wsrc[2][ex, a0 * 128:a1 * 128, :].rearrange("(fc p) n -> p fc n", p=128)),
                    reads=[bwg_], writes=[bwd], dma=bwd)
            for fb in range(NFB):
                wg, bwg = wgr.next()
                wu, bwu = wur.next()
                S.op('sp', lambda e, wg=wg, ex=ex, fb=fb: e.dma_start(
                    out=wg[:], in_=wsrc[0][ex, :, fb * 256:(fb + 1) * 256].rearrange("(kc p) f -> p kc f", p=128)),
                    reads=[bwg_], writes=[bwg], dma=bwg)
                S.op('sp', lambda e, wu=wu, ex=ex, fb=fb: e.dma_start(
                    out=wu[:], in_=wsrc[1][ex, :, fb * 256:(fb + 1) * 256].rearrange("(kc p) f -> p kc f", p=128)),
                    reads=[bwg_], writes=[bwu], dma=bwu)
                for fi in range(2):
                    fc = fb * 2 + fi
                    pg_, bpg = pgr.next()
                    pu_, bpu = pur.next()

                    def mmg(e, pg_=pg_, wg=wg, fi=fi, hT=hT):
                        for kc in range(8):
                            ins = e.matmul(pg_[:], lhsT=wg[:, kc, fi * 128:(fi + 1) * 128], rhs=hT[:, kc, :],
                                           start=(kc == 0), stop=(kc == 7))
                        return ins

                    def mmu(e, pu_=pu_, wu=wu, fi=fi, hT=hT):
                        for kc in range(8):
                            ins = e.matmul(pu_[:], lhsT=wu[:, kc, fi * 128:(fi + 1) * 128], rhs=hT[:, kc, :],
                                           start=(kc == 0), stop=(kc == 7))
                        return ins
                    S.op('pe', mmg, reads=[bwg, bhT], writes=[bpg], self_ok=True)
                    S.op('pe', mmu, reads=[bwu, bhT], writes=[bpu], self_ok=True)
                    sg, bsg = sgr.next()
                    S.op('act', lambda e, sg=sg, pg_=pg_: e.activation(out=sg[:], in_=pg_[:], func=AF.Silu),
                         reads=[bpg], writes=[bsg])
                    S.op('dve', lambda e, sg=sg, pu_=pu_, fc=fc: e.tensor_tensor(out=hid[:, fc, :], in0=pu_[:],
                                                                                 in1=sg[:], op=ALU.mult),
                         reads=[bsg, bpu], writes=[bhid])
            for t in range(4):
                for half in range(2):
                    py_, bpy = pyr.next()
                    hs = slice(half * 512, (half + 1) * 512)

                    def mmd(e, py_=py_, t=t, hs=hs):
                        for fc in range(NFC):
                            ins = e.matmul(py_[:], lhsT=hid[:, fc, t * 128:(t + 1) * 128], rhs=wd[:, fc, hs],
                                           start=(fc == 0), stop=(fc == NFC - 1))
                        return ins
                    S.op('pe', mmd, reads=[bhid, bwd], writes=[bpy], self_ok=True)
                    if not moe:
                        S.op('act', lambda e, py_=py_, t=t, hs=hs: e.activation(out=yacc[:, t, hs], in_=py_[:],
                                                                                func=AF.Copy),
                             reads=[bpy], writes=[byacc])
                    elif ex == 0:
                        S.op('act', lambda e, py_=py_, t=t, hs=hs, cmb=cmb: e.activation(
                            out=yacc[:, t, hs], in_=py_[:], func=AF.Copy, scale=cmb[:, t, 0:1]),
                            reads=[bpy, bcmb], writes=[byacc])
                    else:
                        S.op('dve', lambda e, py_=py_, t=t, hs=hs, cmb=cmb, ex=ex: e.scalar_tensor_tensor(
                            out=yacc[:, t, hs], in0=py_[:], scalar=cmb[:, t, ex:ex + 1], in1=yacc[:, t, hs],
                            op0=ALU.mult, op1=ALU.add), reads=[bpy, bcmb, byacc], writes=[byacc])
        for t in range(4):
            b, tile = tiles[gi * 4 + t]
            j = 2 if tile < 2 else b
            xt, bx = xr.next()
            S.op('sp', lambda e, xt=xt, b=b, tile=tile: e.dma_start(out=xt[:], in_=res_src(C, l, 1, b, tile)),
                 writes=[bx], dma=bx)
            if moe:
                dst = C.y_out[b, (tile - 2) * 128:(tile - 1) * 128, :]
            else:
                dst = C.xs[b, tile * 128:(tile + 1) * 128, :]

            post_norm_res(Ph, yacc[:, t, :], byacc, xt, bx, C.GG2, C.bGG2, j, junkr, str_, ttr, dst)
    Ph.close()


U32 = mybir.dt.uint32
I32 = mybir.dt.int32
NTOK = NB * LLAT
CAPE = NTOK
GS = 512
NGRP = CAPE // GS


def phase_moe_pre(C, l, tiles):
    S = C.S
    Ph = Phase(C, f"mpre{l}")
    xr = Ph.rot(3, [128, D], F32, 'x')
    junkr = Ph.rot(1, [128, D], BF16, 'junk')
    h32r = Ph.rot(2, [128, D], F32, 'h32')
    hbr = Ph.rot(3, [128, D], BF16, 'hb')
    hTr = Ph.rot(2, [128, 8, 128], F32, 'hT32')
    str_ = Ph.rot(4, [128, 4], F32, 'pst')
    rsr = Ph.rot(4, [128, 80], F32, 'rst')
    selr = Ph.rot(2, [128, NE], BF16, 'selb')
    recr = Ph.rot(8, [128, 4], U32, 'rec')
    slur = Ph.rot(4, [128, 2], U32, 'slu')
    wr, bwr = Ph.sbb([128, 8, NE], F32, 'wr')
    base, bbase = Ph.sbb([128, NE], F32, 'base')
    nid, bnid = Ph.sbb([128, 64], F32, 'nid')
    eoff, beoff = Ph.sbb([128, NE], F32, 'eoff')
    thr, bthr = Ph.sbb([128, NGRP], F32, 'thr')
    trif, btrif = Ph.sbb([128, 128], F32, 'trif')
    trib, btrib = Ph.sbb([128, 128], BF16, 'trib')
    oneb, boneb = Ph.sbb([128, 128], BF16, 'oneb')
    flf, bflf = Ph.sbb([128, NE, NGRP], F32, 'flf')
    fli, bfli = Ph.sbb([128, NE, NGRP], I32, 'fli')
    p32r = Ph.rot(2, [128, 8, 128], F32, 'p32', psum=True)
    plg = Ph.rot(2, [128, 512], F32, 'plg', psum=True)
    blst = S.buf('lst')
    S.op('sp', lambda e: e.dma_start(out=C.lst[:, :], in_=C.lst_init[:, :]), writes=[blst], dma=blst)
    S.op('sp', lambda e: e.dma_start(out=wr[:], in_=C.moe_wr.rearrange("(kc p) n -> p kc n", p=128)), writes=[bwr],
         dma=bwr)
    S.op('sp', lambda e: e.dma_start(out=nid[:], in_=C.nidf[:, :]), writes=[bnid], dma=bnid)
    S.op('sp', lambda e: e.dma_start(out=eoff[:], in_=C.eoff[:, :]), writes=[beoff], dma=beoff)
    S.op('sp', lambda e: e.dma_start(out=thr[:], in_=C.thr[:, :]), writes=[bthr], dma=bthr)
    S.op('sp', lambda e: e.dma_start(out=trif[:], in_=C.tri_in[:, 2, :]), writes=[btrif], dma=btrif)
    S.op('dve', lambda e: e.tensor_copy(out=trib[:], in_=trif[:]), reads=[btrif], writes=[btrib])
    S.op('pool', lambda e: e.memset(oneb[:], 1.0), writes=[boneb])
    S.op('pool', lambda e: e.memset(base[:], 0.0), writes=[bbase])
    for k, (b, tile) in enumerate(tiles):
        xt, bx = xr.next()
        S.op('sp', lambda e, xt=xt, b=b, tile=tile: e.dma_start(out=xt[:], in_=res_src(C, l, 1, b, tile)), writes=[bx],
             dma=bx)
        junk, bj = junkr.next()
        st, bst = str_.next()
        h32, bh32 = h32r.next()
        hb, bhb = hbr.next()
        S.op('act', lambda e, junk=junk, xt=xt, st=st: e.activation(out=junk[:], in_=xt[:], func=AF.Square,
                                                                     accum_out=st[:, 0:1]), reads=[bx], writes=[bj, bst])
        S.op('dve', lambda e, st=st: e.tensor_scalar(out=st[:, 1:2], in0=st[:, 0:1], scalar1=1.0 / D, scalar2=EPS,
                                                     op0=ALU.mult, op1=ALU.add), reads=[bst], writes=[bst])
        S.op('pool', lambda e, st=st: e.tensor_tensor(out=st[:, 2:3], in0=st[:, 1:2], in1=C.neghalf[:, 0:1],
                                                      op=ALU.pow), reads=[bst], writes=[bst])
        S.op('dve', lambda e, h32=h32, xt=xt, st=st, b=b: e.scalar_tensor_tensor(
            out=h32[:], in0=xt[:], scalar=st[:, 2:3], in1=C.G2row[:, b, :], op0=ALU.mult, op1=ALU.mult),
            reads=[bx, bst, C.bG2row], writes=[bh32])
        S.op('pool', lambda e, h32=h32, b=b: e.tensor_tensor(out=h32[:], in0=h32[:], in1=C.S2row[:, b, :], op=ALU.add),
             reads=[bh32, C.bG2row], writes=[bh32])
        S.op('act', lambda e, hb=hb, h32=h32: e.activation(out=hb[:], in_=h32[:], func=AF.Copy), reads=[bh32],
             writes=[bhb])
        S.op('act', lambda e, hb=hb, k=k: e.dma_start(out=C.h2tok[k * 128:(k + 1) * 128, :], in_=hb[:]), reads=[bhb],
             dma=bhb)
        p32, bp32 = p32r.next()

        def tr32(e, p32=p32, h32=h32):
            for kc in range(8):
                ins = e.transpose(out=p32[:, kc, :], in_=h32[:, kc * 128:(kc + 1) * 128], identity=C.ident32[:])
            return ins
        S.op('pe', tr32, reads=[bh32], writes=[bp32], self_ok=True)
        hT, bhT = hTr.next()
        S.op('dve', lambda e, hT=hT, p32=p32: e.tensor_copy(out=hT[:, 0:4, :], in_=p32[:, 0:4, :]), reads=[bp32],
             writes=[bhT])
        S.op('act', lambda e, hT=hT, p32=p32: e.activation(out=hT[:, 4:8, :], in_=p32[:, 4:8, :], func=AF.Copy),
             reads=[bp32], writes=[bhT])
        pl, bpl = plg.next()

        def mm(e, pl=pl, hT=hT):
            for kc in range(8):
                ins = e.matmul(pl[:, 0:NE], lhsT=hT[:, kc, :], rhs=wr[:, kc, :], start=(kc == 0), stop=(kc == 7))
            return ins
        S.op('pe', mm, reads=[bhT, bwr], writes=[bpl], self_ok=True)
        r, br = rsr.next()
        LG, M1, M2, NM1, DEN, RDEN = r[:, 0:8], r[:, 8:9], r[:, 9:10], r[:, 10:11], r[:, 11:12], r[:, 12:13]
        EQ1, SEL, WN, MSK, OH1, SV, TMP = r[:, 16:24], r[:, 24:32], r[:, 32:40], r[:, 40:48], r[:, 48:56], r[:, 56:64], \
            r[:, 64:72]
        SL0, SL1, W0, W1, D1 = r[:, 72:73], r[:, 73:74], r[:, 74:75], r[:, 75:76], r[:, 76:77]
        selb, bselb = selr.next()
        dv = lambda f_, extra=(): S.op('dve', f_, reads=[br] + list(extra), writes=[br])
        dv(lambda e, LG=LG, pl=pl: e.tensor_copy(out=LG, in_=pl[:, 0:NE]), [bpl])
        dv(lambda e, LG=LG, M1=M1: e.reduce_max(out=M1, in_=LG, axis=AX.X))
        dv(lambda e, EQ1=EQ1, LG=LG, M1=M1: e.tensor_scalar(out=EQ1, in0=LG, scalar1=M1, scalar2=None,
                                                            op0=ALU.is_equal))
        dv(lambda e, MSK=MSK, EQ1=EQ1, LG=LG: e.scalar_tensor_tensor(out=MSK, in0=EQ1, scalar=-1e30, in1=LG,
                                                                     op0=ALU.mult, op1=ALU.add))
        dv(lambda e, MSK=MSK, M2=M2: e.reduce_max(out=M2, in_=MSK, axis=AX.X))
        dv(lambda e, SEL=SEL, LG=LG, M2=M2: e.tensor_scalar(out=SEL, in0=LG, scalar1=M2, scalar2=None, op0=ALU.is_ge))
        dv(lambda e, NM1=NM1, M1=M1: e.tensor_scalar(out=NM1, in0=M1, scalar1=-1.0, scalar2=None, op0=ALU.mult))
        S.op('act', lambda e, WN=WN, LG=LG, NM1=NM1: e.activation(out=WN, in_=LG, func=AF.Exp, bias=NM1, scale=1.0),
             reads=[br], writes=[br])
        dv(lambda e, WN=WN, SEL=SEL: e.tensor_tensor(out=WN, in0=WN, in1=SEL, op=ALU.mult))
        dv(lambda e, WN=WN, DEN=DEN: e.reduce_sum(out=DEN, in_=WN, axis=AX.X))
        dv(lambda e, DEN=DEN, RDEN=RDEN: e.reciprocal(out=RDEN, in_=DEN))
        dv(lambda e, WN=WN, RDEN=RDEN: e.tensor_scalar(out=WN, in0=WN, scalar1=RDEN, scalar2=None, op0=ALU.mult))
        dv(lambda e, OH1=OH1, SEL=SEL, EQ1=EQ1: e.tensor_tensor(out=OH1, in0=SEL, in1=EQ1, op=ALU.subtract))
        S.op('dve', lambda e, selb=selb, SEL=SEL: e.tensor_copy(out=selb[:], in_=SEL), reads=[br], writes=[bselb])
        pc, bpc = plg.next()

        def mmc(e, pc=pc, selb=selb):
            e.matmul(pc[:, 0:NE], lhsT=trib[:], rhs=selb[:], start=True, stop=True)
            return e.matmul(pc[:, 8:8 + NE], lhsT=oneb[:], rhs=selb[:], start=True, stop=True)
        S.op('pe', mmc, reads=[bselb, btrib, boneb], writes=[bpc], self_ok=True)
        dv(lambda e, SV=SV, pc=pc: e.tensor_tensor(out=SV, in0=pc[:, 0:NE], in1=base[:], op=ALU.add), [bpc, bbase])
        dv(lambda e, SV=SV: e.tensor_tensor(out=SV, in0=SV, in1=eoff[:], op=ALU.add), [beoff])
        S.op('dve', lambda e, pc=pc: e.tensor_tensor(out=base[:], in0=pc[:, 8:8 + NE], in1=base[:], op=ALU.add),
             reads=[bpc, br], writes=[bbase])
        for (OH, SL, W) in [(EQ1, SL0, W0), (OH1, SL1, W1)]:
            dv(lambda e, TMP=TMP, OH=OH, SV=SV: e.tensor_tensor(out=TMP, in0=OH, in1=SV, op=ALU.mult))
            dv(lambda e, TMP=TMP, SL=SL: e.reduce_sum(out=SL, in_=TMP, axis=AX.X))
            dv(lambda e, TMP=TMP, OH=OH, WN=WN: e.tensor_tensor(out=TMP, in0=OH, in1=WN, op=ALU.mult))
            dv(lambda e, TMP=TMP, W=W: e.reduce_sum(out=W, in_=TMP, axis=AX.X))
        dv(lambda e, D1=D1, k=k: e.tensor_scalar(out=D1, in0=nid[:, k:k + 1], scalar1=float(NTOK), scalar2=None,
                                                 op0=ALU.add), [bnid])
        slu, bslu = slur.next()
        S.op('dve', lambda e, slu=slu, SL0=SL0: e.tensor_copy(out=slu[:, 0:1], in_=SL0), reads=[br], writes=[bslu])
        S.op('dve', lambda e, slu=slu, SL1=SL1: e.tensor_copy(out=slu[:, 1:2], in_=SL1), reads=[br], writes=[bslu])
        for rk, (DD, WW) in enumerate([(None, W0), (D1, W1)]):
            rec, brec = recr.next()
            S.op('pool', lambda e, rec=rec: e.memset(rec[:], 0), writes=[brec])
            rw = lambda f_: S.op('dve', f_, reads=[br, bnid], writes=[brec])
            rw(lambda e, rec=rec, k=k: e.tensor_copy(out=rec[:, 0:1], in_=nid[:, k:k + 1]))
            if DD is None:
                rw(lambda e, rec=rec, k=k: e.tensor_copy(out=rec[:, 1:2], in_=nid[:, k:k + 1]))
            else:
                rw(lambda e, rec=rec, DD=DD: e.tensor_copy(out=rec[:, 1:2], in_=DD))
            rw(lambda e, rec=rec, WW=WW: e.tensor_copy(out=rec[:, 2:3].bitcast(F32), in_=WW))
            S.op('pool', lambda e, rec=rec, slu=slu, rk=rk: e.indirect_dma_start(
                out=C.lst[:, :], out_offset=bass.IndirectOffsetOnAxis(ap=slu[:, rk:rk + 1], axis=0),
                in_=rec[:], in_offset=None, bounds_check=S.breg(e, NE * CAPE - 1), oob_is_err=False),
                reads=[brec, bslu, blst], dma=brec)
    for ex in range(NE):
        S.op('dve', lambda e, ex=ex: e.tensor_scalar(out=flf[:, ex, :], in0=thr[:], scalar1=base[:, ex:ex + 1],
                                                     scalar2=None, op0=ALU.is_lt), reads=[bbase, bthr], writes=[bflf])
    S.op('dve', lambda e: e.tensor_copy(out=fli[:], in_=flf[:]), reads=[bflf], writes=[bfli])
    S.op('sp', lambda e: e.dma_start(out=C.flags[0:1, :], in_=fli[0:1, :, :].rearrange("p a b -> p (a b)")),
         reads=[bfli], dma=bfli)
    Ph.close()


def phase_moe_sparse(C, l):
    import os
    SPS = int(os.environ.get('SP_STG', '9'))
    S = C.S
    Ph = Phase(C, f"moe{l}")
    NFC = F_MOE // 128
    NFB = F_MOE // 256
    wsrc = C.moe_bf
    hid, bhid = Ph.sbb([128, NFC, 512], BF16, 'hid')
    wd, bwd = Ph.sbb([128, NFC, D], BF16, 'wd')
    wgr = Ph.rot(3, [128, 8, 256], BF16, 'wg')
    wur = Ph.rot(3, [128, 8, 256], BF16, 'wu')
    htr = Ph.rot(2, [128, 4, D], BF16, 'htok')
    hTr = Ph.rot(2, [128, 8, 512], BF16, 'hT')
    recr = Ph.rot(2, [128, 4, 4], U32, 'recs')
    sgr = Ph.rot(2, [128, 512], F32, 'sg')
    yscr = Ph.rot(2, [128, D], F32, 'ysc')
    ptrr = Ph.rot(2, [128, 8, 128], BF16, 'ptr', psum=True)
    pgr = Ph.rot(2, [128, 512], F32, 'pg', psum=True)
    pur = Ph.rot(1, [128, 512], F32, 'pu', psum=True)
    pyr = Ph.rot(2, [128, 512], F32, 'py', psum=True)
    for t_, b_ in zip(htr.t, htr.b):
        S.op('pool', lambda e, t_=t_: e.memset(t_[:], 0.0), writes=[b_])
    for ex in range(NE):
        hh = NFC // 2
        for (a0, a1) in [(0, hh), (hh, NFC)]:
            S.op('sp', lambda e, ex=ex, a0=a0, a1=a1: e.dma_start(
                out=wd[:, a0:a1, :], in_=wsrc[2][ex, a0 * 128:a1 * 128, :].rearrange("(fc p) n -> p fc n", p=128)),
                reads=[C.bmoe], writes=[bwd], dma=bwd)
        for g in range(NGRP):
            S.cond_begin(C.flags[0:1, ex * NGRP + g:ex * NGRP + g + 1])
            recs, brecs = recr.next()
            r0 = ex * CAPE + g * GS
            S.op('sp', lambda e, recs=recs, r0=r0: e.dma_start(
                out=recs[:], in_=C.lst[r0:r0 + GS, :].rearrange("(t p) c -> p t c", p=128)), writes=[brecs], dma=brecs)
            ht, bht = htr.next()
            for t in range(4):
                S.op('pool', lambda e, ht=ht, recs=recs, t=t: e.indirect_dma_start(
                    out=ht[:, t, :], out_offset=None, in_=C.h2tok[:, :],
                    in_offset=bass.IndirectOffsetOnAxis(ap=recs[:, t, 0:1], axis=0), bounds_check=S.breg(e, NTOK - 1),
                    oob_is_err=False), reads=[brecs], writes=[bht], dma=bht)
            hT, bhT = hTr.next()
            for t in range(4 if SPS >= 2 else 0):
                ptr, bptr = ptrr.next()

                def tr(e, ptr=ptr, ht=ht, t=t):
                    for kc in range(8):
                        ins = e.transpose(out=ptr[:, kc, :], in_=ht[:, t, kc * 128:(kc + 1) * 128], identity=C.ident[:])
                    return ins
                S.op('pe', tr, reads=[bht], writes=[bptr], self_ok=True)
                if t % 2 == 0:
                    S.op('dve', lambda e, hT=hT, ptr=ptr, t=t: e.tensor_copy(out=hT[:, :, t * 128:(t + 1) * 128],
                                                                             in_=ptr[:]), reads=[bptr], writes=[bhT])
                else:
                    S.op('act', lambda e, hT=hT, ptr=ptr, t=t: e.activation(out=hT[:, :, t * 128:(t + 1) * 128],
                                                                            in_=ptr[:], func=AF.Copy), reads=[bptr],
                         writes=[bhT])
            for fb in range(NFB if SPS >= 3 else 0):
                wg, bwg = wgr.next()
                wu, bwu = wur.next()
                S.op('sp', lambda e, wg=wg, ex=ex, fb=fb: e.dma_start(
                    out=wg[:], in_=wsrc[0][ex, :, fb * 256:(fb + 1) * 256].rearrange("(kc p) f -> p kc f", p=128)),
                    reads=[C.bmoe], writes=[bwg], dma=bwg)
                S.op('sp', lambda e, wu=wu, ex=ex, fb=fb: e.dma_start(
                    out=wu[:], in_=wsrc[1][ex, :, fb * 256:(fb + 1) * 256].rearrange("(kc p) f -> p kc f", p=128)),
                    reads=[C.bmoe], writes=[bwu], dma=bwu)
                for fi in range(2):
                    fc = fb * 2 + fi
                    pg_, bpg = pgr.next()
                    pu_, bpu = pur.next()

                    def mmg(e, pg_=pg_, wg=wg, fi=fi, hT=hT):
                        for kc in range(8):
                            ins = e.matmul(pg_[:], lhsT=wg[:, kc, fi * 128:(fi + 1) * 128], rhs=hT[:, kc, :],
                                           start=(kc == 0), stop=(kc == 7))
                        return ins

                    def mmu(e, pu_=pu_, wu=wu, fi=fi, hT=hT):
                        for kc in range(8):
                            ins = e.matmul(pu_[:], lhsT=wu[:, kc, fi * 128:(fi + 1) * 128], rhs=hT[:, kc, :],
                                           start=(kc == 0), stop=(kc == 7))
                        return ins
                    S.op('pe', mmg, reads=[bwg, bhT], writes=[bpg], self_ok=True)
                    S.op('pe', mmu, reads=[bwu, bhT], writes=[bpu], self_ok=True)
                    sg, bsg = sgr.next()
                    S.op('act', lambda e, sg=sg, pg_=pg_: e.activation(out=sg[:], in_=pg_[:], func=AF.Silu),
                         reads=[bpg], writes=[bsg])
                    S.op('dve', lambda e, sg=sg, pu_=pu_, fc=fc: e.tensor_tensor(out=hid[:, fc, :], in0=pu_[:],
                                                                                 in1=sg[:], op=ALU.mult),
                         reads=[bsg, bpu], writes=[bhid])
            for t in range(4 if SPS >= 4 else 0):
                ysc, bysc = yscr.next()
                for half in range(2):
                    py_, bpy = pyr.next()
                    hs = slice(half * 512, (half + 1) * 512)

                    def mmd(e, py_=py_, t=t, hs=hs):
                        for fc in range(NFC):
                            ins = e.matmul(py_[:], lhsT=hid[:, fc, t * 128:(t + 1) * 128], rhs=wd[:, fc, hs],
                                           start=(fc == 0), stop=(fc == NFC - 1))
                        return ins
                    S.op('pe', mmd, reads=[bhid, bwd], writes=[bpy], self_ok=True)
                    S.op('act', lambda e, py_=py_, ysc=ysc, hs=hs, recs=recs, t=t: e.activation(
                        out=ysc[:, hs], in_=py_[:], func=AF.Copy, scale=recs[:, t, 2:3].bitcast(F32)),
                        reads=[bpy, brecs], writes=[bysc])
                if SPS >= 5:
                  S.op('pool', lambda e, ysc=ysc, recs=recs, t=t: e.indirect_dma_start(
                    out=C.Ymoe[:, :], out_offset=bass.IndirectOffsetOnAxis(ap=recs[:, t, 1:2], axis=0), in_=ysc[:],
                    in_offset=None, bounds_check=S.breg(e, 2 * NTOK - 1), oob_is_err=False), reads=[bysc, brecs], dma=bysc)
            S.cond_end()
    Ph.close()


def phase_moe_post(C, l, tiles):
    S = C.S
    Ph = Phase(C, f"mpost{l}")
    xr = Ph.rot(4, [128, D], F32, 'x')
    y1r = Ph.rot(4, [128, D], F32, 'y1')
    y2r = Ph.rot(4, [128, D], F32, 'y2')
    ttr = Ph.rot(3, [128, D], F32, 'tt')
    junkr = Ph.rot(2, [128, D], BF16, 'junk')
    str_ = Ph.rot(8, [128, 4], F32, 'fst')
    for k, (b, tile) in enumerate(tiles):
        xt, bx = xr.next()
        y1, by1 = y1r.next()
        y2, by2 = y2r.next()
        S.op('sp', lambda e, xt=xt, b=b, tile=tile: e.dma_start(out=xt[:], in_=res_src(C, l, 1, b, tile)), writes=[bx],
             dma=bx)
        S.op('sp', lambda e, y1=y1, k=k: e.dma_start(out=y1[:], in_=C.Ymoe[k * 128:(k + 1) * 128, :]), writes=[by1],
             dma=by1)
        S.op('sp', lambda e, y2=y2, k=k: e.dma_start(out=y2[:], in_=C.Ymoe[NTOK + k * 128:NTOK + (k + 1) * 128, :]),
             writes=[by2], dma=by2)
        S.op('pool', lambda e, y1=y1, y2=y2: e.tensor_tensor(out=y1[:], in0=y1[:], in1=y2[:], op=ALU.add),
             reads=[by1, by2], writes=[by1])
        dst = C.y_out[b, (tile - 2) * 128:(tile - 1) * 128, :]
        post_norm_res(Ph, y1[:], by1, xt, bx, C.GG2, C.bGG2, b, junkr, str_, ttr, dst)
    Ph.close()


SPARSE_MOE = True


def build_program(debug=False, upto=None, skip=()):
    nc = bass.Bass("TRN2", target_bir_lowering=False)
    C = Ctx()
    C.nc = nc
    C.debug = debug
    L = 2
    C.x_in = _dram_in(nc, "x", [NB, LLAT, D])
    C.ctx_in = _dram_in(nc, "ctx", [NB, LCTX, D])
    C.cT = _dram_in(nc, "cT", [128, 8, 3])
    C.w_ada = _dram_in(nc, "w_ada", [L, D, 6 * D])
    C.badaT3 = _dram_in(nc, "badaT3", [L, 128, 48, 3])
    C.gpre3 = _dram_in(nc, "gpre3", [L, 128, 2, 8, 3])
    C.rowc = _dram_in(nc, "rowc", [L, 128, 7, D])
    C.w_in = _dram_in(nc, "w_in", [L, D, PROJ])
    C.rope_cos = _dram_in(nc, "rope_cos", [128, 32, 64])
    C.rope_sin = _dram_in(nc, "rope_sin", [128, 32, 64])
    C.ident_in = _dram_in(nc, "ident", [128, 128], BF16)
    C.ident32_in = _dram_in(nc, "ident32", [128, 128], F32)
    C.tri_in = _dram_in(nc, "tri", [128, 4, 128], F32)
    C.wgate = _dram_in(nc, "gla_w_gate", [L, 2, 16, 256])
    C.bgate = _dram_in(nc, "gla_b_gate", [L, 2, 1, 256])
    C.gnormB = _dram_in(nc, "gnormB", [L, 128, 512])
    C.natb = _dram_in(nc, "natb", [L, 8, 128, 5, 5, 128])
    C.w_out = _dram_in(nc, "w_out", [L, D, D])
    C.ffn_wg = _dram_in(nc, "ffn_w_gate", [1, D, F_FFN])
    C.ffn_wu = _dram_in(nc, "ffn_w_up", [1, D, F_FFN])
    C.ffn_wd = _dram_in(nc, "ffn_w_down", [1, F_FFN, D])
    C.moe_wr = _dram_in(nc, "moe_w_router", [D, NE])
    C.moe_wg = _dram_in(nc, "moe_w_gate", [NE, D, F_MOE])
    C.moe_wu = _dram_in(nc, "moe_w_up", [NE, D, F_MOE])
    C.moe_wd = _dram_in(nc, "moe_w_down", [NE, F_MOE, D])
    C.lst_init = _dram_in(nc, "lst_init", [NE * CAPE, 4], U32)
    C.nidf = _dram_in(nc, "nidf", [128, 64])
    C.eoff = _dram_in(nc, "eoff", [128, NE])
    C.thr = _dram_in(nc, "thr", [128, NGRP])
    C.lst = _dram_tmp(nc, "lst", [NE * CAPE, 4], U32)
    C.flags = _dram_tmp(nc, "flags", [1, NE * NGRP], I32)
    C.h2tok = _dram_tmp(nc, "h2tok", [NTOK, D], BF16)
    C.Ymoe = _dram_tmp(nc, "Ymoe", [2 * NTOK, D], F32)
    C.y_out = nc.dram_tensor("y", [NB, LLAT, D], F32, kind="ExternalOutput").ap()
    dbg = debug
    C.xs = _dram_tmp(nc, "xs", [NB, LT, D], F32, dbg)
    C.tokmaj = _dram_tmp(nc, "tokmaj", [NB, LT, 2048], BF16, dbg)
    C.nqkT = _dram_tmp(nc, "nqkT", [NB, 1024, LT], BF16, dbg)
    C.lrT = _dram_tmp(nc, "lrT", [NB, 2, 16, LT], F32, dbg)
    C.cat = _dram_tmp(nc, "cat", [NB, LT, D], BF16, dbg)
    C.h2T = _dram_tmp(nc, "h2T", [17, 128, 8, 512], BF16, dbg)
    C.comb = _dram_tmp(nc, "comb", [17, 128, 4, NE], F32, dbg)
    C.ffn_bf = [_dram_tmp(nc, "ffn_wg_bf", [1, D, F_FFN], BF16), _dram_tmp(nc, "ffn_wu_bf", [1, D, F_FFN], BF16),
                _dram_tmp(nc, "ffn_wd_bf", [1, F_FFN, D], BF16)]
    C.moe_bf = [_dram_tmp(nc, "moe_wg_bf", [NE, D, F_MOE], BF16), _dram_tmp(nc, "moe_wu_bf", [NE, D, F_MOE], BF16),
                _dram_tmp(nc, "moe_wd_bf", [NE, F_MOE, D], BF16)]
    if debug:
        C.dbg = nc.dram_tensor("dbg", [128, 8192], F32, kind="ExternalOutput").ap()
    with ExitStack() as gs:
        S = Sched(nc, gs)
        C.S = S
        S.bounds = [NE * CAPE - 1, NTOK - 1, 2 * NTOK - 1]
        GP = Phase(C, "glob")
        C.ident, bid = GP.sbb([128, 128], BF16, 'ident')
        C.ident32, bid32 = GP.sbb([128, 128], F32, 'ident32')
        C.ones, bones = GP.sbb([128, 128], F32, 'ones')
        C.neghalf, bnh = GP.sbb([128, 4], F32, 'neghalf')
        S.op('sp', lambda e: e.dma_start(out=C.ident[:], in_=C.ident_in[:, :]), writes=[bid], dma=bid)
        S.op('sp', lambda e: e.dma_start(out=C.ident32[:], in_=C.ident32_in[:, :]), writes=[bid32], dma=bid32)
        S.op('pool', lambda e: e.memset(C.ones[:], 1.0), writes=[bones])
        S.op('pool', lambda e: e.memset(C.neghalf[:], -0.5), writes=[bnh])
        C.bffn = Buf('ffn_bf')
        C.bmoe = Buf('moe_bf')
        if upto is None or upto >= 5:
            for src, dst in zip([C.ffn_wg, C.ffn_wu, C.ffn_wd], C.ffn_bf):
                S.op('pool', lambda e, src=src, dst=dst: e.dma_start(out=dst[0], in_=src[0]), writes=[C.bffn],
                     dma=C.bffn, track=False)
        C.moe_cast_pending = (upto is None or upto >= 6)
        S.flush()
        for l in range(L):
            LP = Phase(C, f"L{l}")
            C.want_rows = (l == L - 1) and SPARSE_MOE
            phase_mod(C, l, LP)
            if debug and l == debug - 1 and upto == 0:
                dump_mod(C)
            if upto is not None and upto == 0:
                LP.st.close()
                break
            if 1 not in skip:
                phase_proj(C, l)
            if upto is not None and upto <= 1:
                LP.st.close()
                break
            last = (l == L - 1)
            phase_gla(C, l, last)
            if upto is not None and upto <= 2:
                LP.st.close()
                break
            phase_na(C, l, last)
            if upto is not None and upto <= 3:
                LP.st.close()
                break
            phase_outproj(C, l, last)
            if upto is not None and upto <= 4:
                LP.st.close()
                break
            if last:
                tiles = [(b, t) for b in range(NB) for t in range(2, NT)]
            else:
                tiles = [(b, t) for b in range(NB) for t in range(NT)]
            if last and SPARSE_MOE:
                import os
                ms = int(os.environ.get('MOE_STOP', '9'))
                phase_moe_pre(C, l, tiles)
                if ms >= 2:
                    phase_moe_sparse(C, l)
                if ms >= 3:
                    phase_moe_post(C, l, tiles)
            else:
                phase_ffn_pre(C, l, last, tiles)
                phase_ffn(C, l, last, tiles)
            if upto is not None and upto <= 5 + l:
                LP.st.close()
                break
            LP.st.close()
        GP.st.close()
    return nc


def dump_mod(C):
    S = C.S
    Ph = Phase(C, "dump")
    o = 0
    for t, n in [(C.G1, 24), (C.SH1, 24), (C.G2, 24), (C.SH2, 24)]:
        S.op('sp', lambda e, t=t, o=o, n=n: e.dma_start(out=C.dbg[:, o:o + n], in_=t[:].rearrange("p a b -> p (a b)")),
             reads=[C.bG1, C.bG2], dma=S.buf())
        o += n
    for t in [C.GG1, C.GG2]:
        S.op('sp', lambda e, t=t, o=o: e.dma_start(out=C.dbg[:, o:o + 3072], in_=t[:].rearrange("p a b -> p (a b)")),
             reads=[C.bGG1, C.bGG2], dma=S.buf())
        o += 3072
    Ph.close()


def _na_bias_tables(rpb):
    L = rpb.shape[0]
    out = np.full((L, 8, 128, 5, 640), NEG, np.float32)
    reps = [0, 1, 10, 30, 31]
    for pi, j in enumerate(reps):
        ts = min(max(j - 2, 0), 27)
        for rq2 in range(2):
            r = 2 * j + rq2
            rs = min(max(r - 4, 0), 56)
            for cq in range(64):
                cs = min(max(cq - 8, 0), 48)
                p = rq2 * 64 + cq
                ck = np.arange(cs, cs + 16)
                for rk in range(rs, rs + 8):
                    slot = rk - 2 * ts
                    out[:, :, p, pi, slot * 64 + ck] = rpb[:, :, rk - r + 7, ck - cq + 15]
    return out


def _tri():
    i = np.arange(128)
    ut = (i[:, None] <= i[None, :]).astype(np.float32)
    lt = (i[:, None] >= i[None, :]).astype(np.float32)
    sut = (i[:, None] < i[None, :]).astype(np.float32)
    slt = (i[:, None] > i[None, :]).astype(np.float32)
    return np.stack([ut, lt, sut, slt], axis=1).copy()


def make_in_maps(inp, n_cores=8):
    import ml_dtypes
    f = lambda a: np.ascontiguousarray(np.asarray(a, dtype=np.float32))
    L = 2
    w_ada = f(inp['w_ada'])
    b_ada = f(inp['b_ada'])
    badaT3 = np.repeat(b_ada.reshape(L, 48, 128).transpose(0, 2, 1)[:, :, :, None], 3, axis=3).copy()
    gp = np.stack([f(inp['g_pre_mix']), f(inp['g_pre_ffn'])], axis=1)
    gpre3 = np.repeat(gp.reshape(L, 2, 8, 128).transpose(0, 3, 1, 2)[..., None], 3, axis=4).copy()
    rows = np.stack([b_ada[:, 2048:3072], b_ada[:, 5120:6144], f(inp['g_post_mix']), f(inp['g_post_ffn']),
                     b_ada[:, 3072:4096], b_ada[:, 4096:5120], f(inp['g_pre_ffn'])], axis=1)
    rowc = np.repeat(rows[:, None, :, :], 128, axis=1).copy()
    cos, sin = _rope_tables()
    gn = f(inp['gla_g_norm'])
    gnormB = np.repeat(np.tile(gn, (1, 4))[:, None, :], 128, axis=1).copy()
    natb = _na_bias_tables(f(inp['na_rpb']))
    natb = np.ascontiguousarray(natb.reshape(L, 8, 128, 5, 5, 128).transpose(0, 1, 5, 3, 4, 2))
    lst_init = np.zeros((NE * CAPE, 4), np.uint32)
    lst_init[:, 0:2] = 1 << 30
    nidf = (np.arange(64)[None, :] * 128 + np.arange(128)[:, None]).astype(np.float32)
    eoff = np.repeat((np.arange(NE) * CAPE).astype(np.float32)[None, :], 128, axis=0)
    thr = np.repeat((np.arange(NGRP) * GS).astype(np.float32)[None, :], 128, axis=0)
    shared = {
        "lst_init": lst_init, "nidf": nidf, "eoff": eoff, "thr": thr,
        "w_ada": w_ada, "badaT3": badaT3, "gpre3": gpre3, "rowc": rowc, "w_in": f(inp['w_in']),
        "rope_cos": cos, "rope_sin": sin, "ident": np.eye(128).astype(ml_dtypes.bfloat16),
        "ident32": np.eye(128, dtype=np.float32), "tri": _tri(),
        "gla_w_gate": f(inp['gla_w_gate']), "gla_b_gate": f(inp['gla_b_gate']).reshape(L, 2, 1, 256),
        "gnormB": gnormB, "natb": natb, "w_out": f(inp['w_out']),
        "ffn_w_gate": f(inp['ffn_w_gate']), "ffn_w_up": f(inp['ffn_w_up']), "ffn_w_down": f(inp['ffn_w_down']),
        "moe_w_router": f(inp['moe_w_router'])[0], "moe_w_gate": f(inp['moe_w_gate'])[0],
        "moe_w_up": f(inp['moe_w_up'])[0], "moe_w_down": f(inp['moe_w_down'])[0],
    }
    x = f(inp['x'])
    c = f(inp['c'])
    ctx = f(inp['ctx'])
    c_ctx = f(inp['c_ctx'])
    maps = []
    for i in range(n_cores):
        cv = np.stack([c[2 * i], c[2 * i + 1], c_ctx], axis=0)
        cT = cv.reshape(3, 8, 128).transpose(2, 1, 0).copy()
        m = dict(shared)
        m["x"] = x[2 * i:2 * i + 2]
        m["ctx"] = ctx[2 * i:2 * i + 2]
        m["cT"] = cT
        maps.append(m)
    return maps


def kernel(**inputs):
    nc = build_program()
    maps = make_in_maps(inputs, 8)
    res = run_bass_kernel_spmd(nc, maps, core_ids=list(range(8)))
    return np.concatenate([np.asarray(r["y"]) for r in res.results], axis=0).astype(np.float32)


def _rope_tables():
    pos = np.arange(LLAT)
    row, col = pos // 64, pos % 64
    half = 16
    inv = (10000.0 ** (-np.arange(half, dtype=np.float32) / half)).astype(np.float32)
    ang_r = row.astype(np.float32)[:, None] * inv[None, :]
    ang_c = col.astype(np.float32)[:, None] * inv[None, :]
    cr, sr, cc, sc = np.cos(ang_r), np.sin(ang_r), np.cos(ang_c), np.sin(ang_c)
    cos = np.concatenate([cr, cr, cc, cc], axis=1).astype(np.float32)
    sin = np.concatenate([-sr, sr, -sc, sc], axis=1).astype(np.float32)
    cos = cos.reshape(32, 128, 64).transpose(1, 0, 2).copy()
    sin = sin.reshape(32, 128, 64).transpose(1, 0, 2).copy()
    return cos, sin
```
